# Optimizing a Trainium2 kernel written in Bass

```python
import math
import jax, jax.numpy as jnp
from jax import lax
import numpy as np

D_MODEL = 2048
BATCH = 2
SEQ = 8192
DEPTH = 1

GRID_W = 64
CTX_LEN = 256
D_MIX = D_MODEL
D_S5 = D_MIX // 2
S5_GROUP = 16
S5_GROUPS = D_S5 // S5_GROUP
S5_STATE = 64
S5_CHUNK = 128
D_ML = D_MIX - D_S5
ML_HEADS = 4
ML_HEAD_DIM = D_ML // ML_HEADS
ML_CHUNK = 64
CONV_K = 5
D_IN = D_S5 + 4 * D_ML + 4 * ML_HEADS
N_EXPERTS = 16
EC_FACTOR = 2
EXPERT_FF = 2048
N_MOD = 6
EPS = 1e-6

kernel_name = "hymba_s5_mlstm_ec_flow_block"

F32 = jnp.float32


def rms_norm(x, g):
    xf = x.astype(F32)
    y = xf * lax.rsqrt(jnp.mean(xf * xf, axis=-1, keepdims=True) + EPS)
    return (y * g.astype(F32)).astype(x.dtype)


def _modulation(cond, ada_w, ada_b):
    m = jax.nn.silu(cond) @ ada_w + ada_b
    return jnp.split(m[..., None, :], N_MOD, axis=-1)


def centred_dwconv(u, w, b):
    ch = u.shape[-1]
    out = lax.conv_general_dilated(
        u, w[:, None, :].astype(u.dtype), window_strides=(1,),
        padding=[(CONV_K // 2, CONV_K // 2)],
        dimension_numbers=("NWC", "WIO", "NWC"), feature_group_count=ch)
    return out + b


def to_colmajor(t, rows):
    b, L, ch = t.shape
    return t.reshape(b, rows, GRID_W, ch).swapaxes(1, 2).reshape(b, L, ch)


def from_colmajor(t, rows):
    b, L, ch = t.shape
    return t.reshape(b, GRID_W, rows, ch).swapaxes(1, 2).reshape(b, L, ch)


def s5_discretise(a_re, a_im, log_dt, b_re, b_im, c_re, c_im):
    lam = lax.complex(jnp.minimum(a_re.astype(F32), -1e-4), a_im.astype(F32))
    dt = jnp.exp(log_dt.astype(F32))[..., None]
    a_bar = jnp.exp(lam * dt)
    bmat = lax.complex(b_re.astype(F32), b_im.astype(F32))
    b_bar = ((a_bar - 1.0) / lam)[..., None] * bmat[None]
    c_mat = lax.complex(c_re.astype(F32), c_im.astype(F32))
    return a_bar, b_bar, c_mat


def _lin_combine(e1, e2):
    a1, b1 = e1
    a2, b2 = e2
    return a1 * a2, a2 * b1 + b2


def s5_scan(u, a_bar, b_bar, c_mat, h0):
    bsz, L = u.shape[:2]
    ub = u.reshape(bsz, L // S5_CHUNK, S5_CHUNK, S5_GROUPS, S5_GROUP).swapaxes(0, 1)

    def step(h, u_blk):
        bu = jnp.einsum("btgc,gpc->btgp", u_blk.astype(jnp.complex64), b_bar)
        bu = bu.at[:, 0].add(a_bar * h)
        a = jnp.broadcast_to(a_bar, bu.shape)
        _, hs = lax.associative_scan(_lin_combine, (a, bu), axis=1)
        y = jnp.einsum("btgp,gcp->btgc", hs, c_mat).real
        return hs[:, -1], y

    h_last, ys = lax.scan(step, h0, ub)
    return ys.swapaxes(0, 1).reshape(u.shape), h_last


def s5_bidir(u, a_bar, b_bar, c_mat, d_skip, h0f, h0b):
    bsz, L, _ = u.shape
    uf = u.astype(F32)
    ug = uf.reshape(bsz, L, S5_GROUPS, S5_GROUP)
    yf, hf = s5_scan(ug, a_bar[0], b_bar[0], c_mat, h0f)
    yb, hb = s5_scan(ug[:, ::-1], a_bar[1], b_bar[1], c_mat, h0b)
    y = (yf + yb[:, ::-1]).reshape(bsz, L, D_S5) + d_skip.astype(F32) * uf
    return y, hf, hb


def s5_glu(y, w, b):
    y = jax.nn.gelu(y)
    return y * jax.nn.sigmoid(y @ w + b)


def mlstm_zero_state(bsz):
    return (jnp.zeros((bsz, ML_HEADS, ML_HEAD_DIM, ML_HEAD_DIM), F32),
            jnp.zeros((bsz, ML_HEADS, ML_HEAD_DIM), F32),
            jnp.zeros((bsz, ML_HEADS), F32))


def mlstm_scan(q, k, v, i_pre, log_f, state):
    bsz, nh, L, dh = q.shape
    nc = L // ML_CHUNK

    def blk(t):
        return jnp.moveaxis(t.reshape(bsz, nh, nc, ML_CHUNK, *t.shape[3:]), 2, 0)

    lower = jnp.tril(jnp.ones((ML_CHUNK, ML_CHUNK), dtype=bool))

    def step(carry, inp):
        c_st, n_st, m_st = carry
        qb, kb, vb, ib, fb = inp
        b = jnp.cumsum(fb, axis=-1)
        d = b[..., :, None] - b[..., None, :] + ib[..., None, :]
        d = jnp.where(lower, d, -jnp.inf)
        inter = b + m_st[..., None]
        m_t = jnp.maximum(inter, jnp.max(d, axis=-1))
        w = jnp.exp(d - m_t[..., None])
        s_inter = jnp.exp(inter - m_t)
        s = jnp.einsum("bhtd,bhsd->bhts", qb, kb) * w
        num = jnp.einsum("bhts,bhsd->bhtd", s, vb) + s_inter[..., None] * jnp.einsum("bhvd,bhtd->bhtv", c_st, qb)
        den = jnp.sum(s, axis=-1) + s_inter * jnp.einsum("bhd,bhtd->bht", n_st, qb)
        h = num / jnp.maximum(jnp.abs(den), jnp.exp(-m_t))[..., None]
        b_T = b[..., -1]
        dT = b_T[..., None] - b + ib
        m_new = jnp.maximum(b_T + m_st, jnp.max(dT, axis=-1))
        wT = jnp.exp(dT - m_new[..., None])
        dec = jnp.exp(b_T + m_st - m_new)
        c_new = dec[..., None, None] * c_st + jnp.einsum("bhsv,bhsd->bhvd", vb * wT[..., None], kb)
        n_new = dec[..., None] * n_st + jnp.einsum("bhs,bhsd->bhd", wT, kb)
        return (c_new, n_new, m_new), h

    state, hs = lax.scan(step, state, (blk(q), blk(k), blk(v), blk(i_pre), blk(log_f)))
    return jnp.moveaxis(hs, 0, 2).reshape(bsz, nh, L, dh), state


def mlstm_inputs(p, conv_w, conv_b, gate_b):
    bsz, L, _ = p.shape
    qk = jax.nn.silu(centred_dwconv(p[..., :2 * D_ML], conv_w, conv_b))

    def heads(t):
        return t.astype(F32).reshape(bsz, L, ML_HEADS, ML_HEAD_DIM).transpose(0, 2, 1, 3)

    q = heads(qk[..., :D_ML]) * ML_HEAD_DIM ** -0.5
    k = heads(qk[..., D_ML:])
    v = heads(p[..., 2 * D_ML:3 * D_ML])
    o = p[..., 3 * D_ML:4 * D_ML]
    g = (p[..., 4 * D_ML:].reshape(bsz, L, 4, ML_HEADS) + gate_b).astype(F32).transpose(0, 2, 3, 1)
    return q, k, v, o, g


def mlstm_bidir(p, conv_w, conv_b, gate_b, state_f, state_b):
    q, k, v, o, g = mlstm_inputs(p, conv_w, conv_b, gate_b)
    hf, sf = mlstm_scan(q, k, v, g[:, 0], jax.nn.log_sigmoid(g[:, 1]), state_f)

    def rev(t):
        return jnp.flip(t, axis=2)

    hb, sb = mlstm_scan(rev(q), rev(k), rev(v), g[:, 2, :, ::-1],
                        jax.nn.log_sigmoid(g[:, 3, :, ::-1]), state_b)
    return hf + rev(hb), o, sf, sb


def mlstm_out(h, o, norm_g):
    bsz, nh, L, dh = h.shape
    hn = h * lax.rsqrt(jnp.mean(h * h, axis=-1, keepdims=True) + EPS)
    hn = hn.transpose(0, 2, 1, 3).reshape(bsz, L, D_ML) * norm_g.astype(F32)
    return hn * jax.nn.sigmoid(o.astype(F32))


def token_mixer(hc, hx, with_ctx_out, w_in, s5_a_re, s5_a_im, s5_log_dt, s5_b_re, s5_b_im,
                s5_c_re, s5_c_im, s5_d, s5_glu_w, s5_glu_b, ml_conv_w, ml_conv_b, ml_gate_b,
                ml_norm_g, w_out):
    bsz, L, _ = hx.shape
    rows = L // GRID_W
    pc = hc @ w_in
    px = hx @ w_in
    a_bar, b_bar, c_mat = s5_discretise(s5_a_re, s5_a_im, s5_log_dt, s5_b_re, s5_b_im, s5_c_re, s5_c_im)
    z5 = jnp.zeros((bsz, S5_GROUPS, S5_STATE), jnp.complex64)
    yc_s5, s5f, s5b = s5_bidir(pc[..., :D_S5], a_bar, b_bar, c_mat, s5_d, z5, z5)
    yx_s5, _, _ = s5_bidir(px[..., :D_S5], a_bar, b_bar, c_mat, s5_d, s5f, s5b)
    zm = mlstm_zero_state(bsz)
    hc_ml, oc, mlf, mlb = mlstm_bidir(pc[..., D_S5:], ml_conv_w, ml_conv_b, ml_gate_b, zm, zm)
    hx_ml, ox, _, _ = mlstm_bidir(to_colmajor(px[..., D_S5:], rows), ml_conv_w, ml_conv_b,
                                  ml_gate_b, mlf, mlb)
    yx = jnp.concatenate([s5_glu(yx_s5, s5_glu_w, s5_glu_b),
                          from_colmajor(mlstm_out(hx_ml, ox, ml_norm_g), rows)], axis=-1)
    yx = yx.astype(hx.dtype) @ w_out
    yc = None
    if with_ctx_out:
        yc = jnp.concatenate([s5_glu(yc_s5, s5_glu_w, s5_glu_b),
                              mlstm_out(hc_ml, oc, ml_norm_g)], axis=-1).astype(hc.dtype) @ w_out
    return yc, yx


def ec_moe(h, router_w, w_gate, w_up, w_down):
    bsz, n, d = h.shape
    cap = EC_FACTOR * n // N_EXPERTS
    aff = jax.nn.softmax((h @ router_w).astype(F32), axis=-1)
    g, idx = lax.top_k(aff.swapaxes(1, 2), cap)
    xs = jax.vmap(lambda hb, ib: hb[ib])(h, idx)
    a = jnp.einsum("becd,edf->becf", xs, w_gate)
    u = jnp.einsum("becd,edf->becf", xs, w_up)
    y = jnp.einsum("becf,efd->becd", jax.nn.silu(a) * u, w_down) * g[..., None].astype(h.dtype)
    return jax.vmap(lambda yb, ib: jnp.zeros((n, d), yb.dtype).at[ib.reshape(-1)].add(yb.reshape(-1, d)))(y, idx)


def setup_inputs(seed: int = 0) -> dict:
    key = jax.random.key(seed)
    ks = jax.random.split(key, 32)

    def nrm(k, shape, s):
        return jax.random.normal(k, shape, F32) * s

    n_idx = jnp.arange(S5_STATE, dtype=F32)
    log_lo, log_hi = math.log(1e-3), math.log(1e-1)
    f_bias = jnp.linspace(3.0, 6.0, ML_HEADS, dtype=F32)
    gate_base = jnp.stack([jnp.zeros((ML_HEADS,), F32), f_bias, jnp.zeros((ML_HEADS,), F32), f_bias])
    return {
        "x": nrm(ks[0], (BATCH, SEQ, D_MODEL), 1.0),
        "c": nrm(ks[1], (BATCH, D_MODEL), 1.0),
        "ctx": nrm(ks[2], (BATCH, CTX_LEN, D_MODEL), 1.0),
        "c_ctx": nrm(ks[3], (D_MODEL,), 1.0),
        "ada_w": nrm(ks[4], (DEPTH, D_MODEL, N_MOD * D_MODEL), 0.5 * D_MODEL ** -0.5),
        "ada_b": nrm(ks[5], (DEPTH, N_MOD * D_MODEL), 0.02),
        "norm_g": 1.0 + nrm(ks[6], (DEPTH, 4, D_MODEL), 0.05),
        "w_in": nrm(ks[7], (DEPTH, D_MODEL, D_IN), D_MODEL ** -0.5),
        "s5_a_re": -0.5 + nrm(ks[8], (DEPTH, 2, S5_GROUPS, S5_STATE), 0.01),
        "s5_a_im": math.pi * n_idx + nrm(ks[9], (DEPTH, 2, S5_GROUPS, S5_STATE), 0.01),
        "s5_log_dt": log_lo + (log_hi - log_lo) * jax.random.uniform(ks[10], (DEPTH, 2, S5_GROUPS), F32),
        "s5_b_re": nrm(ks[11], (DEPTH, S5_GROUPS, S5_STATE, S5_GROUP), (2 * S5_GROUP) ** -0.5),
        "s5_b_im": nrm(ks[12], (DEPTH, S5_GROUPS, S5_STATE, S5_GROUP), (2 * S5_GROUP) ** -0.5),
        "s5_c_re": nrm(ks[13], (DEPTH, S5_GROUPS, S5_GROUP, S5_STATE), 0.5),
        "s5_c_im": nrm(ks[14], (DEPTH, S5_GROUPS, S5_GROUP, S5_STATE), 0.5),
        "s5_d": nrm(ks[15], (DEPTH, D_S5), 0.5),
        "s5_glu_w": nrm(ks[16], (DEPTH, D_S5, D_S5), D_S5 ** -0.5),
        "s5_glu_b": nrm(ks[17], (DEPTH, D_S5), 0.02),
        "ml_conv_w": nrm(ks[18], (DEPTH, CONV_K, 2 * D_ML), CONV_K ** -0.5),
        "ml_conv_b": nrm(ks[19], (DEPTH, 2 * D_ML), 0.02),
        "ml_gate_b": gate_base[None] + nrm(ks[20], (DEPTH, 4, ML_HEADS), 0.1),
        "ml_norm_g": 1.0 + nrm(ks[21], (DEPTH, D_ML), 0.05),
        "w_out": nrm(ks[22], (DEPTH, D_MIX, D_MODEL), D_MIX ** -0.5),
        "router_w": nrm(ks[23], (DEPTH, D_MODEL, N_EXPERTS), D_MODEL ** -0.5),
        "exp_w_gate": nrm(ks[24], (DEPTH, N_EXPERTS, D_MODEL, EXPERT_FF), D_MODEL ** -0.5),
        "exp_w_up": nrm(ks[25], (DEPTH, N_EXPERTS, D_MODEL, EXPERT_FF), D_MODEL ** -0.5),
        "exp_w_down": nrm(ks[26], (DEPTH, N_EXPERTS, EXPERT_FF, D_MODEL), EXPERT_FF ** -0.5),
    }


def reference(x, c, ctx, c_ctx, ada_w, ada_b, norm_g, w_in, s5_a_re, s5_a_im, s5_log_dt,
              s5_b_re, s5_b_im, s5_c_re, s5_c_im, s5_d, s5_glu_w, s5_glu_b, ml_conv_w,
              ml_conv_b, ml_gate_b, ml_norm_g, w_out, router_w, exp_w_gate, exp_w_up, exp_w_down):
    for li in range(DEPTH):
        with_ctx_out = li + 1 < DEPTH
        sh1, sc1, g1, sh2, sc2, g2 = _modulation(c, ada_w[li], ada_b[li])
        csh1, csc1, cg1, csh2, csc2, cg2 = _modulation(c_ctx, ada_w[li], ada_b[li])
        hx = rms_norm(x, norm_g[li, 0]) * (1.0 + sc1) + sh1
        hc = rms_norm(ctx, norm_g[li, 0]) * (1.0 + csc1) + csh1
        yc, yx = token_mixer(hc, hx, with_ctx_out, w_in[li], s5_a_re[li], s5_a_im[li], s5_log_dt[li],
                             s5_b_re[li], s5_b_im[li], s5_c_re[li], s5_c_im[li], s5_d[li],
                             s5_glu_w[li], s5_glu_b[li], ml_conv_w[li], ml_conv_b[li],
                             ml_gate_b[li], ml_norm_g[li], w_out[li])
        x = x + g1 * rms_norm(yx, norm_g[li, 1])
        hx = rms_norm(x, norm_g[li, 2]) * (1.0 + sc2) + sh2
        x = x + g2 * rms_norm(ec_moe(hx, router_w[li], exp_w_gate[li], exp_w_up[li], exp_w_down[li]), norm_g[li, 3])
        if with_ctx_out:
            ctx = ctx + cg1 * rms_norm(yc, norm_g[li, 1])
            hc = rms_norm(ctx, norm_g[li, 2]) * (1.0 + csc2) + csh2
            ctx = ctx + cg2 * rms_norm(ec_moe(hc, router_w[li], exp_w_gate[li], exp_w_up[li], exp_w_down[li]), norm_g[li, 3])
    return x
```

```python
from contextlib import ExitStack
import math
import numpy as np
import ml_dtypes
import concourse.bass as bass
import concourse.mybir as mybir
from concourse.bass_utils import run_bass_kernel_spmd

F32 = mybir.dt.float32
BF16 = mybir.dt.bfloat16
I32 = mybir.dt.int32
U32 = mybir.dt.uint32
ALU = mybir.AluOpType
AF = mybir.ActivationFunctionType
AX = mybir.AxisListType

ENGS = ("sp", "act", "dve", "pool", "pe")

D = 2048
L = 8192
CTX = 256
NT = L + CTX
NTILE = NT // 128
GW = 64
D_S5 = 1024
D_ML = 1024
NH = 4
DH = 256
D_IN = D_S5 + 4 * D_ML + 16
NE = 16
CAPE = 1024
FF = 2048
EPS = 1e-6
OWN = 2048
CAPL = 384


class _Op:
    __slots__ = ("eng", "fn", "deps", "dma", "needed", "sem", "val", "prev_val", "inc")

    def __init__(self, eng, fn, deps, dma, inc):
        self.eng = eng
        self.fn = fn
        self.deps = deps
        self.dma = dma
        self.needed = False
        self.sem = None
        self.val = 0
        self.prev_val = 0
        self.inc = inc


class Prog:
    def __init__(self, nc, n_dma_sems=8):
        self.nc = nc
        self.ops = []
        self.pending = []
        self.last_w = {}
        self.readers = {}
        self.st = ExitStack()
        self.esem = {e: self.st.enter_context(nc.semaphore("es_" + e)) for e in ENGS}
        self.ecnt = {e: 0 for e in ENGS}
        self.dsems = [self.st.enter_context(nc.semaphore("ds%d" % i)) for i in range(n_dma_sems)]
        self.dval = [0] * n_dma_sems
        self.drr = 0
        self.waited = {e: {} for e in ENGS}
        self.n_ins = 0

    def add(self, eng, fn, r=(), w=(), dma=False, inc=16):
        deps = set()
        for x in r:
            if x in self.last_w:
                deps.add(self.last_w[x])
        for x in w:
            if x in self.last_w:
                deps.add(self.last_w[x])
            deps.update(self.readers.get(x, {}).values())
        op = _Op(eng, fn, deps, dma, inc)
        oid = len(self.ops)
        self.ops.append(op)
        self.pending.append(oid)
        rk = ("dma", oid) if dma else eng
        for x in r:
            self.readers.setdefault(x, {})[rk] = oid
        for x in w:
            self.last_w[x] = oid
            self.readers[x] = {}
        return oid

    def dma(self, eng, out, in_, r=(), w=(), **kw):
        return self.add(eng, lambda e: e.dma_start(out=out, in_=in_, **kw), r=r, w=w, dma=True)

    def flush(self, final_wait=()):
        ops = self.ops
        pend = self.pending
        self.pending = []
        live = set(self.last_w.values())
        for lst in self.readers.values():
            live.update(lst.values())
        for oid in pend:
            for d in ops[oid].deps:
                ops[d].needed = True
        for oid in pend:
            op = ops[oid]
            if op.dma or oid in live:
                op.needed = True
        for oid in pend:
            op = ops[oid]
            if op.dma:
                k = self.drr
                self.drr = (self.drr + 1) % len(self.dsems)
                op.sem = k
                op.prev_val = self.dval[k]
                self.dval[k] += op.inc
                op.val = self.dval[k]
            elif op.needed:
                self.ecnt[op.eng] += 1
                op.val = self.ecnt[op.eng]
        per = {e: [] for e in ENGS}
        for oid in pend:
            per[ops[oid].eng].append(oid)
        fw = list(final_wait)

        def run(ename, e):
            wd = self.waited[ename]

            def wait(key, sem, val):
                if wd.get(key, 0) >= val:
                    return
                wd[key] = val
                e.wait_ge(sem, val)
                self.n_ins += 1

            def wait_op(p):
                if p.dma:
                    wait(("d", p.sem), self.dsems[p.sem], p.val)
                else:
                    assert p.val > 0, "dependency on unsignalled op"
                    wait(("e", p.eng), self.esem[p.eng], p.val)

            for oid in per[ename]:
                op = ops[oid]
                for d in sorted(op.deps):
                    wait_op(ops[d])
                self.n_ins += 1
                if op.dma:
                    if op.prev_val > 0:
                        wait(("d", op.sem), self.dsems[op.sem], op.prev_val)
                    ins = op.fn(e)
                    ins.then_inc(self.dsems[op.sem], op.inc)
                else:
                    ins = op.fn(e)
                    if op.needed:
                        ins.then_inc(self.esem[ename], 1)
            if ename == "sp":
                for d in fw:
                    wait_op(ops[d])

        with self.nc.Block() as block:
            @block.sync
            def _(e):
                run("sp", e)

            @block.scalar
            def _(e):
                run("act", e)

            @block.vector
            def _(e):
                run("dve", e)

            @block.gpsimd
            def _(e):
                run("pool", e)

            @block.tensor
            def _(e):
                run("pe", e)

    def close(self):
        self.st.close()


class K:
    def __init__(self, dbg=()):
        self.nc = bass.Bass("TRN2", target_bir_lowering=False)
        self.P = Prog(self.nc)
        self.dbg = set(dbg)
        self.t = {}
        self._rr = 0

    def din(self, name, shape, dt=F32):
        a = self.nc.dram_tensor(name, list(shape), dt, kind="ExternalInput").ap()
        self.t[name] = a
        return a

    def dout(self, name, shape, dt=F32):
        a = self.nc.dram_tensor(name, list(shape), dt, kind="ExternalOutput").ap()
        self.t[name] = a
        return a

    def dscr(self, name, shape, dt=F32):
        kind = "ExternalOutput" if name in self.dbg else "Internal"
        a = self.nc.dram_tensor(name, list(shape), dt, kind=kind).ap()
        self.t[name] = a
        return a

    def q(self):
        self._rr ^= 1
        return "sp" if self._rr else "act"


def declare_io(k):
    k.din("x", [L, D])
    k.din("ctx", [CTX, D])
    k.din("cond", [128, 16, 2])
    k.din("ada_w", [D, 6 * D])
    k.din("ada_b", [1, 6 * D])
    k.din("norm_g", [4, D])
    k.din("w_in", [D, D_IN])
    k.din("ident_bf", [128, 128], BF16)
    k.din("ident_f", [128, 128])
    k.dout("out", [OWN, D])
    k.dscr("MODS", [6, 128, D])
    k.dscr("MODS2", [4, 128, D])
    k.dscr("HXT", [2, 16, 128, NT], BF16)
    k.dscr("U5", [8, 128, NT], BF16)
    k.dscr("QKPRE", [16, 128, NT], BF16)
    k.dscr("V", [NT, D_ML], BF16)
    k.dscr("SO", [NT, D_ML], BF16)
    k.dscr("GATES", [128, NTILE, 16])


def stage0(k):
    nc, P, t = k.nc, k.P, k.t
    with ExitStack() as st:
        sb = lambda n, s, d=F32: st.enter_context(nc.sbuf_tensor(n, s, d))
        cond = sb("s0_cond", [128, 16, 2])
        sil = sb("s0_sil", [128, 16, 2])
        lx = sb("s0_lx", [128, 16, 128])
        lc = sb("s0_lc", [128, 16, 128])
        wt = [sb("s0_w%d" % i, [128, 16, 512]) for i in range(2)]
        bt = [sb("s0_b%d" % i, [128, 512]) for i in range(2)]
        mx = sb("s0_mx", [128, 6, D])
        mc = sb("s0_mc", [128, 2, D])
        ng = sb("s0_ng", [128, 4, D])
        o1 = sb("s0_o1", [128, D])
        ps = [st.enter_context(nc.psum_tensor("s0_ps%d" % i, [128, 512], F32)) for i in range(2)]

        P.dma("sp", cond[:], t["cond"][:, :, :], w=["cond"])
        P.dma("act", ng[:], t["norm_g"].rearrange("(o g) d -> o g d", o=1).broadcast_to([128, 4, D]), w=["ng"])
        P.add("act", lambda e: e.activation(out=sil[:], in_=cond[:], func=AF.Silu), r=["cond"], w=["sil"])
        P.add("dve", lambda e: e.tensor_copy(lx[:], sil[:, :, 0:1].broadcast_to([128, 16, 128])), r=["sil"], w=["lx"])
        P.add("dve", lambda e: e.tensor_copy(lc[:], sil[:, :, 1:2].broadcast_to([128, 16, 128])), r=["sil"], w=["lc"])
        adaw = t["ada_w"].rearrange("(kc p) n -> p kc n", p=128)
        for nb in range(24):
            b = nb % 2
            P.dma("sp" if b else "act", wt[b][:], adaw[:, :, nb * 512:(nb + 1) * 512], w=[("w", b)])
            P.dma("pool", bt[b][:], t["ada_b"][:, nb * 512:(nb + 1) * 512].broadcast_to([128, 512]), w=[("b", b)])
            for which in range(2 if nb < 8 else 1):
                lhs = lx if which == 0 else lc
                pst = ps[which]
                for kc in range(16):
                    P.add("pe", lambda e, lhs=lhs, kc=kc, b=b, pst=pst: e.matmul(
                        pst[:], lhs[:, kc, :], wt[b][:, kc, :], start=(kc == 0), stop=(kc == 15)),
                        r=["lx", "lc", ("w", b)], w=[("ps", which)])
                dst = mx if which == 0 else mc
                ch, off = divmod(nb * 512, D)
                P.add("dve", lambda e, dst=dst, ch=ch, off=off, pst=pst, b=b: e.tensor_tensor(
                    dst[:, ch, off:off + 512], pst[:], bt[b][:], ALU.add),
                    r=[("ps", which), ("b", b)], w=[("m", which)])
        mods = t["MODS"]
        mods2 = t["MODS2"]

        def emit(dst_ap, fn, key):
            P.add("dve", fn, r=[("m", 0), ("m", 1), "ng"], w=["o1"])
            P.dma("sp", dst_ap, o1[:], r=["o1"], w=[key])

        emit(mods[0], lambda e: e.scalar_tensor_tensor(o1[:], mx[:, 1, :], 1.0, ng[:, 0, :], ALU.add, ALU.mult), "MODS0")
        emit(mods[1], lambda e: e.tensor_copy(o1[:], mx[:, 0, :]), "MODS1")
        emit(mods[2], lambda e: e.scalar_tensor_tensor(o1[:], mc[:, 1, :], 1.0, ng[:, 0, :], ALU.add, ALU.mult), "MODS2")
        emit(mods[3], lambda e: e.tensor_copy(o1[:], mc[:, 0, :]), "MODS3")
        emit(mods2[0], lambda e: e.tensor_tensor(o1[:], mx[:, 2, :], ng[:, 1, :], ALU.mult), "M2_0")
        emit(mods2[1], lambda e: e.scalar_tensor_tensor(o1[:], mx[:, 4, :], 1.0, ng[:, 2, :], ALU.add, ALU.mult), "M2_1")
        emit(mods2[2], lambda e: e.tensor_copy(o1[:], mx[:, 3, :]), "M2_2")
        emit(mods2[3], lambda e: e.tensor_tensor(o1[:], mx[:, 5, :], ng[:, 3, :], ALU.mult), "M2_3")
        P.flush()


def tok_src(k, order, ti):
    t = k.t
    if ti < 2:
        return t["ctx"][ti * 128:(ti + 1) * 128, :]
    xi = ti - 2
    if order == 0:
        return t["x"][xi * 128:(xi + 1) * 128, :]
    return t["x"].rearrange("(r w) d -> w r d", w=GW)[xi]


def stage1(k, orders=(0, 1)):
    nc, P, t = k.nc, k.P, k.t
    with ExitStack() as st:
        sb = lambda n, s, d=F32: st.enter_context(nc.sbuf_tensor(n, s, d))
        A = [sb("s1_A%d" % i, [128, D]) for i in range(4)]
        ident = sb("s1_id", [128, 128], BF16)
        xt = [sb("s1_x%d" % i, [128, D]) for i in range(2)]
        junk = sb("s1_junk", [128, D])
        t1 = [sb("s1_t%d" % i, [128, D]) for i in range(2)]
        hx = [sb("s1_hx%d" % i, [128, D], BF16) for i in range(2)]
        ss = [sb("s1_ss%d" % i, [128, 2]) for i in range(2)]
        blk = [sb("s1_blk%d" % i, [128, 16, 512], BF16) for i in range(2)]
        ps = [st.enter_context(nc.psum_tensor("s1_ps%d" % i, [128, 16, 128], BF16)) for i in range(2)]
        epst = sb("s1_eps", [128, 1])
        P.add("pool", lambda e: e.memset(epst[:], EPS), w=["epst"])
        for i in range(4):
            P.dma("sp", A[i][:], t["MODS"][i], r=["MODS%d" % i], w=[("A", i)])
        P.dma("act", ident[:], t["ident_bf"][:, :], w=["ident"])
        for order in orders:
            groups = [(0, 2)] + [(2 + 4 * g, 4) for g in range(16)]
            for gi, (t0, n) in enumerate(groups):
                bb = gi % 2
                for j in range(n):
                    ti = t0 + j
                    b = ti % 2
                    a_i, b_i = (2, 3) if ti < 2 else (0, 1)
                    P.dma(k.q(), xt[b][:], tok_src(k, order, ti), w=[("xt", b)])
                    P.add("act", lambda e, b=b: e.activation(out=junk[:], in_=xt[b][:], func=AF.Square,
                                                             scale=float(D ** -0.5), accum_out=ss[b][:, 0:1]),
                          r=[("xt", b)], w=["junk", ("ss", b)])
                    P.add("act", lambda e, b=b: e.activation(out=ss[b][:, 1:2], in_=ss[b][:, 0:1], func=AF.Sqrt,
                                                             bias=epst[:, 0:1], scale=1.0),
                          r=[("ss", b), "epst"], w=[("ss1", b)])
                    P.add("dve", lambda e, b=b: e.reciprocal(ss[b][:, 1:2], ss[b][:, 1:2]),
                          r=[("ss1", b)], w=[("ss1", b)])
                    P.add("dve", lambda e, b=b, a_i=a_i: e.scalar_tensor_tensor(
                        t1[b][:], xt[b][:], ss[b][:, 1:2], A[a_i][:], ALU.mult, ALU.mult),
                        r=[("xt", b), ("ss1", b), ("A", a_i)], w=[("t1", b)])
                    P.add("pool", lambda e, b=b, b_i=b_i: e.tensor_tensor(hx[b][:], t1[b][:], A[b_i][:], ALU.add),
                          r=[("t1", b), ("A", b_i)], w=[("hx", b)])
                    for kc in range(16):
                        P.add("pe", lambda e, b=b, kc=kc: e.transpose(ps[b][:, kc, :], hx[b][:, kc * 128:(kc + 1) * 128],
                                                                      ident[:]),
                              r=[("hx", b), "ident"], w=[("ps", b)])
                    P.add("act", lambda e, b=b, bb=bb, j=j: e.copy(blk[bb][:, :, j * 128:(j + 1) * 128], ps[b][:]),
                          r=[("ps", b)], w=[("blk", bb)])
                dst = t["HXT"][order].rearrange("kc p t -> p kc t")[:, :, t0 * 128:(t0 + n) * 128]
                P.dma(k.q(), dst, blk[bb][:, :, 0:n * 128], r=[("blk", bb)], w=[("HXT", order, gi)])
        P.flush()


TGROUPS = [(0, 256)] + [(256 + 512 * g, 512) for g in range(16)]


def stage2(k):
    nc, P, t = k.nc, k.P, k.t
    win = t["w_in"].rearrange("(kc p) n -> p kc n", p=128)
    for pname, order, c0, ncb, dst in (("A", 0, 0, 8, "U5"), ("B", 1, D_S5, 16, "QKPRE")):
        with ExitStack() as st:
            sb = lambda n, s, d=F32: st.enter_context(nc.sbuf_tensor(n, s, d))
            W = sb("s2%s_w" % pname, [128, 16, ncb * 128], BF16)
            hb = [sb("s2%s_h%d" % (pname, i), [128, 16, 512], BF16) for i in range(2)]
            ob = [sb("s2%s_o%d" % (pname, i), [128, ncb, 512], BF16) for i in range(2)]
            ps = [st.enter_context(nc.psum_tensor("s2%s_ps%d" % (pname, i), [128, 512], F32)) for i in range(4)]
            for kc in range(16):
                for c1 in range(0, ncb * 128, 1024):
                    P.dma("pool", W[:, kc, c1:c1 + 1024], win[:, kc, c0 + c1:c0 + c1 + 1024], w=[("W", kc, c1 // 1024)])
            hsrc = t["HXT"][order].rearrange("kc p t -> p kc t")
            dview = t[dst].rearrange("cb p t -> p cb t")
            for gi, (t0, gt) in enumerate(TGROUPS):
                b = gi % 2
                P.dma(k.q(), hb[b][:, :, 0:gt], hsrc[:, :, t0:t0 + gt], r=[("HXT", order, gi)], w=[("hb", b)])
                for cb in range(ncb):
                    pi = cb % 4
                    for kc in range(16):
                        P.add("pe", lambda e, pi=pi, kc=kc, cb=cb, b=b, gt=gt: e.matmul(
                            ps[pi][:, 0:gt], W[:, kc, cb * 128:(cb + 1) * 128], hb[b][:, kc, 0:gt],
                            start=(kc == 0), stop=(kc == 15)),
                            r=[("W", kc, cb // 8), ("hb", b)], w=[("ps", pi)])
                    eng = "act" if cb % 2 == 0 else "dve"
                    if eng == "act":
                        P.add("act", lambda e, pi=pi, cb=cb, b=b, gt=gt: e.copy(ob[b][:, cb, 0:gt], ps[pi][:, 0:gt]),
                              r=[("ps", pi)], w=[("ob", b)])
                    else:
                        P.add("dve", lambda e, pi=pi, cb=cb, b=b, gt=gt: e.tensor_copy(ob[b][:, cb, 0:gt], ps[pi][:, 0:gt]),
                              r=[("ps", pi)], w=[("ob", b)])
                P.dma(k.q(), dview[:, :, t0:t0 + gt], ob[b][:, :, 0:gt], r=[("ob", b)], w=[(dst, gi)])
            P.flush()
    with ExitStack() as st:
        sb = lambda n, s, d=F32: st.enter_context(nc.sbuf_tensor(n, s, d))
        W = sb("s2C_w", [128, 16, 2 * D_ML + 16], BF16)
        hb = [sb("s2C_h%d" % i, [128, 16, 512], BF16) for i in range(2)]
        vo = [sb("s2C_vo%d" % i, [128, 2 * D_ML], BF16) for i in range(2)]
        gt_sb = sb("s2C_g", [128, NTILE, 16])
        ps = [st.enter_context(nc.psum_tensor("s2C_ps%d" % i, [128, 512], F32)) for i in range(4)]
        psg = st.enter_context(nc.psum_tensor("s2C_psg", [128, 16], F32))
        c0 = D_S5 + 2 * D_ML
        for kc in range(16):
            for c1, cn in ((0, 1024), (1024, 1024), (2048, 16)):
                P.dma("pool", W[:, kc, c1:c1 + cn], win[:, kc, c0 + c1:c0 + c1 + cn], w=[("W", kc, c1 // 1024)])
        hsrc = t["HXT"][1].rearrange("kc p t -> p kc t")
        for gi, (t0, gtk) in enumerate(TGROUPS):
            b = gi % 2
            P.dma(k.q(), hb[b][:, :, 0:gtk], hsrc[:, :, t0:t0 + gtk], r=[("HXT", 1, gi)], w=[("hb", b)])
            for j in range(gtk // 128):
                ti = t0 // 128 + j
                vb = ti % 2
                for nb in range(4):
                    for kc in range(16):
                        P.add("pe", lambda e, nb=nb, kc=kc, b=b, j=j: e.matmul(
                            ps[nb][:], hb[b][:, kc, j * 128:(j + 1) * 128], W[:, kc, nb * 512:(nb + 1) * 512],
                            start=(kc == 0), stop=(kc == 15)),
                            r=[("W", kc, nb // 2), ("hb", b)], w=[("ps", nb)])
                    if nb < 2:
                        P.add("dve", lambda e, nb=nb, vb=vb: e.tensor_copy(vo[vb][:, nb * 512:(nb + 1) * 512], ps[nb][:]),
                              r=[("ps", nb)], w=[("vo", vb, nb)])
                    else:
                        P.add("act", lambda e, nb=nb, vb=vb: e.activation(out=vo[vb][:, nb * 512:(nb + 1) * 512],
                                                                          in_=ps[nb][:], func=AF.Sigmoid),
                              r=[("ps", nb)], w=[("vo", vb, nb)])
                for kc in range(16):
                    P.add("pe", lambda e, kc=kc, b=b, j=j: e.matmul(
                        psg[:], hb[b][:, kc, j * 128:(j + 1) * 128], W[:, kc, 2 * D_ML:2 * D_ML + 16],
                        start=(kc == 0), stop=(kc == 15)),
                        r=[("W", kc, 2), ("hb", b)], w=["psg"])
                P.add("dve", lambda e, ti=ti: e.tensor_copy(gt_sb[:, ti, :], psg[:]), r=["psg"], w=["gt_sb"])
                P.dma(k.q(), t["V"][ti * 128:(ti + 1) * 128, :], vo[vb][:, 0:D_ML],
                      r=[("vo", vb, 0), ("vo", vb, 1)], w=[("V", ti)])
                P.dma(k.q(), t["SO"][ti * 128:(ti + 1) * 128, :], vo[vb][:, D_ML:2 * D_ML],
                      r=[("vo", vb, 2), ("vo", vb, 3)], w=[("SO", ti)])
        P.dma("sp", t["GATES"][:, :, :], gt_sb[:], r=["gt_sb"], w=["GATES"])
        P.flush()


TS5 = 16
NCH = NT // TS5
TWO_PI = 2.0 * math.pi


def emit_sincos(P, x, osin, ocos, tf, ti, tm, cpi0, rk, wk_sin, wk_cos, tkey):
    PI_ = math.pi
    for which, out, off, wk in ((0, osin, 0.0, wk_sin), (1, ocos, 0.5 * PI_, wk_cos)):
        kt = (tkey, "tf")
        P.add("dve", lambda e, off=off: e.tensor_scalar(tf, x, off, 1.0 / TWO_PI, ALU.add, ALU.mult), r=rk, w=[kt])
        P.add("dve", lambda e: e.tensor_copy(ti, tf), r=[kt], w=[(tkey, "ti")])
        P.add("dve", lambda e: e.tensor_copy(tf, ti), r=[(tkey, "ti")], w=[kt])
        P.add("dve", lambda e, off=off: e.tensor_scalar_add(tm, x, off), r=rk, w=[(tkey, "tm")])
        P.add("dve", lambda e: e.scalar_tensor_tensor(tf, tf, -TWO_PI, tm, ALU.mult, ALU.add), r=[kt, (tkey, "tm")], w=[kt])
        P.add("dve", lambda e: e.tensor_single_scalar(tm, tf, PI_, ALU.is_gt), r=[kt], w=[(tkey, "tm")])
        P.add("dve", lambda e: e.scalar_tensor_tensor(tf, tm, -TWO_PI, tf, ALU.mult, ALU.add), r=[kt, (tkey, "tm")], w=[kt])
        P.add("dve", lambda e: e.tensor_single_scalar(tm, tf, -PI_, ALU.is_lt), r=[kt], w=[(tkey, "tm")])
        P.add("dve", lambda e: e.scalar_tensor_tensor(tf, tm, TWO_PI, tf, ALU.mult, ALU.add), r=[kt, (tkey, "tm")], w=[kt])
        P.add("dve", lambda e: e.tensor_scalar(tf, tf, -3.14159, 3.14159, ALU.max, ALU.min), r=[kt], w=[kt])
        P.add("act", lambda e, out=out: e.activation(out=out, in_=tf, func=AF.Sin, bias=cpi0, scale=1.0),
              r=[kt, "cpi"], w=wk)


def declare_s5(k):
    k.din("s5p", [8, 128, 24])
    k.din("s5b", [8, 128, 2, 4, 16])
    k.din("s5c", [8, 128, 2, 4, 16])
    k.din("s5d", [8, 128, 1])
    k.din("c_jidx", [128, 17])
    k.din("c_parmask", [128, 2])
    k.din("c_blockmask", [128, 128])
    k.din("c_midx", [128, 2, NCH])
    k.din("c_rowmask", [128, 4])
    k.dscr("S5T_LAG", [8, 128, 2, 16, 128], BF16)
    k.dscr("S5T_BDT", [8, 128, 2, 16, 2, 128], BF16)
    k.dscr("S5T_MG", [8, 128, 2, 2, 16, 128], BF16)
    k.dscr("S5T_RT", [8, 128, 16])
    k.dscr("S5T_DIAG", [8, 128, 128], BF16)
    k.dscr("YG", [8, 128, L], BF16)


def stage3a(k):
    nc, P, t = k.nc, k.P, k.t
    with ExitStack() as st:
        sb = lambda n, s, d=F32: st.enter_context(nc.sbuf_tensor(n, s, d))
        jidx = sb("a_jidx", [128, 17])
        parm = sb("a_parm", [128, 2])
        bmask = sb("a_bmask", [128, 128])
        identf = sb("a_idf", [128, 128])
        cpi = sb("a_cpi", [128, 2])
        prm = sb("a_prm", [128, 24])
        bb = sb("a_bb", [128, 2, 4, 16])
        cc = sb("a_cc", [128, 2, 4, 16])
        dd = sb("a_dd", [128, 1])
        sm = sb("a_sm", [128, 12, 8])
        JL = sb("a_JL", [128, 17, 8])
        JA = sb("a_JA", [128, 17, 8])
        T1 = sb("a_T1", [128, 17, 8])
        T2 = sb("a_T2", [128, 17, 8])
        TI = sb("a_TI", [128, 17, 8], I32)
        PR = sb("a_PR", [128, 17, 8])
        PI = sb("a_PI", [128, 17, 8])
        BB = sb("a_BB", [128, 2, 8, 16])
        tb = sb("a_tb", [128, 2, 8, 16])
        Wr = sb("a_Wr", [128, 16, 8, 16])
        Wi = sb("a_Wi", [128, 16, 8, 16])
        Wt = sb("a_Wt", [128, 16, 8, 16])
        MWr = sb("a_MWr", [128, 128, 2, 16])
        MWi = sb("a_MWi", [128, 128, 2, 16])
        MC = sb("a_MC", [128, 2, 4, 2, 16])
        Gt = sb("a_Gt", [128, 16, 4, 16])
        Gt2 = sb("a_Gt2", [128, 16, 4, 16])
        MG = sb("a_MG", [128, 2, 2, 16, 128], BF16)
        LAG = sb("a_LAG", [128, 2, 16, 128], BF16)
        BDT = sb("a_BDT", [128, 2, 16, 2, 128], BF16)
        DG = sb("a_DG", [128, 128], BF16)
        RT = sb("a_RT", [128, 16])
        ps = [st.enter_context(nc.psum_tensor("a_ps%d" % i, [128, 128], F32)) for i in range(4)]

        P.dma("sp", jidx[:], t["c_jidx"][:, :], w=["jidx"])
        P.dma("sp", parm[:], t["c_parmask"][:, :], w=["parm"])
        P.dma("sp", bmask[:], t["c_blockmask"][:, :], w=["bmask"])
        P.dma("sp", identf[:], t["ident_f"][:, :], w=["identf"])
        P.add("pool", lambda e: e.memset(cpi[:], 0.0), w=["cpi"])

        def V(fn, r, w, eng="dve"):
            P.add(eng, fn, r=r, w=w)

        for blk in range(8):
            P.dma("sp", prm[:], t["s5p"][blk], w=["prm"])
            P.dma("act", bb[:], t["s5b"][blk], w=["bb"])
            P.dma("sp", cc[:], t["s5c"][blk], w=["cc"])
            P.dma("act", dd[:], t["s5d"][blk], w=["dd"])
            are, aim, ldt = prm[:, 0:8], prm[:, 8:16], prm[:, 16:24]
            dt_, lr, lrdt, ang = sm[:, 0, :], sm[:, 1, :], sm[:, 2, :], sm[:, 3, :]
            nr, den, cr, ci, tmp, tmp2 = sm[:, 4, :], sm[:, 5, :], sm[:, 6, :], sm[:, 7, :], sm[:, 8, :], sm[:, 9, :]
            V(lambda e: e.activation(out=dt_, in_=ldt, func=AF.Exp), ["prm"], ["sm0"], "act")
            V(lambda e: e.tensor_scalar_min(lr, are, -1e-4), ["prm"], ["sm1"])
            V(lambda e: e.tensor_tensor(lrdt, lr, dt_, ALU.mult), ["sm0", "sm1"], ["sm2"])
            V(lambda e: e.tensor_tensor(ang, aim, dt_, ALU.mult), ["sm0", "prm"], ["sm3"])
            b17 = lambda a: a.unsqueeze(1).broadcast_to([128, 17, 8])
            j17 = jidx[:].unsqueeze(2).broadcast_to([128, 17, 8])
            V(lambda e: e.tensor_tensor(JL[:], j17, b17(lrdt), ALU.mult), ["jidx", "sm2"], ["JL"])
            V(lambda e: e.activation(out=JL[:], in_=JL[:], func=AF.Exp), ["JL"], ["JL"], "act")
            V(lambda e: e.tensor_tensor(JA[:], j17, b17(ang), ALU.mult), ["jidx", "sm3"], ["JA"])
            f2 = lambda a: a.rearrange("p j g -> p (j g)")
            emit_sincos(P, f2(JA[:]), f2(PI[:]), f2(PR[:]), f2(T1[:]), f2(TI[:]), f2(T2[:]), cpi[:, 1:2],
                        ["JA"], ["PI"], ["PR"], "sc_a")
            V(lambda e: e.tensor_tensor(PR[:], PR[:], JL[:], ALU.mult), ["PR", "JL"], ["PR"])
            V(lambda e: e.tensor_tensor(PI[:], PI[:], JL[:], ALU.mult), ["PI", "JL"], ["PI"])
            V(lambda e: e.tensor_scalar_add(nr, PR[:, 1, :], -1.0), ["PR"], ["sm4"])
            V(lambda e: e.tensor_tensor(den, lr, lr, ALU.mult), ["sm1"], ["sm5"])
            V(lambda e: e.tensor_tensor(tmp, aim, aim, ALU.mult), ["prm"], ["sm8"])
            V(lambda e: e.tensor_tensor(den, den, tmp, ALU.add), ["sm5", "sm8"], ["sm5"])
            V(lambda e: e.reciprocal(den, den), ["sm5"], ["sm5"])
            V(lambda e: e.tensor_tensor(cr, nr, lr, ALU.mult), ["sm4", "sm1"], ["sm6"])
            V(lambda e: e.tensor_tensor(tmp, PI[:, 1, :], aim, ALU.mult), ["PI", "prm", "sm5"], ["sm8"])
            V(lambda e: e.tensor_tensor(cr, cr, tmp, ALU.add), ["sm6", "sm8"], ["sm6"])
            V(lambda e: e.tensor_tensor(cr, cr, den, ALU.mult), ["sm6", "sm5"], ["sm6"])
            V(lambda e: e.tensor_tensor(ci, PI[:, 1, :], lr, ALU.mult), ["PI", "sm1"], ["sm7"])
            V(lambda e: e.tensor_tensor(tmp2, nr, aim, ALU.mult), ["sm4", "prm"], ["sm9"])
            V(lambda e: e.tensor_tensor(ci, ci, tmp2, ALU.subtract), ["sm7", "sm9"], ["sm7"])
            V(lambda e: e.tensor_tensor(ci, ci, den, ALU.mult), ["sm7", "sm5"], ["sm7"])
            for d_ in range(2):
                crd = cr[:, d_ * 4:(d_ + 1) * 4].unsqueeze(2).broadcast_to([128, 4, 16])
                cid = ci[:, d_ * 4:(d_ + 1) * 4].unsqueeze(2).broadcast_to([128, 4, 16])
                o_r = BB[:, 0, d_ * 4:(d_ + 1) * 4, :]
                o_i = BB[:, 1, d_ * 4:(d_ + 1) * 4, :]
                t_r = tb[:, 0, d_ * 4:(d_ + 1) * 4, :]
                t_i = tb[:, 1, d_ * 4:(d_ + 1) * 4, :]
                V(lambda e, o_r=o_r, crd=crd: e.tensor_tensor(o_r, crd, bb[:, 0], ALU.mult), ["sm6", "bb"], [("BB", d_)])
                V(lambda e, t_r=t_r, cid=cid: e.tensor_tensor(t_r, cid, bb[:, 1], ALU.mult), ["sm7", "bb"], [("tb", d_)])
                V(lambda e, o_r=o_r, t_r=t_r: e.tensor_tensor(o_r, o_r, t_r, ALU.subtract), [("BB", d_), ("tb", d_)], [("BB", d_)])
                V(lambda e, o_i=o_i, crd=crd: e.tensor_tensor(o_i, crd, bb[:, 1], ALU.mult), ["sm6", "bb"], [("BBi", d_)])
                V(lambda e, t_i=t_i, cid=cid: e.tensor_tensor(t_i, cid, bb[:, 0], ALU.mult), ["sm7", "bb"], [("tbi", d_)])
                V(lambda e, o_i=o_i, t_i=t_i: e.tensor_tensor(o_i, o_i, t_i, ALU.add), [("BBi", d_), ("tbi", d_)], [("BBi", d_)])
            BBk = [("BB", 0), ("BB", 1), ("BBi", 0), ("BBi", 1)]
            pr16 = PR[:, 0:16, :].unsqueeze(3).broadcast_to([128, 16, 8, 16])
            pi16 = PI[:, 0:16, :].unsqueeze(3).broadcast_to([128, 16, 8, 16])
            bbr = BB[:, 0].unsqueeze(1).broadcast_to([128, 16, 8, 16])
            bbi = BB[:, 1].unsqueeze(1).broadcast_to([128, 16, 8, 16])
            V(lambda e: e.tensor_tensor(Wr[:], pr16, bbr, ALU.mult), ["PR"] + BBk, ["Wr"])
            V(lambda e: e.tensor_tensor(Wt[:], pi16, bbi, ALU.mult), ["PI"] + BBk, ["Wt"])
            V(lambda e: e.tensor_tensor(Wr[:], Wr[:], Wt[:], ALU.subtract), ["Wr", "Wt"], ["Wr"])
            V(lambda e: e.tensor_tensor(Wi[:], pr16, bbi, ALU.mult), ["PR"] + BBk, ["Wi"])
            V(lambda e: e.tensor_tensor(Wt[:], pi16, bbr, ALU.mult), ["PI", "Wr"] + BBk, ["Wt"])
            V(lambda e: e.tensor_tensor(Wi[:], Wi[:], Wt[:], ALU.add), ["Wi", "Wt"], ["Wi"])
            pm = parm[:].unsqueeze(1).unsqueeze(3).broadcast_to([128, 128, 2, 16])
            wrv = Wr[:].rearrange("p j g c -> p (j g) c").unsqueeze(2).broadcast_to([128, 128, 2, 16])
            wiv = Wi[:].rearrange("p j g c -> p (j g) c").unsqueeze(2).broadcast_to([128, 128, 2, 16])
            V(lambda e: e.tensor_tensor(MWr[:], wrv, pm, ALU.mult), ["Wr", "parm"], ["MWr"])
            V(lambda e: e.tensor_tensor(MWi[:], wiv, pm, ALU.mult), ["Wi", "parm"], ["MWi"], "pool")
            pm2 = parm[:].unsqueeze(1).unsqueeze(3).broadcast_to([128, 4, 2, 16])
            V(lambda e: e.tensor_tensor(MC[:, 0], cc[:, 0].unsqueeze(2).broadcast_to([128, 4, 2, 16]), pm2, ALU.mult),
              ["cc", "parm"], ["MC0"])
            V(lambda e: e.tensor_tensor(MC[:, 1], cc[:, 1].unsqueeze(2).broadcast_to([128, 4, 2, 16]), pm2, ALU.mult),
              ["cc", "parm"], ["MC1"])
            mc1f = MC[:, 1].rearrange("p q a c -> p (q a c)")
            V(lambda e: e.tensor_scalar_mul(mc1f, mc1f, -1.0), ["MC1"], ["MC1"])
            mwr = MWr[:].rearrange("p (j d q) a c -> p j d (q a c)", j=16, d=2)
            mwi = MWi[:].rearrange("p (j d q) a c -> p j d (q a c)", j=16, d=2)
            mcr = MC[:, 0].rearrange("p q a c -> p (q a c)")
            mci = MC[:, 1].rearrange("p q a c -> p (q a c)")
            n = 0
            for d_ in range(2):
                for j in range(16):
                    pa = ps[n % 4]
                    n += 1
                    P.add("pe", lambda e, pa=pa, j=j, d_=d_: e.matmul(pa[:], mwr[:, j, d_, :], mcr, start=True, stop=False),
                          r=["MWr", "MC0"], w=[("ps", id(pa))])
                    P.add("pe", lambda e, pa=pa, j=j, d_=d_: e.matmul(pa[:], mwi[:, j, d_, :], mci, start=False, stop=True),
                          r=["MWi", "MC1"], w=[("ps", id(pa))])
                    V(lambda e, pa=pa, j=j, d_=d_: e.tensor_tensor(LAG[:, d_, j, :], pa[:], bmask[:], ALU.mult),
                      [("ps", id(pa)), "bmask"], ["LAG"])
                    for ri, mw in ((0, mwr), (1, mwi)):
                        pb = ps[n % 4]
                        n += 1
                        P.add("pe", lambda e, pb=pb, mw=mw, j=j, d_=d_: e.transpose(pb[:], mw[:, j, d_, :], identf[:]),
                              r=["MWr", "MWi", "identf"], w=[("ps", id(pb))])
                        V(lambda e, pb=pb, ri=ri, j=j, d_=d_: e.copy(BDT[:, d_, j, ri, :], pb[:]),
                          [("ps", id(pb))], ["BDT"], "act")
            for d_ in range(2):
                prj = PR[:, 1:17, d_ * 4:(d_ + 1) * 4].unsqueeze(3).broadcast_to([128, 16, 4, 16])
                pij = PI[:, 1:17, d_ * 4:(d_ + 1) * 4].unsqueeze(3).broadcast_to([128, 16, 4, 16])
                crv = cc[:, 0].unsqueeze(1).broadcast_to([128, 16, 4, 16])
                civ = cc[:, 1].unsqueeze(1).broadcast_to([128, 16, 4, 16])
                pm3 = parm[:].unsqueeze(1).unsqueeze(3).broadcast_to([128, 64, 2, 16])
                V(lambda e, prj=prj, crv=crv: e.tensor_tensor(Gt[:], prj, crv, ALU.mult), ["PR", "cc"], ["Gt"])
                V(lambda e, pij=pij, civ=civ: e.tensor_tensor(Gt2[:], pij, civ, ALU.mult), ["PI", "cc"], ["Gt2"])
                V(lambda e: e.tensor_tensor(Gt[:], Gt[:], Gt2[:], ALU.subtract), ["Gt", "Gt2"], ["Gt"])
                gv = Gt[:].rearrange("p j q c -> p (j q) c").unsqueeze(2).broadcast_to([128, 64, 2, 16])
                ov = MG[:, 0, d_].rearrange("p j (q a c) -> p (j q) a c", q=4, a=2)
                V(lambda e, ov=ov, gv=gv, pm3=pm3: e.tensor_tensor(ov, gv, pm3, ALU.mult), ["Gt", "parm"], [("MG", 0, d_)])
                V(lambda e, pij=pij, crv=crv: e.tensor_tensor(Gt[:], pij, crv, ALU.mult), ["PI", "cc", ("MG", 0, d_)], ["Gt"])
                V(lambda e, prj=prj, civ=civ: e.tensor_tensor(Gt2[:], prj, civ, ALU.mult), ["PR", "cc"], ["Gt2"])
                V(lambda e: e.tensor_tensor(Gt[:], Gt[:], Gt2[:], ALU.add), ["Gt", "Gt2"], ["Gt"])
                ov2 = MG[:, 1, d_].rearrange("p j (q a c) -> p (j q) a c", q=4, a=2)
                gtf = Gt[:].rearrange("p j q c -> p (j q c)")
                V(lambda e, gtf=gtf: e.tensor_scalar_mul(gtf, gtf, -1.0), ["Gt"], ["Gt"])
                V(lambda e, ov2=ov2, gv=gv, pm3=pm3: e.tensor_tensor(ov2, gv, pm3, ALU.mult),
                  ["Gt", "parm"], [("MG", 1, d_)])
            V(lambda e: e.tensor_copy(RT[:, 0:8], JL[:, 16, :]), ["JL"], ["RT0"])
            V(lambda e: e.tensor_copy(RT[:, 8:16], JA[:, 16, :]), ["JA"], ["RT1"])
            V(lambda e: e.tensor_scalar_mul(DG[:], identf[:], dd[:, 0:1]), ["identf", "dd"], ["DG"])
            P.dma("sp", t["S5T_LAG"][blk], LAG[:], r=["LAG"], w=[("S5T", blk)])
            P.dma("act", t["S5T_BDT"][blk], BDT[:], r=["BDT"], w=[("S5T", blk)])
            P.dma("sp", t["S5T_MG"][blk], MG[:], r=[("MG", a, b) for a in range(2) for b in range(2)], w=[("S5T", blk)])
            P.dma("act", t["S5T_RT"][blk], RT[:], r=["RT0", "RT1"], w=[("S5T", blk)])
            P.dma("sp", t["S5T_DIAG"][blk], DG[:], r=["DG"], w=[("S5T", blk)])
        P.flush()


def build(upto=99, dbg=(), only=None):
    k = K(dbg)
    declare_io(k)
    declare_s5(k)
    declare_ml(k)
    declare_s5post(k)
    declare_moe(k)
    stages = [stage0, stage1, stage2, stage3a, stage3b, stage4a, stage4b, stage4c, stage5, stage6, stage7, stage8, stage9]
    for i, f in enumerate(stages):
        if (only is None and i <= upto) or (only is not None and i in only):
            f(k)
    return k


S5_BLOCKS = list(range(8))
S5_PH = {'X', 'dem', 'inter', 'lag'}
S5_CUT = 9


def stage3b(k, blocks=None):
    blocks = S5_BLOCKS if blocks is None else blocks
    nc, P, t = k.nc, k.P, k.t
    NX = L // TS5
    NC = CTX // TS5
    with ExitStack() as st:
        sb = lambda n, s, d=F32: st.enter_context(nc.sbuf_tensor(n, s, d))
        Ub = sb("b_U", [128, NT], BF16)
        LAG = sb("b_LAG", [128, 2, 16, 128], BF16)
        BDT = sb("b_BDT", [128, 2, 16, 2, 128], BF16)
        MG = sb("b_MG", [128, 2, 2, 16, 128], BF16)
        DG = sb("b_DG", [128, 128], BF16)
        RT = sb("b_RT", [128, 16])
        midx = sb("b_midx", [128, 2, NCH])
        BDQ = [sb("b_BDQ%d" % i, [128, 16, 2, 128], BF16) for i in range(2)]
        rowm = sb("b_rowm", [128, 4])
        cpi = sb("b_cpi", [128, 2])
        Xr = sb("b_Xr", [128, 4, NCH])
        Xi = sb("b_Xi", [128, 4, NCH])
        Ec = sb("b_Ec", [128, 4, NCH])
        Es = sb("b_Es", [128, 4, NCH])
        tf = sb("b_tf", [128, 4, NCH])
        tm = sb("b_tm", [128, 4, NCH])
        ti = sb("b_ti", [128, 4, NCH], I32)
        Hb = sb("b_Hb", [128, 2, 2, 4, NCH], BF16)
        YI = sb("b_YI", [128, NX, TS5])
        ys = [sb("b_ys%d" % i, [128, 512]) for i in range(2)]
        y2 = [sb("b_y2%d" % i, [128, 512]) for i in range(2)]
        yo = [sb("b_yo%d" % i, [128, 512], BF16) for i in range(2)]
        psx = [st.enter_context(nc.psum_tensor("b_psx%d" % i, [128, 512], F32)) for i in range(4)]
        psc = [st.enter_context(nc.psum_tensor("b_psc%d" % i, [128, 2, 16], F32)) for i in range(2)]
        psy = [st.enter_context(nc.psum_tensor("b_psy%d" % i, [128, 512], F32)) for i in range(2)]
        P.dma("sp", midx[:], t["c_midx"][:, :, :], w=["midx"])
        P.dma("sp", rowm[:], t["c_rowmask"][:, :], w=["rowm"])
        P.add("pool", lambda e: e.memset(cpi[:], 0.0), w=["cpi"])
        f2 = lambda a: a.rearrange("p q n -> p (q n)")
        for blk in blocks:
            P.dma("sp", Ub[:], t["U5"][blk], r=[("U5", g) for g in range(17)], w=["Ub"])
            P.dma("act", LAG[:], t["S5T_LAG"][blk], r=[("S5T", blk)], w=["LAG"])
            P.dma("sp", BDT[:], t["S5T_BDT"][blk], r=[("S5T", blk)], w=["BDT"])
            P.dma("act", MG[:], t["S5T_MG"][blk], r=[("S5T", blk)], w=["MG"])
            P.dma("sp", DG[:], t["S5T_DIAG"][blk], r=[("S5T", blk)], w=["DG"])
            P.dma("act", RT[:], t["S5T_RT"][blk], r=[("S5T", blk)], w=["RT"])
            for d_ in range(2):
                xo, co = (NC, 0) if d_ == 0 else (0, NX)
                for q in (range(4) if 'X' in S5_PH else ()):
                    pb = q % 2
                    bq = BDQ[pb]
                    P.add("dve", lambda e, bq=bq, q=q, d_=d_: e.tensor_scalar_mul(
                        bq[:].rearrange("p j r c -> p (j r c)"), BDT[:, d_].rearrange("p j r c -> p (j r c)"),
                        rowm[:, q:q + 1]), r=["BDT", "rowm"], w=[("BDQ", pb)])
                    for ri in (range(2) if S5_CUT >= 2 else ()):
                        px_ = psx[pb * 2 + ri]
                        for s in range(TS5):
                            j = (TS5 - 1 - s) if d_ == 0 else s
                            lhs = bq[:, j, ri, :]
                            P.add("pe", lambda e, px_=px_, lhs=lhs, q=q, s=s: e.matmul(
                                px_[:], lhs, Ub[:, CTX + s:NT:TS5],
                                start=(s == 0), stop=(s == TS5 - 1)),
                                r=[("BDQ", pb), "Ub"], w=[("psx", pb, ri)])
                            P.add("pe", lambda e, pb=pb, ri=ri, lhs=lhs, q=q, s=s: e.matmul(
                                psc[pb][:, ri, :], lhs, Ub[:, s:CTX:TS5],
                                start=(s == 0), stop=(s == TS5 - 1)),
                                r=[("BDQ", pb), "Ub"], w=[("psc", pb, ri)])
                        X = Xr if ri == 0 else Xi
                        if S5_CUT < 3:
                            continue
                        if S5_CUT != 4:
                            P.add("dve", lambda e, X=X, q=q, px_=px_, xo=xo: e.tensor_copy(X[:, q, xo:xo + NX], px_[:]),
                                  r=[("psx", pb, ri)], w=[("X", ri)])
                        if S5_CUT != 5:
                            P.add("dve", lambda e, X=X, q=q, pb=pb, ri=ri, co=co: e.tensor_copy(X[:, q, co:co + NC], psc[pb][:, ri, :]),
                                  r=[("psc", pb, ri)], w=[("X", ri)])
                if 'dem' not in S5_PH:
                    continue
                for q in range(4):
                    P.add("dve", lambda e, q=q, d_=d_: e.tensor_scalar_mul(Ec[:, q, :], midx[:, d_, :], RT[:, 8 + d_ * 4 + q:9 + d_ * 4 + q]),
                          r=["midx", "RT", "Hdone"], w=["ANG"])
                emit_sincos(P, f2(Ec[:]), f2(Es[:]), f2(Ec[:]), f2(tf[:]), f2(ti[:]), f2(tm[:]), cpi[:, 0:1],
                            ["ANG"], ["Es"], ["Ec", "ANG"], "sc_b")
                V = lambda fn, r, w, eng="dve": P.add(eng, fn, r=r, w=w)
                V(lambda e: e.tensor_tensor(f2(tf[:]), f2(Ec[:]), f2(Xr[:]), ALU.mult), ["Ec", ("X", 0), ("sc_b", "tf")], [("sc_b", "tf")])
                V(lambda e: e.tensor_tensor(f2(tm[:]), f2(Es[:]), f2(Xi[:]), ALU.mult), ["Es", ("X", 1), ("sc_b", "tm")], [("sc_b", "tm")], "pool")
                V(lambda e: e.tensor_tensor(f2(tf[:]), f2(tf[:]), f2(tm[:]), ALU.add), [("sc_b", "tf"), ("sc_b", "tm")], [("sc_b", "tf")])
                V(lambda e: e.tensor_tensor(f2(tm[:]), f2(Ec[:]), f2(Xi[:]), ALU.mult), ["Ec", ("X", 1), ("sc_b", "tf")], [("sc_b", "tm")])
                V(lambda e: e.tensor_tensor(f2(Xr[:]), f2(Es[:]), f2(Xr[:]), ALU.mult), ["Es", ("X", 0), ("sc_b", "tf")], [("X", 0)], "pool")
                V(lambda e: e.tensor_tensor(f2(tm[:]), f2(tm[:]), f2(Xr[:]), ALU.subtract), [("sc_b", "tm"), ("X", 0)], [("sc_b", "tm")])
                for q in range(4):
                    rq = RT[:, d_ * 4 + q:d_ * 4 + q + 1].broadcast_to([128, NCH])
                    for src, dst, kk in ((tf, Xr, 0), (tm, Xi, 1)):
                        if d_ == 0:
                            o_, i_ = dst[:, q, :], src[:, q, :]
                        else:
                            o_, i_ = dst[:, q, ::-1], src[:, q, ::-1]
                        V(lambda e, o_=o_, i_=i_, rq=rq: e.tensor_tensor_scan(o_, rq, i_, 0.0, ALU.mult, ALU.add),
                          ["RT", ("sc_b", "tf"), ("sc_b", "tm"), ("X", kk)], [("X", kk)])
                hr = Hb[:, 0, d_].rearrange("p q n -> p (q n)")
                hi = Hb[:, 1, d_].rearrange("p q n -> p (q n)")
                V(lambda e: e.tensor_tensor(f2(tf[:]), f2(Ec[:]), f2(Xr[:]), ALU.mult), ["Ec", ("X", 0), ("sc_b", "tf")], [("sc_b", "tf")])
                V(lambda e: e.tensor_tensor(f2(tm[:]), f2(Es[:]), f2(Xi[:]), ALU.mult), ["Es", ("X", 1), ("sc_b", "tm")], [("sc_b", "tm")], "pool")
                V(lambda e, hr=hr: e.tensor_tensor(hr, f2(tf[:]), f2(tm[:]), ALU.subtract), [("sc_b", "tf"), ("sc_b", "tm")], [("Hb", d_, 0)])
                V(lambda e: e.tensor_tensor(f2(tf[:]), f2(Ec[:]), f2(Xi[:]), ALU.mult), ["Ec", ("X", 1), ("Hb", d_, 0)], [("sc_b", "tf")])
                V(lambda e: e.tensor_tensor(f2(tm[:]), f2(Es[:]), f2(Xr[:]), ALU.mult), ["Es", ("X", 0), ("Hb", d_, 0)], [("sc_b", "tm")], "pool")
                V(lambda e, hi=hi: e.tensor_tensor(hi, f2(tf[:]), f2(tm[:]), ALU.add), [("sc_b", "tf"), ("sc_b", "tm")], [("Hb", d_, 1), "Hdone"])
            hbk = [("Hb", a, b) for a in range(2) for b in range(2)]
            for s in (range(TS5) if 'inter' in S5_PH else ()):
                pp = psy[s % 2]
                for q in range(4):
                    n_mm = 0
                    for d_ in range(2):
                        j = (s + 1) if d_ == 0 else (TS5 - s)
                        c0 = (NC - 1) if d_ == 0 else 1
                        for ri in range(2):
                            P.add("pe", lambda e, pp=pp, q=q, d_=d_, ri=ri, j=j, c0=c0, n_mm=n_mm: e.matmul(
                                pp[32 * q:32 * q + 32, :], MG[:, ri, d_, j - 1, 32 * q:32 * q + 32],
                                Hb[:, ri, d_, q, c0:c0 + NX], start=(n_mm == 0), stop=(n_mm == 3),
                                tile_position=(0, 32 * q)),
                                r=["MG"] + hbk, w=[("psy", s % 2)])
                            n_mm += 1
                P.add("act", lambda e, pp=pp, s=s: e.copy(YI[:, :, s], pp[:]), r=[("psy", s % 2)], w=["YI"])
            for tb in (range(16) if 'lag' in S5_PH else ()):
                pp = psy[tb % 2]
                b = tb % 2
                u0 = CTX + tb * 512
                uv = Ub[:, u0:u0 + 512].rearrange("p (n s) -> p n s", s=TS5)
                pv = pp[:].rearrange("p (n s) -> p n s", s=TS5)
                P.add("pe", lambda e, pp=pp, u0=u0: e.matmul(pp[:], DG[:], Ub[:, u0:u0 + 512], start=True, stop=False),
                      r=["DG", "Ub"], w=[("psy", b)])
                for j in range(TS5):
                    P.add("pe", lambda e, pv=pv, uv=uv, j=j: e.matmul(pv[:, :, j:TS5], LAG[:, 0, j, :], uv[:, :, 0:TS5 - j],
                                                                      start=False, stop=False),
                          r=["LAG", "Ub"], w=[("psy", b)])
                    P.add("pe", lambda e, pv=pv, uv=uv, j=j: e.matmul(pv[:, :, 0:TS5 - j], LAG[:, 1, j, :], uv[:, :, j:TS5],
                                                                      start=False, stop=(j == TS5 - 1)),
                          r=["LAG", "Ub"], w=[("psy", b)])
                if S5_CUT < 2:
                    continue
                yiv = YI[:, tb * 32:(tb + 1) * 32, :].rearrange("p n s -> p (n s)")
                P.add("dve", lambda e, pp=pp, b=b, yiv=yiv: e.tensor_tensor(ys[b][:], pp[:], yiv, ALU.add),
                      r=[("psy", b), "YI"], w=[("ys", b)])
                P.add("act", lambda e, b=b: e.activation(out=y2[b][:], in_=ys[b][:], func=AF.Square), r=[("ys", b)], w=[("y2", b)])
                P.add("dve", lambda e, b=b: e.tensor_scalar(y2[b][:], y2[b][:], 0.044715, 1.0, ALU.mult, ALU.add),
                      r=[("y2", b)], w=[("y2", b)])
                P.add("dve", lambda e, b=b: e.tensor_tensor(y2[b][:], y2[b][:], ys[b][:], ALU.mult), r=[("y2", b), ("ys", b)], w=[("y2", b)])
                P.add("act", lambda e, b=b: e.activation(out=y2[b][:], in_=y2[b][:], func=AF.Sigmoid, scale=1.5957691216),
                      r=[("y2", b)], w=[("y2", b)])
                P.add("dve", lambda e, b=b: e.tensor_tensor(yo[b][:], y2[b][:], ys[b][:], ALU.mult), r=[("y2", b), ("ys", b)], w=[("yo", b)])
                if S5_CUT < 3:
                    continue
                P.dma("sp", t["YG"][blk][:, tb * 512:(tb + 1) * 512], yo[b][:], r=[("yo", b)], w=[("YG", blk, tb)])
        P.flush()


def declare_ml(k):
    k.din("convw", [16, 128, 5])
    k.din("convb", [16, 128, 1])
    k.din("gateb", [128, 16])
    k.din("mlng", [128, D_ML])
    k.din("c_tri", [128, 128])
    k.din("c_trit", [128, 128])
    k.din("c_ones", [128, 128])
    k.dscr("QKT", [16, 128, NT], BF16)
    k.dscr("HDIR", [NH, 2, L, DH])
    k.dscr("YML", [L, D_ML], BF16)


def stage4a(k):
    nc, P, t = k.nc, k.P, k.t
    PADW = 2 + CTX + 2 + 2 + L + 2
    with ExitStack() as st:
        sb = lambda n, s, d=F32: st.enter_context(nc.sbuf_tensor(n, s, d))
        identf = sb("c_idf", [128, 128])
        pp = [sb("c_pp%d" % i, [128, PADW], BF16) for i in range(2)]
        cw = [sb("c_cw%d" % i, [128, 5]) for i in range(2)]
        cb_ = [sb("c_cb%d" % i, [128, 1]) for i in range(2)]
        dg = [sb("c_dg%d" % i, [128, 5, 128], BF16) for i in range(2)]
        of = [sb("c_of%d" % i, [128, 512]) for i in range(2)]
        ob = [sb("c_ob%d" % i, [128, NT], BF16) for i in range(2)]
        ps = [st.enter_context(nc.psum_tensor("c_ps%d" % i, [128, 512], F32)) for i in range(2)]
        P.dma("sp", identf[:], t["ident_f"][:, :], w=["identf"])
        for b in range(2):
            for (a0, a1) in ((0, 2), (2 + CTX, 2 + CTX + 4), (PADW - 2, PADW)):
                P.add("pool", lambda e, b=b, a0=a0, a1=a1: e.memset(pp[b][:, a0:a1], 0.0), w=[("pad", b)])
        segs = [(0, CTX, 2)] + [(CTX + 512 * i, 512, 2 + CTX + 4 + 512 * i) for i in range(16)]
        for cb in range(16):
            b = cb % 2
            P.dma("sp", pp[b][:, 2:2 + CTX], t["QKPRE"][cb][:, 0:CTX], r=[("QKPRE", 0), ("pad", b)], w=[("pp", b)])
            P.dma("act", pp[b][:, 2 + CTX + 4:2 + CTX + 4 + L], t["QKPRE"][cb][:, CTX:NT],
                  r=[("QKPRE", g) for g in range(1, 17)] + [("pad", b)], w=[("pp", b)])
            P.dma("sp", cw[b][:], t["convw"][cb], w=[("cw", b)])
            P.dma("sp", cb_[b][:], t["convb"][cb], w=[("cb", b)])
            for kk in range(5):
                P.add("dve", lambda e, b=b, kk=kk: e.tensor_scalar_mul(dg[b][:, kk, :], identf[:], cw[b][:, kk:kk + 1]),
                      r=["identf", ("cw", b)], w=[("dg", b)])
            for si, (t0, n, po) in enumerate(segs):
                pb = si % 2
                for kk in range(5):
                    P.add("pe", lambda e, pb=pb, b=b, kk=kk, po=po, n=n: e.matmul(
                        ps[pb][:, 0:n], dg[b][:, kk, :], pp[b][:, po + kk - 2:po + kk - 2 + n], start=(kk == 0), stop=(kk == 4)),
                        r=[("dg", b), ("pp", b)], w=[("ps", pb)])
                if cb < 8:
                    P.add("act", lambda e, pb=pb, b=b, n=n: e.activation(out=of[pb][:, 0:n], in_=ps[pb][:, 0:n], func=AF.Silu,
                                                                         bias=cb_[b][:, 0:1], scale=1.0),
                          r=[("ps", pb), ("cb", b)], w=[("of", pb)])
                    P.add("dve", lambda e, pb=pb, b=b, n=n, t0=t0: e.tensor_scalar_mul(ob[b][:, t0:t0 + n], of[pb][:, 0:n], 1.0 / 16.0),
                          r=[("of", pb)], w=[("ob", b)])
                else:
                    P.add("act", lambda e, pb=pb, b=b, n=n, t0=t0: e.activation(out=ob[b][:, t0:t0 + n], in_=ps[pb][:, 0:n], func=AF.Silu,
                                                                               bias=cb_[b][:, 0:1], scale=1.0),
                          r=[("ps", pb), ("cb", b)], w=[("ob", b)])
            P.dma(k.q(), t["QKT"][cb], ob[b][:], r=[("ob", b)], w=[("QKT", cb)])
        P.flush()


def stage4b(k):
    nc, P, t = k.nc, k.P, k.t
    NG_ = NTILE * NH
    with ExitStack() as st:
        sb = lambda n, s, d=F32: st.enter_context(nc.sbuf_tensor(n, s, d))
        G = sb("m_G", [128, NTILE, 16])
        gb = sb("m_gb", [128, 16])
        tri = sb("m_tri", [128, 128])
        trit = sb("m_trit", [128, 128])
        ones = sb("m_ones", [128, 128])
        trib = sb("m_trib", [128, 2, 128], BF16)
        identb = sb("m_idb", [128, 128], BF16)
        one1 = sb("m_one1", [128, 1])
        LF = sb("m_LF", [128, 2, NTILE, NH])
        SC = sb("m_SC", [128, 2, 4, NTILE, NH])
        tmpg = sb("m_tmpg", [128, NTILE, NH])
        pso_full = [st.enter_context(nc.psum_tensor("m_pso%d" % i, [128, NG_], F32)) for i in range(2)]
        psg = pso_full
        P.dma("sp", G[:], t["GATES"][:, :, :], r=["GATES"], w=["G"])
        P.dma("sp", gb[:], t["gateb"][:, :], w=["gb"])
        P.dma("act", tri[:], t["c_tri"][:, :], w=["tri"])
        P.dma("act", trit[:], t["c_trit"][:, :], w=["trit"])
        P.dma("sp", ones[:], t["c_ones"][:, :], w=["ones"])
        P.dma("sp", identb[:], t["ident_bf"][:, :], w=["identb"])
        P.add("pool", lambda e: e.memset(one1[:], 1.0), w=["one1"])
        P.add("dve", lambda e: e.tensor_copy(trib[:, 0, :], tri[:]), r=["tri"], w=["trib"])
        P.add("dve", lambda e: e.tensor_copy(trib[:, 1, :], trit[:]), r=["trit"], w=["trib"])
        P.add("dve", lambda e: e.tensor_tensor(G[:], G[:], gb[:].unsqueeze(1).broadcast_to([128, NTILE, 16]), ALU.add),
              r=["G", "gb"], w=["G"])
        for d_ in range(2):
            ipre = G[:, :, 8 * d_:8 * d_ + 4]
            fpre = G[:, :, 8 * d_ + 4:8 * d_ + 8]
            lf = LF[:, d_]
            P.add("act", lambda e, lf=lf, fpre=fpre: e.activation(out=lf, in_=fpre, func=AF.Exp, scale=-1.0), r=["G"], w=[("LF", d_)])
            P.add("act", lambda e, lf=lf: e.activation(out=lf, in_=lf, func=AF.Ln, bias=one1[:, 0:1], scale=1.0),
                  r=[("LF", d_), "one1"], w=[("LF", d_)])
            lff = lf.rearrange("p n h -> p (n h)")
            P.add("dve", lambda e, lff=lff: e.tensor_scalar_mul(lff, lff, -1.0), r=[("LF", d_)], w=[("LF", d_)])
            m_ = tri if d_ == 0 else trit
            P.add("pe", lambda e, m_=m_, lff=lff: e.matmul(psg[0][:], m_[:], lff, start=True, stop=True),
                  r=["tri", "trit", ("LF", d_)], w=["psg0"])
            P.add("pe", lambda e, lff=lff: e.matmul(psg[1][:], ones[:], lff, start=True, stop=True),
                  r=["ones", ("LF", d_)], w=["psg1"])
            u_ = SC[:, d_, 0].rearrange("p n h -> p (n h)")
            fl_ = SC[:, d_, 1].rearrange("p n h -> p (n h)")
            dec_ = SC[:, d_, 2].rearrange("p n h -> p (n h)")
            wt_ = SC[:, d_, 3].rearrange("p n h -> p (n h)")
            tg = tmpg[:].rearrange("p n h -> p (n h)")
            P.add("dve", lambda e, ipre=ipre: e.tensor_copy(tmpg[:], ipre), r=["G"], w=["tmpg"])
            P.add("dve", lambda e, tg=tg: e.tensor_tensor(tg, tg, psg[0][:], ALU.subtract), r=["tmpg", "psg0"], w=["tmpg"])
            P.add("act", lambda e, u_=u_, tg=tg: e.activation(out=u_, in_=tg, func=AF.Exp), r=["tmpg"], w=[("SC", d_)])
            P.add("act", lambda e, fl_=fl_: e.activation(out=fl_, in_=psg[0][:], func=AF.Exp, scale=-1.0), r=["psg0"], w=[("SC", d_)])
            P.add("act", lambda e, dec_=dec_: e.activation(out=dec_, in_=psg[1][:], func=AF.Exp), r=["psg1"], w=[("SC", d_)])
            P.add("dve", lambda e, wt_=wt_, u_=u_, dec_=dec_: e.tensor_tensor(wt_, u_, dec_, ALU.mult), r=[("SC", d_)], w=[("SC", d_)])
        QT = sb("m_QT", [128, 2, NT], BF16)
        KT = sb("m_KT", [128, 2, NT], BF16)
        Vh = sb("m_Vh", [128, NTILE, DH + 1], BF16)
        CT = [sb("m_CT%d" % i, [128, 2, DH + 1]) for i in range(2)]
        CTb = [sb("m_CTb%d" % i, [128, 2, DH + 1], BF16) for i in range(2)]
        kt = [sb("m_kt%d" % i, [128, DH], BF16) for i in range(2)]
        SW = [sb("m_SW%d" % i, [128, 128], BF16) for i in range(2)]
        vw = [sb("m_vw%d" % i, [128, DH + 1], BF16) for i in range(2)]
        dn = [sb("m_dn%d" % i, [128, 2]) for i in range(2)]
        ho = [sb("m_ho%d" % i, [128, DH]) for i in range(2)]
        pst = [st.enter_context(nc.psum_tensor("m_pst%d" % i, [128, DH], BF16)) for i in range(2)]
        pss = [st.enter_context(nc.psum_tensor("m_pss%d" % i, [128, 128], F32)) for i in range(2)]
        pso = [pso_full[i][:, 0:DH + 1] for i in range(2)]
        psc_ = [st.enter_context(nc.psum_tensor("m_psc%d" % i, [128, DH + 1], F32)) for i in range(2)]
        for h in range(NH):
            for dc in range(2):
                P.dma("sp", QT[:, dc, :], t["QKT"][2 * h + dc], r=[("QKT", 2 * h + dc)], w=["QT"])
                P.dma("act", KT[:, dc, :], t["QKT"][8 + 2 * h + dc], r=[("QKT", 8 + 2 * h + dc)], w=["KT"])
            for n0 in range(0, NTILE, 11):
                P.dma(k.q(), Vh[:, n0:n0 + 11, 0:DH],
                      t["V"].rearrange("(n p) c -> p n c", p=128)[:, n0:n0 + 11, h * DH:(h + 1) * DH],
                      r=[("V", ti) for ti in range(n0, n0 + 11)], w=["Vh"])
            P.add("pool", lambda e: e.memset(Vh[:, :, DH:DH + 1], 1.0), w=["Vh1"])
            for d_ in range(2):
                P.add("pool", lambda e, d_=d_: e.memset(CT[d_][:], 0.0), w=[("CT", d_)])
                P.add("pool", lambda e, d_=d_: e.memset(CTb[d_][:], 0.0), w=[("CTb", d_)])
            order_f = list(range(NTILE))
            order_b = [1, 0] + list(range(NTILE - 1, 1, -1))
            for step in range(NTILE):
                for d_ in range(2):
                    ti = order_f[step] if d_ == 0 else order_b[step]
                    c0 = ti * 128
                    col = ti * NH + h
                    sc = lambda kind, d_=d_, col=col: SC[:, d_, kind].rearrange("p n h -> p (n h)")[:, col:col + 1]
                    for dc in range(2):
                        P.add("pe", lambda e, d_=d_, dc=dc, c0=c0: e.transpose(pst[d_][:, dc * 128:(dc + 1) * 128],
                                                                               KT[:, dc, c0:c0 + 128], identb[:]),
                              r=["KT", "identb"], w=[("pst", d_)])
                    P.add("act", lambda e, d_=d_: e.copy(kt[d_][:], pst[d_][:]), r=[("pst", d_)], w=[("kt", d_)])
                    if ti >= 2:
                        for dc in range(2):
                            P.add("pe", lambda e, d_=d_, dc=dc, c0=c0: e.matmul(pss[d_][:], KT[:, dc, c0:c0 + 128], QT[:, dc, c0:c0 + 128],
                                                                                start=(dc == 0), stop=(dc == 1)),
                                  r=["KT", "QT"], w=[("pss", d_)])
                        P.add("dve", lambda e, d_=d_, sc=sc: e.scalar_tensor_tensor(SW[d_][:], pss[d_][:], sc(0), trib[:, d_, :],
                                                                                    ALU.mult, ALU.mult),
                              r=[("pss", d_), ("SC", d_), "trib"], w=[("SW", d_)])
                        P.add("pe", lambda e, d_=d_, ti=ti: e.matmul(pso[d_][:], SW[d_][:], Vh[:, ti, :], start=True, stop=False),
                              r=[("SW", d_), "Vh", "Vh1"], w=[("pso", d_), "psg%d" % d_])
                        for dc in range(2):
                            P.add("pe", lambda e, d_=d_, dc=dc, c0=c0: e.matmul(pso[d_][:], QT[:, dc, c0:c0 + 128], CTb[d_][:, dc, :],
                                                                                start=False, stop=(dc == 1)),
                                  r=["QT", ("CTb", d_)], w=[("pso", d_)])
                        P.add("act", lambda e, d_=d_: e.activation(out=dn[d_][:, 0:1], in_=pso[d_][:, DH:DH + 1], func=AF.Abs),
                              r=[("pso", d_)], w=[("dn", d_)])
                        P.add("dve", lambda e, d_=d_, sc=sc: e.tensor_tensor(dn[d_][:, 0:1], dn[d_][:, 0:1], sc(1), ALU.max),
                              r=[("dn", d_), ("SC", d_)], w=[("dn", d_)])
                        P.add("dve", lambda e, d_=d_: e.reciprocal(dn[d_][:, 1:2], dn[d_][:, 0:1]), r=[("dn", d_)], w=[("dn1", d_)])
                        P.add("act", lambda e, d_=d_: e.activation(out=ho[d_][:], in_=pso[d_][:, 0:DH], func=AF.Copy,
                                                                   scale=dn[d_][:, 1:2]),
                              r=[("pso", d_), ("dn1", d_)], w=[("ho", d_)])
                        P.dma("sp", t["HDIR"][h, d_, (ti - 2) * 128:(ti - 1) * 128, :], ho[d_][:], r=[("ho", d_)],
                              w=[("HDIR", h, d_, ti)])
                    P.add("dve", lambda e, d_=d_, ti=ti, sc=sc: e.tensor_scalar_mul(vw[d_][:], Vh[:, ti, :], sc(3)),
                          r=["Vh", "Vh1", ("SC", d_)], w=[("vw", d_)])
                    for dc in range(2):
                        pc = psc_[dc]
                        P.add("pe", lambda e, d_=d_, dc=dc, pc=pc: e.matmul(pc[:], kt[d_][:, dc * 128:(dc + 1) * 128], vw[d_][:],
                                                                            start=True, stop=True),
                              r=[("kt", d_), ("vw", d_)], w=[("psc", dc)])
                        P.add("dve", lambda e, d_=d_, dc=dc, pc=pc, sc=sc: e.scalar_tensor_tensor(
                            CT[d_][:, dc, :], CT[d_][:, dc, :], sc(2), pc[:], ALU.mult, ALU.add),
                            r=[("psc", dc), ("CT", d_), ("SC", d_)], w=[("CT", d_)])
                        P.add("act", lambda e, d_=d_, dc=dc: e.copy(CTb[d_][:, dc, :], CT[d_][:, dc, :]),
                              r=[("CT", d_)], w=[("CTb", d_)])
        P.flush()


def stage4c(k):
    nc, P, t = k.nc, k.P, k.t
    with ExitStack() as st:
        sb = lambda n, s, d=F32: st.enter_context(nc.sbuf_tensor(n, s, d))
        ng = sb("n_ng", [128, D_ML])
        epst = sb("n_eps", [128, 1])
        hf = [sb("n_hf%d" % i, [128, NH, DH]) for i in range(2)]
        hb = [sb("n_hb%d" % i, [128, NH, DH]) for i in range(2)]
        so = [sb("n_so%d" % i, [128, D_ML], BF16) for i in range(2)]
        junk = sb("n_junk", [128, DH])
        ss = [sb("n_ss%d" % i, [128, 2, NH]) for i in range(2)]
        yo = [sb("n_yo%d" % i, [128, D_ML], BF16) for i in range(2)]
        P.dma("sp", ng[:], t["mlng"][:, :], w=["ng"])
        P.add("pool", lambda e: e.memset(epst[:], EPS), w=["epst"])
        for i in range(L // 128):
            b = i % 2
            P.dma("sp", hf[b][:], t["HDIR"][:, 0, i * 128:(i + 1) * 128, :].rearrange("h p d -> p h d"),
                  r=[("HDIR", h, 0, i + 2) for h in range(NH)], w=[("hf", b)])
            P.dma("act", hb[b][:], t["HDIR"][:, 1, i * 128:(i + 1) * 128, :].rearrange("h p d -> p h d"),
                  r=[("HDIR", h, 1, i + 2) for h in range(NH)], w=[("hb", b)])
            P.dma("sp", so[b][:], t["SO"][(i + 2) * 128:(i + 3) * 128, :], r=[("SO", i + 2)], w=[("so", b)])
            hff = hf[b][:].rearrange("p h d -> p (h d)")
            hbf = hb[b][:].rearrange("p h d -> p (h d)")
            P.add("dve", lambda e, hff=hff, hbf=hbf: e.tensor_tensor(hff, hff, hbf, ALU.add), r=[("hf", b), ("hb", b)], w=[("hf", b)])
            for h in range(NH):
                P.add("act", lambda e, b=b, h=h: e.activation(out=junk[:], in_=hf[b][:, h, :], func=AF.Square, scale=float(DH ** -0.5),
                                                              accum_out=ss[b][:, 0, h:h + 1]),
                      r=[("hf", b)], w=["junk", ("ss", b)])
            P.add("act", lambda e, b=b: e.activation(out=ss[b][:, 1, :], in_=ss[b][:, 0, :], func=AF.Sqrt, bias=epst[:, 0:1], scale=1.0),
                  r=[("ss", b), "epst"], w=[("ss1", b)])
            P.add("dve", lambda e, b=b: e.reciprocal(ss[b][:, 1, :], ss[b][:, 1, :]), r=[("ss1", b)], w=[("ss1", b)])
            for h in range(NH):
                P.add("dve", lambda e, b=b, h=h: e.scalar_tensor_tensor(hf[b][:, h, :], hf[b][:, h, :], ss[b][:, 1, h:h + 1],
                                                                        ng[:, h * DH:(h + 1) * DH], ALU.mult, ALU.mult),
                      r=[("hf", b), ("ss1", b), "ng"], w=[("hf", b)])
            P.add("pool", lambda e, b=b, hff=hff: e.tensor_tensor(yo[b][:], hff, so[b][:], ALU.mult), r=[("hf", b), ("so", b)], w=[("yo", b)])
            P.dma("act", t["YML"][i * 128:(i + 1) * 128, :], yo[b][:], r=[("yo", b)], w=[("YML", i)])
        P.flush()


def declare_s5post(k):
    k.din("gluw", [D_S5, D_S5])
    k.din("glub", [128, 8])
    k.din("w_out", [D, D])
    k.din("rw", [128, 16, NE])
    k.dscr("HX2", [L, D], BF16)
    k.dscr("X1", [L, D])
    k.dscr("AFFD", [128, L // 128, NE])


def stage5(k):
    nc, P, t = k.nc, k.P, k.t
    with ExitStack() as st:
        sb = lambda n, s, d=F32: st.enter_context(nc.sbuf_tensor(n, s, d))
        Wg = sb("p_Wg", [128, 8, D_S5], BF16)
        Wo = sb("p_Wo", [128, 16, D], BF16)
        M2 = [sb("p_M%d" % i, [128, D]) for i in range(3)]
        RW = sb("p_RW", [128, 16, NE])
        glub = sb("p_glub", [128, 8])
        identb = sb("p_idb", [128, 128], BF16)
        identf = sb("p_idf", [128, 128])
        epst = sb("p_eps", [128, 1])
        ygT = [sb("p_yg%d" % i, [128, 8, 512], BF16) for i in range(2)]
        sig = sb("p_sig", [128, 512], BF16)
        yglu = sb("p_yglu", [128, 8, 512], BF16)
        yml = [sb("p_yml%d" % i, [128, D_ML], BF16) for i in range(2)]
        ymlT = [sb("p_ymlT%d" % i, [128, 8, 128], BF16) for i in range(2)]
        yx = sb("p_yx", [128, D])
        xt = sb("p_xt", [128, D])
        x1 = sb("p_x1", [128, D])
        hx2 = sb("p_hx2", [128, D])
        hx2b = sb("p_hx2b", [128, D], BF16)
        junk = sb("p_junk", [128, D], BF16)
        hx2T = sb("p_hx2T", [128, 16, 128])
        ss = sb("p_ss", [128, 8])
        AFF = sb("p_AFF", [128, L // 128, NE])
        pw = [st.enter_context(nc.psum_tensor("p_pw%d" % i, [128, 512], F32)) for i in range(4)]
        pz = [st.enter_context(nc.psum_tensor("p_pz%d" % i, [128, 512], F32)) for i in range(2)]
        pt = st.enter_context(nc.psum_tensor("p_pt", [128, 8, 128], BF16))
        pl = st.enter_context(nc.psum_tensor("p_pl", [128, NE], F32))
        gw = t["gluw"].rearrange("(c p) n -> p c n", p=128)
        for c in range(8):
            P.dma("pool", Wg[:, c, :], gw[:, c, :], w=[("Wg", c)])
        wo = t["w_out"].rearrange("(c p) n -> p c n", p=128)
        for c in range(16):
            for h_ in range(2):
                P.dma("pool", Wo[:, c, h_ * 1024:(h_ + 1) * 1024], wo[:, c, h_ * 1024:(h_ + 1) * 1024], w=[("Wo", c, h_)])
        for i in range(3):
            P.dma("sp", M2[i][:], t["MODS2"][i], r=["M2_%d" % i], w=[("M2", i)])
        P.dma("sp", RW[:], t["rw"][:, :, :], w=["RW"])
        P.dma("sp", glub[:], t["glub"][:, :], w=["glub"])
        P.dma("act", identb[:], t["ident_bf"][:, :], w=["identb"])
        P.dma("act", identf[:], t["ident_f"][:, :], w=["identf"])
        P.add("pool", lambda e: e.memset(epst[:], EPS), w=["epst"])
        ygsrc = t["YG"].rearrange("c p t -> p c t")
        ymlsrc = t["YML"].rearrange("(w r) c -> r w c", r=128)
        ymlkeys = [("YML", i) for i in range(L // 128)]
        for g5 in range(16):
            gb_ = g5 % 2
            P.dma(k.q(), ygT[gb_][:], ygsrc[:, :, g5 * 512:(g5 + 1) * 512], r=[("YG", blk, g5) for blk in range(8)], w=[("ygT", gb_)])
            for c2 in range(8):
                pzz = pz[c2 % 2]
                for c in range(8):
                    P.add("pe", lambda e, pzz=pzz, c=c, c2=c2, gb_=gb_: e.matmul(pzz[:], Wg[:, c, c2 * 128:(c2 + 1) * 128], ygT[gb_][:, c, :],
                                                                                 start=(c == 0), stop=(c == 7)),
                          r=[("Wg", c), ("ygT", gb_)], w=[("pz", c2 % 2)])
                P.add("act", lambda e, pzz=pzz, c2=c2: e.activation(out=sig[:], in_=pzz[:], func=AF.Sigmoid, bias=glub[:, c2:c2 + 1], scale=1.0),
                      r=[("pz", c2 % 2), "glub"], w=["sig"])
                P.add("dve", lambda e, c2=c2, gb_=gb_: e.tensor_tensor(yglu[:, c2, :], ygT[gb_][:, c2, :], sig[:], ALU.mult),
                      r=["sig", ("ygT", gb_)], w=["yglu"])
            for j in range(4):
                ti = g5 * 4 + j
                tb_ = ti % 2
                r0 = ti * 2
                for a in range(2):
                    P.dma(k.q(), yml[tb_][64 * a:64 * (a + 1), :], ymlsrc[r0 + a], r=ymlkeys, w=[("yml", tb_)])
                for c in range(8):
                    P.add("pe", lambda e, c=c, tb_=tb_: e.transpose(pt[:, c, :], yml[tb_][:, c * 128:(c + 1) * 128], identb[:]),
                          r=[("yml", tb_), "identb"], w=["pt"])
                P.add("act", lambda e, tb_=tb_: e.copy(ymlT[tb_][:], pt[:]), r=["pt"], w=[("ymlT", tb_)])
                for nb in range(4):
                    for c in range(16):
                        lhs = yglu[:, c, j * 128:(j + 1) * 128] if c < 8 else ymlT[tb_][:, c - 8, :]
                        P.add("pe", lambda e, nb=nb, c=c, lhs=lhs: e.matmul(pw[nb][:], lhs, Wo[:, c, nb * 512:(nb + 1) * 512],
                                                                            start=(c == 0), stop=(c == 15)),
                              r=["yglu", ("ymlT", tb_), ("Wo", c, nb // 2)], w=[("pw", nb)])
                    if nb % 2 == 0:
                        P.add("act", lambda e, nb=nb: e.copy(yx[:, nb * 512:(nb + 1) * 512], pw[nb][:]), r=[("pw", nb)], w=[("yx", nb)])
                    else:
                        P.add("dve", lambda e, nb=nb: e.tensor_copy(yx[:, nb * 512:(nb + 1) * 512], pw[nb][:]), r=[("pw", nb)], w=[("yx", nb)])
                yxk = [("yx", nb) for nb in range(4)]
                P.dma("sp", xt[:], t["x"][ti * 128:(ti + 1) * 128, :], w=["xt"])
                P.add("act", lambda e: e.activation(out=junk[:], in_=yx[:], func=AF.Square, scale=float(D ** -0.5), accum_out=ss[:, 0:1]),
                      r=yxk, w=["junk", "ss0"])
                P.add("act", lambda e: e.activation(out=ss[:, 1:2], in_=ss[:, 0:1], func=AF.Sqrt, bias=epst[:, 0:1], scale=1.0),
                      r=["ss0", "epst"], w=["ss1"])
                P.add("dve", lambda e: e.reciprocal(ss[:, 1:2], ss[:, 1:2]), r=["ss1"], w=["ss1"])
                P.add("dve", lambda e: e.scalar_tensor_tensor(x1[:], yx[:], ss[:, 1:2], M2[0][:], ALU.mult, ALU.mult),
                      r=yxk + ["ss1", ("M2", 0)], w=["x1"])
                P.add("pool", lambda e: e.tensor_tensor(x1[:], x1[:], xt[:], ALU.add), r=["x1", "xt"], w=["x1"])
                P.dma("act", t["X1"][ti * 128:(ti + 1) * 128, :], x1[:], r=["x1"], w=[("X1", ti)])
                P.add("act", lambda e: e.activation(out=junk[:], in_=x1[:], func=AF.Square, scale=float(D ** -0.5), accum_out=ss[:, 2:3]),
                      r=["x1"], w=["junk", "ss2"])
                P.add("act", lambda e: e.activation(out=ss[:, 3:4], in_=ss[:, 2:3], func=AF.Sqrt, bias=epst[:, 0:1], scale=1.0),
                      r=["ss2", "epst"], w=["ss3"])
                P.add("dve", lambda e: e.reciprocal(ss[:, 3:4], ss[:, 3:4]), r=["ss3"], w=["ss3"])
                P.add("dve", lambda e: e.scalar_tensor_tensor(hx2[:], x1[:], ss[:, 3:4], M2[1][:], ALU.mult, ALU.mult),
                      r=["x1", "ss3", ("M2", 1)], w=["hx2"])
                P.add("pool", lambda e: e.tensor_tensor(hx2[:], hx2[:], M2[2][:], ALU.add), r=["hx2", ("M2", 2)], w=["hx2"])
                P.add("act", lambda e: e.copy(hx2b[:], hx2[:]), r=["hx2"], w=["hx2b"])
                P.dma("sp", t["HX2"][ti * 128:(ti + 1) * 128, :], hx2b[:], r=["hx2b"], w=[("HX2", ti)])
                for g4 in range(4):
                    pzz = pz[g4 % 2]
                    for c in range(4):
                        kc = g4 * 4 + c
                        P.add("pe", lambda e, pzz=pzz, c=c, kc=kc: e.transpose(pzz[:, c * 128:(c + 1) * 128], hx2[:, kc * 128:(kc + 1) * 128], identf[:]),
                              r=["hx2", "identf"], w=[("pz", g4 % 2)])
                    P.add("dve", lambda e, pzz=pzz, g4=g4: e.tensor_copy(hx2T[:, g4 * 4:(g4 + 1) * 4, :].rearrange("p c t -> p (c t)"), pzz[:]),
                          r=[("pz", g4 % 2)], w=[("hx2T", g4)])
                for kc in range(16):
                    P.add("pe", lambda e, kc=kc: e.matmul(pl[:], hx2T[:, kc, :], RW[:, kc, :], start=(kc == 0), stop=(kc == 15)),
                          r=[("hx2T", kc // 4), "RW"], w=["pl"])
                P.add("dve", lambda e: e.tensor_reduce(ss[:, 4:5], pl[:], AX.X, ALU.max), r=["pl"], w=["ss4"])
                P.add("dve", lambda e: e.tensor_scalar_mul(ss[:, 4:5], ss[:, 4:5], -1.0), r=["ss4"], w=["ss4"])
                P.add("act", lambda e, ti=ti: e.activation(out=AFF[:, ti, :], in_=pl[:], func=AF.Exp, bias=ss[:, 4:5], scale=1.0,
                                                           accum_out=ss[:, 5:6]),
                      r=["pl", "ss4"], w=["AFF", "ss5"])
                P.add("dve", lambda e: e.reciprocal(ss[:, 5:6], ss[:, 5:6]), r=["ss5"], w=["ss5"])
                P.add("dve", lambda e, ti=ti: e.tensor_scalar_mul(AFF[:, ti, :], AFF[:, ti, :], ss[:, 5:6]), r=["AFF", "ss5"], w=["AFF"])
        P.dma("sp", t["AFFD"][:, :, :], AFF[:], r=["AFF"], w=["AFFD"])
        P.flush()


def declare_moe(k):
    k.din("ewg", [NE, D, FF])
    k.din("ewu", [NE, D, FF])
    k.din("ewd", [NE, FF, D])
    k.din("ownidx", [128, OWN // 128], I32)
    k.din("c_slt", [128, 128], BF16)
    k.din("c_onesb", [128, 128], BF16)
    k.din("c_iota", [128, CAPL])
    k.dscr("AFFT", [L, NE])
    k.dscr("TH", [128, NE])
    k.dscr("YGALL", [NE, 3, 128, D], BF16)
    k.dscr("OHTALL", [OWN // 128, 128, NE * 3, 128], BF16)
    k.dscr("MOEO", [OWN, D])


def stage6(k):
    nc, P, t = k.nc, k.P, k.t
    NTL = L // 128
    with ExitStack() as st:
        sb = lambda n, s, d=F32: st.enter_context(nc.sbuf_tensor(n, s, d))
        A = sb("t_A", [128, NTL, NE])
        cmp_ = sb("t_cmp", [128, NTL, NE], BF16)
        onesb = sb("t_onesb", [128, 128])
        v = sb("t_v", [128, 8, NE])
        cntp = sb("t_cntp", [128, NE])
        pc = st.enter_context(nc.psum_tensor("t_pc", [128, NE], F32))
        P.dma("sp", A[:], t["AFFD"][:, :, :], r=["AFFD"], w=["A"])
        P.dma("act", t["AFFT"].rearrange("(n p) e -> p n e", p=128), A[:], r=["A"], w=["AFFT"])
        P.dma("sp", onesb[:], t["c_ones"][:, :], w=["onesb"])
        P.add("pool", lambda e: e.memset(v[:, 0, :], 0.0), w=["lo"])
        P.add("pool", lambda e: e.memset(v[:, 1, :], 1.0), w=["hi"])
        lo, hi, th, ge, d1, d2 = (v[:, i, :] for i in range(6))
        for it in range(30):
            P.add("dve", lambda e: e.tensor_tensor(th, lo, hi, ALU.add), r=["lo", "hi"], w=["th"])
            P.add("dve", lambda e: e.tensor_scalar_mul(th, th, 0.5), r=["th"], w=["th"])
            P.add("dve", lambda e: e.tensor_tensor(cmp_[:], A[:], th.unsqueeze(1).broadcast_to([128, NTL, NE]), ALU.is_ge),
                  r=["A", "th"], w=["cmp"])
            P.add("dve", lambda e: e.tensor_reduce(cntp[:], cmp_[:].rearrange("p n e -> p e n"), AX.X, ALU.add), r=["cmp"], w=["cntp"])
            P.add("pe", lambda e: e.matmul(pc[:], onesb[:], cntp[:], start=True, stop=True), r=["onesb", "cntp"], w=["pc"])
            P.add("dve", lambda e: e.tensor_single_scalar(ge, pc[:], float(CAPE) - 0.5, ALU.is_gt), r=["pc"], w=["ge"])
            P.add("dve", lambda e: e.tensor_tensor(d1, th, lo, ALU.subtract), r=["th", "lo"], w=["d1"])
            P.add("dve", lambda e: e.tensor_tensor(d1, d1, ge, ALU.mult), r=["d1", "ge"], w=["d1"])
            P.add("dve", lambda e: e.tensor_tensor(d2, hi, th, ALU.subtract), r=["th", "hi"], w=["d2"])
            P.add("dve", lambda e: e.tensor_tensor(d2, d2, ge, ALU.mult), r=["d2", "ge"], w=["d2"])
            P.add("dve", lambda e: e.tensor_tensor(lo, lo, d1, ALU.add), r=["lo", "d1"], w=["lo"])
            P.add("dve", lambda e: e.tensor_tensor(hi, th, d2, ALU.add), r=["th", "d2"], w=["hi"])
        P.dma("sp", t["TH"][:, :], lo, r=["lo"], w=["TH"])
        P.flush()


def stage7(k):
    nc, P, t = k.nc, k.P, k.t
    NO = OWN // 128
    with ExitStack() as st:
        sb = lambda n, s, d=F32: st.enter_context(nc.sbuf_tensor(n, s, d))
        oidx = sb("e_oidx", [128, NO], I32)
        HX = sb("e_HX", [128, NO, D], BF16)
        Ao = sb("e_Ao", [128, NO, NE])
        th = sb("e_th", [128, NE])
        sel = sb("e_sel", [128, NO, NE])
        selb = sb("e_selb", [128, NO, NE], BF16)
        slot = sb("e_slot", [128, NO, NE])
        cum = sb("e_cum", [128, NO, NE])
        tot = sb("e_tot", [128, NO, NE])
        AHL = sb("e_AHL", [128, NO, NE, 2], BF16)
        ares = sb("e_ares", [128, NO, NE])
        slt = sb("e_slt", [128, 128], BF16)
        onesb = sb("e_onesb", [128, 128], BF16)
        identb = sb("e_idb", [128, 128], BF16)
        iota = sb("e_iota", [128, CAPL])
        onef = sb("e_onef", [128, 1])
        OH = sb("e_OH", [128, NO, CAPL], BF16)
        XT = sb("e_XT", [128, 16, CAPL], BF16)
        HT = sb("e_HT", [128, 16, CAPL], BF16)
        Wg = [sb("e_Wg%d" % i, [128, 16, 256], BF16) for i in range(2)]
        Wu = [sb("e_Wu%d" % i, [128, 16, 256], BF16) for i in range(2)]
        Wd = [sb("e_Wd%d" % i, [128, 16, 512], BF16) for i in range(2)]
        asb = sb("e_asb", [128, CAPL])
        gs = sb("e_gs", [128, 3, 2])
        Yg = sb("e_Yg", [128, 3, D], BF16)
        OHT = sb("e_OHT", [128, NO, 3, 128], BF16)
        pa = [st.enter_context(nc.psum_tensor("e_pa%d" % i, [128, 512], F32)) for i in range(2)]
        pu = [st.enter_context(nc.psum_tensor("e_pu%d" % i, [128, 512], F32)) for i in range(2)]
        py = [st.enter_context(nc.psum_tensor("e_py%d" % i, [128, 512], F32)) for i in range(2)]
        pg = st.enter_context(nc.psum_tensor("e_pg", [128, 3, 2], F32))
        pt = st.enter_context(nc.psum_tensor("e_pt", [128, 3, 128], BF16))
        P.dma("sp", oidx[:], t["ownidx"][:, :], w=["oidx"])
        P.dma("sp", th[:], t["TH"][:, :], r=["TH"], w=["th"])
        P.dma("act", slt[:], t["c_slt"][:, :], w=["slt"])
        P.dma("act", onesb[:], t["c_onesb"][:, :], w=["onesb"])
        P.dma("act", identb[:], t["ident_bf"][:, :], w=["identb"])
        P.dma("sp", iota[:], t["c_iota"][:, :], w=["iota"])
        P.add("pool", lambda e: e.memset(onef[:], 1.0), w=["onef"])
        hx2keys = [("HX2", ti) for ti in range(L // 128)]
        for i in range(NO):
            P.add("pool", lambda e, i=i: e.indirect_dma_start(out=HX[:, i, :], out_offset=None, in_=t["HX2"][:, :],
                                                             in_offset=bass.IndirectOffsetOnAxis(ap=oidx[:, i:i + 1], axis=0)),
                  r=["oidx"] + hx2keys, w=[("HX", i)], dma=True)
            P.add("pool", lambda e, i=i: e.indirect_dma_start(out=Ao[:, i, :], out_offset=None, in_=t["AFFT"][:, :],
                                                             in_offset=bass.IndirectOffsetOnAxis(ap=oidx[:, i:i + 1], axis=0)),
                  r=["oidx", "AFFT"], w=["Ao"], dma=True)
        fl = lambda a: a.rearrange("p n e -> p (n e)")
        P.add("dve", lambda e: e.tensor_tensor(sel[:], Ao[:], th[:].unsqueeze(1).broadcast_to([128, NO, NE]), ALU.is_ge),
              r=["Ao", "th"], w=["sel"])
        P.add("dve", lambda e: e.tensor_copy(selb[:], sel[:]), r=["sel"], w=["selb"])
        P.add("pe", lambda e: e.matmul(pa[0][:, 0:NO * NE], slt[:], fl(selb[:]), start=True, stop=True), r=["slt", "selb"], w=["pa0"])
        P.add("pe", lambda e: e.matmul(pu[0][:, 0:NO * NE], onesb[:], fl(selb[:]), start=True, stop=True), r=["onesb", "selb"], w=["pu0"])
        P.add("dve", lambda e: e.tensor_copy(fl(tot[:]), pu[0][:, 0:NO * NE]), r=["pu0"], w=["tot"])
        for e_ in range(NE):
            P.add("dve", lambda e, e_=e_: e.tensor_tensor_scan(cum[:, :, e_], onef[:, 0:1].broadcast_to([128, NO]), tot[:, :, e_], 0.0,
                                                               ALU.mult, ALU.add),
                  r=["tot", "onef"], w=["cum"])
        P.add("dve", lambda e: e.tensor_tensor(fl(slot[:]), fl(cum[:]), fl(tot[:]), ALU.subtract), r=["cum", "tot"], w=["slot"])
        P.add("dve", lambda e: e.tensor_tensor(fl(slot[:]), fl(slot[:]), pa[0][:, 0:NO * NE], ALU.add), r=["slot", "pa0"], w=["slot"])
        P.add("dve", lambda e: e.scalar_tensor_tensor(fl(slot[:]), fl(slot[:]), 1.0, fl(sel[:]), ALU.add, ALU.mult), r=["slot", "sel"], w=["slot"])
        P.add("dve", lambda e: e.tensor_scalar_add(fl(slot[:]), fl(slot[:]), -1.0), r=["slot"], w=["slot"])
        P.add("dve", lambda e: e.tensor_copy(AHL[:, :, :, 0], Ao[:]), r=["Ao"], w=["AHL0"])
        P.add("dve", lambda e: e.tensor_copy(ares[:], AHL[:, :, :, 0]), r=["AHL0"], w=["ares"])
        P.add("dve", lambda e: e.tensor_tensor(ares[:], Ao[:], ares[:], ALU.subtract), r=["Ao", "ares"], w=["ares"])
        P.add("dve", lambda e: e.tensor_copy(AHL[:, :, :, 1], ares[:]), r=["ares"], w=["AHL1"])
        hxk = [("HX", i) for i in range(NO)]
        nld = 0
        for e_ in range(NE):
            for i in range(NO):
                P.add("dve", lambda e, i=i, e_=e_: e.tensor_single_scalar(OH[:, i, :], iota[:], slot[:, i, e_:e_ + 1], ALU.is_equal),
                      r=["iota", "slot"], w=[("OH", i)])
            ohk = [("OH", i) for i in range(NO)]
            for kc in range(16):
                pp = pa[kc % 2]
                for i in range(NO):
                    P.add("pe", lambda e, pp=pp, i=i, kc=kc: e.matmul(pp[:, 0:CAPL], HX[:, i, kc * 128:(kc + 1) * 128], OH[:, i, :],
                                                                      start=(i == 0), stop=(i == NO - 1)),
                          r=[("HX", i), ("OH", i)], w=[("pa", kc % 2)])
                if kc % 2 == 0:
                    P.add("act", lambda e, pp=pp, kc=kc: e.copy(XT[:, kc, :], pp[:, 0:CAPL]), r=[("pa", kc % 2)], w=[("XT", kc)])
                else:
                    P.add("dve", lambda e, pp=pp, kc=kc: e.tensor_copy(XT[:, kc, :], pp[:, 0:CAPL]), r=[("pa", kc % 2)], w=[("XT", kc)])
            for sc in range(3):
                for i in range(NO):
                    P.add("pe", lambda e, sc=sc, i=i, e_=e_: e.matmul(pg[:, sc, :], OH[:, i, sc * 128:(sc + 1) * 128], AHL[:, i, e_, :],
                                                                      start=(i == 0), stop=(i == NO - 1)),
                          r=[("OH", i), "AHL0", "AHL1"], w=["pg"])
            P.add("dve", lambda e: e.tensor_copy(gs[:], pg[:]), r=["pg"], w=["gs"])
            P.add("dve", lambda e: e.tensor_tensor(gs[:, :, 0], gs[:, :, 0], gs[:, :, 1], ALU.add), r=["gs"], w=["gs"])
            for i in range(NO):
                for sc in range(3):
                    P.add("pe", lambda e, i=i, sc=sc: e.transpose(pt[:, sc, :], OH[:, i, sc * 128:(sc + 1) * 128], identb[:]),
                          r=[("OH", i), "identb"], w=["pt"])
                P.add("act", lambda e, i=i: e.copy(OHT[:, i, :, :], pt[:]), r=["pt"], w=[("OHT", i)])
                P.dma("sp", t["OHTALL"][i][:, e_ * 3:(e_ + 1) * 3, :], OHT[:, i, :, :], r=[("OHT", i)], w=[("OHTALL", i, e_)])
            xtk = [("XT", kc) for kc in range(16)]
            wgv = t["ewg"][e_].rearrange("(kc p) f -> p kc f", p=128)
            wuv = t["ewu"][e_].rearrange("(kc p) f -> p kc f", p=128)
            for fb in range(8):
                wb = nld % 2
                nld += 1
                P.dma("pool", Wg[wb][:], wgv[:, :, fb * 256:(fb + 1) * 256], w=[("Wg", wb)])
                P.dma("pool", Wu[wb][:], wuv[:, :, fb * 256:(fb + 1) * 256], w=[("Wu", wb)])
                for f2_ in range(2):
                    fc = fb * 2 + f2_
                    pb = fc % 2
                    for kc in range(16):
                        P.add("pe", lambda e, pb=pb, wb=wb, kc=kc, f2_=f2_: e.matmul(pa[pb][:, 0:CAPL], Wg[wb][:, kc, f2_ * 128:(f2_ + 1) * 128],
                                                                                     XT[:, kc, :], start=(kc == 0), stop=(kc == 15)),
                              r=[("Wg", wb), ("XT", kc)], w=[("pa", pb)])
                    for kc in range(16):
                        P.add("pe", lambda e, pb=pb, wb=wb, kc=kc, f2_=f2_: e.matmul(pu[pb][:, 0:CAPL], Wu[wb][:, kc, f2_ * 128:(f2_ + 1) * 128],
                                                                                     XT[:, kc, :], start=(kc == 0), stop=(kc == 15)),
                              r=[("Wu", wb), ("XT", kc)], w=[("pu", pb)])
                    P.add("act", lambda e, pb=pb: e.activation(out=asb[:], in_=pa[pb][:, 0:CAPL], func=AF.Silu), r=[("pa", pb)], w=["asb"])
                    P.add("dve", lambda e, pb=pb, fc=fc: e.tensor_tensor(HT[:, fc, :], asb[:], pu[pb][:, 0:CAPL], ALU.mult),
                          r=["asb", ("pu", pb)], w=[("HT", fc)])
            wdv = t["ewd"][e_].rearrange("(fc p) d -> p fc d", p=128)
            for db in range(4):
                wb = db % 2
                P.dma("pool", Wd[wb][:], wdv[:, :, db * 512:(db + 1) * 512], w=[("Wd", wb)])
                for sc in range(3):
                    pp = py[sc % 2]
                    for fc in range(16):
                        P.add("pe", lambda e, pp=pp, wb=wb, fc=fc, sc=sc: e.matmul(pp[:], HT[:, fc, sc * 128:(sc + 1) * 128], Wd[wb][:, fc, :],
                                                                                   start=(fc == 0), stop=(fc == 15)),
                              r=[("HT", fc), ("Wd", wb)], w=[("py", sc % 2)])
                    P.add("act", lambda e, pp=pp, sc=sc, db=db: e.activation(out=Yg[:, sc, db * 512:(db + 1) * 512], in_=pp[:], func=AF.Copy,
                                                                             scale=gs[:, sc, 0:1]),
                          r=[("py", sc % 2), "gs"], w=["Yg"])
            P.dma("act", t["YGALL"][e_].rearrange("c s d -> s c d"), Yg[:], r=["Yg"], w=[("YGALL", e_)])
        P.flush()


def stage8(k):
    nc, P, t = k.nc, k.P, k.t
    NO = OWN // 128
    with ExitStack() as st:
        sb = lambda n, s, d=F32: st.enter_context(nc.sbuf_tensor(n, s, d))
        YGb = sb("f_YG", [128, NE * 3, 512], BF16)
        OT = [sb("f_OT%d" % i, [128, NE * 3, 128], BF16) for i in range(2)]
        ob = [sb("f_ob%d" % i, [128, 512]) for i in range(2)]
        ps = [st.enter_context(nc.psum_tensor("f_ps%d" % i, [128, 512], F32)) for i in range(2)]
        ygv = t["YGALL"].rearrange("e c s d -> s (e c) d")
        for db in range(4):
            for e_ in range(NE):
                P.dma(k.q(), YGb[:, e_ * 3:(e_ + 1) * 3, :], ygv[:, e_ * 3:(e_ + 1) * 3, db * 512:(db + 1) * 512],
                      r=[("YGALL", e_)], w=[("YGb", e_)])
            for i in range(NO):
                b = i % 2
                P.dma(k.q(), OT[b][:], t["OHTALL"][i], r=[("OHTALL", i, e_) for e_ in range(NE)], w=[("OT", b)])
                for j in range(NE * 3):
                    P.add("pe", lambda e, b=b, j=j: e.matmul(ps[b][:], OT[b][:, j, :], YGb[:, j, :], start=(j == 0), stop=(j == NE * 3 - 1)),
                          r=[("OT", b), ("YGb", j // 3)], w=[("ps", b)])
                P.add("act", lambda e, b=b: e.copy(ob[b][:], ps[b][:]), r=[("ps", b)], w=[("ob", b)])
                P.dma("sp", t["MOEO"][i * 128:(i + 1) * 128, db * 512:(db + 1) * 512], ob[b][:], r=[("ob", b)], w=[("MOEO", i, db)])
        P.flush()


def stage9(k):
    nc, P, t = k.nc, k.P, k.t
    NO = OWN // 128
    with ExitStack() as st:
        sb = lambda n, s, d=F32: st.enter_context(nc.sbuf_tensor(n, s, d))
        GN3 = sb("g_GN3", [128, D])
        oidx = sb("g_oidx", [128, NO], I32)
        epst = sb("g_eps", [128, 1])
        mo = [sb("g_mo%d" % i, [128, D]) for i in range(2)]
        x1 = [sb("g_x1%d" % i, [128, D]) for i in range(2)]
        junk = sb("g_junk", [128, D], BF16)
        ss = [sb("g_ss%d" % i, [128, 2]) for i in range(2)]
        P.dma("sp", GN3[:], t["MODS2"][3], r=["M2_3"], w=["GN3"])
        P.dma("sp", oidx[:], t["ownidx"][:, :], w=["oidx"])
        P.add("pool", lambda e: e.memset(epst[:], EPS), w=["epst"])
        x1keys = [("X1", ti) for ti in range(L // 128)]
        outs = []
        for i in range(NO):
            b = i % 2
            P.dma("sp", mo[b][:], t["MOEO"][i * 128:(i + 1) * 128, :], r=[("MOEO", i, db) for db in range(4)], w=[("mo", b)])
            P.add("pool", lambda e, b=b, i=i: e.indirect_dma_start(out=x1[b][:], out_offset=None, in_=t["X1"][:, :],
                                                                  in_offset=bass.IndirectOffsetOnAxis(ap=oidx[:, i:i + 1], axis=0)),
                  r=["oidx"] + x1keys, w=[("x1", b)], dma=True)
            P.add("act", lambda e, b=b: e.activation(out=junk[:], in_=mo[b][:], func=AF.Square, scale=float(D ** -0.5), accum_out=ss[b][:, 0:1]),
                  r=[("mo", b)], w=["junk", ("ss", b)])
            P.add("act", lambda e, b=b: e.activation(out=ss[b][:, 1:2], in_=ss[b][:, 0:1], func=AF.Sqrt, bias=epst[:, 0:1], scale=1.0),
                  r=[("ss", b), "epst"], w=[("ss1", b)])
            P.add("dve", lambda e, b=b: e.reciprocal(ss[b][:, 1:2], ss[b][:, 1:2]), r=[("ss1", b)], w=[("ss1", b)])
            P.add("dve", lambda e, b=b: e.scalar_tensor_tensor(mo[b][:], mo[b][:], ss[b][:, 1:2], GN3[:], ALU.mult, ALU.mult),
                  r=[("mo", b), ("ss1", b), "GN3"], w=[("mo", b)])
            P.add("pool", lambda e, b=b: e.tensor_tensor(mo[b][:], mo[b][:], x1[b][:], ALU.add), r=[("mo", b), ("x1", b)], w=[("mo", b)])
            outs.append(P.dma("sp", t["out"][i * 128:(i + 1) * 128, :], mo[b][:], r=[("mo", b)], w=[("out", i)]))
        P.flush(final_wait=outs)


def _host_s5(inp):
    are, aim, ldt = inp["s5_a_re"][0], inp["s5_a_im"][0], inp["s5_log_dt"][0]
    bre, bim = inp["s5_b_re"][0], inp["s5_b_im"][0]
    cre, cim = inp["s5_c_re"][0], inp["s5_c_im"][0]

    def gp(a):
        sh = a.shape[:-2]
        a = a.reshape(*sh, 8, 4, 2, 64)
        a = np.moveaxis(a, [-4, -2, -1, -3], [0, 1, 2, -1])
        return a.reshape(8, 128, *sh, 4)

    s5p = np.concatenate([gp(are).reshape(8, 128, 8), gp(aim).reshape(8, 128, 8),
                          gp(np.broadcast_to(ldt[..., None], (2, 64, 64))).reshape(8, 128, 8)], -1)

    def bc(r, i, cfirst):
        out = []
        for a in (r, i):
            if cfirst:
                a = a.transpose(0, 2, 1)
            a = a.reshape(8, 4, 2, 64, 16).transpose(0, 2, 3, 1, 4).reshape(8, 128, 4, 16)
            out.append(a)
        return np.ascontiguousarray(np.stack(out, 2))

    d = {"s5p": np.ascontiguousarray(s5p.astype(np.float32)), "s5b": bc(bre, bim, False),
         "s5c": bc(cre, cim, True), "s5d": np.ascontiguousarray(inp["s5_d"][0].reshape(8, 128, 1))}
    d["c_jidx"] = np.broadcast_to(np.arange(17, dtype=np.float32), (128, 17)).copy()
    pm = np.zeros((128, 2), np.float32)
    pm[:64, 0] = 1
    pm[64:, 1] = 1
    d["c_parmask"] = pm
    d["c_blockmask"] = np.kron(np.eye(8, dtype=np.float32), np.ones((16, 16), np.float32))
    m = np.arange(NCH, dtype=np.float32)
    d["c_midx"] = np.broadcast_to(np.stack([m, m[::-1]]), (128, 2, NCH)).copy()
    d["c_rowmask"] = np.kron(np.eye(4, dtype=np.float32), np.ones((32, 1), np.float32))
    return d


def _core_inputs(inp, core, shared):
    b, j = divmod(core, 4)
    cond = np.stack([inp["c"][b], inp["c_ctx"]], -1).reshape(16, 128, 2).transpose(1, 0, 2)
    d = {"x": inp["x"][b], "ctx": inp["ctx"][b], "cond": np.ascontiguousarray(cond),
         "xown": inp["x"][b][j * OWN:(j + 1) * OWN]}
    d.update(shared)
    return d


def _host_rest(inp):
    d = {}
    d["convw"] = np.ascontiguousarray(inp["ml_conv_w"][0].T.reshape(16, 128, 5))
    d["convb"] = np.ascontiguousarray(inp["ml_conv_b"][0].reshape(16, 128, 1))
    d["gateb"] = np.broadcast_to(inp["ml_gate_b"][0].reshape(1, 16), (128, 16)).copy()
    d["mlng"] = np.broadcast_to(inp["ml_norm_g"][0][None], (128, D_ML)).copy()
    tri = np.triu(np.ones((128, 128), np.float32))
    d["c_tri"] = tri
    d["c_trit"] = np.ascontiguousarray(tri.T)
    d["c_ones"] = np.ones((128, 128), np.float32)
    d["gluw"] = inp["s5_glu_w"][0]
    d["w_out"] = inp["w_out"][0]
    d["glub"] = np.ascontiguousarray(inp["s5_glu_b"][0].reshape(8, 128).T)
    d["rw"] = np.ascontiguousarray(inp["router_w"][0].reshape(16, 128, 16).transpose(1, 0, 2))
    d["ewg"] = inp["exp_w_gate"][0]
    d["ewu"] = inp["exp_w_up"][0]
    d["ewd"] = inp["exp_w_down"][0]
    d["c_slt"] = np.triu(np.ones((128, 128), np.float32), 1).astype(ml_dtypes.bfloat16)
    d["c_onesb"] = np.ones((128, 128), ml_dtypes.bfloat16)
    d["c_iota"] = np.broadcast_to(np.arange(CAPL, dtype=np.float32), (128, CAPL)).copy()
    return d


def build_full():
    k = K()
    declare_io(k)
    declare_s5(k)
    declare_ml(k)
    declare_s5post(k)
    declare_moe(k)
    for f in (stage0, stage1, stage2, stage3a, stage3b, stage4a, stage4b, stage4c, stage5, stage6, stage7, stage8, stage9):
        f(k)
    return k


def host_inputs(inp, cores):
    shared = {"ada_w": inp["ada_w"][0], "ada_b": inp["ada_b"][0][None], "norm_g": inp["norm_g"][0],
              "w_in": inp["w_in"][0], "ident_bf": np.eye(128, dtype=ml_dtypes.bfloat16),
              "ident_f": np.eye(128, dtype=np.float32)}
    shared.update(_host_s5(inp))
    shared.update(_host_rest(inp))
    maps = []
    for core in cores:
        b, j = divmod(core, 4)
        cond = np.stack([inp["c"][b], inp["c_ctx"]], -1).reshape(16, 128, 2).transpose(1, 0, 2)
        d = {"x": inp["x"][b], "ctx": inp["ctx"][b], "cond": np.ascontiguousarray(cond)}
        d["ownidx"] = (j * OWN + np.arange(OWN, dtype=np.int32).reshape(OWN // 128, 128).T).astype(np.int32).copy()
        d.update(shared)
        maps.append(d)
    return maps


def kernel(**inputs):
    inp = {k_: np.asarray(v) for k_, v in inputs.items()}
    k = build_full()
    names = set(k.t.keys())
    in_maps = [{n: np.ascontiguousarray(v) for n, v in m.items() if n in names} for m in host_inputs(inp, range(8))]
    res = run_bass_kernel_spmd(k.nc, in_maps, core_ids=list(range(8)))
    out = np.zeros((2, L, D), np.float32)
    for core in range(8):
        b, j = divmod(core, 4)
        out[b, j * OWN:(j + 1) * OWN] = np.asarray(res.results[core]["out"])
    return out
```

```python
from contextlib import ExitStack
import math
import numpy as np
import ml_dtypes
import concourse.bass as bass
import concourse.mybir as mybir
from concourse.bass_utils import run_bass_kernel_spmd

F32 = mybir.dt.float32
BF16 = mybir.dt.bfloat16
I32 = mybir.dt.int32
U32 = mybir.dt.uint32
ALU = mybir.AluOpType
AF = mybir.ActivationFunctionType
AX = mybir.AxisListType

ENGS = ("sp", "act", "dve", "pool", "pe")

D = 2048
L = 8192
CTX = 256
NT = L + CTX
NTILE = NT // 128
GW = 64
D_S5 = 1024
D_ML = 1024
NH = 4
DH = 256
D_IN = D_S5 + 4 * D_ML + 16
NE = 16
CAPE = 1024
FF = 2048
EPS = 1e-6
OWN = 2048
CAPL = 384


class _Op:
    __slots__ = ("eng", "fn", "deps", "dma", "needed", "sem", "val", "prev_val", "inc")

    def __init__(self, eng, fn, deps, dma, inc):
        self.eng = eng
        self.fn = fn
        self.deps = deps
        self.dma = dma
        self.needed = False
        self.sem = None
        self.val = 0
        self.prev_val = 0
        self.inc = inc


class Prog:
    def __init__(self, nc, n_dma_sems=8):
        self.nc = nc
        self.ops = []
        self.pending = []
        self.last_w = {}
        self.readers = {}
        self.st = ExitStack()
        self.esem = {e: self.st.enter_context(nc.semaphore("es_" + e)) for e in ENGS}
        self.ecnt = {e: 0 for e in ENGS}
        self.dsems = [self.st.enter_context(nc.semaphore("ds%d" % i)) for i in range(n_dma_sems)]
        self.dval = [0] * n_dma_sems
        self.drr = 0
        self.waited = {e: {} for e in ENGS}
        self.n_ins = 0

    def add(self, eng, fn, r=(), w=(), dma=False, inc=16):
        deps = set()
        for x in r:
            if x in self.last_w:
                deps.add(self.last_w[x])
        for x in w:
            if x in self.last_w:
                deps.add(self.last_w[x])
            deps.update(self.readers.get(x, {}).values())
        if eng == "pe" and not dma:
            deps = {d for d in deps if self.ops[d].dma or self.ops[d].eng != "pe"}
        op = _Op(eng, fn, deps, dma, inc)
        oid = len(self.ops)
        self.ops.append(op)
        self.pending.append(oid)
        rk = ("dma", oid) if dma else eng
        for x in r:
            self.readers.setdefault(x, {})[rk] = oid
        for x in w:
            self.last_w[x] = oid
            self.readers[x] = {}
        return oid

    def dma(self, eng, out, in_, r=(), w=(), **kw):
        return self.add(eng, lambda e: e.dma_start(out=out, in_=in_, **kw), r=r, w=w, dma=True)

    def flush(self, final_wait=()):
        ops = self.ops
        pend = self.pending
        self.pending = []
        live = set(self.last_w.values())
        for lst in self.readers.values():
            live.update(lst.values())
        for oid in pend:
            for d in ops[oid].deps:
                ops[d].needed = True
        for oid in pend:
            op = ops[oid]
            if op.dma or oid in live:
                op.needed = True
        for oid in pend:
            op = ops[oid]
            if op.dma:
                k = self.drr
                self.drr = (self.drr + 1) % len(self.dsems)
                op.sem = k
                op.prev_val = self.dval[k]
                self.dval[k] += op.inc
                op.val = self.dval[k]
            elif op.needed:
                self.ecnt[op.eng] += 1
                op.val = self.ecnt[op.eng]
        per = {e: [] for e in ENGS}
        for oid in pend:
            per[ops[oid].eng].append(oid)
        fw = list(final_wait)

        def run(ename, e):
            wd = self.waited[ename]

            def wait(key, sem, val):
                if wd.get(key, 0) >= val:
                    return
                wd[key] = val
                e.wait_ge(sem, val)
                self.n_ins += 1

            def wait_op(p):
                if p.dma:
                    wait(("d", p.sem), self.dsems[p.sem], p.val)
                else:
                    assert p.val > 0, "dependency on unsignalled op"
                    wait(("e", p.eng), self.esem[p.eng], p.val)

            for oid in per[ename]:
                op = ops[oid]
                for d in sorted(op.deps):
                    wait_op(ops[d])
                self.n_ins += 1
                if op.dma:
                    if op.prev_val > 0:
                        wait(("d", op.sem), self.dsems[op.sem], op.prev_val)
                    ins = op.fn(e)
                    ins.then_inc(self.dsems[op.sem], op.inc)
                else:
                    ins = op.fn(e)
                    if op.needed:
                        ins.then_inc(self.esem[ename], 1)
            if ename == "sp":
                for d in fw:
                    wait_op(ops[d])

        with self.nc.Block() as block:
            @block.sync
            def _(e):
                run("sp", e)

            @block.scalar
            def _(e):
                run("act", e)

            @block.vector
            def _(e):
                run("dve", e)

            @block.gpsimd
            def _(e):
                run("pool", e)

            @block.tensor
            def _(e):
                run("pe", e)

    def close(self):
        self.st.close()


class K:
    def __init__(self, dbg=()):
        self.nc = bass.Bass("TRN2", target_bir_lowering=False)
        self.P = Prog(self.nc)
        self.dbg = set(dbg)
        self.t = {}
        self._rr = 0

    def din(self, name, shape, dt=F32):
        a = self.nc.dram_tensor(name, list(shape), dt, kind="ExternalInput").ap()
        self.t[name] = a
        return a

    def dout(self, name, shape, dt=F32):
        a = self.nc.dram_tensor(name, list(shape), dt, kind="ExternalOutput").ap()
        self.t[name] = a
        return a

    def dscr(self, name, shape, dt=F32):
        kind = "ExternalOutput" if name in self.dbg else "Internal"
        a = self.nc.dram_tensor(name, list(shape), dt, kind=kind).ap()
        self.t[name] = a
        return a

    def q(self):
        self._rr ^= 1
        return "sp" if self._rr else "act"


def declare_io(k):
    k.din("x", [L, D])
    k.din("ctx", [CTX, D])
    k.din("cond", [128, 16, 2])
    k.din("ada_w", [D, 6 * D])
    k.din("ada_b", [1, 6 * D])
    k.din("norm_g", [4, D])
    k.din("w_in", [D, D_IN])
    k.din("ident_bf", [128, 128], BF16)
    k.din("ident_f", [128, 128])
    k.dout("out", [OWN, D])
    k.dscr("MODS", [6, 128, D])
    k.dscr("MODS2", [4, 128, D])
    k.dscr("HXT", [2, 16, 128, NT], BF16)
    k.dscr("U5", [8, 128, NT], BF16)
    k.dscr("QKPRE", [16, 128, NT], BF16)
    k.dscr("V", [NT, D_ML], BF16)
    k.dscr("SO", [NT, D_ML], BF16)
    k.dscr("GATES", [128, NTILE, 16])


def stage0(k):
    nc, P, t = k.nc, k.P, k.t
    with ExitStack() as st:
        sb = lambda n, s, d=F32: st.enter_context(nc.sbuf_tensor(n, s, d))
        cond = sb("s0_cond", [128, 16, 2])
        sil = sb("s0_sil", [128, 16, 2])
        lx = sb("s0_lx", [128, 16, 128])
        lc = sb("s0_lc", [128, 16, 128])
        wt = [sb("s0_w%d" % i, [128, 16, 512]) for i in range(2)]
        bt = [sb("s0_b%d" % i, [128, 512]) for i in range(2)]
        mx = sb("s0_mx", [128, 6, D])
        mc = sb("s0_mc", [128, 2, D])
        ng = sb("s0_ng", [128, 4, D])
        o1 = sb("s0_o1", [128, D])
        ps = [st.enter_context(nc.psum_tensor("s0_ps%d" % i, [128, 512], F32)) for i in range(2)]

        P.dma("sp", cond[:], t["cond"][:, :, :], w=["cond"])
        P.dma("act", ng[:], t["norm_g"].rearrange("(o g) d -> o g d", o=1).broadcast_to([128, 4, D]), w=["ng"])
        P.add("act", lambda e: e.activation(out=sil[:], in_=cond[:], func=AF.Silu), r=["cond"], w=["sil"])
        P.add("dve", lambda e: e.tensor_copy(lx[:], sil[:, :, 0:1].broadcast_to([128, 16, 128])), r=["sil"], w=["lx"])
        P.add("dve", lambda e: e.tensor_copy(lc[:], sil[:, :, 1:2].broadcast_to([128, 16, 128])), r=["sil"], w=["lc"])
        adaw = t["ada_w"].rearrange("(kc p) n -> p kc n", p=128)
        for nb in range(24):
            b = nb % 2
            P.dma("sp" if b else "act", wt[b][:], adaw[:, :, nb * 512:(nb + 1) * 512], w=[("w", b)])
            P.dma("pool", bt[b][:], t["ada_b"][:, nb * 512:(nb + 1) * 512].broadcast_to([128, 512]), w=[("b", b)])
            for which in range(2 if nb < 8 else 1):
                lhs = lx if which == 0 else lc
                pst = ps[which]
                for kc in range(16):
                    P.add("pe", lambda e, lhs=lhs, kc=kc, b=b, pst=pst: e.matmul(
                        pst[:], lhs[:, kc, :], wt[b][:, kc, :], start=(kc == 0), stop=(kc == 15)),
                        r=["lx", "lc", ("w", b)], w=[("ps", which)])
                dst = mx if which == 0 else mc
                ch, off = divmod(nb * 512, D)
                P.add("dve", lambda e, dst=dst, ch=ch, off=off, pst=pst, b=b: e.tensor_tensor(
                    dst[:, ch, off:off + 512], pst[:], bt[b][:], ALU.add),
                    r=[("ps", which), ("b", b)], w=[("m", which)])
        mods = t["MODS"]
        mods2 = t["MODS2"]

        def emit(dst_ap, fn, key):
            P.add("dve", fn, r=[("m", 0), ("m", 1), "ng"], w=["o1"])
            P.dma("sp", dst_ap, o1[:], r=["o1"], w=[key])

        emit(mods[0], lambda e: e.scalar_tensor_tensor(o1[:], mx[:, 1, :], 1.0, ng[:, 0, :], ALU.add, ALU.mult), "MODS0")
        emit(mods[1], lambda e: e.tensor_copy(o1[:], mx[:, 0, :]), "MODS1")
        emit(mods[2], lambda e: e.scalar_tensor_tensor(o1[:], mc[:, 1, :], 1.0, ng[:, 0, :], ALU.add, ALU.mult), "MODS2")
        emit(mods[3], lambda e: e.tensor_copy(o1[:], mc[:, 0, :]), "MODS3")
        emit(mods2[0], lambda e: e.tensor_tensor(o1[:], mx[:, 2, :], ng[:, 1, :], ALU.mult), "M2_0")
        emit(mods2[1], lambda e: e.scalar_tensor_tensor(o1[:], mx[:, 4, :], 1.0, ng[:, 2, :], ALU.add, ALU.mult), "M2_1")
        emit(mods2[2], lambda e: e.tensor_copy(o1[:], mx[:, 3, :]), "M2_2")
        emit(mods2[3], lambda e: e.tensor_tensor(o1[:], mx[:, 5, :], ng[:, 3, :], ALU.mult), "M2_3")
        P.flush()


def tok_src(k, order, ti):
    t = k.t
    if ti < 2:
        return t["ctx"][ti * 128:(ti + 1) * 128, :]
    xi = ti - 2
    if order == 0:
        return t["x"][xi * 128:(xi + 1) * 128, :]
    return t["x"].rearrange("(r w) d -> w r d", w=GW)[xi]


def stage1(k, orders=(0, 1)):
    nc, P, t = k.nc, k.P, k.t
    with ExitStack() as st:
        sb = lambda n, s, d=F32: st.enter_context(nc.sbuf_tensor(n, s, d))
        A = [sb("s1_A%d" % i, [128, D]) for i in range(4)]
        ident = sb("s1_id", [128, 128], BF16)
        xt = [sb("s1_x%d" % i, [128, D]) for i in range(2)]
        junk = sb("s1_junk", [128, D])
        t1 = [sb("s1_t%d" % i, [128, D]) for i in range(2)]
        hx = [sb("s1_hx%d" % i, [128, D], BF16) for i in range(2)]
        ss = [sb("s1_ss%d" % i, [128, 2]) for i in range(2)]
        blk = [sb("s1_blk%d" % i, [128, 16, 512], BF16) for i in range(2)]
        ps = [st.enter_context(nc.psum_tensor("s1_ps%d" % i, [128, 16, 128], BF16)) for i in range(2)]
        epst = sb("s1_eps", [128, 1])
        P.add("pool", lambda e: e.memset(epst[:], EPS), w=["epst"])
        for i in range(4):
            P.dma("sp", A[i][:], t["MODS"][i], r=["MODS%d" % i], w=[("A", i)])
        P.dma("act", ident[:], t["ident_bf"][:, :], w=["ident"])
        for order in orders:
            groups = [(0, 2)] + [(2 + 4 * g, 4) for g in range(16)]
            for gi, (t0, n) in enumerate(groups):
                bb = gi % 2
                for j in range(n):
                    ti = t0 + j
                    b = ti % 2
                    a_i, b_i = (2, 3) if ti < 2 else (0, 1)
                    P.dma(k.q(), xt[b][:], tok_src(k, order, ti), w=[("xt", b)])
                    P.add("act", lambda e, b=b: e.activation(out=junk[:], in_=xt[b][:], func=AF.Square,
                                                             scale=float(D ** -0.5), accum_out=ss[b][:, 0:1]),
                          r=[("xt", b)], w=["junk", ("ss", b)])
                    P.add("act", lambda e, b=b: e.activation(out=ss[b][:, 1:2], in_=ss[b][:, 0:1], func=AF.Sqrt,
                                                             bias=epst[:, 0:1], scale=1.0),
                          r=[("ss", b), "epst"], w=[("ss1", b)])
                    P.add("dve", lambda e, b=b: e.reciprocal(ss[b][:, 1:2], ss[b][:, 1:2]),
                          r=[("ss1", b)], w=[("ss1", b)])
                    P.add("dve", lambda e, b=b, a_i=a_i: e.scalar_tensor_tensor(
                        t1[b][:], xt[b][:], ss[b][:, 1:2], A[a_i][:], ALU.mult, ALU.mult),
                        r=[("xt", b), ("ss1", b), ("A", a_i)], w=[("t1", b)])
                    P.add("pool", lambda e, b=b, b_i=b_i: e.tensor_tensor(hx[b][:], t1[b][:], A[b_i][:], ALU.add),
                          r=[("t1", b), ("A", b_i)], w=[("hx", b)])
                    for kc in range(16):
                        P.add("pe", lambda e, b=b, kc=kc: e.transpose(ps[b][:, kc, :], hx[b][:, kc * 128:(kc + 1) * 128],
                                                                      ident[:]),
                              r=[("hx", b), "ident"], w=[("ps", b)])
                    P.add("act", lambda e, b=b, bb=bb, j=j: e.copy(blk[bb][:, :, j * 128:(j + 1) * 128], ps[b][:]),
                          r=[("ps", b)], w=[("blk", bb)])
                dst = t["HXT"][order].rearrange("kc p t -> p kc t")[:, :, t0 * 128:(t0 + n) * 128]
                P.dma(k.q(), dst, blk[bb][:, :, 0:n * 128], r=[("blk", bb)], w=[("HXT", order, gi)])
        P.flush()


TGROUPS = [(0, 256)] + [(256 + 512 * g, 512) for g in range(16)]


def stage2(k):
    nc, P, t = k.nc, k.P, k.t
    win = t["w_in"].rearrange("(kc p) n -> p kc n", p=128)
    for pname, order, c0, ncb, dst in (("A", 0, 0, 8, "U5"), ("B", 1, D_S5, 16, "QKPRE")):
        with ExitStack() as st:
            sb = lambda n, s, d=F32: st.enter_context(nc.sbuf_tensor(n, s, d))
            W = sb("s2%s_w" % pname, [128, 16, ncb * 128], BF16)
            hb = [sb("s2%s_h%d" % (pname, i), [128, 16, 512], BF16) for i in range(2)]
            ob = [sb("s2%s_o%d" % (pname, i), [128, ncb, 512], BF16) for i in range(2)]
            ps = [st.enter_context(nc.psum_tensor("s2%s_ps%d" % (pname, i), [128, 512], F32)) for i in range(4)]
            for kc in range(16):
                for c1 in range(0, ncb * 128, 1024):
                    P.dma("pool", W[:, kc, c1:c1 + 1024], win[:, kc, c0 + c1:c0 + c1 + 1024], w=[("W", kc, c1 // 1024)])
            hsrc = t["HXT"][order].rearrange("kc p t -> p kc t")
            dview = t[dst].rearrange("cb p t -> p cb t")
            for gi, (t0, gt) in enumerate(TGROUPS):
                b = gi % 2
                P.dma(k.q(), hb[b][:, :, 0:gt], hsrc[:, :, t0:t0 + gt], r=[("HXT", order, gi)], w=[("hb", b)])
                for cb in range(ncb):
                    pi = cb % 4
                    for kc in range(16):
                        P.add("pe", lambda e, pi=pi, kc=kc, cb=cb, b=b, gt=gt: e.matmul(
                            ps[pi][:, 0:gt], W[:, kc, cb * 128:(cb + 1) * 128], hb[b][:, kc, 0:gt],
                            start=(kc == 0), stop=(kc == 15)),
                            r=[("W", kc, cb // 8), ("hb", b)], w=[("ps", pi)])
                    eng = "act" if cb % 2 == 0 else "dve"
                    if eng == "act":
                        P.add("act", lambda e, pi=pi, cb=cb, b=b, gt=gt: e.copy(ob[b][:, cb, 0:gt], ps[pi][:, 0:gt]),
                              r=[("ps", pi)], w=[("ob", b)])
                    else:
                        P.add("dve", lambda e, pi=pi, cb=cb, b=b, gt=gt: e.tensor_copy(ob[b][:, cb, 0:gt], ps[pi][:, 0:gt]),
                              r=[("ps", pi)], w=[("ob", b)])
                P.dma(k.q(), dview[:, :, t0:t0 + gt], ob[b][:, :, 0:gt], r=[("ob", b)], w=[(dst, gi)])
            P.flush()
    with ExitStack() as st:
        sb = lambda n, s, d=F32: st.enter_context(nc.sbuf_tensor(n, s, d))
        W = sb("s2C_w", [128, 16, 2 * D_ML + 16], BF16)
        hb = [sb("s2C_h%d" % i, [128, 16, 512], BF16) for i in range(2)]
        vo = [sb("s2C_vo%d" % i, [128, 2 * D_ML], BF16) for i in range(2)]
        gt_sb = sb("s2C_g", [128, NTILE, 16])
        ps = [st.enter_context(nc.psum_tensor("s2C_ps%d" % i, [128, 512], F32)) for i in range(4)]
        psg = st.enter_context(nc.psum_tensor("s2C_psg", [128, 16], F32))
        c0 = D_S5 + 2 * D_ML
        for kc in range(16):
            for c1, cn in ((0, 1024), (1024, 1024), (2048, 16)):
                P.dma("pool", W[:, kc, c1:c1 + cn], win[:, kc, c0 + c1:c0 + c1 + cn], w=[("W", kc, c1 // 1024)])
        hsrc = t["HXT"][1].rearrange("kc p t -> p kc t")
        for gi, (t0, gtk) in enumerate(TGROUPS):
            b = gi % 2
            P.dma(k.q(), hb[b][:, :, 0:gtk], hsrc[:, :, t0:t0 + gtk], r=[("HXT", 1, gi)], w=[("hb", b)])
            for j in range(gtk // 128):
                ti = t0 // 128 + j
                vb = ti % 2
                for nb in range(4):
                    for kc in range(16):
                        P.add("pe", lambda e, nb=nb, kc=kc, b=b, j=j: e.matmul(
                            ps[nb][:], hb[b][:, kc, j * 128:(j + 1) * 128], W[:, kc, nb * 512:(nb + 1) * 512],
                            start=(kc == 0), stop=(kc == 15)),
                            r=[("W", kc, nb // 2), ("hb", b)], w=[("ps", nb)])
                    if nb < 2:
                        P.add("dve", lambda e, nb=nb, vb=vb: e.tensor_copy(vo[vb][:, nb * 512:(nb + 1) * 512], ps[nb][:]),
                              r=[("ps", nb)], w=[("vo", vb, nb)])
                    else:
                        P.add("act", lambda e, nb=nb, vb=vb: e.activation(out=vo[vb][:, nb * 512:(nb + 1) * 512],
                                                                          in_=ps[nb][:], func=AF.Sigmoid),
                              r=[("ps", nb)], w=[("vo", vb, nb)])
                for kc in range(16):
                    P.add("pe", lambda e, kc=kc, b=b, j=j: e.matmul(
                        psg[:], hb[b][:, kc, j * 128:(j + 1) * 128], W[:, kc, 2 * D_ML:2 * D_ML + 16],
                        start=(kc == 0), stop=(kc == 15)),
                        r=[("W", kc, 2), ("hb", b)], w=["psg"])
                P.add("dve", lambda e, ti=ti: e.tensor_copy(gt_sb[:, ti, :], psg[:]), r=["psg"], w=["gt_sb"])
                P.dma(k.q(), t["V"][ti * 128:(ti + 1) * 128, :], vo[vb][:, 0:D_ML],
                      r=[("vo", vb, 0), ("vo", vb, 1)], w=[("V", ti)])
                P.dma(k.q(), t["SO"][ti * 128:(ti + 1) * 128, :], vo[vb][:, D_ML:2 * D_ML],
                      r=[("vo", vb, 2), ("vo", vb, 3)], w=[("SO", ti)])
        P.dma("sp", t["GATES"][:, :, :], gt_sb[:], r=["gt_sb"], w=["GATES"])
        P.flush()


TS5 = 16
NCH = NT // TS5
TWO_PI = 2.0 * math.pi


def emit_sincos(P, x, osin, ocos, tf, ti, tm, cpi0, rk, wk_sin, wk_cos, tkey):
    PI_ = math.pi
    for which, out, off, wk in ((0, osin, 0.0, wk_sin), (1, ocos, 0.5 * PI_, wk_cos)):
        kt = (tkey, "tf")
        P.add("dve", lambda e, off=off: e.tensor_scalar(tf, x, off, 1.0 / TWO_PI, ALU.add, ALU.mult), r=rk, w=[kt])
        P.add("dve", lambda e: e.tensor_copy(ti, tf), r=[kt], w=[(tkey, "ti")])
        P.add("dve", lambda e: e.tensor_copy(tf, ti), r=[(tkey, "ti")], w=[kt])
        P.add("dve", lambda e, off=off: e.tensor_scalar_add(tm, x, off), r=rk, w=[(tkey, "tm")])
        P.add("dve", lambda e: e.scalar_tensor_tensor(tf, tf, -TWO_PI, tm, ALU.mult, ALU.add), r=[kt, (tkey, "tm")], w=[kt])
        P.add("dve", lambda e: e.tensor_single_scalar(tm, tf, PI_, ALU.is_gt), r=[kt], w=[(tkey, "tm")])
        P.add("dve", lambda e: e.scalar_tensor_tensor(tf, tm, -TWO_PI, tf, ALU.mult, ALU.add), r=[kt, (tkey, "tm")], w=[kt])
        P.add("dve", lambda e: e.tensor_single_scalar(tm, tf, -PI_, ALU.is_lt), r=[kt], w=[(tkey, "tm")])
        P.add("dve", lambda e: e.scalar_tensor_tensor(tf, tm, TWO_PI, tf, ALU.mult, ALU.add), r=[kt, (tkey, "tm")], w=[kt])
        P.add("dve", lambda e: e.tensor_scalar(tf, tf, -3.14159, 3.14159, ALU.max, ALU.min), r=[kt], w=[kt])
        P.add("act", lambda e, out=out: e.activation(out=out, in_=tf, func=AF.Sin, bias=cpi0, scale=1.0),
              r=[kt, "cpi"], w=wk)


def declare_s5(k):
    k.din("s5p", [8, 128, 24])
    k.din("s5b", [8, 128, 2, 4, 16])
    k.din("s5c", [8, 128, 2, 4, 16])
    k.din("s5d", [8, 128, 1])
    k.din("c_jidx", [128, 17])
    k.din("c_parmask", [128, 2])
    k.din("c_blockmask", [128, 128])
    k.din("c_midx", [128, 2, NCH])
    k.din("c_rowmask", [128, 4])
    k.dscr("S5T_LAG", [8, 128, 2, 16, 128], BF16)
    k.dscr("S5T_BDT", [8, 128, 2, 16, 2, 128], BF16)
    k.dscr("S5T_MG", [8, 128, 2, 2, 16, 128], BF16)
    k.dscr("S5T_RT", [8, 128, 16])
    k.dscr("S5T_DIAG", [8, 128, 128], BF16)
    k.dscr("YG", [8, 128, L], BF16)


def stage3a(k):
    nc, P, t = k.nc, k.P, k.t
    with ExitStack() as st:
        sb = lambda n, s, d=F32: st.enter_context(nc.sbuf_tensor(n, s, d))
        jidx = sb("a_jidx", [128, 17])
        parm = sb("a_parm", [128, 2])
        bmask = sb("a_bmask", [128, 128])
        identf = sb("a_idf", [128, 128])
        cpi = sb("a_cpi", [128, 2])
        prm = sb("a_prm", [128, 24])
        bb = sb("a_bb", [128, 2, 4, 16])
        cc = sb("a_cc", [128, 2, 4, 16])
        dd = sb("a_dd", [128, 1])
        sm = sb("a_sm", [128, 12, 8])
        JL = sb("a_JL", [128, 17, 8])
        JA = sb("a_JA", [128, 17, 8])
        T1 = sb("a_T1", [128, 17, 8])
        T2 = sb("a_T2", [128, 17, 8])
        TI = sb("a_TI", [128, 17, 8], I32)
        PR = sb("a_PR", [128, 17, 8])
        PI = sb("a_PI", [128, 17, 8])
        BB = sb("a_BB", [128, 2, 8, 16])
        tb = sb("a_tb", [128, 2, 8, 16])
        Wr = sb("a_Wr", [128, 16, 8, 16])
        Wi = sb("a_Wi", [128, 16, 8, 16])
        Wt = sb("a_Wt", [128, 16, 8, 16])
        MWr = sb("a_MWr", [128, 128, 2, 16])
        MWi = sb("a_MWi", [128, 128, 2, 16])
        MC = sb("a_MC", [128, 2, 4, 2, 16])
        Gt = sb("a_Gt", [128, 16, 4, 16])
        Gt2 = sb("a_Gt2", [128, 16, 4, 16])
        MG = sb("a_MG", [128, 2, 2, 16, 128], BF16)
        LAG = sb("a_LAG", [128, 2, 16, 128], BF16)
        BDT = sb("a_BDT", [128, 2, 16, 2, 128], BF16)
        DG = sb("a_DG", [128, 128], BF16)
        RT = sb("a_RT", [128, 16])
        ps = [st.enter_context(nc.psum_tensor("a_ps%d" % i, [128, 128], F32)) for i in range(4)]

        P.dma("sp", jidx[:], t["c_jidx"][:, :], w=["jidx"])
        P.dma("sp", parm[:], t["c_parmask"][:, :], w=["parm"])
        P.dma("sp", bmask[:], t["c_blockmask"][:, :], w=["bmask"])
        P.dma("sp", identf[:], t["ident_f"][:, :], w=["identf"])
        P.add("pool", lambda e: e.memset(cpi[:], 0.0), w=["cpi"])

        def V(fn, r, w, eng="dve"):
            P.add(eng, fn, r=r, w=w)

        for blk in range(8):
            P.dma("sp", prm[:], t["s5p"][blk], w=["prm"])
            P.dma("act", bb[:], t["s5b"][blk], w=["bb"])
            P.dma("sp", cc[:], t["s5c"][blk], w=["cc"])
            P.dma("act", dd[:], t["s5d"][blk], w=["dd"])
            are, aim, ldt = prm[:, 0:8], prm[:, 8:16], prm[:, 16:24]
            dt_, lr, lrdt, ang = sm[:, 0, :], sm[:, 1, :], sm[:, 2, :], sm[:, 3, :]
            nr, den, cr, ci, tmp, tmp2 = sm[:, 4, :], sm[:, 5, :], sm[:, 6, :], sm[:, 7, :], sm[:, 8, :], sm[:, 9, :]
            V(lambda e: e.activation(out=dt_, in_=ldt, func=AF.Exp), ["prm"], ["sm0"], "act")
            V(lambda e: e.tensor_scalar_min(lr, are, -1e-4), ["prm"], ["sm1"])
            V(lambda e: e.tensor_tensor(lrdt, lr, dt_, ALU.mult), ["sm0", "sm1"], ["sm2"])
            V(lambda e: e.tensor_tensor(ang, aim, dt_, ALU.mult), ["sm0", "prm"], ["sm3"])
            b17 = lambda a: a.unsqueeze(1).broadcast_to([128, 17, 8])
            j17 = jidx[:].unsqueeze(2).broadcast_to([128, 17, 8])
            V(lambda e: e.tensor_tensor(JL[:], j17, b17(lrdt), ALU.mult), ["jidx", "sm2"], ["JL"])
            V(lambda e: e.activation(out=JL[:], in_=JL[:], func=AF.Exp), ["JL"], ["JL"], "act")
            V(lambda e: e.tensor_tensor(JA[:], j17, b17(ang), ALU.mult), ["jidx", "sm3"], ["JA"])
            f2 = lambda a: a.rearrange("p j g -> p (j g)")
            emit_sincos(P, f2(JA[:]), f2(PI[:]), f2(PR[:]), f2(T1[:]), f2(TI[:]), f2(T2[:]), cpi[:, 1:2],
                        ["JA"], ["PI"], ["PR"], "sc_a")
            V(lambda e: e.tensor_tensor(PR[:], PR[:], JL[:], ALU.mult), ["PR", "JL"], ["PR"])
            V(lambda e: e.tensor_tensor(PI[:], PI[:], JL[:], ALU.mult), ["PI", "JL"], ["PI"])
            V(lambda e: e.tensor_scalar_add(nr, PR[:, 1, :], -1.0), ["PR"], ["sm4"])
            V(lambda e: e.tensor_tensor(den, lr, lr, ALU.mult), ["sm1"], ["sm5"])
            V(lambda e: e.tensor_tensor(tmp, aim, aim, ALU.mult), ["prm"], ["sm8"])
            V(lambda e: e.tensor_tensor(den, den, tmp, ALU.add), ["sm5", "sm8"], ["sm5"])
            V(lambda e: e.reciprocal(den, den), ["sm5"], ["sm5"])
            V(lambda e: e.tensor_tensor(cr, nr, lr, ALU.mult), ["sm4", "sm1"], ["sm6"])
            V(lambda e: e.tensor_tensor(tmp, PI[:, 1, :], aim, ALU.mult), ["PI", "prm", "sm5"], ["sm8"])
            V(lambda e: e.tensor_tensor(cr, cr, tmp, ALU.add), ["sm6", "sm8"], ["sm6"])
            V(lambda e: e.tensor_tensor(cr, cr, den, ALU.mult), ["sm6", "sm5"], ["sm6"])
            V(lambda e: e.tensor_tensor(ci, PI[:, 1, :], lr, ALU.mult), ["PI", "sm1"], ["sm7"])
            V(lambda e: e.tensor_tensor(tmp2, nr, aim, ALU.mult), ["sm4", "prm"], ["sm9"])
            V(lambda e: e.tensor_tensor(ci, ci, tmp2, ALU.subtract), ["sm7", "sm9"], ["sm7"])
            V(lambda e: e.tensor_tensor(ci, ci, den, ALU.mult), ["sm7", "sm5"], ["sm7"])
            for d_ in range(2):
                crd = cr[:, d_ * 4:(d_ + 1) * 4].unsqueeze(2).broadcast_to([128, 4, 16])
                cid = ci[:, d_ * 4:(d_ + 1) * 4].unsqueeze(2).broadcast_to([128, 4, 16])
                o_r = BB[:, 0, d_ * 4:(d_ + 1) * 4, :]
                o_i = BB[:, 1, d_ * 4:(d_ + 1) * 4, :]
                t_r = tb[:, 0, d_ * 4:(d_ + 1) * 4, :]
                t_i = tb[:, 1, d_ * 4:(d_ + 1) * 4, :]
                V(lambda e, o_r=o_r, crd=crd: e.tensor_tensor(o_r, crd, bb[:, 0], ALU.mult), ["sm6", "bb"], [("BB", d_)])
                V(lambda e, t_r=t_r, cid=cid: e.tensor_tensor(t_r, cid, bb[:, 1], ALU.mult), ["sm7", "bb"], [("tb", d_)])
                V(lambda e, o_r=o_r, t_r=t_r: e.tensor_tensor(o_r, o_r, t_r, ALU.subtract), [("BB", d_), ("tb", d_)], [("BB", d_)])
                V(lambda e, o_i=o_i, crd=crd: e.tensor_tensor(o_i, crd, bb[:, 1], ALU.mult), ["sm6", "bb"], [("BBi", d_)])
                V(lambda e, t_i=t_i, cid=cid: e.tensor_tensor(t_i, cid, bb[:, 0], ALU.mult), ["sm7", "bb"], [("tbi", d_)])
                V(lambda e, o_i=o_i, t_i=t_i: e.tensor_tensor(o_i, o_i, t_i, ALU.add), [("BBi", d_), ("tbi", d_)], [("BBi", d_)])
            BBk = [("BB", 0), ("BB", 1), ("BBi", 0), ("BBi", 1)]
            pr16 = PR[:, 0:16, :].unsqueeze(3).broadcast_to([128, 16, 8, 16])
            pi16 = PI[:, 0:16, :].unsqueeze(3).broadcast_to([128, 16, 8, 16])
            bbr = BB[:, 0].unsqueeze(1).broadcast_to([128, 16, 8, 16])
            bbi = BB[:, 1].unsqueeze(1).broadcast_to([128, 16, 8, 16])
            V(lambda e: e.tensor_tensor(Wr[:], pr16, bbr, ALU.mult), ["PR"] + BBk, ["Wr"])
            V(lambda e: e.tensor_tensor(Wt[:], pi16, bbi, ALU.mult), ["PI"] + BBk, ["Wt"])
            V(lambda e: e.tensor_tensor(Wr[:], Wr[:], Wt[:], ALU.subtract), ["Wr", "Wt"], ["Wr"])
            V(lambda e: e.tensor_tensor(Wi[:], pr16, bbi, ALU.mult), ["PR"] + BBk, ["Wi"])
            V(lambda e: e.tensor_tensor(Wt[:], pi16, bbr, ALU.mult), ["PI", "Wr"] + BBk, ["Wt"])
            V(lambda e: e.tensor_tensor(Wi[:], Wi[:], Wt[:], ALU.add), ["Wi", "Wt"], ["Wi"])
            pm = parm[:].unsqueeze(1).unsqueeze(3).broadcast_to([128, 128, 2, 16])
            wrv = Wr[:].rearrange("p j g c -> p (j g) c").unsqueeze(2).broadcast_to([128, 128, 2, 16])
            wiv = Wi[:].rearrange("p j g c -> p (j g) c").unsqueeze(2).broadcast_to([128, 128, 2, 16])
            V(lambda e: e.tensor_tensor(MWr[:], wrv, pm, ALU.mult), ["Wr", "parm"], ["MWr"])
            V(lambda e: e.tensor_tensor(MWi[:], wiv, pm, ALU.mult), ["Wi", "parm"], ["MWi"], "pool")
            pm2 = parm[:].unsqueeze(1).unsqueeze(3).broadcast_to([128, 4, 2, 16])
            V(lambda e: e.tensor_tensor(MC[:, 0], cc[:, 0].unsqueeze(2).broadcast_to([128, 4, 2, 16]), pm2, ALU.mult),
              ["cc", "parm"], ["MC0"])
            V(lambda e: e.tensor_tensor(MC[:, 1], cc[:, 1].unsqueeze(2).broadcast_to([128, 4, 2, 16]), pm2, ALU.mult),
              ["cc", "parm"], ["MC1"])
            mc1f = MC[:, 1].rearrange("p q a c -> p (q a c)")
            V(lambda e: e.tensor_scalar_mul(mc1f, mc1f, -1.0), ["MC1"], ["MC1"])
            mwr = MWr[:].rearrange("p (j d q) a c -> p j d (q a c)", j=16, d=2)
            mwi = MWi[:].rearrange("p (j d q) a c -> p j d (q a c)", j=16, d=2)
            mcr = MC[:, 0].rearrange("p q a c -> p (q a c)")
            mci = MC[:, 1].rearrange("p q a c -> p (q a c)")
            n = 0
            for d_ in range(2):
                for j in range(16):
                    pa = ps[n % 4]
                    n += 1
                    P.add("pe", lambda e, pa=pa, j=j, d_=d_: e.matmul(pa[:], mwr[:, j, d_, :], mcr, start=True, stop=False),
                          r=["MWr", "MC0"], w=[("ps", id(pa))])
                    P.add("pe", lambda e, pa=pa, j=j, d_=d_: e.matmul(pa[:], mwi[:, j, d_, :], mci, start=False, stop=True),
                          r=["MWi", "MC1"], w=[("ps", id(pa))])
                    V(lambda e, pa=pa, j=j, d_=d_: e.tensor_tensor(LAG[:, d_, j, :], pa[:], bmask[:], ALU.mult),
                      [("ps", id(pa)), "bmask"], ["LAG"])
                    for ri, mw in ((0, mwr), (1, mwi)):
                        pb = ps[n % 4]
                        n += 1
                        P.add("pe", lambda e, pb=pb, mw=mw, j=j, d_=d_: e.transpose(pb[:], mw[:, j, d_, :], identf[:]),
                              r=["MWr", "MWi", "identf"], w=[("ps", id(pb))])
                        V(lambda e, pb=pb, ri=ri, j=j, d_=d_: e.copy(BDT[:, d_, j, ri, :], pb[:]),
                          [("ps", id(pb))], ["BDT"], "act")
            for d_ in range(2):
                prj = PR[:, 1:17, d_ * 4:(d_ + 1) * 4].unsqueeze(3).broadcast_to([128, 16, 4, 16])
                pij = PI[:, 1:17, d_ * 4:(d_ + 1) * 4].unsqueeze(3).broadcast_to([128, 16, 4, 16])
                crv = cc[:, 0].unsqueeze(1).broadcast_to([128, 16, 4, 16])
                civ = cc[:, 1].unsqueeze(1).broadcast_to([128, 16, 4, 16])
                pm3 = parm[:].unsqueeze(1).unsqueeze(3).broadcast_to([128, 64, 2, 16])
                V(lambda e, prj=prj, crv=crv: e.tensor_tensor(Gt[:], prj, crv, ALU.mult), ["PR", "cc"], ["Gt"])
                V(lambda e, pij=pij, civ=civ: e.tensor_tensor(Gt2[:], pij, civ, ALU.mult), ["PI", "cc"], ["Gt2"])
                V(lambda e: e.tensor_tensor(Gt[:], Gt[:], Gt2[:], ALU.subtract), ["Gt", "Gt2"], ["Gt"])
                gv = Gt[:].rearrange("p j q c -> p (j q) c").unsqueeze(2).broadcast_to([128, 64, 2, 16])
                ov = MG[:, 0, d_].rearrange("p j (q a c) -> p (j q) a c", q=4, a=2)
                V(lambda e, ov=ov, gv=gv, pm3=pm3: e.tensor_tensor(ov, gv, pm3, ALU.mult), ["Gt", "parm"], [("MG", 0, d_)])
                V(lambda e, pij=pij, crv=crv: e.tensor_tensor(Gt[:], pij, crv, ALU.mult), ["PI", "cc", ("MG", 0, d_)], ["Gt"])
                V(lambda e, prj=prj, civ=civ: e.tensor_tensor(Gt2[:], prj, civ, ALU.mult), ["PR", "cc"], ["Gt2"])
                V(lambda e: e.tensor_tensor(Gt[:], Gt[:], Gt2[:], ALU.add), ["Gt", "Gt2"], ["Gt"])
                ov2 = MG[:, 1, d_].rearrange("p j (q a c) -> p (j q) a c", q=4, a=2)
                gtf = Gt[:].rearrange("p j q c -> p (j q c)")
                V(lambda e, gtf=gtf: e.tensor_scalar_mul(gtf, gtf, -1.0), ["Gt"], ["Gt"])
                V(lambda e, ov2=ov2, gv=gv, pm3=pm3: e.tensor_tensor(ov2, gv, pm3, ALU.mult),
                  ["Gt", "parm"], [("MG", 1, d_)])
            V(lambda e: e.tensor_copy(RT[:, 0:8], JL[:, 16, :]), ["JL"], ["RT0"])
            V(lambda e: e.tensor_copy(RT[:, 8:16], JA[:, 16, :]), ["JA"], ["RT1"])
            V(lambda e: e.tensor_scalar_mul(DG[:], identf[:], dd[:, 0:1]), ["identf", "dd"], ["DG"])
            P.dma("sp", t["S5T_LAG"][blk], LAG[:], r=["LAG"], w=[("S5T", blk)])
            P.dma("act", t["S5T_BDT"][blk], BDT[:], r=["BDT"], w=[("S5T", blk)])
            P.dma("sp", t["S5T_MG"][blk], MG[:], r=[("MG", a, b) for a in range(2) for b in range(2)], w=[("S5T", blk)])
            P.dma("act", t["S5T_RT"][blk], RT[:], r=["RT0", "RT1"], w=[("S5T", blk)])
            P.dma("sp", t["S5T_DIAG"][blk], DG[:], r=["DG"], w=[("S5T", blk)])
        P.flush()


def build(upto=99, dbg=(), only=None):
    k = K(dbg)
    declare_io(k)
    declare_s5(k)
    declare_ml(k)
    declare_s5post(k)
    declare_moe(k)
    stages = [stage0, stage1, stage2, stage3a, stage3b, stage4a, stage4b, stage4c, stage5, stage6, stage7, stage8, stage9]
    for i, f in enumerate(stages):
        if (only is None and i <= upto) or (only is not None and i in only):
            f(k)
    return k


S5_BLOCKS = list(range(8))
S5_PH = {'X', 'dem', 'inter', 'lag'}
S5_CUT = 9


def stage3b(k, blocks=None):
    blocks = S5_BLOCKS if blocks is None else blocks
    nc, P, t = k.nc, k.P, k.t
    NX = L // TS5
    NC = CTX // TS5
    with ExitStack() as st:
        sb = lambda n, s, d=F32: st.enter_context(nc.sbuf_tensor(n, s, d))
        Ub = sb("b_U", [128, NT], BF16)
        LAG = sb("b_LAG", [128, 2, 16, 128], BF16)
        BDT = sb("b_BDT", [128, 2, 16, 2, 128], BF16)
        MG = sb("b_MG", [128, 2, 2, 16, 128], BF16)
        DG = sb("b_DG", [128, 128], BF16)
        RT = sb("b_RT", [128, 16])
        midx = sb("b_midx", [128, 2, NCH])
        BDQ = [sb("b_BDQ%d" % i, [128, 16, 2, 128], BF16) for i in range(2)]
        rowm = sb("b_rowm", [128, 4])
        cpi = sb("b_cpi", [128, 2])
        Xr = sb("b_Xr", [128, 4, NCH])
        Xi = sb("b_Xi", [128, 4, NCH])
        Ec = sb("b_Ec", [128, 4, NCH])
        Es = sb("b_Es", [128, 4, NCH])
        tf = sb("b_tf", [128, 4, NCH])
        tm = sb("b_tm", [128, 4, NCH])
        ANGS = sb("b_ANGS", [128, 4, 49])
        SINS = sb("b_SINS", [128, 4, 49])
        COSS = sb("b_COSS", [128, 4, 49])
        stf = sb("b_stf", [128, 4, 49])
        stm = sb("b_stm", [128, 4, 49])
        sti = sb("b_sti", [128, 4, 49], I32)
        Hb = sb("b_Hb", [128, 2, 2, 4, NCH], BF16)
        YI = sb("b_YI", [128, NX, TS5])
        ys = [sb("b_ys%d" % i, [128, 512]) for i in range(2)]
        y2 = [sb("b_y2%d" % i, [128, 512]) for i in range(2)]
        yo = [sb("b_yo%d" % i, [128, 512], BF16) for i in range(2)]
        psx = [st.enter_context(nc.psum_tensor("b_psx%d" % i, [128, 512], F32)) for i in range(4)]
        psc = [st.enter_context(nc.psum_tensor("b_psc%d" % i, [128, 2, 16], F32)) for i in range(2)]
        psy = [st.enter_context(nc.psum_tensor("b_psy%d" % i, [128, 512], F32)) for i in range(2)]
        P.dma("sp", midx[:], t["c_midx"][:, :, :], w=["midx"])
        P.dma("sp", rowm[:], t["c_rowmask"][:, :], w=["rowm"])
        P.add("pool", lambda e: e.memset(cpi[:], 0.0), w=["cpi"])
        f2 = lambda a: a.rearrange("p q n -> p (q n)")
        for blk in blocks:
            P.dma("sp", Ub[:], t["U5"][blk], r=[("U5", g) for g in range(17)], w=["Ub"])
            P.dma("act", LAG[:], t["S5T_LAG"][blk], r=[("S5T", blk)], w=["LAG"])
            P.dma("sp", BDT[:], t["S5T_BDT"][blk], r=[("S5T", blk)], w=["BDT"])
            P.dma("act", MG[:], t["S5T_MG"][blk], r=[("S5T", blk)], w=["MG"])
            P.dma("sp", DG[:], t["S5T_DIAG"][blk], r=[("S5T", blk)], w=["DG"])
            P.dma("act", RT[:], t["S5T_RT"][blk], r=[("S5T", blk)], w=["RT"])
            for d_ in range(2):
                xo, co = (NC, 0) if d_ == 0 else (0, NX)
                for q in (range(4) if 'X' in S5_PH else ()):
                    pb = q % 2
                    bq = BDQ[pb]
                    P.add("dve", lambda e, bq=bq, q=q, d_=d_: e.tensor_scalar_mul(
                        bq[:].rearrange("p j r c -> p (j r c)"), BDT[:, d_].rearrange("p j r c -> p (j r c)"),
                        rowm[:, q:q + 1]), r=["BDT", "rowm"], w=[("BDQ", pb)])
                    for ri in (range(2) if S5_CUT >= 2 else ()):
                        px_ = psx[pb * 2 + ri]
                        for s in range(TS5):
                            j = (TS5 - 1 - s) if d_ == 0 else s
                            lhs = bq[:, j, ri, :]
                            P.add("pe", lambda e, px_=px_, lhs=lhs, q=q, s=s: e.matmul(
                                px_[:], lhs, Ub[:, CTX + s:NT:TS5],
                                start=(s == 0), stop=(s == TS5 - 1)),
                                r=[("BDQ", pb), "Ub"], w=[("psx", pb, ri)])
                            P.add("pe", lambda e, pb=pb, ri=ri, lhs=lhs, q=q, s=s: e.matmul(
                                psc[pb][:, ri, :], lhs, Ub[:, s:CTX:TS5],
                                start=(s == 0), stop=(s == TS5 - 1)),
                                r=[("BDQ", pb), "Ub"], w=[("psc", pb, ri)])
                        X = Xr if ri == 0 else Xi
                        if S5_CUT < 3:
                            continue
                        if S5_CUT != 4:
                            P.add("dve", lambda e, X=X, q=q, px_=px_, xo=xo: e.tensor_copy(X[:, q, xo:xo + NX], px_[:]),
                                  r=[("psx", pb, ri)], w=[("X", ri)])
                        if S5_CUT != 5:
                            P.add("dve", lambda e, X=X, q=q, pb=pb, ri=ri, co=co: e.tensor_copy(X[:, q, co:co + NC], psc[pb][:, ri, :]),
                                  r=[("psc", pb, ri)], w=[("X", ri)])
                if 'dem' not in S5_PH:
                    continue
                V = lambda fn, r, w, eng="dve": P.add(eng, fn, r=r, w=w)
                for q in range(4):
                    thq = RT[:, 8 + d_ * 4 + q:9 + d_ * 4 + q]
                    V(lambda e, q=q, thq=thq: e.tensor_scalar_mul(ANGS[:, q, 0:16], midx[:, 0, 0:16], thq), ["midx", "RT"], ["ANGS"])
                    V(lambda e, q=q, thq=thq: e.tensor_scalar(ANGS[:, q, 16:49], midx[:, 0, 0:33], thq, 16.0, ALU.mult, ALU.mult),
                      ["midx", "RT"], ["ANGS"])
                fs = lambda a: a.rearrange("p q n -> p (q n)")
                emit_sincos(P, fs(ANGS[:]), fs(SINS[:]), fs(COSS[:]), fs(stf[:]), fs(sti[:]), fs(stm[:]), cpi[:, 0:1],
                            ["ANGS"], ["SINS"], ["COSS"], "sc_s")
                if d_ == 0:
                    c0, s0 = COSS[:, :, 0:16], SINS[:, :, 0:16]
                    c1, s1 = COSS[:, :, 16:49], SINS[:, :, 16:49]
                else:
                    c0, s0 = COSS[:, :, 15::-1] if False else COSS[:, :, 0:16][:, :, ::-1], SINS[:, :, 0:16][:, :, ::-1]
                    c1, s1 = COSS[:, :, 16:49][:, :, ::-1], SINS[:, :, 16:49][:, :, ::-1]
                b0 = lambda a: a.unsqueeze(2).broadcast_to([128, 4, 33, 16])
                b1 = lambda a: a.unsqueeze(3).broadcast_to([128, 4, 33, 16])
                v4 = lambda a: a.rearrange("p q (a b) -> p q a b", b=16)
                V(lambda e, c0=c0, c1=c1: e.tensor_tensor(v4(Ec[:]), b1(c1), b0(c0), ALU.mult), ["COSS", "SINS", "Hdone"], ["Ec"])
                V(lambda e, s0=s0, s1=s1: e.tensor_tensor(v4(tf[:]), b1(s1), b0(s0), ALU.mult), ["COSS", "SINS", "Hdone"], [("sc_b", "tf")], "pool")
                V(lambda e: e.tensor_tensor(f2(Ec[:]), f2(Ec[:]), f2(tf[:]), ALU.subtract), ["Ec", ("sc_b", "tf")], ["Ec"])
                V(lambda e, c0=c0, s1=s1: e.tensor_tensor(v4(Es[:]), b1(s1), b0(c0), ALU.mult), ["COSS", "SINS", "Hdone"], ["Es"])
                V(lambda e, s0=s0, c1=c1: e.tensor_tensor(v4(tm[:]), b1(c1), b0(s0), ALU.mult), ["COSS", "SINS", "Hdone"], [("sc_b", "tm")], "pool")
                V(lambda e: e.tensor_tensor(f2(Es[:]), f2(Es[:]), f2(tm[:]), ALU.add), ["Es", ("sc_b", "tm")], ["Es"])
                V(lambda e: e.tensor_tensor(f2(tf[:]), f2(Ec[:]), f2(Xr[:]), ALU.mult), ["Ec", ("X", 0), ("sc_b", "tf")], [("sc_b", "tf")])
                V(lambda e: e.tensor_tensor(f2(tm[:]), f2(Es[:]), f2(Xi[:]), ALU.mult), ["Es", ("X", 1), ("sc_b", "tm")], [("sc_b", "tm")], "pool")
                V(lambda e: e.tensor_tensor(f2(tf[:]), f2(tf[:]), f2(tm[:]), ALU.add), [("sc_b", "tf"), ("sc_b", "tm")], [("sc_b", "tf")])
                V(lambda e: e.tensor_tensor(f2(tm[:]), f2(Ec[:]), f2(Xi[:]), ALU.mult), ["Ec", ("X", 1), ("sc_b", "tf")], [("sc_b", "tm")])
                V(lambda e: e.tensor_tensor(f2(Xr[:]), f2(Es[:]), f2(Xr[:]), ALU.mult), ["Es", ("X", 0), ("sc_b", "tf")], [("X", 0)], "pool")
                V(lambda e: e.tensor_tensor(f2(tm[:]), f2(tm[:]), f2(Xr[:]), ALU.subtract), [("sc_b", "tm"), ("X", 0)], [("sc_b", "tm")])
                for q in range(4):
                    rq = RT[:, d_ * 4 + q:d_ * 4 + q + 1].broadcast_to([128, NCH])
                    for src, dst, kk in ((tf, Xr, 0), (tm, Xi, 1)):
                        if d_ == 0:
                            o_, i_ = dst[:, q, :], src[:, q, :]
                        else:
                            o_, i_ = dst[:, q, ::-1], src[:, q, ::-1]
                        V(lambda e, o_=o_, i_=i_, rq=rq: e.tensor_tensor_scan(o_, rq, i_, 0.0, ALU.mult, ALU.add),
                          ["RT", ("sc_b", "tf"), ("sc_b", "tm"), ("X", kk)], [("X", kk)])
                hr = Hb[:, 0, d_].rearrange("p q n -> p (q n)")
                hi = Hb[:, 1, d_].rearrange("p q n -> p (q n)")
                V(lambda e: e.tensor_tensor(f2(tf[:]), f2(Ec[:]), f2(Xr[:]), ALU.mult), ["Ec", ("X", 0), ("sc_b", "tf")], [("sc_b", "tf")])
                V(lambda e: e.tensor_tensor(f2(tm[:]), f2(Es[:]), f2(Xi[:]), ALU.mult), ["Es", ("X", 1), ("sc_b", "tm")], [("sc_b", "tm")], "pool")
                V(lambda e, hr=hr: e.tensor_tensor(hr, f2(tf[:]), f2(tm[:]), ALU.subtract), [("sc_b", "tf"), ("sc_b", "tm")], [("Hb", d_, 0)])
                V(lambda e: e.tensor_tensor(f2(tf[:]), f2(Ec[:]), f2(Xi[:]), ALU.mult), ["Ec", ("X", 1), ("Hb", d_, 0)], [("sc_b", "tf")])
                V(lambda e: e.tensor_tensor(f2(tm[:]), f2(Es[:]), f2(Xr[:]), ALU.mult), ["Es", ("X", 0), ("Hb", d_, 0)], [("sc_b", "tm")], "pool")
                V(lambda e, hi=hi: e.tensor_tensor(hi, f2(tf[:]), f2(tm[:]), ALU.add), [("sc_b", "tf"), ("sc_b", "tm")], [("Hb", d_, 1), "Hdone"])
            hbk = [("Hb", a, b) for a in range(2) for b in range(2)]
            for s in (range(TS5) if 'inter' in S5_PH else ()):
                pp = psy[s % 2]
                for q in range(4):
                    n_mm = 0
                    for d_ in range(2):
                        j = (s + 1) if d_ == 0 else (TS5 - s)
                        c0 = (NC - 1) if d_ == 0 else 1
                        for ri in range(2):
                            P.add("pe", lambda e, pp=pp, q=q, d_=d_, ri=ri, j=j, c0=c0, n_mm=n_mm: e.matmul(
                                pp[32 * q:32 * q + 32, :], MG[:, ri, d_, j - 1, 32 * q:32 * q + 32],
                                Hb[:, ri, d_, q, c0:c0 + NX], start=(n_mm == 0), stop=(n_mm == 3),
                                tile_position=(0, 32 * q)),
                                r=["MG"] + hbk, w=[("psy", s % 2)])
                            n_mm += 1
                P.add("act", lambda e, pp=pp, s=s: e.copy(YI[:, :, s], pp[:]), r=[("psy", s % 2)], w=["YI"])
            for tb in (range(16) if 'lag' in S5_PH else ()):
                pp = psy[tb % 2]
                b = tb % 2
                u0 = CTX + tb * 512
                uv = Ub[:, u0:u0 + 512].rearrange("p (n s) -> p n s", s=TS5)
                pv = pp[:].rearrange("p (n s) -> p n s", s=TS5)
                P.add("pe", lambda e, pp=pp, u0=u0: e.matmul(pp[:], DG[:], Ub[:, u0:u0 + 512], start=True, stop=False),
                      r=["DG", "Ub"], w=[("psy", b)])
                for j in range(TS5):
                    P.add("pe", lambda e, pv=pv, uv=uv, j=j: e.matmul(pv[:, :, j:TS5], LAG[:, 0, j, :], uv[:, :, 0:TS5 - j],
                                                                      start=False, stop=False),
                          r=["LAG", "Ub"], w=[("psy", b)])
                    P.add("pe", lambda e, pv=pv, uv=uv, j=j: e.matmul(pv[:, :, 0:TS5 - j], LAG[:, 1, j, :], uv[:, :, j:TS5],
                                                                      start=False, stop=(j == TS5 - 1)),
                          r=["LAG", "Ub"], w=[("psy", b)])
                if S5_CUT < 2:
                    continue
                yiv = YI[:, tb * 32:(tb + 1) * 32, :].rearrange("p n s -> p (n s)")
                P.add("dve", lambda e, pp=pp, b=b, yiv=yiv: e.tensor_tensor(ys[b][:], pp[:], yiv, ALU.add),
                      r=[("psy", b), "YI"], w=[("ys", b)])
                P.add("act", lambda e, b=b: e.activation(out=y2[b][:], in_=ys[b][:], func=AF.Square), r=[("ys", b)], w=[("y2", b)])
                P.add("dve", lambda e, b=b: e.tensor_scalar(y2[b][:], y2[b][:], 0.044715, 1.0, ALU.mult, ALU.add),
                      r=[("y2", b)], w=[("y2", b)])
                P.add("dve", lambda e, b=b: e.tensor_tensor(y2[b][:], y2[b][:], ys[b][:], ALU.mult), r=[("y2", b), ("ys", b)], w=[("y2", b)])
                P.add("act", lambda e, b=b: e.activation(out=y2[b][:], in_=y2[b][:], func=AF.Sigmoid, scale=1.5957691216),
                      r=[("y2", b)], w=[("y2", b)])
                P.add("dve", lambda e, b=b: e.tensor_tensor(yo[b][:], y2[b][:], ys[b][:], ALU.mult), r=[("y2", b), ("ys", b)], w=[("yo", b)])
                if S5_CUT < 3:
                    continue
                P.dma("sp", t["YG"][blk][:, tb * 512:(tb + 1) * 512], yo[b][:], r=[("yo", b)], w=[("YG", blk, tb)])
        P.flush()


def declare_ml(k):
    k.din("convw", [16, 128, 5])
    k.din("convb", [16, 128, 1])
    k.din("gateb", [128, 16])
    k.din("mlng", [128, D_ML])
    k.din("c_tri", [128, 128])
    k.din("c_trit", [128, 128])
    k.din("c_ones", [128, 128])
    k.dscr("QKT", [16, 128, NT], BF16)
    k.dscr("HDIR", [NH, 2, L, DH])
    k.dscr("YML", [L, D_ML], BF16)


def stage4a(k):
    nc, P, t = k.nc, k.P, k.t
    PADW = 2 + CTX + 2 + 2 + L + 2
    with ExitStack() as st:
        sb = lambda n, s, d=F32: st.enter_context(nc.sbuf_tensor(n, s, d))
        identf = sb("c_idf", [128, 128])
        pp = [sb("c_pp%d" % i, [128, PADW], BF16) for i in range(2)]
        cw = [sb("c_cw%d" % i, [128, 5]) for i in range(2)]
        cb_ = [sb("c_cb%d" % i, [128, 1]) for i in range(2)]
        dg = [sb("c_dg%d" % i, [128, 5, 128], BF16) for i in range(2)]
        of = [sb("c_of%d" % i, [128, 512]) for i in range(2)]
        ob = [sb("c_ob%d" % i, [128, NT], BF16) for i in range(2)]
        ps = [st.enter_context(nc.psum_tensor("c_ps%d" % i, [128, 512], F32)) for i in range(2)]
        P.dma("sp", identf[:], t["ident_f"][:, :], w=["identf"])
        for b in range(2):
            for (a0, a1) in ((0, 2), (2 + CTX, 2 + CTX + 4), (PADW - 2, PADW)):
                P.add("pool", lambda e, b=b, a0=a0, a1=a1: e.memset(pp[b][:, a0:a1], 0.0), w=[("pad", b)])
        segs = [(0, CTX, 2)] + [(CTX + 512 * i, 512, 2 + CTX + 4 + 512 * i) for i in range(16)]
        for cb in range(16):
            b = cb % 2
            P.dma("sp", pp[b][:, 2:2 + CTX], t["QKPRE"][cb][:, 0:CTX], r=[("QKPRE", 0), ("pad", b)], w=[("pp", b)])
            P.dma("act", pp[b][:, 2 + CTX + 4:2 + CTX + 4 + L], t["QKPRE"][cb][:, CTX:NT],
                  r=[("QKPRE", g) for g in range(1, 17)] + [("pad", b)], w=[("pp", b)])
            P.dma("sp", cw[b][:], t["convw"][cb], w=[("cw", b)])
            P.dma("sp", cb_[b][:], t["convb"][cb], w=[("cb", b)])
            for kk in range(5):
                P.add("dve", lambda e, b=b, kk=kk: e.tensor_scalar_mul(dg[b][:, kk, :], identf[:], cw[b][:, kk:kk + 1]),
                      r=["identf", ("cw", b)], w=[("dg", b)])
            for si, (t0, n, po) in enumerate(segs):
                pb = si % 2
                for kk in range(5):
                    P.add("pe", lambda e, pb=pb, b=b, kk=kk, po=po, n=n: e.matmul(
                        ps[pb][:, 0:n], dg[b][:, kk, :], pp[b][:, po + kk - 2:po + kk - 2 + n], start=(kk == 0), stop=(kk == 4)),
                        r=[("dg", b), ("pp", b)], w=[("ps", pb)])
                if cb < 8:
                    P.add("act", lambda e, pb=pb, b=b, n=n: e.activation(out=of[pb][:, 0:n], in_=ps[pb][:, 0:n], func=AF.Silu,
                                                                         bias=cb_[b][:, 0:1], scale=1.0),
                          r=[("ps", pb), ("cb", b)], w=[("of", pb)])
                    P.add("dve", lambda e, pb=pb, b=b, n=n, t0=t0: e.tensor_scalar_mul(ob[b][:, t0:t0 + n], of[pb][:, 0:n], 1.0 / 16.0),
                          r=[("of", pb)], w=[("ob", b)])
                else:
                    P.add("act", lambda e, pb=pb, b=b, n=n, t0=t0: e.activation(out=ob[b][:, t0:t0 + n], in_=ps[pb][:, 0:n], func=AF.Silu,
                                                                               bias=cb_[b][:, 0:1], scale=1.0),
                          r=[("ps", pb), ("cb", b)], w=[("ob", b)])
            P.dma(k.q(), t["QKT"][cb], ob[b][:], r=[("ob", b)], w=[("QKT", cb)])
        P.flush()


def stage4b(k):
    nc, P, t = k.nc, k.P, k.t
    NG_ = NTILE * NH
    with ExitStack() as st:
        sb = lambda n, s, d=F32: st.enter_context(nc.sbuf_tensor(n, s, d))
        G = sb("m_G", [128, NTILE, 16])
        gb = sb("m_gb", [128, 16])
        tri = sb("m_tri", [128, 128])
        trit = sb("m_trit", [128, 128])
        ones = sb("m_ones", [128, 128])
        trib = sb("m_trib", [128, 2, 128], BF16)
        identb = sb("m_idb", [128, 128], BF16)
        one1 = sb("m_one1", [128, 1])
        LF = sb("m_LF", [128, 2, NTILE, NH])
        SC = sb("m_SC", [128, 2, 4, NTILE, NH])
        tmpg = sb("m_tmpg", [128, NTILE, NH])
        pso_full = [st.enter_context(nc.psum_tensor("m_pso%d" % i, [128, NG_], F32)) for i in range(2)]
        psg = pso_full
        P.dma("sp", G[:], t["GATES"][:, :, :], r=["GATES"], w=["G"])
        P.dma("sp", gb[:], t["gateb"][:, :], w=["gb"])
        P.dma("act", tri[:], t["c_tri"][:, :], w=["tri"])
        P.dma("act", trit[:], t["c_trit"][:, :], w=["trit"])
        P.dma("sp", ones[:], t["c_ones"][:, :], w=["ones"])
        P.dma("sp", identb[:], t["ident_bf"][:, :], w=["identb"])
        P.add("pool", lambda e: e.memset(one1[:], 1.0), w=["one1"])
        P.add("dve", lambda e: e.tensor_copy(trib[:, 0, :], tri[:]), r=["tri"], w=["trib"])
        P.add("dve", lambda e: e.tensor_copy(trib[:, 1, :], trit[:]), r=["trit"], w=["trib"])
        P.add("dve", lambda e: e.tensor_tensor(G[:], G[:], gb[:].unsqueeze(1).broadcast_to([128, NTILE, 16]), ALU.add),
              r=["G", "gb"], w=["G"])
        for d_ in range(2):
            ipre = G[:, :, 8 * d_:8 * d_ + 4]
            fpre = G[:, :, 8 * d_ + 4:8 * d_ + 8]
            lf = LF[:, d_]
            P.add("act", lambda e, lf=lf, fpre=fpre: e.activation(out=lf, in_=fpre, func=AF.Exp, scale=-1.0), r=["G"], w=[("LF", d_)])
            P.add("act", lambda e, lf=lf: e.activation(out=lf, in_=lf, func=AF.Ln, bias=one1[:, 0:1], scale=1.0),
                  r=[("LF", d_), "one1"], w=[("LF", d_)])
            lff = lf.rearrange("p n h -> p (n h)")
            P.add("dve", lambda e, lff=lff: e.tensor_scalar_mul(lff, lff, -1.0), r=[("LF", d_)], w=[("LF", d_)])
            m_ = tri if d_ == 0 else trit
            P.add("pe", lambda e, m_=m_, lff=lff: e.matmul(psg[0][:], m_[:], lff, start=True, stop=True),
                  r=["tri", "trit", ("LF", d_)], w=["psg0"])
            P.add("pe", lambda e, lff=lff: e.matmul(psg[1][:], ones[:], lff, start=True, stop=True),
                  r=["ones", ("LF", d_)], w=["psg1"])
            u_ = SC[:, d_, 0].rearrange("p n h -> p (n h)")
            fl_ = SC[:, d_, 1].rearrange("p n h -> p (n h)")
            dec_ = SC[:, d_, 2].rearrange("p n h -> p (n h)")
            wt_ = SC[:, d_, 3].rearrange("p n h -> p (n h)")
            tg = tmpg[:].rearrange("p n h -> p (n h)")
            P.add("dve", lambda e, ipre=ipre: e.tensor_copy(tmpg[:], ipre), r=["G"], w=["tmpg"])
            P.add("dve", lambda e, tg=tg: e.tensor_tensor(tg, tg, psg[0][:], ALU.subtract), r=["tmpg", "psg0"], w=["tmpg"])
            P.add("act", lambda e, u_=u_, tg=tg: e.activation(out=u_, in_=tg, func=AF.Exp), r=["tmpg"], w=[("SC", d_)])
            P.add("act", lambda e, fl_=fl_: e.activation(out=fl_, in_=psg[0][:], func=AF.Exp, scale=-1.0), r=["psg0"], w=[("SC", d_)])
            P.add("act", lambda e, dec_=dec_: e.activation(out=dec_, in_=psg[1][:], func=AF.Exp), r=["psg1"], w=[("SC", d_)])
            P.add("dve", lambda e, wt_=wt_, u_=u_, dec_=dec_: e.tensor_tensor(wt_, u_, dec_, ALU.mult), r=[("SC", d_)], w=[("SC", d_)])
        QT = sb("m_QT", [128, 2, NT], BF16)
        KT = sb("m_KT", [128, 2, NT], BF16)
        Vh = sb("m_Vh", [128, NTILE, DH + 1], BF16)
        CT = [sb("m_CT%d" % i, [128, 2, DH + 1]) for i in range(2)]
        CTb = [sb("m_CTb%d" % i, [128, 2, DH + 1], BF16) for i in range(2)]
        kt = [sb("m_kt%d" % i, [128, DH], BF16) for i in range(2)]
        SW = [sb("m_SW%d" % i, [128, 128], BF16) for i in range(2)]
        vw = [sb("m_vw%d" % i, [128, DH + 1], BF16) for i in range(2)]
        dn = [sb("m_dn%d" % i, [128, 2]) for i in range(2)]
        ho = [sb("m_ho%d" % i, [128, DH]) for i in range(2)]
        pst = [st.enter_context(nc.psum_tensor("m_pst%d" % i, [128, DH], BF16)) for i in range(2)]
        pss = [st.enter_context(nc.psum_tensor("m_pss%d" % i, [128, 128], F32)) for i in range(2)]
        pso = [pso_full[i][:, 0:DH + 1] for i in range(2)]
        psc_ = [st.enter_context(nc.psum_tensor("m_psc%d" % i, [128, DH + 1], F32)) for i in range(2)]
        for h in range(NH):
            for dc in range(2):
                P.dma("sp", QT[:, dc, :], t["QKT"][2 * h + dc], r=[("QKT", 2 * h + dc)], w=["QT"])
                P.dma("act", KT[:, dc, :], t["QKT"][8 + 2 * h + dc], r=[("QKT", 8 + 2 * h + dc)], w=["KT"])
            for n0 in range(0, NTILE, 11):
                P.dma(k.q(), Vh[:, n0:n0 + 11, 0:DH],
                      t["V"].rearrange("(n p) c -> p n c", p=128)[:, n0:n0 + 11, h * DH:(h + 1) * DH],
                      r=[("V", ti) for ti in range(n0, n0 + 11)], w=["Vh"])
            P.add("pool", lambda e: e.memset(Vh[:, :, DH:DH + 1], 1.0), w=["Vh1"])
            for d_ in range(2):
                P.add("pool", lambda e, d_=d_: e.memset(CT[d_][:], 0.0), w=[("CT", d_)])
                P.add("pool", lambda e, d_=d_: e.memset(CTb[d_][:], 0.0), w=[("CTb", d_)])
            order_f = list(range(NTILE))
            order_b = [1, 0] + list(range(NTILE - 1, 1, -1))
            for step in range(NTILE):
                for d_ in range(2):
                    ti = order_f[step] if d_ == 0 else order_b[step]
                    c0 = ti * 128
                    col = ti * NH + h
                    sc = lambda kind, d_=d_, col=col: SC[:, d_, kind].rearrange("p n h -> p (n h)")[:, col:col + 1]
                    for dc in range(2):
                        P.add("pe", lambda e, d_=d_, dc=dc, c0=c0: e.transpose(pst[d_][:, dc * 128:(dc + 1) * 128],
                                                                               KT[:, dc, c0:c0 + 128], identb[:]),
                              r=["KT", "identb"], w=[("pst", d_)])
                    P.add("act", lambda e, d_=d_: e.copy(kt[d_][:], pst[d_][:]), r=[("pst", d_)], w=[("kt", d_)])
                    if ti >= 2:
                        for dc in range(2):
                            P.add("pe", lambda e, d_=d_, dc=dc, c0=c0: e.matmul(pss[d_][:], KT[:, dc, c0:c0 + 128], QT[:, dc, c0:c0 + 128],
                                                                                start=(dc == 0), stop=(dc == 1)),
                                  r=["KT", "QT"], w=[("pss", d_)])
                        P.add("dve", lambda e, d_=d_, sc=sc: e.scalar_tensor_tensor(SW[d_][:], pss[d_][:], sc(0), trib[:, d_, :],
                                                                                    ALU.mult, ALU.mult),
                              r=[("pss", d_), ("SC", d_), "trib"], w=[("SW", d_)])
                        P.add("pe", lambda e, d_=d_, ti=ti: e.matmul(pso[d_][:], SW[d_][:], Vh[:, ti, :], start=True, stop=False),
                              r=[("SW", d_), "Vh", "Vh1"], w=[("pso", d_), "psg%d" % d_])
                        for dc in range(2):
                            P.add("pe", lambda e, d_=d_, dc=dc, c0=c0: e.matmul(pso[d_][:], QT[:, dc, c0:c0 + 128], CTb[d_][:, dc, :],
                                                                                start=False, stop=(dc == 1)),
                                  r=["QT", ("CTb", d_)], w=[("pso", d_)])
                        P.add("act", lambda e, d_=d_: e.activation(out=dn[d_][:, 0:1], in_=pso[d_][:, DH:DH + 1], func=AF.Abs),
                              r=[("pso", d_)], w=[("dn", d_)])
                        P.add("dve", lambda e, d_=d_, sc=sc: e.tensor_tensor(dn[d_][:, 0:1], dn[d_][:, 0:1], sc(1), ALU.max),
                              r=[("dn", d_), ("SC", d_)], w=[("dn", d_)])
                        P.add("dve", lambda e, d_=d_: e.reciprocal(dn[d_][:, 1:2], dn[d_][:, 0:1]), r=[("dn", d_)], w=[("dn1", d_)])
                        P.add("act", lambda e, d_=d_: e.activation(out=ho[d_][:], in_=pso[d_][:, 0:DH], func=AF.Copy,
                                                                   scale=dn[d_][:, 1:2]),
                              r=[("pso", d_), ("dn1", d_)], w=[("ho", d_)])
                        P.dma("sp", t["HDIR"][h, d_, (ti - 2) * 128:(ti - 1) * 128, :], ho[d_][:], r=[("ho", d_)],
                              w=[("HDIR", h, d_, ti)])
                    P.add("dve", lambda e, d_=d_, ti=ti, sc=sc: e.tensor_scalar_mul(vw[d_][:], Vh[:, ti, :], sc(3)),
                          r=["Vh", "Vh1", ("SC", d_)], w=[("vw", d_)])
                    for dc in range(2):
                        pc = psc_[dc]
                        P.add("pe", lambda e, d_=d_, dc=dc, pc=pc: e.matmul(pc[:], kt[d_][:, dc * 128:(dc + 1) * 128], vw[d_][:],
                                                                            start=True, stop=True),
                              r=[("kt", d_), ("vw", d_)], w=[("psc", dc)])
                        P.add("dve", lambda e, d_=d_, dc=dc, pc=pc, sc=sc: e.scalar_tensor_tensor(
                            CT[d_][:, dc, :], CT[d_][:, dc, :], sc(2), pc[:], ALU.mult, ALU.add),
                            r=[("psc", dc), ("CT", d_), ("SC", d_)], w=[("CT", d_)])
                        P.add("act", lambda e, d_=d_, dc=dc: e.copy(CTb[d_][:, dc, :], CT[d_][:, dc, :]),
                              r=[("CT", d_)], w=[("CTb", d_)])
        P.flush()


def stage4c(k):
    nc, P, t = k.nc, k.P, k.t
    with ExitStack() as st:
        sb = lambda n, s, d=F32: st.enter_context(nc.sbuf_tensor(n, s, d))
        ng = sb("n_ng", [128, D_ML])
        epst = sb("n_eps", [128, 1])
        hf = [sb("n_hf%d" % i, [128, NH, DH]) for i in range(2)]
        hb = [sb("n_hb%d" % i, [128, NH, DH]) for i in range(2)]
        so = [sb("n_so%d" % i, [128, D_ML], BF16) for i in range(2)]
        junk = sb("n_junk", [128, DH])
        ss = [sb("n_ss%d" % i, [128, 2, NH]) for i in range(2)]
        yo = [sb("n_yo%d" % i, [128, D_ML], BF16) for i in range(2)]
        P.dma("sp", ng[:], t["mlng"][:, :], w=["ng"])
        P.add("pool", lambda e: e.memset(epst[:], EPS), w=["epst"])
        for i in range(L // 128):
            b = i % 2
            P.dma("sp", hf[b][:], t["HDIR"][:, 0, i * 128:(i + 1) * 128, :].rearrange("h p d -> p h d"),
                  r=[("HDIR", h, 0, i + 2) for h in range(NH)], w=[("hf", b)])
            P.dma("act", hb[b][:], t["HDIR"][:, 1, i * 128:(i + 1) * 128, :].rearrange("h p d -> p h d"),
                  r=[("HDIR", h, 1, i + 2) for h in range(NH)], w=[("hb", b)])
            P.dma("sp", so[b][:], t["SO"][(i + 2) * 128:(i + 3) * 128, :], r=[("SO", i + 2)], w=[("so", b)])
            hff = hf[b][:].rearrange("p h d -> p (h d)")
            hbf = hb[b][:].rearrange("p h d -> p (h d)")
            P.add("dve", lambda e, hff=hff, hbf=hbf: e.tensor_tensor(hff, hff, hbf, ALU.add), r=[("hf", b), ("hb", b)], w=[("hf", b)])
            for h in range(NH):
                P.add("act", lambda e, b=b, h=h: e.activation(out=junk[:], in_=hf[b][:, h, :], func=AF.Square, scale=float(DH ** -0.5),
                                                              accum_out=ss[b][:, 0, h:h + 1]),
                      r=[("hf", b)], w=["junk", ("ss", b)])
            P.add("act", lambda e, b=b: e.activation(out=ss[b][:, 1, :], in_=ss[b][:, 0, :], func=AF.Sqrt, bias=epst[:, 0:1], scale=1.0),
                  r=[("ss", b), "epst"], w=[("ss1", b)])
            P.add("dve", lambda e, b=b: e.reciprocal(ss[b][:, 1, :], ss[b][:, 1, :]), r=[("ss1", b)], w=[("ss1", b)])
            for h in range(NH):
                P.add("dve", lambda e, b=b, h=h: e.scalar_tensor_tensor(hf[b][:, h, :], hf[b][:, h, :], ss[b][:, 1, h:h + 1],
                                                                        ng[:, h * DH:(h + 1) * DH], ALU.mult, ALU.mult),
                      r=[("hf", b), ("ss1", b), "ng"], w=[("hf", b)])
            P.add("pool", lambda e, b=b, hff=hff: e.tensor_tensor(yo[b][:], hff, so[b][:], ALU.mult), r=[("hf", b), ("so", b)], w=[("yo", b)])
            P.dma("act", t["YML"][i * 128:(i + 1) * 128, :], yo[b][:], r=[("yo", b)], w=[("YML", i)])
        P.flush()


def declare_s5post(k):
    k.din("gluw", [D_S5, D_S5])
    k.din("glub", [128, 8])
    k.din("w_out", [D, D])
    k.din("rw", [128, 16, NE])
    k.dscr("HX2", [L, D], BF16)
    k.dscr("X1", [L, D])
    k.dscr("AFFD", [128, L // 128, NE])


def stage5(k):
    nc, P, t = k.nc, k.P, k.t
    with ExitStack() as st:
        sb = lambda n, s, d=F32: st.enter_context(nc.sbuf_tensor(n, s, d))
        Wg = sb("p_Wg", [128, 8, D_S5], BF16)
        Wo = sb("p_Wo", [128, 16, D], BF16)
        M2 = [sb("p_M%d" % i, [128, D]) for i in range(3)]
        RW = sb("p_RW", [128, 16, NE])
        glub = sb("p_glub", [128, 8])
        identb = sb("p_idb", [128, 128], BF16)
        identf = sb("p_idf", [128, 128])
        epst = sb("p_eps", [128, 1])
        ygT = [sb("p_yg%d" % i, [128, 8, 512], BF16) for i in range(2)]
        sig = sb("p_sig", [128, 512], BF16)
        yglu = sb("p_yglu", [128, 8, 512], BF16)
        yml = [sb("p_yml%d" % i, [128, D_ML], BF16) for i in range(2)]
        ymlT = [sb("p_ymlT%d" % i, [128, 8, 128], BF16) for i in range(2)]
        yx = sb("p_yx", [128, D])
        xt = sb("p_xt", [128, D])
        x1 = sb("p_x1", [128, D])
        hx2 = sb("p_hx2", [128, D])
        hx2b = sb("p_hx2b", [128, D], BF16)
        junk = sb("p_junk", [128, D], BF16)
        hx2T = sb("p_hx2T", [128, 16, 128])
        ss = sb("p_ss", [128, 8])
        AFF = sb("p_AFF", [128, L // 128, NE])
        pw = [st.enter_context(nc.psum_tensor("p_pw%d" % i, [128, 512], F32)) for i in range(4)]
        pz = [st.enter_context(nc.psum_tensor("p_pz%d" % i, [128, 512], F32)) for i in range(2)]
        pt = st.enter_context(nc.psum_tensor("p_pt", [128, 8, 128], BF16))
        pl = st.enter_context(nc.psum_tensor("p_pl", [128, NE], F32))
        gw = t["gluw"].rearrange("(c p) n -> p c n", p=128)
        for c in range(8):
            P.dma("pool", Wg[:, c, :], gw[:, c, :], w=[("Wg", c)])
        wo = t["w_out"].rearrange("(c p) n -> p c n", p=128)
        for c in range(16):
            for h_ in range(2):
                P.dma("pool", Wo[:, c, h_ * 1024:(h_ + 1) * 1024], wo[:, c, h_ * 1024:(h_ + 1) * 1024], w=[("Wo", c, h_)])
        for i in range(3):
            P.dma("sp", M2[i][:], t["MODS2"][i], r=["M2_%d" % i], w=[("M2", i)])
        P.dma("sp", RW[:], t["rw"][:, :, :], w=["RW"])
        P.dma("sp", glub[:], t["glub"][:, :], w=["glub"])
        P.dma("act", identb[:], t["ident_bf"][:, :], w=["identb"])
        P.dma("act", identf[:], t["ident_f"][:, :], w=["identf"])
        P.add("pool", lambda e: e.memset(epst[:], EPS), w=["epst"])
        ygsrc = t["YG"].rearrange("c p t -> p c t")
        ymlsrc = t["YML"].rearrange("(w r) c -> r w c", r=128)
        ymlkeys = [("YML", i) for i in range(L // 128)]
        for g5 in range(16):
            gb_ = g5 % 2
            P.dma(k.q(), ygT[gb_][:], ygsrc[:, :, g5 * 512:(g5 + 1) * 512], r=[("YG", blk, g5) for blk in range(8)], w=[("ygT", gb_)])
            for c2 in range(8):
                pzz = pz[c2 % 2]
                for c in range(8):
                    P.add("pe", lambda e, pzz=pzz, c=c, c2=c2, gb_=gb_: e.matmul(pzz[:], Wg[:, c, c2 * 128:(c2 + 1) * 128], ygT[gb_][:, c, :],
                                                                                 start=(c == 0), stop=(c == 7)),
                          r=[("Wg", c), ("ygT", gb_)], w=[("pz", c2 % 2)])
                P.add("act", lambda e, pzz=pzz, c2=c2: e.activation(out=sig[:], in_=pzz[:], func=AF.Sigmoid, bias=glub[:, c2:c2 + 1], scale=1.0),
                      r=[("pz", c2 % 2), "glub"], w=["sig"])
                P.add("dve", lambda e, c2=c2, gb_=gb_: e.tensor_tensor(yglu[:, c2, :], ygT[gb_][:, c2, :], sig[:], ALU.mult),
                      r=["sig", ("ygT", gb_)], w=["yglu"])
            for j in range(4):
                ti = g5 * 4 + j
                tb_ = ti % 2
                r0 = ti * 2
                for a in range(2):
                    P.dma(k.q(), yml[tb_][64 * a:64 * (a + 1), :], ymlsrc[r0 + a], r=ymlkeys, w=[("yml", tb_)])
                for c in range(8):
                    P.add("pe", lambda e, c=c, tb_=tb_: e.transpose(pt[:, c, :], yml[tb_][:, c * 128:(c + 1) * 128], identb[:]),
                          r=[("yml", tb_), "identb"], w=["pt"])
                P.add("act", lambda e, tb_=tb_: e.copy(ymlT[tb_][:], pt[:]), r=["pt"], w=[("ymlT", tb_)])
                for nb in range(4):
                    for c in range(16):
                        lhs = yglu[:, c, j * 128:(j + 1) * 128] if c < 8 else ymlT[tb_][:, c - 8, :]
                        P.add("pe", lambda e, nb=nb, c=c, lhs=lhs: e.matmul(pw[nb][:], lhs, Wo[:, c, nb * 512:(nb + 1) * 512],
                                                                            start=(c == 0), stop=(c == 15)),
                              r=["yglu", ("ymlT", tb_), ("Wo", c, nb // 2)], w=[("pw", nb)])
                    if nb % 2 == 0:
                        P.add("act", lambda e, nb=nb: e.copy(yx[:, nb * 512:(nb + 1) * 512], pw[nb][:]), r=[("pw", nb)], w=[("yx", nb)])
                    else:
                        P.add("dve", lambda e, nb=nb: e.tensor_copy(yx[:, nb * 512:(nb + 1) * 512], pw[nb][:]), r=[("pw", nb)], w=[("yx", nb)])
                yxk = [("yx", nb) for nb in range(4)]
                P.dma("sp", xt[:], t["x"][ti * 128:(ti + 1) * 128, :], w=["xt"])
                P.add("act", lambda e: e.activation(out=junk[:], in_=yx[:], func=AF.Square, scale=float(D ** -0.5), accum_out=ss[:, 0:1]),
                      r=yxk, w=["junk", "ss0"])
                P.add("act", lambda e: e.activation(out=ss[:, 1:2], in_=ss[:, 0:1], func=AF.Sqrt, bias=epst[:, 0:1], scale=1.0),
                      r=["ss0", "epst"], w=["ss1"])
                P.add("dve", lambda e: e.reciprocal(ss[:, 1:2], ss[:, 1:2]), r=["ss1"], w=["ss1"])
                P.add("dve", lambda e: e.scalar_tensor_tensor(x1[:], yx[:], ss[:, 1:2], M2[0][:], ALU.mult, ALU.mult),
                      r=yxk + ["ss1", ("M2", 0)], w=["x1"])
                P.add("pool", lambda e: e.tensor_tensor(x1[:], x1[:], xt[:], ALU.add), r=["x1", "xt"], w=["x1"])
                P.dma("act", t["X1"][ti * 128:(ti + 1) * 128, :], x1[:], r=["x1"], w=[("X1", ti)])
                P.add("act", lambda e: e.activation(out=junk[:], in_=x1[:], func=AF.Square, scale=float(D ** -0.5), accum_out=ss[:, 2:3]),
                      r=["x1"], w=["junk", "ss2"])
                P.add("act", lambda e: e.activation(out=ss[:, 3:4], in_=ss[:, 2:3], func=AF.Sqrt, bias=epst[:, 0:1], scale=1.0),
                      r=["ss2", "epst"], w=["ss3"])
                P.add("dve", lambda e: e.reciprocal(ss[:, 3:4], ss[:, 3:4]), r=["ss3"], w=["ss3"])
                P.add("dve", lambda e: e.scalar_tensor_tensor(hx2[:], x1[:], ss[:, 3:4], M2[1][:], ALU.mult, ALU.mult),
                      r=["x1", "ss3", ("M2", 1)], w=["hx2"])
                P.add("pool", lambda e: e.tensor_tensor(hx2[:], hx2[:], M2[2][:], ALU.add), r=["hx2", ("M2", 2)], w=["hx2"])
                P.add("act", lambda e: e.copy(hx2b[:], hx2[:]), r=["hx2"], w=["hx2b"])
                P.dma("sp", t["HX2"][ti * 128:(ti + 1) * 128, :], hx2b[:], r=["hx2b"], w=[("HX2", ti)])
                for g4 in range(4):
                    pzz = pz[g4 % 2]
                    for c in range(4):
                        kc = g4 * 4 + c
                        P.add("pe", lambda e, pzz=pzz, c=c, kc=kc: e.transpose(pzz[:, c * 128:(c + 1) * 128], hx2[:, kc * 128:(kc + 1) * 128], identf[:]),
                              r=["hx2", "identf"], w=[("pz", g4 % 2)])
                    P.add("dve", lambda e, pzz=pzz, g4=g4: e.tensor_copy(hx2T[:, g4 * 4:(g4 + 1) * 4, :].rearrange("p c t -> p (c t)"), pzz[:]),
                          r=[("pz", g4 % 2)], w=[("hx2T", g4)])
                for kc in range(16):
                    P.add("pe", lambda e, kc=kc: e.matmul(pl[:], hx2T[:, kc, :], RW[:, kc, :], start=(kc == 0), stop=(kc == 15)),
                          r=[("hx2T", kc // 4), "RW"], w=["pl"])
                P.add("dve", lambda e: e.tensor_reduce(ss[:, 4:5], pl[:], AX.X, ALU.max), r=["pl"], w=["ss4"])
                P.add("dve", lambda e: e.tensor_scalar_mul(ss[:, 4:5], ss[:, 4:5], -1.0), r=["ss4"], w=["ss4"])
                P.add("act", lambda e, ti=ti: e.activation(out=AFF[:, ti, :], in_=pl[:], func=AF.Exp, bias=ss[:, 4:5], scale=1.0,
                                                           accum_out=ss[:, 5:6]),
                      r=["pl", "ss4"], w=["AFF", "ss5"])
                P.add("dve", lambda e: e.reciprocal(ss[:, 5:6], ss[:, 5:6]), r=["ss5"], w=["ss5"])
                P.add("dve", lambda e, ti=ti: e.tensor_scalar_mul(AFF[:, ti, :], AFF[:, ti, :], ss[:, 5:6]), r=["AFF", "ss5"], w=["AFF"])
        P.dma("sp", t["AFFD"][:, :, :], AFF[:], r=["AFF"], w=["AFFD"])
        P.flush()


def declare_moe(k):
    k.din("ewg", [NE, D, FF])
    k.din("ewu", [NE, D, FF])
    k.din("ewd", [NE, FF, D])
    k.din("ownidx", [128, OWN // 128], I32)
    k.din("c_slt", [128, 128], BF16)
    k.din("c_onesb", [128, 128], BF16)
    k.din("c_iota", [128, CAPL])
    k.dscr("AFFT", [L, NE])
    k.dscr("TH", [128, NE])
    k.dscr("YGALL", [NE, 3, 128, D], BF16)
    k.dscr("OHTALL", [OWN // 128, 128, NE * 3, 128], BF16)
    k.dscr("MOEO", [OWN, D])


def stage6(k):
    nc, P, t = k.nc, k.P, k.t
    NTL = L // 128
    with ExitStack() as st:
        sb = lambda n, s, d=F32: st.enter_context(nc.sbuf_tensor(n, s, d))
        A = sb("t_A", [128, NTL, NE])
        cmp_ = sb("t_cmp", [128, NTL, NE], BF16)
        onesb = sb("t_onesb", [128, 128])
        v = sb("t_v", [128, 8, NE])
        cntp = sb("t_cntp", [128, NE])
        pc = st.enter_context(nc.psum_tensor("t_pc", [128, NE], F32))
        P.dma("sp", A[:], t["AFFD"][:, :, :], r=["AFFD"], w=["A"])
        P.dma("act", t["AFFT"].rearrange("(n p) e -> p n e", p=128), A[:], r=["A"], w=["AFFT"])
        P.dma("sp", onesb[:], t["c_ones"][:, :], w=["onesb"])
        P.add("pool", lambda e: e.memset(v[:, 0, :], 0.0), w=["lo"])
        P.add("pool", lambda e: e.memset(v[:, 1, :], 1.0), w=["hi"])
        lo, hi, th, ge, d1, d2 = (v[:, i, :] for i in range(6))
        for it in range(30):
            P.add("dve", lambda e: e.tensor_tensor(th, lo, hi, ALU.add), r=["lo", "hi"], w=["th"])
            P.add("dve", lambda e: e.tensor_scalar_mul(th, th, 0.5), r=["th"], w=["th"])
            P.add("dve", lambda e: e.tensor_tensor(cmp_[:], A[:], th.unsqueeze(1).broadcast_to([128, NTL, NE]), ALU.is_ge),
                  r=["A", "th"], w=["cmp"])
            P.add("dve", lambda e: e.tensor_reduce(cntp[:], cmp_[:].rearrange("p n e -> p e n"), AX.X, ALU.add), r=["cmp"], w=["cntp"])
            P.add("pe", lambda e: e.matmul(pc[:], onesb[:], cntp[:], start=True, stop=True), r=["onesb", "cntp"], w=["pc"])
            P.add("dve", lambda e: e.tensor_single_scalar(ge, pc[:], float(CAPE) - 0.5, ALU.is_gt), r=["pc"], w=["ge"])
            P.add("dve", lambda e: e.tensor_tensor(d1, th, lo, ALU.subtract), r=["th", "lo"], w=["d1"])
            P.add("dve", lambda e: e.tensor_tensor(d1, d1, ge, ALU.mult), r=["d1", "ge"], w=["d1"])
            P.add("dve", lambda e: e.tensor_tensor(d2, hi, th, ALU.subtract), r=["th", "hi"], w=["d2"])
            P.add("dve", lambda e: e.tensor_tensor(d2, d2, ge, ALU.mult), r=["d2", "ge"], w=["d2"])
            P.add("dve", lambda e: e.tensor_tensor(lo, lo, d1, ALU.add), r=["lo", "d1"], w=["lo"])
            P.add("dve", lambda e: e.tensor_tensor(hi, th, d2, ALU.add), r=["th", "d2"], w=["hi"])
        P.dma("sp", t["TH"][:, :], lo, r=["lo"], w=["TH"])
        P.flush()


def stage7(k):
    nc, P, t = k.nc, k.P, k.t
    NO = OWN // 128
    with ExitStack() as st:
        sb = lambda n, s, d=F32: st.enter_context(nc.sbuf_tensor(n, s, d))
        oidx = sb("e_oidx", [128, NO], I32)
        HX = sb("e_HX", [128, NO, D], BF16)
        Ao = sb("e_Ao", [128, NO, NE])
        th = sb("e_th", [128, NE])
        sel = sb("e_sel", [128, NO, NE])
        selb = sb("e_selb", [128, NO, NE], BF16)
        slot = sb("e_slot", [128, NO, NE])
        cum = sb("e_cum", [128, NO, NE])
        tot = sb("e_tot", [128, NO, NE])
        AHL = sb("e_AHL", [128, NO, NE, 2], BF16)
        ares = sb("e_ares", [128, NO, NE])
        slt = sb("e_slt", [128, 128], BF16)
        onesb = sb("e_onesb", [128, 128], BF16)
        identb = sb("e_idb", [128, 128], BF16)
        iota = sb("e_iota", [128, CAPL])
        onef = sb("e_onef", [128, 1])
        OH = sb("e_OH", [128, NO, CAPL], BF16)
        XT = sb("e_XT", [128, 16, CAPL], BF16)
        HT = sb("e_HT", [128, 16, CAPL], BF16)
        Wg = [sb("e_Wg%d" % i, [128, 16, 256], BF16) for i in range(2)]
        Wu = [sb("e_Wu%d" % i, [128, 16, 256], BF16) for i in range(2)]
        Wd = [sb("e_Wd%d" % i, [128, 16, 512], BF16) for i in range(2)]
        asb = sb("e_asb", [128, CAPL])
        gs = sb("e_gs", [128, 3, 2])
        Yg = sb("e_Yg", [128, 3, D], BF16)
        OHT = sb("e_OHT", [128, NO, 3, 128], BF16)
        pa = [st.enter_context(nc.psum_tensor("e_pa%d" % i, [128, 512], F32)) for i in range(2)]
        pu = [st.enter_context(nc.psum_tensor("e_pu%d" % i, [128, 512], F32)) for i in range(2)]
        py = [st.enter_context(nc.psum_tensor("e_py%d" % i, [128, 512], F32)) for i in range(2)]
        pg = st.enter_context(nc.psum_tensor("e_pg", [128, 3, 2], F32))
        pt = st.enter_context(nc.psum_tensor("e_pt", [128, 3, 128], BF16))
        P.dma("sp", oidx[:], t["ownidx"][:, :], w=["oidx"])
        P.dma("sp", th[:], t["TH"][:, :], r=["TH"], w=["th"])
        P.dma("act", slt[:], t["c_slt"][:, :], w=["slt"])
        P.dma("act", onesb[:], t["c_onesb"][:, :], w=["onesb"])
        P.dma("act", identb[:], t["ident_bf"][:, :], w=["identb"])
        P.dma("sp", iota[:], t["c_iota"][:, :], w=["iota"])
        P.add("pool", lambda e: e.memset(onef[:], 1.0), w=["onef"])
        hx2keys = [("HX2", ti) for ti in range(L // 128)]
        for i in range(NO):
            P.add("pool", lambda e, i=i: e.indirect_dma_start(out=HX[:, i, :], out_offset=None, in_=t["HX2"][:, :],
                                                             in_offset=bass.IndirectOffsetOnAxis(ap=oidx[:, i:i + 1], axis=0)),
                  r=["oidx"] + hx2keys, w=[("HX", i)], dma=True)
            P.add("pool", lambda e, i=i: e.indirect_dma_start(out=Ao[:, i, :], out_offset=None, in_=t["AFFT"][:, :],
                                                             in_offset=bass.IndirectOffsetOnAxis(ap=oidx[:, i:i + 1], axis=0)),
                  r=["oidx", "AFFT"], w=["Ao"], dma=True)
        fl = lambda a: a.rearrange("p n e -> p (n e)")
        P.add("dve", lambda e: e.tensor_tensor(sel[:], Ao[:], th[:].unsqueeze(1).broadcast_to([128, NO, NE]), ALU.is_ge),
              r=["Ao", "th"], w=["sel"])
        P.add("dve", lambda e: e.tensor_copy(selb[:], sel[:]), r=["sel"], w=["selb"])
        P.add("pe", lambda e: e.matmul(pa[0][:, 0:NO * NE], slt[:], fl(selb[:]), start=True, stop=True), r=["slt", "selb"], w=["pa0"])
        P.add("pe", lambda e: e.matmul(pu[0][:, 0:NO * NE], onesb[:], fl(selb[:]), start=True, stop=True), r=["onesb", "selb"], w=["pu0"])
        P.add("dve", lambda e: e.tensor_copy(fl(tot[:]), pu[0][:, 0:NO * NE]), r=["pu0"], w=["tot"])
        for e_ in range(NE):
            P.add("dve", lambda e, e_=e_: e.tensor_tensor_scan(cum[:, :, e_], onef[:, 0:1].broadcast_to([128, NO]), tot[:, :, e_], 0.0,
                                                               ALU.mult, ALU.add),
                  r=["tot", "onef"], w=["cum"])
        P.add("dve", lambda e: e.tensor_tensor(fl(slot[:]), fl(cum[:]), fl(tot[:]), ALU.subtract), r=["cum", "tot"], w=["slot"])
        P.add("dve", lambda e: e.tensor_tensor(fl(slot[:]), fl(slot[:]), pa[0][:, 0:NO * NE], ALU.add), r=["slot", "pa0"], w=["slot"])
        P.add("dve", lambda e: e.scalar_tensor_tensor(fl(slot[:]), fl(slot[:]), 1.0, fl(sel[:]), ALU.add, ALU.mult), r=["slot", "sel"], w=["slot"])
        P.add("dve", lambda e: e.tensor_scalar_add(fl(slot[:]), fl(slot[:]), -1.0), r=["slot"], w=["slot"])
        P.add("dve", lambda e: e.tensor_copy(AHL[:, :, :, 0], Ao[:]), r=["Ao"], w=["AHL0"])
        P.add("dve", lambda e: e.tensor_copy(ares[:], AHL[:, :, :, 0]), r=["AHL0"], w=["ares"])
        P.add("dve", lambda e: e.tensor_tensor(ares[:], Ao[:], ares[:], ALU.subtract), r=["Ao", "ares"], w=["ares"])
        P.add("dve", lambda e: e.tensor_copy(AHL[:, :, :, 1], ares[:]), r=["ares"], w=["AHL1"])
        hxk = [("HX", i) for i in range(NO)]
        nld = 0
        for e_ in range(NE):
            for i in range(NO):
                P.add("dve", lambda e, i=i, e_=e_: e.tensor_single_scalar(OH[:, i, :], iota[:], slot[:, i, e_:e_ + 1], ALU.is_equal),
                      r=["iota", "slot"], w=[("OH", i)])
            ohk = [("OH", i) for i in range(NO)]
            for kc in range(16):
                pp = pa[kc % 2]
                for i in range(NO):
                    P.add("pe", lambda e, pp=pp, i=i, kc=kc: e.matmul(pp[:, 0:CAPL], HX[:, i, kc * 128:(kc + 1) * 128], OH[:, i, :],
                                                                      start=(i == 0), stop=(i == NO - 1)),
                          r=[("HX", i), ("OH", i)], w=[("pa", kc % 2)])
                if kc % 2 == 0:
                    P.add("act", lambda e, pp=pp, kc=kc: e.copy(XT[:, kc, :], pp[:, 0:CAPL]), r=[("pa", kc % 2)], w=[("XT", kc)])
                else:
                    P.add("dve", lambda e, pp=pp, kc=kc: e.tensor_copy(XT[:, kc, :], pp[:, 0:CAPL]), r=[("pa", kc % 2)], w=[("XT", kc)])
            for sc in range(3):
                for i in range(NO):
                    P.add("pe", lambda e, sc=sc, i=i, e_=e_: e.matmul(pg[:, sc, :], OH[:, i, sc * 128:(sc + 1) * 128], AHL[:, i, e_, :],
                                                                      start=(i == 0), stop=(i == NO - 1)),
                          r=[("OH", i), "AHL0", "AHL1"], w=["pg"])
            P.add("dve", lambda e: e.tensor_copy(gs[:], pg[:]), r=["pg"], w=["gs"])
            P.add("dve", lambda e: e.tensor_tensor(gs[:, :, 0], gs[:, :, 0], gs[:, :, 1], ALU.add), r=["gs"], w=["gs"])
            for i in range(NO):
                for sc in range(3):
                    P.add("pe", lambda e, i=i, sc=sc: e.transpose(pt[:, sc, :], OH[:, i, sc * 128:(sc + 1) * 128], identb[:]),
                          r=[("OH", i), "identb"], w=["pt"])
                P.add("act", lambda e, i=i: e.copy(OHT[:, i, :, :], pt[:]), r=["pt"], w=[("OHT", i)])
                P.dma("sp", t["OHTALL"][i][:, e_ * 3:(e_ + 1) * 3, :], OHT[:, i, :, :], r=[("OHT", i)], w=[("OHTALL", i, e_)])
            xtk = [("XT", kc) for kc in range(16)]
            wgv = t["ewg"][e_].rearrange("(kc p) f -> p kc f", p=128)
            wuv = t["ewu"][e_].rearrange("(kc p) f -> p kc f", p=128)
            for fb in range(8):
                wb = nld % 2
                nld += 1
                P.dma("pool", Wg[wb][:], wgv[:, :, fb * 256:(fb + 1) * 256], w=[("Wg", wb)])
                P.dma("pool", Wu[wb][:], wuv[:, :, fb * 256:(fb + 1) * 256], w=[("Wu", wb)])
                for f2_ in range(2):
                    fc = fb * 2 + f2_
                    pb = fc % 2
                    for kc in range(16):
                        P.add("pe", lambda e, pb=pb, wb=wb, kc=kc, f2_=f2_: e.matmul(pa[pb][:, 0:CAPL], Wg[wb][:, kc, f2_ * 128:(f2_ + 1) * 128],
                                                                                     XT[:, kc, :], start=(kc == 0), stop=(kc == 15)),
                              r=[("Wg", wb), ("XT", kc)], w=[("pa", pb)])
                    for kc in range(16):
                        P.add("pe", lambda e, pb=pb, wb=wb, kc=kc, f2_=f2_: e.matmul(pu[pb][:, 0:CAPL], Wu[wb][:, kc, f2_ * 128:(f2_ + 1) * 128],
                                                                                     XT[:, kc, :], start=(kc == 0), stop=(kc == 15)),
                              r=[("Wu", wb), ("XT", kc)], w=[("pu", pb)])
                    P.add("act", lambda e, pb=pb: e.activation(out=asb[:], in_=pa[pb][:, 0:CAPL], func=AF.Silu), r=[("pa", pb)], w=["asb"])
                    P.add("dve", lambda e, pb=pb, fc=fc: e.tensor_tensor(HT[:, fc, :], asb[:], pu[pb][:, 0:CAPL], ALU.mult),
                          r=["asb", ("pu", pb)], w=[("HT", fc)])
            wdv = t["ewd"][e_].rearrange("(fc p) d -> p fc d", p=128)
            for db in range(4):
                wb = db % 2
                P.dma("pool", Wd[wb][:], wdv[:, :, db * 512:(db + 1) * 512], w=[("Wd", wb)])
                for sc in range(3):
                    pp = py[sc % 2]
                    for fc in range(16):
                        P.add("pe", lambda e, pp=pp, wb=wb, fc=fc, sc=sc: e.matmul(pp[:], HT[:, fc, sc * 128:(sc + 1) * 128], Wd[wb][:, fc, :],
                                                                                   start=(fc == 0), stop=(fc == 15)),
                              r=[("HT", fc), ("Wd", wb)], w=[("py", sc % 2)])
                    P.add("act", lambda e, pp=pp, sc=sc, db=db: e.activation(out=Yg[:, sc, db * 512:(db + 1) * 512], in_=pp[:], func=AF.Copy,
                                                                             scale=gs[:, sc, 0:1]),
                          r=[("py", sc % 2), "gs"], w=["Yg"])
            P.dma("act", t["YGALL"][e_].rearrange("c s d -> s c d"), Yg[:], r=["Yg"], w=[("YGALL", e_)])
        P.flush()


def stage8(k):
    nc, P, t = k.nc, k.P, k.t
    NO = OWN // 128
    with ExitStack() as st:
        sb = lambda n, s, d=F32: st.enter_context(nc.sbuf_tensor(n, s, d))
        YGb = sb("f_YG", [128, NE * 3, 512], BF16)
        OT = [sb("f_OT%d" % i, [128, NE * 3, 128], BF16) for i in range(2)]
        ob = [sb("f_ob%d" % i, [128, 512]) for i in range(2)]
        ps = [st.enter_context(nc.psum_tensor("f_ps%d" % i, [128, 512], F32)) for i in range(2)]
        ygv = t["YGALL"].rearrange("e c s d -> s (e c) d")
        for db in range(4):
            for e_ in range(NE):
                P.dma(k.q(), YGb[:, e_ * 3:(e_ + 1) * 3, :], ygv[:, e_ * 3:(e_ + 1) * 3, db * 512:(db + 1) * 512],
                      r=[("YGALL", e_)], w=[("YGb", e_)])
            for i in range(NO):
                b = i % 2
                P.dma(k.q(), OT[b][:], t["OHTALL"][i], r=[("OHTALL", i, e_) for e_ in range(NE)], w=[("OT", b)])
                for j in range(NE * 3):
                    P.add("pe", lambda e, b=b, j=j: e.matmul(ps[b][:], OT[b][:, j, :], YGb[:, j, :], start=(j == 0), stop=(j == NE * 3 - 1)),
                          r=[("OT", b), ("YGb", j // 3)], w=[("ps", b)])
                P.add("act", lambda e, b=b: e.copy(ob[b][:], ps[b][:]), r=[("ps", b)], w=[("ob", b)])
                P.dma("sp", t["MOEO"][i * 128:(i + 1) * 128, db * 512:(db + 1) * 512], ob[b][:], r=[("ob", b)], w=[("MOEO", i, db)])
        P.flush()


def stage9(k):
    nc, P, t = k.nc, k.P, k.t
    NO = OWN // 128
    with ExitStack() as st:
        sb = lambda n, s, d=F32: st.enter_context(nc.sbuf_tensor(n, s, d))
        GN3 = sb("g_GN3", [128, D])
        oidx = sb("g_oidx", [128, NO], I32)
        epst = sb("g_eps", [128, 1])
        mo = [sb("g_mo%d" % i, [128, D]) for i in range(2)]
        x1 = [sb("g_x1%d" % i, [128, D]) for i in range(2)]
        junk = sb("g_junk", [128, D], BF16)
        ss = [sb("g_ss%d" % i, [128, 2]) for i in range(2)]
        P.dma("sp", GN3[:], t["MODS2"][3], r=["M2_3"], w=["GN3"])
        P.dma("sp", oidx[:], t["ownidx"][:, :], w=["oidx"])
        P.add("pool", lambda e: e.memset(epst[:], EPS), w=["epst"])
        x1keys = [("X1", ti) for ti in range(L // 128)]
        outs = []
        for i in range(NO):
            b = i % 2
            P.dma("sp", mo[b][:], t["MOEO"][i * 128:(i + 1) * 128, :], r=[("MOEO", i, db) for db in range(4)], w=[("mo", b)])
            P.add("pool", lambda e, b=b, i=i: e.indirect_dma_start(out=x1[b][:], out_offset=None, in_=t["X1"][:, :],
                                                                  in_offset=bass.IndirectOffsetOnAxis(ap=oidx[:, i:i + 1], axis=0)),
                  r=["oidx"] + x1keys, w=[("x1", b)], dma=True)
            P.add("act", lambda e, b=b: e.activation(out=junk[:], in_=mo[b][:], func=AF.Square, scale=float(D ** -0.5), accum_out=ss[b][:, 0:1]),
                  r=[("mo", b)], w=["junk", ("ss", b)])
            P.add("act", lambda e, b=b: e.activation(out=ss[b][:, 1:2], in_=ss[b][:, 0:1], func=AF.Sqrt, bias=epst[:, 0:1], scale=1.0),
                  r=[("ss", b), "epst"], w=[("ss1", b)])
            P.add("dve", lambda e, b=b: e.reciprocal(ss[b][:, 1:2], ss[b][:, 1:2]), r=[("ss1", b)], w=[("ss1", b)])
            P.add("dve", lambda e, b=b: e.scalar_tensor_tensor(mo[b][:], mo[b][:], ss[b][:, 1:2], GN3[:], ALU.mult, ALU.mult),
                  r=[("mo", b), ("ss1", b), "GN3"], w=[("mo", b)])
            P.add("pool", lambda e, b=b: e.tensor_tensor(mo[b][:], mo[b][:], x1[b][:], ALU.add), r=[("mo", b), ("x1", b)], w=[("mo", b)])
            outs.append(P.dma("sp", t["out"][i * 128:(i + 1) * 128, :], mo[b][:], r=[("mo", b)], w=[("out", i)]))
        P.flush(final_wait=outs)


def _host_s5(inp):
    are, aim, ldt = inp["s5_a_re"][0], inp["s5_a_im"][0], inp["s5_log_dt"][0]
    bre, bim = inp["s5_b_re"][0], inp["s5_b_im"][0]
    cre, cim = inp["s5_c_re"][0], inp["s5_c_im"][0]

    def gp(a):
        sh = a.shape[:-2]
        a = a.reshape(*sh, 8, 4, 2, 64)
        a = np.moveaxis(a, [-4, -2, -1, -3], [0, 1, 2, -1])
        return a.reshape(8, 128, *sh, 4)

    s5p = np.concatenate([gp(are).reshape(8, 128, 8), gp(aim).reshape(8, 128, 8),
                          gp(np.broadcast_to(ldt[..., None], (2, 64, 64))).reshape(8, 128, 8)], -1)

    def bc(r, i, cfirst):
        out = []
        for a in (r, i):
            if cfirst:
                a = a.transpose(0, 2, 1)
            a = a.reshape(8, 4, 2, 64, 16).transpose(0, 2, 3, 1, 4).reshape(8, 128, 4, 16)
            out.append(a)
        return np.ascontiguousarray(np.stack(out, 2))

    d = {"s5p": np.ascontiguousarray(s5p.astype(np.float32)), "s5b": bc(bre, bim, False),
         "s5c": bc(cre, cim, True), "s5d": np.ascontiguousarray(inp["s5_d"][0].reshape(8, 128, 1))}
    d["c_jidx"] = np.broadcast_to(np.arange(17, dtype=np.float32), (128, 17)).copy()
    pm = np.zeros((128, 2), np.float32)
    pm[:64, 0] = 1
    pm[64:, 1] = 1
    d["c_parmask"] = pm
    d["c_blockmask"] = np.kron(np.eye(8, dtype=np.float32), np.ones((16, 16), np.float32))
    m = np.arange(NCH, dtype=np.float32)
    d["c_midx"] = np.broadcast_to(np.stack([m, m[::-1]]), (128, 2, NCH)).copy()
    d["c_rowmask"] = np.kron(np.eye(4, dtype=np.float32), np.ones((32, 1), np.float32))
    return d


def _core_inputs(inp, core, shared):
    b, j = divmod(core, 4)
    cond = np.stack([inp["c"][b], inp["c_ctx"]], -1).reshape(16, 128, 2).transpose(1, 0, 2)
    d = {"x": inp["x"][b], "ctx": inp["ctx"][b], "cond": np.ascontiguousarray(cond),
         "xown": inp["x"][b][j * OWN:(j + 1) * OWN]}
    d.update(shared)
    return d


def _host_rest(inp):
    d = {}
    d["convw"] = np.ascontiguousarray(inp["ml_conv_w"][0].T.reshape(16, 128, 5))
    d["convb"] = np.ascontiguousarray(inp["ml_conv_b"][0].reshape(16, 128, 1))
    d["gateb"] = np.broadcast_to(inp["ml_gate_b"][0].reshape(1, 16), (128, 16)).copy()
    d["mlng"] = np.broadcast_to(inp["ml_norm_g"][0][None], (128, D_ML)).copy()
    tri = np.triu(np.ones((128, 128), np.float32))
    d["c_tri"] = tri
    d["c_trit"] = np.ascontiguousarray(tri.T)
    d["c_ones"] = np.ones((128, 128), np.float32)
    d["gluw"] = inp["s5_glu_w"][0]
    d["w_out"] = inp["w_out"][0]
    d["glub"] = np.ascontiguousarray(inp["s5_glu_b"][0].reshape(8, 128).T)
    d["rw"] = np.ascontiguousarray(inp["router_w"][0].reshape(16, 128, 16).transpose(1, 0, 2))
    d["ewg"] = inp["exp_w_gate"][0]
    d["ewu"] = inp["exp_w_up"][0]
    d["ewd"] = inp["exp_w_down"][0]
    d["c_slt"] = np.triu(np.ones((128, 128), np.float32), 1).astype(ml_dtypes.bfloat16)
    d["c_onesb"] = np.ones((128, 128), ml_dtypes.bfloat16)
    d["c_iota"] = np.broadcast_to(np.arange(CAPL, dtype=np.float32), (128, CAPL)).copy()
    return d


def build_full():
    k = K()
    declare_io(k)
    declare_s5(k)
    declare_ml(k)
    declare_s5post(k)
    declare_moe(k)
    for f in (stage0, stage1, stage2, stage3a, stage3b, stage4a, stage4b, stage4c, stage5, stage6, stage7, stage8, stage9):
        f(k)
    return k


def host_inputs(inp, cores):
    shared = {"ada_w": inp["ada_w"][0], "ada_b": inp["ada_b"][0][None], "norm_g": inp["norm_g"][0],
              "w_in": inp["w_in"][0], "ident_bf": np.eye(128, dtype=ml_dtypes.bfloat16),
              "ident_f": np.eye(128, dtype=np.float32)}
    shared.update(_host_s5(inp))
    shared.update(_host_rest(inp))
    maps = []
    for core in cores:
        b, j = divmod(core, 4)
        cond = np.stack([inp["c"][b], inp["c_ctx"]], -1).reshape(16, 128, 2).transpose(1, 0, 2)
        d = {"x": inp["x"][b], "ctx": inp["ctx"][b], "cond": np.ascontiguousarray(cond)}
        d["ownidx"] = (j * OWN + np.arange(OWN, dtype=np.int32).reshape(OWN // 128, 128).T).astype(np.int32).copy()
        d.update(shared)
        maps.append(d)
    return maps


def kernel(**inputs):
    inp = {k_: np.asarray(v) for k_, v in inputs.items()}
    k = build_full()
    names = set(k.t.keys())
    in_maps = [{n: np.ascontiguousarray(v) for n, v in m.items() if n in names} for m in host_inputs(inp, range(8))]
    res = run_bass_kernel_spmd(k.nc, in_maps, core_ids=list(range(8)))
    out = np.zeros((2, L, D), np.float32)
    for core in range(8):
        b, j = divmod(core, 4)
        out[b, j * OWN:(j + 1) * OWN] = np.asarray(res.results[core]["out"])
    return out
```

```python
from contextlib import ExitStack
import math
import numpy as np
import ml_dtypes
import concourse.bass as bass
import concourse.mybir as mybir
from concourse.bass_utils import run_bass_kernel_spmd

F32 = mybir.dt.float32
BF16 = mybir.dt.bfloat16
I32 = mybir.dt.int32
U32 = mybir.dt.uint32
ALU = mybir.AluOpType
AF = mybir.ActivationFunctionType
AX = mybir.AxisListType

ENGS = ("sp", "act", "dve", "pool", "pe")

D = 2048
L = 8192
CTX = 256
NT = L + CTX
NTILE = NT // 128
GW = 64
D_S5 = 1024
D_ML = 1024
NH = 4
DH = 256
D_IN = D_S5 + 4 * D_ML + 16
NE = 16
CAPE = 1024
FF = 2048
EPS = 1e-6
OWN = 2048
CAPL = 384


class _Op:
    __slots__ = ("eng", "fn", "deps", "dma", "needed", "sem", "val", "prev_val", "inc")

    def __init__(self, eng, fn, deps, dma, inc):
        self.eng = eng
        self.fn = fn
        self.deps = deps
        self.dma = dma
        self.needed = False
        self.sem = None
        self.val = 0
        self.prev_val = 0
        self.inc = inc


class Prog:
    def __init__(self, nc, n_dma_sems=8):
        self.nc = nc
        self.ops = []
        self.pending = []
        self.last_w = {}
        self.readers = {}
        self.st = ExitStack()
        self.esem = {e: self.st.enter_context(nc.semaphore("es_" + e)) for e in ENGS}
        self.ecnt = {e: 0 for e in ENGS}
        self.dsems = [self.st.enter_context(nc.semaphore("ds%d" % i)) for i in range(n_dma_sems)]
        self.dval = [0] * n_dma_sems
        self.drr = 0
        self.waited = {e: {} for e in ENGS}
        self.n_ins = 0

    def add(self, eng, fn, r=(), w=(), dma=False, inc=16):
        deps = set()
        for x in r:
            if x in self.last_w:
                deps.add(self.last_w[x])
        for x in w:
            if x in self.last_w:
                deps.add(self.last_w[x])
            deps.update(self.readers.get(x, {}).values())
        if eng == "pe" and not dma:
            deps = {d for d in deps if self.ops[d].dma or self.ops[d].eng != "pe"}
        op = _Op(eng, fn, deps, dma, inc)
        oid = len(self.ops)
        self.ops.append(op)
        self.pending.append(oid)
        rk = ("dma", oid) if dma else eng
        for x in r:
            self.readers.setdefault(x, {})[rk] = oid
        for x in w:
            self.last_w[x] = oid
            self.readers[x] = {}
        return oid

    def dma(self, eng, out, in_, r=(), w=(), **kw):
        return self.add(eng, lambda e: e.dma_start(out=out, in_=in_, **kw), r=r, w=w, dma=True)

    def flush(self, final_wait=()):
        ops = self.ops
        pend = self.pending
        self.pending = []
        live = set(self.last_w.values())
        for lst in self.readers.values():
            live.update(lst.values())
        for oid in pend:
            for d in ops[oid].deps:
                ops[d].needed = True
        for oid in pend:
            op = ops[oid]
            if op.dma or oid in live:
                op.needed = True
        for oid in pend:
            op = ops[oid]
            if op.dma:
                k = self.drr
                self.drr = (self.drr + 1) % len(self.dsems)
                op.sem = k
                op.prev_val = self.dval[k]
                self.dval[k] += op.inc
                op.val = self.dval[k]
            elif op.needed:
                self.ecnt[op.eng] += 1
                op.val = self.ecnt[op.eng]
        per = {e: [] for e in ENGS}
        for oid in pend:
            per[ops[oid].eng].append(oid)
        fw = list(final_wait)

        def run(ename, e):
            wd = self.waited[ename]

            def wait(key, sem, val):
                if wd.get(key, 0) >= val:
                    return
                wd[key] = val
                e.wait_ge(sem, val)
                self.n_ins += 1

            def wait_op(p):
                if p.dma:
                    wait(("d", p.sem), self.dsems[p.sem], p.val)
                else:
                    assert p.val > 0, "dependency on unsignalled op"
                    wait(("e", p.eng), self.esem[p.eng], p.val)

            for oid in per[ename]:
                op = ops[oid]
                for d in sorted(op.deps):
                    wait_op(ops[d])
                self.n_ins += 1
                if op.dma:
                    if op.prev_val > 0:
                        wait(("d", op.sem), self.dsems[op.sem], op.prev_val)
                    ins = op.fn(e)
                    ins.then_inc(self.dsems[op.sem], op.inc)
                else:
                    ins = op.fn(e)
                    if op.needed:
                        ins.then_inc(self.esem[ename], 1)
            if ename == "sp":
                for d in fw:
                    wait_op(ops[d])

        with self.nc.Block() as block:
            @block.sync
            def _(e):
                run("sp", e)

            @block.scalar
            def _(e):
                run("act", e)

            @block.vector
            def _(e):
                run("dve", e)

            @block.gpsimd
            def _(e):
                run("pool", e)

            @block.tensor
            def _(e):
                run("pe", e)

    def close(self):
        self.st.close()


class K:
    def __init__(self, dbg=()):
        self.nc = bass.Bass("TRN2", target_bir_lowering=False)
        self.P = Prog(self.nc)
        self.dbg = set(dbg)
        self.t = {}
        self._rr = 0

    def din(self, name, shape, dt=F32):
        a = self.nc.dram_tensor(name, list(shape), dt, kind="ExternalInput").ap()
        self.t[name] = a
        return a

    def dout(self, name, shape, dt=F32):
        a = self.nc.dram_tensor(name, list(shape), dt, kind="ExternalOutput").ap()
        self.t[name] = a
        return a

    def dscr(self, name, shape, dt=F32):
        kind = "ExternalOutput" if name in self.dbg else "Internal"
        a = self.nc.dram_tensor(name, list(shape), dt, kind=kind).ap()
        self.t[name] = a
        return a

    def q(self):
        return "sp"


def declare_io(k):
    k.din("x", [L, D])
    k.din("ctx", [CTX, D])
    k.din("cond", [128, 16, 2])
    k.din("ada_w", [D, 6 * D])
    k.din("ada_b", [1, 6 * D])
    k.din("norm_g", [4, D])
    k.din("w_in", [D, D_IN])
    k.din("ident_bf", [128, 128], BF16)
    k.din("ident_f", [128, 128])
    k.dout("out", [OWN, D])
    k.dscr("MODS", [6, 128, D])
    k.dscr("MODS2", [4, 128, D])
    k.dscr("HXT", [2, 16, 128, NT], BF16)
    k.dscr("U5", [8, 128, NT], BF16)
    k.dscr("QKPRE", [16, 128, NT], BF16)
    k.dscr("V", [NT, D_ML], BF16)
    k.dscr("SO", [NT, D_ML], BF16)
    k.dscr("GATES", [128, NTILE, 16])


def stage0(k):
    nc, P, t = k.nc, k.P, k.t
    with ExitStack() as st:
        sb = lambda n, s, d=F32: st.enter_context(nc.sbuf_tensor(n, s, d))
        cond = sb("s0_cond", [128, 16, 2])
        sil = sb("s0_sil", [128, 16, 2])
        lx = sb("s0_lx", [128, 16, 128])
        lc = sb("s0_lc", [128, 16, 128])
        wt = [sb("s0_w%d" % i, [128, 16, 512]) for i in range(2)]
        bt = [sb("s0_b%d" % i, [128, 512]) for i in range(2)]
        mx = sb("s0_mx", [128, 6, D])
        mc = sb("s0_mc", [128, 2, D])
        ng = sb("s0_ng", [128, 4, D])
        o1 = sb("s0_o1", [128, D])
        ps = [st.enter_context(nc.psum_tensor("s0_ps%d" % i, [128, 512], F32)) for i in range(2)]

        P.dma("sp", cond[:], t["cond"][:, :, :], w=["cond"])
        P.dma("act", ng[:], t["norm_g"].rearrange("(o g) d -> o g d", o=1).broadcast_to([128, 4, D]), w=["ng"])
        P.add("act", lambda e: e.activation(out=sil[:], in_=cond[:], func=AF.Silu), r=["cond"], w=["sil"])
        P.add("dve", lambda e: e.tensor_copy(lx[:], sil[:, :, 0:1].broadcast_to([128, 16, 128])), r=["sil"], w=["lx"])
        P.add("dve", lambda e: e.tensor_copy(lc[:], sil[:, :, 1:2].broadcast_to([128, 16, 128])), r=["sil"], w=["lc"])
        adaw = t["ada_w"].rearrange("(kc p) n -> p kc n", p=128)
        for nb in range(24):
            b = nb % 2
            P.dma("sp" if b else "act", wt[b][:], adaw[:, :, nb * 512:(nb + 1) * 512], w=[("w", b)])
            P.dma("pool", bt[b][:], t["ada_b"][:, nb * 512:(nb + 1) * 512].broadcast_to([128, 512]), w=[("b", b)])
            for which in range(2 if nb < 8 else 1):
                lhs = lx if which == 0 else lc
                pst = ps[which]
                for kc in range(16):
                    P.add("pe", lambda e, lhs=lhs, kc=kc, b=b, pst=pst: e.matmul(
                        pst[:], lhs[:, kc, :], wt[b][:, kc, :], start=(kc == 0), stop=(kc == 15)),
                        r=["lx", "lc", ("w", b)], w=[("ps", which)])
                dst = mx if which == 0 else mc
                ch, off = divmod(nb * 512, D)
                P.add("dve", lambda e, dst=dst, ch=ch, off=off, pst=pst, b=b: e.tensor_tensor(
                    dst[:, ch, off:off + 512], pst[:], bt[b][:], ALU.add),
                    r=[("ps", which), ("b", b)], w=[("m", which)])
        mods = t["MODS"]
        mods2 = t["MODS2"]

        def emit(dst_ap, fn, key):
            P.add("dve", fn, r=[("m", 0), ("m", 1), "ng"], w=["o1"])
            P.dma("sp", dst_ap, o1[:], r=["o1"], w=[key])

        emit(mods[0], lambda e: e.scalar_tensor_tensor(o1[:], mx[:, 1, :], 1.0, ng[:, 0, :], ALU.add, ALU.mult), "MODS0")
        emit(mods[1], lambda e: e.tensor_copy(o1[:], mx[:, 0, :]), "MODS1")
        emit(mods[2], lambda e: e.scalar_tensor_tensor(o1[:], mc[:, 1, :], 1.0, ng[:, 0, :], ALU.add, ALU.mult), "MODS2")
        emit(mods[3], lambda e: e.tensor_copy(o1[:], mc[:, 0, :]), "MODS3")
        emit(mods2[0], lambda e: e.tensor_tensor(o1[:], mx[:, 2, :], ng[:, 1, :], ALU.mult), "M2_0")
        emit(mods2[1], lambda e: e.scalar_tensor_tensor(o1[:], mx[:, 4, :], 1.0, ng[:, 2, :], ALU.add, ALU.mult), "M2_1")
        emit(mods2[2], lambda e: e.tensor_copy(o1[:], mx[:, 3, :]), "M2_2")
        emit(mods2[3], lambda e: e.tensor_tensor(o1[:], mx[:, 5, :], ng[:, 3, :], ALU.mult), "M2_3")
        P.flush()


def tok_src(k, order, ti):
    t = k.t
    if ti < 2:
        return t["ctx"][ti * 128:(ti + 1) * 128, :]
    xi = ti - 2
    if order == 0:
        return t["x"][xi * 128:(xi + 1) * 128, :]
    return t["x"].rearrange("(r w) d -> w r d", w=GW)[xi]


def stage1(k, orders=(0, 1)):
    nc, P, t = k.nc, k.P, k.t
    with ExitStack() as st:
        sb = lambda n, s, d=F32: st.enter_context(nc.sbuf_tensor(n, s, d))
        A = [sb("s1_A%d" % i, [128, D]) for i in range(4)]
        ident = sb("s1_id", [128, 128], BF16)
        xt = [sb("s1_x%d" % i, [128, D]) for i in range(2)]
        junk = sb("s1_junk", [128, D])
        t1 = [sb("s1_t%d" % i, [128, D]) for i in range(2)]
        hx = [sb("s1_hx%d" % i, [128, D], BF16) for i in range(2)]
        ss = [sb("s1_ss%d" % i, [128, 2]) for i in range(2)]
        blk = [sb("s1_blk%d" % i, [128, 16, 512], BF16) for i in range(2)]
        ps = [st.enter_context(nc.psum_tensor("s1_ps%d" % i, [128, 16, 128], BF16)) for i in range(2)]
        epst = sb("s1_eps", [128, 1])
        P.add("pool", lambda e: e.memset(epst[:], EPS), w=["epst"])
        for i in range(4):
            P.dma("sp", A[i][:], t["MODS"][i], r=["MODS%d" % i], w=[("A", i)])
        P.dma("act", ident[:], t["ident_bf"][:, :], w=["ident"])
        for order in orders:
            groups = [(0, 2)] + [(2 + 4 * g, 4) for g in range(16)]
            for gi, (t0, n) in enumerate(groups):
                bb = gi % 2
                for j in range(n):
                    ti = t0 + j
                    b = ti % 2
                    a_i, b_i = (2, 3) if ti < 2 else (0, 1)
                    P.dma(k.q(), xt[b][:], tok_src(k, order, ti), w=[("xt", b)])
                    P.add("act", lambda e, b=b: e.activation(out=junk[:], in_=xt[b][:], func=AF.Square,
                                                             scale=float(D ** -0.5), accum_out=ss[b][:, 0:1]),
                          r=[("xt", b)], w=["junk", ("ss", b)])
                    P.add("act", lambda e, b=b: e.activation(out=ss[b][:, 1:2], in_=ss[b][:, 0:1], func=AF.Sqrt,
                                                             bias=epst[:, 0:1], scale=1.0),
                          r=[("ss", b), "epst"], w=[("ss1", b)])
                    P.add("dve", lambda e, b=b: e.reciprocal(ss[b][:, 1:2], ss[b][:, 1:2]),
                          r=[("ss1", b)], w=[("ss1", b)])
                    P.add("dve", lambda e, b=b, a_i=a_i: e.scalar_tensor_tensor(
                        t1[b][:], xt[b][:], ss[b][:, 1:2], A[a_i][:], ALU.mult, ALU.mult),
                        r=[("xt", b), ("ss1", b), ("A", a_i)], w=[("t1", b)])
                    P.add("pool", lambda e, b=b, b_i=b_i: e.tensor_tensor(hx[b][:], t1[b][:], A[b_i][:], ALU.add),
                          r=[("t1", b), ("A", b_i)], w=[("hx", b)])
                    for kc in range(16):
                        P.add("pe", lambda e, b=b, kc=kc: e.transpose(ps[b][:, kc, :], hx[b][:, kc * 128:(kc + 1) * 128],
                                                                      ident[:]),
                              r=[("hx", b), "ident"], w=[("ps", b)])
                    P.add("act", lambda e, b=b, bb=bb, j=j: e.copy(blk[bb][:, :, j * 128:(j + 1) * 128], ps[b][:]),
                          r=[("ps", b)], w=[("blk", bb)])
                dst = t["HXT"][order].rearrange("kc p t -> p kc t")[:, :, t0 * 128:(t0 + n) * 128]
                P.dma(k.q(), dst, blk[bb][:, :, 0:n * 128], r=[("blk", bb)], w=[("HXT", order, gi)])
        P.flush()


TGROUPS = [(0, 256)] + [(256 + 512 * g, 512) for g in range(16)]


def stage2(k):
    nc, P, t = k.nc, k.P, k.t
    win = t["w_in"].rearrange("(kc p) n -> p kc n", p=128)
    for pname, order, c0, ncb, dst in (("A", 0, 0, 8, "U5"), ("B", 1, D_S5, 16, "QKPRE")):
        with ExitStack() as st:
            sb = lambda n, s, d=F32: st.enter_context(nc.sbuf_tensor(n, s, d))
            W = sb("s2%s_w" % pname, [128, 16, ncb * 128], BF16)
            hb = [sb("s2%s_h%d" % (pname, i), [128, 16, 512], BF16) for i in range(2)]
            ob = [sb("s2%s_o%d" % (pname, i), [128, ncb, 512], BF16) for i in range(2)]
            ps = [st.enter_context(nc.psum_tensor("s2%s_ps%d" % (pname, i), [128, 512], F32)) for i in range(4)]
            for kc in range(16):
                for c1 in range(0, ncb * 128, 1024):
                    P.dma("pool", W[:, kc, c1:c1 + 1024], win[:, kc, c0 + c1:c0 + c1 + 1024], w=[("W", kc, c1 // 1024)])
            hsrc = t["HXT"][order].rearrange("kc p t -> p kc t")
            dview = t[dst].rearrange("cb p t -> p cb t")
            for gi, (t0, gt) in enumerate(TGROUPS):
                b = gi % 2
                P.dma(k.q(), hb[b][:, :, 0:gt], hsrc[:, :, t0:t0 + gt], r=[("HXT", order, gi)], w=[("hb", b)])
                for cb in range(ncb):
                    pi = cb % 4
                    for kc in range(16):
                        P.add("pe", lambda e, pi=pi, kc=kc, cb=cb, b=b, gt=gt: e.matmul(
                            ps[pi][:, 0:gt], W[:, kc, cb * 128:(cb + 1) * 128], hb[b][:, kc, 0:gt],
                            start=(kc == 0), stop=(kc == 15)),
                            r=[("W", kc, cb // 8), ("hb", b)], w=[("ps", pi)])
                    eng = "act" if cb % 2 == 0 else "dve"
                    if eng == "act":
                        P.add("act", lambda e, pi=pi, cb=cb, b=b, gt=gt: e.copy(ob[b][:, cb, 0:gt], ps[pi][:, 0:gt]),
                              r=[("ps", pi)], w=[("ob", b)])
                    else:
                        P.add("dve", lambda e, pi=pi, cb=cb, b=b, gt=gt: e.tensor_copy(ob[b][:, cb, 0:gt], ps[pi][:, 0:gt]),
                              r=[("ps", pi)], w=[("ob", b)])
                P.dma(k.q(), dview[:, :, t0:t0 + gt], ob[b][:, :, 0:gt], r=[("ob", b)], w=[(dst, gi)])
            P.flush()
    with ExitStack() as st:
        sb = lambda n, s, d=F32: st.enter_context(nc.sbuf_tensor(n, s, d))
        W = sb("s2C_w", [128, 16, 2 * D_ML + 16], BF16)
        hb = [sb("s2C_h%d" % i, [128, 16, 512], BF16) for i in range(2)]
        vo = [sb("s2C_vo%d" % i, [128, 2 * D_ML], BF16) for i in range(2)]
        gt_sb = sb("s2C_g", [128, NTILE, 16])
        ps = [st.enter_context(nc.psum_tensor("s2C_ps%d" % i, [128, 512], F32)) for i in range(4)]
        psg = st.enter_context(nc.psum_tensor("s2C_psg", [128, 16], F32))
        c0 = D_S5 + 2 * D_ML
        for kc in range(16):
            for c1, cn in ((0, 1024), (1024, 1024), (2048, 16)):
                P.dma("pool", W[:, kc, c1:c1 + cn], win[:, kc, c0 + c1:c0 + c1 + cn], w=[("W", kc, c1 // 1024)])
        hsrc = t["HXT"][1].rearrange("kc p t -> p kc t")
        for gi, (t0, gtk) in enumerate(TGROUPS):
            b = gi % 2
            P.dma(k.q(), hb[b][:, :, 0:gtk], hsrc[:, :, t0:t0 + gtk], r=[("HXT", 1, gi)], w=[("hb", b)])
            for j in range(gtk // 128):
                ti = t0 // 128 + j
                vb = ti % 2
                for nb in range(4):
                    for kc in range(16):
                        P.add("pe", lambda e, nb=nb, kc=kc, b=b, j=j: e.matmul(
                            ps[nb][:], hb[b][:, kc, j * 128:(j + 1) * 128], W[:, kc, nb * 512:(nb + 1) * 512],
                            start=(kc == 0), stop=(kc == 15)),
                            r=[("W", kc, nb // 2), ("hb", b)], w=[("ps", nb)])
                    if nb < 2:
                        P.add("dve", lambda e, nb=nb, vb=vb: e.tensor_copy(vo[vb][:, nb * 512:(nb + 1) * 512], ps[nb][:]),
                              r=[("ps", nb)], w=[("vo", vb, nb)])
                    else:
                        P.add("act", lambda e, nb=nb, vb=vb: e.activation(out=vo[vb][:, nb * 512:(nb + 1) * 512],
                                                                          in_=ps[nb][:], func=AF.Sigmoid),
                              r=[("ps", nb)], w=[("vo", vb, nb)])
                for kc in range(16):
                    P.add("pe", lambda e, kc=kc, b=b, j=j: e.matmul(
                        psg[:], hb[b][:, kc, j * 128:(j + 1) * 128], W[:, kc, 2 * D_ML:2 * D_ML + 16],
                        start=(kc == 0), stop=(kc == 15)),
                        r=[("W", kc, 2), ("hb", b)], w=["psg"])
                P.add("dve", lambda e, ti=ti: e.tensor_copy(gt_sb[:, ti, :], psg[:]), r=["psg"], w=["gt_sb"])
                P.dma(k.q(), t["V"][ti * 128:(ti + 1) * 128, :], vo[vb][:, 0:D_ML],
                      r=[("vo", vb, 0), ("vo", vb, 1)], w=[("V", ti)])
                P.dma(k.q(), t["SO"][ti * 128:(ti + 1) * 128, :], vo[vb][:, D_ML:2 * D_ML],
                      r=[("vo", vb, 2), ("vo", vb, 3)], w=[("SO", ti)])
        P.dma("sp", t["GATES"][:, :, :], gt_sb[:], r=["gt_sb"], w=["GATES"])
        P.flush()


TS5 = 16
NCH = NT // TS5
TWO_PI = 2.0 * math.pi


def emit_sincos(P, x, osin, ocos, tf, ti, tm, cpi0, rk, wk_sin, wk_cos, tkey):
    PI_ = math.pi
    for which, out, off, wk in ((0, osin, 0.0, wk_sin), (1, ocos, 0.5 * PI_, wk_cos)):
        kt = (tkey, "tf")
        P.add("dve", lambda e, off=off: e.tensor_scalar(tf, x, off, 1.0 / TWO_PI, ALU.add, ALU.mult), r=rk, w=[kt])
        P.add("dve", lambda e: e.tensor_copy(ti, tf), r=[kt], w=[(tkey, "ti")])
        P.add("dve", lambda e: e.tensor_copy(tf, ti), r=[(tkey, "ti")], w=[kt])
        P.add("dve", lambda e, off=off: e.tensor_scalar_add(tm, x, off), r=rk, w=[(tkey, "tm")])
        P.add("dve", lambda e: e.scalar_tensor_tensor(tf, tf, -TWO_PI, tm, ALU.mult, ALU.add), r=[kt, (tkey, "tm")], w=[kt])
        P.add("dve", lambda e: e.tensor_single_scalar(tm, tf, PI_, ALU.is_gt), r=[kt], w=[(tkey, "tm")])
        P.add("dve", lambda e: e.scalar_tensor_tensor(tf, tm, -TWO_PI, tf, ALU.mult, ALU.add), r=[kt, (tkey, "tm")], w=[kt])
        P.add("dve", lambda e: e.tensor_single_scalar(tm, tf, -PI_, ALU.is_lt), r=[kt], w=[(tkey, "tm")])
        P.add("dve", lambda e: e.scalar_tensor_tensor(tf, tm, TWO_PI, tf, ALU.mult, ALU.add), r=[kt, (tkey, "tm")], w=[kt])
        P.add("dve", lambda e: e.tensor_scalar(tf, tf, -3.14159, 3.14159, ALU.max, ALU.min), r=[kt], w=[kt])
        P.add("act", lambda e, out=out: e.activation(out=out, in_=tf, func=AF.Sin, bias=cpi0, scale=1.0),
              r=[kt, "cpi"], w=wk)


def declare_s5(k):
    k.din("s5p", [8, 128, 24])
    k.din("s5b", [8, 128, 2, 4, 16])
    k.din("s5c", [8, 128, 2, 4, 16])
    k.din("s5d", [8, 128, 1])
    k.din("c_jidx", [128, 17])
    k.din("c_parmask", [128, 2])
    k.din("c_blockmask", [128, 128])
    k.din("c_midx", [128, 2, NCH])
    k.din("c_rowmask", [128, 4])
    k.dscr("S5T_LAG", [8, 128, 2, 16, 128], BF16)
    k.dscr("S5T_BDT", [8, 128, 2, 16, 2, 128], BF16)
    k.dscr("S5T_MG", [8, 128, 2, 2, 16, 128], BF16)
    k.dscr("S5T_RT", [8, 128, 16])
    k.dscr("S5T_DIAG", [8, 128, 128], BF16)
    k.dscr("YG", [8, 128, L], BF16)


def stage3a(k):
    nc, P, t = k.nc, k.P, k.t
    with ExitStack() as st:
        sb = lambda n, s, d=F32: st.enter_context(nc.sbuf_tensor(n, s, d))
        jidx = sb("a_jidx", [128, 17])
        parm = sb("a_parm", [128, 2])
        bmask = sb("a_bmask", [128, 128])
        identf = sb("a_idf", [128, 128])
        cpi = sb("a_cpi", [128, 2])
        prm = sb("a_prm", [128, 24])
        bb = sb("a_bb", [128, 2, 4, 16])
        cc = sb("a_cc", [128, 2, 4, 16])
        dd = sb("a_dd", [128, 1])
        sm = sb("a_sm", [128, 12, 8])
        JL = sb("a_JL", [128, 17, 8])
        JA = sb("a_JA", [128, 17, 8])
        T1 = sb("a_T1", [128, 17, 8])
        T2 = sb("a_T2", [128, 17, 8])
        TI = sb("a_TI", [128, 17, 8], I32)
        PR = sb("a_PR", [128, 17, 8])
        PI = sb("a_PI", [128, 17, 8])
        BB = sb("a_BB", [128, 2, 8, 16])
        tb = sb("a_tb", [128, 2, 8, 16])
        Wr = sb("a_Wr", [128, 16, 8, 16])
        Wi = sb("a_Wi", [128, 16, 8, 16])
        Wt = sb("a_Wt", [128, 16, 8, 16])
        MWr = sb("a_MWr", [128, 128, 2, 16])
        MWi = sb("a_MWi", [128, 128, 2, 16])
        MC = sb("a_MC", [128, 2, 4, 2, 16])
        Gt = sb("a_Gt", [128, 16, 4, 16])
        Gt2 = sb("a_Gt2", [128, 16, 4, 16])
        MG = sb("a_MG", [128, 2, 2, 16, 128], BF16)
        LAG = sb("a_LAG", [128, 2, 16, 128], BF16)
        BDT = sb("a_BDT", [128, 2, 16, 2, 128], BF16)
        DG = sb("a_DG", [128, 128], BF16)
        RT = sb("a_RT", [128, 16])
        ps = [st.enter_context(nc.psum_tensor("a_ps%d" % i, [128, 128], F32)) for i in range(4)]

        P.dma("sp", jidx[:], t["c_jidx"][:, :], w=["jidx"])
        P.dma("sp", parm[:], t["c_parmask"][:, :], w=["parm"])
        P.dma("sp", bmask[:], t["c_blockmask"][:, :], w=["bmask"])
        P.dma("sp", identf[:], t["ident_f"][:, :], w=["identf"])
        P.add("pool", lambda e: e.memset(cpi[:], 0.0), w=["cpi"])

        def V(fn, r, w, eng="dve"):
            P.add(eng, fn, r=r, w=w)

        for blk in range(8):
            P.dma("sp", prm[:], t["s5p"][blk], w=["prm"])
            P.dma("act", bb[:], t["s5b"][blk], w=["bb"])
            P.dma("sp", cc[:], t["s5c"][blk], w=["cc"])
            P.dma("act", dd[:], t["s5d"][blk], w=["dd"])
            are, aim, ldt = prm[:, 0:8], prm[:, 8:16], prm[:, 16:24]
            dt_, lr, lrdt, ang = sm[:, 0, :], sm[:, 1, :], sm[:, 2, :], sm[:, 3, :]
            nr, den, cr, ci, tmp, tmp2 = sm[:, 4, :], sm[:, 5, :], sm[:, 6, :], sm[:, 7, :], sm[:, 8, :], sm[:, 9, :]
            V(lambda e: e.activation(out=dt_, in_=ldt, func=AF.Exp), ["prm"], ["sm0"], "act")
            V(lambda e: e.tensor_scalar_min(lr, are, -1e-4), ["prm"], ["sm1"])
            V(lambda e: e.tensor_tensor(lrdt, lr, dt_, ALU.mult), ["sm0", "sm1"], ["sm2"])
            V(lambda e: e.tensor_tensor(ang, aim, dt_, ALU.mult), ["sm0", "prm"], ["sm3"])
            b17 = lambda a: a.unsqueeze(1).broadcast_to([128, 17, 8])
            j17 = jidx[:].unsqueeze(2).broadcast_to([128, 17, 8])
            V(lambda e: e.tensor_tensor(JL[:], j17, b17(lrdt), ALU.mult), ["jidx", "sm2"], ["JL"])
            V(lambda e: e.activation(out=JL[:], in_=JL[:], func=AF.Exp), ["JL"], ["JL"], "act")
            V(lambda e: e.tensor_tensor(JA[:], j17, b17(ang), ALU.mult), ["jidx", "sm3"], ["JA"])
            f2 = lambda a: a.rearrange("p j g -> p (j g)")
            emit_sincos(P, f2(JA[:]), f2(PI[:]), f2(PR[:]), f2(T1[:]), f2(TI[:]), f2(T2[:]), cpi[:, 1:2],
                        ["JA"], ["PI"], ["PR"], "sc_a")
            V(lambda e: e.tensor_tensor(PR[:], PR[:], JL[:], ALU.mult), ["PR", "JL"], ["PR"])
            V(lambda e: e.tensor_tensor(PI[:], PI[:], JL[:], ALU.mult), ["PI", "JL"], ["PI"])
            V(lambda e: e.tensor_scalar_add(nr, PR[:, 1, :], -1.0), ["PR"], ["sm4"])
            V(lambda e: e.tensor_tensor(den, lr, lr, ALU.mult), ["sm1"], ["sm5"])
            V(lambda e: e.tensor_tensor(tmp, aim, aim, ALU.mult), ["prm"], ["sm8"])
            V(lambda e: e.tensor_tensor(den, den, tmp, ALU.add), ["sm5", "sm8"], ["sm5"])
            V(lambda e: e.reciprocal(den, den), ["sm5"], ["sm5"])
            V(lambda e: e.tensor_tensor(cr, nr, lr, ALU.mult), ["sm4", "sm1"], ["sm6"])
            V(lambda e: e.tensor_tensor(tmp, PI[:, 1, :], aim, ALU.mult), ["PI", "prm", "sm5"], ["sm8"])
            V(lambda e: e.tensor_tensor(cr, cr, tmp, ALU.add), ["sm6", "sm8"], ["sm6"])
            V(lambda e: e.tensor_tensor(cr, cr, den, ALU.mult), ["sm6", "sm5"], ["sm6"])
            V(lambda e: e.tensor_tensor(ci, PI[:, 1, :], lr, ALU.mult), ["PI", "sm1"], ["sm7"])
            V(lambda e: e.tensor_tensor(tmp2, nr, aim, ALU.mult), ["sm4", "prm"], ["sm9"])
            V(lambda e: e.tensor_tensor(ci, ci, tmp2, ALU.subtract), ["sm7", "sm9"], ["sm7"])
            V(lambda e: e.tensor_tensor(ci, ci, den, ALU.mult), ["sm7", "sm5"], ["sm7"])
            for d_ in range(2):
                crd = cr[:, d_ * 4:(d_ + 1) * 4].unsqueeze(2).broadcast_to([128, 4, 16])
                cid = ci[:, d_ * 4:(d_ + 1) * 4].unsqueeze(2).broadcast_to([128, 4, 16])
                o_r = BB[:, 0, d_ * 4:(d_ + 1) * 4, :]
                o_i = BB[:, 1, d_ * 4:(d_ + 1) * 4, :]
                t_r = tb[:, 0, d_ * 4:(d_ + 1) * 4, :]
                t_i = tb[:, 1, d_ * 4:(d_ + 1) * 4, :]
                V(lambda e, o_r=o_r, crd=crd: e.tensor_tensor(o_r, crd, bb[:, 0], ALU.mult), ["sm6", "bb"], [("BB", d_)])
                V(lambda e, t_r=t_r, cid=cid: e.tensor_tensor(t_r, cid, bb[:, 1], ALU.mult), ["sm7", "bb"], [("tb", d_)])
                V(lambda e, o_r=o_r, t_r=t_r: e.tensor_tensor(o_r, o_r, t_r, ALU.subtract), [("BB", d_), ("tb", d_)], [("BB", d_)])
                V(lambda e, o_i=o_i, crd=crd: e.tensor_tensor(o_i, crd, bb[:, 1], ALU.mult), ["sm6", "bb"], [("BBi", d_)])
                V(lambda e, t_i=t_i, cid=cid: e.tensor_tensor(t_i, cid, bb[:, 0], ALU.mult), ["sm7", "bb"], [("tbi", d_)])
                V(lambda e, o_i=o_i, t_i=t_i: e.tensor_tensor(o_i, o_i, t_i, ALU.add), [("BBi", d_), ("tbi", d_)], [("BBi", d_)])
            BBk = [("BB", 0), ("BB", 1), ("BBi", 0), ("BBi", 1)]
            pr16 = PR[:, 0:16, :].unsqueeze(3).broadcast_to([128, 16, 8, 16])
            pi16 = PI[:, 0:16, :].unsqueeze(3).broadcast_to([128, 16, 8, 16])
            bbr = BB[:, 0].unsqueeze(1).broadcast_to([128, 16, 8, 16])
            bbi = BB[:, 1].unsqueeze(1).broadcast_to([128, 16, 8, 16])
            V(lambda e: e.tensor_tensor(Wr[:], pr16, bbr, ALU.mult), ["PR"] + BBk, ["Wr"])
            V(lambda e: e.tensor_tensor(Wt[:], pi16, bbi, ALU.mult), ["PI"] + BBk, ["Wt"])
            V(lambda e: e.tensor_tensor(Wr[:], Wr[:], Wt[:], ALU.subtract), ["Wr", "Wt"], ["Wr"])
            V(lambda e: e.tensor_tensor(Wi[:], pr16, bbi, ALU.mult), ["PR"] + BBk, ["Wi"])
            V(lambda e: e.tensor_tensor(Wt[:], pi16, bbr, ALU.mult), ["PI", "Wr"] + BBk, ["Wt"])
            V(lambda e: e.tensor_tensor(Wi[:], Wi[:], Wt[:], ALU.add), ["Wi", "Wt"], ["Wi"])
            pm = parm[:].unsqueeze(1).unsqueeze(3).broadcast_to([128, 128, 2, 16])
            wrv = Wr[:].rearrange("p j g c -> p (j g) c").unsqueeze(2).broadcast_to([128, 128, 2, 16])
            wiv = Wi[:].rearrange("p j g c -> p (j g) c").unsqueeze(2).broadcast_to([128, 128, 2, 16])
            V(lambda e: e.tensor_tensor(MWr[:], wrv, pm, ALU.mult), ["Wr", "parm"], ["MWr"])
            V(lambda e: e.tensor_tensor(MWi[:], wiv, pm, ALU.mult), ["Wi", "parm"], ["MWi"], "pool")
            pm2 = parm[:].unsqueeze(1).unsqueeze(3).broadcast_to([128, 4, 2, 16])
            V(lambda e: e.tensor_tensor(MC[:, 0], cc[:, 0].unsqueeze(2).broadcast_to([128, 4, 2, 16]), pm2, ALU.mult),
              ["cc", "parm"], ["MC0"])
            V(lambda e: e.tensor_tensor(MC[:, 1], cc[:, 1].unsqueeze(2).broadcast_to([128, 4, 2, 16]), pm2, ALU.mult),
              ["cc", "parm"], ["MC1"])
            mc1f = MC[:, 1].rearrange("p q a c -> p (q a c)")
            V(lambda e: e.tensor_scalar_mul(mc1f, mc1f, -1.0), ["MC1"], ["MC1"])
            mwr = MWr[:].rearrange("p (j d q) a c -> p j d (q a c)", j=16, d=2)
            mwi = MWi[:].rearrange("p (j d q) a c -> p j d (q a c)", j=16, d=2)
            mcr = MC[:, 0].rearrange("p q a c -> p (q a c)")
            mci = MC[:, 1].rearrange("p q a c -> p (q a c)")
            n = 0
            for d_ in range(2):
                for j in range(16):
                    pa = ps[n % 4]
                    n += 1
                    P.add("pe", lambda e, pa=pa, j=j, d_=d_: e.matmul(pa[:], mwr[:, j, d_, :], mcr, start=True, stop=False),
                          r=["MWr", "MC0"], w=[("ps", id(pa))])
                    P.add("pe", lambda e, pa=pa, j=j, d_=d_: e.matmul(pa[:], mwi[:, j, d_, :], mci, start=False, stop=True),
                          r=["MWi", "MC1"], w=[("ps", id(pa))])
                    V(lambda e, pa=pa, j=j, d_=d_: e.tensor_tensor(LAG[:, d_, j, :], pa[:], bmask[:], ALU.mult),
                      [("ps", id(pa)), "bmask"], ["LAG"])
                    for ri, mw in ((0, mwr), (1, mwi)):
                        pb = ps[n % 4]
                        n += 1
                        P.add("pe", lambda e, pb=pb, mw=mw, j=j, d_=d_: e.transpose(pb[:], mw[:, j, d_, :], identf[:]),
                              r=["MWr", "MWi", "identf"], w=[("ps", id(pb))])
                        V(lambda e, pb=pb, ri=ri, j=j, d_=d_: e.copy(BDT[:, d_, j, ri, :], pb[:]),
                          [("ps", id(pb))], ["BDT"], "act")
            for d_ in range(2):
                prj = PR[:, 1:17, d_ * 4:(d_ + 1) * 4].unsqueeze(3).broadcast_to([128, 16, 4, 16])
                pij = PI[:, 1:17, d_ * 4:(d_ + 1) * 4].unsqueeze(3).broadcast_to([128, 16, 4, 16])
                crv = cc[:, 0].unsqueeze(1).broadcast_to([128, 16, 4, 16])
                civ = cc[:, 1].unsqueeze(1).broadcast_to([128, 16, 4, 16])
                pm3 = parm[:].unsqueeze(1).unsqueeze(3).broadcast_to([128, 64, 2, 16])
                V(lambda e, prj=prj, crv=crv: e.tensor_tensor(Gt[:], prj, crv, ALU.mult), ["PR", "cc"], ["Gt"])
                V(lambda e, pij=pij, civ=civ: e.tensor_tensor(Gt2[:], pij, civ, ALU.mult), ["PI", "cc"], ["Gt2"])
                V(lambda e: e.tensor_tensor(Gt[:], Gt[:], Gt2[:], ALU.subtract), ["Gt", "Gt2"], ["Gt"])
                gv = Gt[:].rearrange("p j q c -> p (j q) c").unsqueeze(2).broadcast_to([128, 64, 2, 16])
                ov = MG[:, 0, d_].rearrange("p j (q a c) -> p (j q) a c", q=4, a=2)
                V(lambda e, ov=ov, gv=gv, pm3=pm3: e.tensor_tensor(ov, gv, pm3, ALU.mult), ["Gt", "parm"], [("MG", 0, d_)])
                V(lambda e, pij=pij, crv=crv: e.tensor_tensor(Gt[:], pij, crv, ALU.mult), ["PI", "cc", ("MG", 0, d_)], ["Gt"])
                V(lambda e, prj=prj, civ=civ: e.tensor_tensor(Gt2[:], prj, civ, ALU.mult), ["PR", "cc"], ["Gt2"])
                V(lambda e: e.tensor_tensor(Gt[:], Gt[:], Gt2[:], ALU.add), ["Gt", "Gt2"], ["Gt"])
                ov2 = MG[:, 1, d_].rearrange("p j (q a c) -> p (j q) a c", q=4, a=2)
                gtf = Gt[:].rearrange("p j q c -> p (j q c)")
                V(lambda e, gtf=gtf: e.tensor_scalar_mul(gtf, gtf, -1.0), ["Gt"], ["Gt"])
                V(lambda e, ov2=ov2, gv=gv, pm3=pm3: e.tensor_tensor(ov2, gv, pm3, ALU.mult),
                  ["Gt", "parm"], [("MG", 1, d_)])
            V(lambda e: e.tensor_copy(RT[:, 0:8], JL[:, 16, :]), ["JL"], ["RT0"])
            V(lambda e: e.tensor_copy(RT[:, 8:16], JA[:, 16, :]), ["JA"], ["RT1"])
            V(lambda e: e.tensor_scalar_mul(DG[:], identf[:], dd[:, 0:1]), ["identf", "dd"], ["DG"])
            P.dma("sp", t["S5T_LAG"][blk], LAG[:], r=["LAG"], w=[("S5T", blk)])
            P.dma("act", t["S5T_BDT"][blk], BDT[:], r=["BDT"], w=[("S5T", blk)])
            P.dma("sp", t["S5T_MG"][blk], MG[:], r=[("MG", a, b) for a in range(2) for b in range(2)], w=[("S5T", blk)])
            P.dma("act", t["S5T_RT"][blk], RT[:], r=["RT0", "RT1"], w=[("S5T", blk)])
            P.dma("sp", t["S5T_DIAG"][blk], DG[:], r=["DG"], w=[("S5T", blk)])
        P.flush()


def build(upto=99, dbg=(), only=None):
    k = K(dbg)
    declare_io(k)
    declare_s5(k)
    declare_ml(k)
    declare_s5post(k)
    declare_moe(k)
    stages = [stage0, stage1, stage2, stage3a, stage3b, stage4a, stage4b, stage4c, stage5, stage6, stage7, stage8, stage9]
    for i, f in enumerate(stages):
        if (only is None and i <= upto) or (only is not None and i in only):
            f(k)
    return k


S5_BLOCKS = list(range(8))
S5_PH = {'X', 'dem', 'inter', 'lag'}
S5_CUT = 9


def stage3b(k, blocks=None):
    blocks = S5_BLOCKS if blocks is None else blocks
    nc, P, t = k.nc, k.P, k.t
    NX = L // TS5
    NC = CTX // TS5
    with ExitStack() as st:
        sb = lambda n, s, d=F32: st.enter_context(nc.sbuf_tensor(n, s, d))
        Ub = sb("b_U", [128, NT], BF16)
        LAG = sb("b_LAG", [128, 2, 16, 128], BF16)
        BDT = sb("b_BDT", [128, 2, 16, 2, 128], BF16)
        MG = sb("b_MG", [128, 2, 2, 16, 128], BF16)
        DG = sb("b_DG", [128, 128], BF16)
        RT = sb("b_RT", [128, 16])
        midx = sb("b_midx", [128, 2, NCH])
        BDQ = [sb("b_BDQ%d" % i, [128, 16, 2, 128], BF16) for i in range(2)]
        rowm = sb("b_rowm", [128, 4])
        cpi = sb("b_cpi", [128, 2])
        Xr = sb("b_Xr", [128, 4, NCH])
        Xi = sb("b_Xi", [128, 4, NCH])
        Ec = sb("b_Ec", [128, 4, NCH])
        Es = sb("b_Es", [128, 4, NCH])
        tf = sb("b_tf", [128, 4, NCH])
        tm = sb("b_tm", [128, 4, NCH])
        ANGS = sb("b_ANGS", [128, 4, 49])
        SINS = sb("b_SINS", [128, 4, 49])
        COSS = sb("b_COSS", [128, 4, 49])
        stf = sb("b_stf", [128, 4, 49])
        stm = sb("b_stm", [128, 4, 49])
        sti = sb("b_sti", [128, 4, 49], I32)
        Hb = sb("b_Hb", [128, 2, 2, 4, NCH], BF16)
        YI = sb("b_YI", [128, NX, TS5])
        Us = YI[:].rearrange("p n s -> p (n s)").bitcast(BF16)[:, 0:TS5 * NCH].rearrange("p (s n) -> p s n", n=NCH)
        ys = [sb("b_ys%d" % i, [128, 512]) for i in range(2)]
        y2 = [sb("b_y2%d" % i, [128, 512]) for i in range(2)]
        yo = [sb("b_yo%d" % i, [128, 512], BF16) for i in range(2)]
        psx = [st.enter_context(nc.psum_tensor("b_psx%d" % i, [128, 512], F32)) for i in range(4)]
        psc = [st.enter_context(nc.psum_tensor("b_psc%d" % i, [128, 2, 16], F32)) for i in range(2)]
        psy = [st.enter_context(nc.psum_tensor("b_psy%d" % i, [128, 512], F32)) for i in range(2)]
        P.dma("sp", midx[:], t["c_midx"][:, :, :], w=["midx"])
        P.dma("sp", rowm[:], t["c_rowmask"][:, :], w=["rowm"])
        P.add("pool", lambda e: e.memset(cpi[:], 0.0), w=["cpi"])
        f2 = lambda a: a.rearrange("p q n -> p (q n)")
        for blk in blocks:
            P.dma("sp", Ub[:], t["U5"][blk], r=[("U5", g) for g in range(17)], w=["Ub"])
            P.dma("act", LAG[:], t["S5T_LAG"][blk], r=[("S5T", blk)], w=["LAG"])
            P.dma("sp", BDT[:], t["S5T_BDT"][blk], r=[("S5T", blk)], w=["BDT"])
            P.dma("act", MG[:], t["S5T_MG"][blk], r=[("S5T", blk)], w=["MG"])
            P.dma("sp", DG[:], t["S5T_DIAG"][blk], r=[("S5T", blk)], w=["DG"])
            P.dma("act", RT[:], t["S5T_RT"][blk], r=[("S5T", blk)], w=["RT"])
            P.add("dve", lambda e: e.tensor_copy(Us, Ub[:].rearrange("p (n s) -> p s n", s=TS5)), r=["Ub"], w=["Us", "YI"])
            for d_ in range(2):
                xo, co = (NC, 0) if d_ == 0 else (0, NX)
                for q in (range(4) if 'X' in S5_PH else ()):
                    pb = q % 2
                    bq = BDQ[pb]
                    P.add("dve", lambda e, bq=bq, q=q, d_=d_: e.tensor_scalar_mul(
                        bq[:].rearrange("p j r c -> p (j r c)"), BDT[:, d_].rearrange("p j r c -> p (j r c)"),
                        rowm[:, q:q + 1]), r=["BDT", "rowm"], w=[("BDQ", pb)])
                    for ri in (range(2) if S5_CUT >= 2 else ()):
                        px_ = psx[pb * 2 + ri]
                        for s in range(TS5):
                            j = (TS5 - 1 - s) if d_ == 0 else s
                            lhs = bq[:, j, ri, :]
                            P.add("pe", lambda e, px_=px_, lhs=lhs, q=q, s=s: e.matmul(
                                px_[:], lhs, Us[:, s, NC:NCH],
                                start=(s == 0), stop=(s == TS5 - 1)),
                                r=[("BDQ", pb), "Us"], w=[("psx", pb, ri)])
                            P.add("pe", lambda e, pb=pb, ri=ri, lhs=lhs, q=q, s=s: e.matmul(
                                psc[pb][:, ri, :], lhs, Us[:, s, 0:NC],
                                start=(s == 0), stop=(s == TS5 - 1)),
                                r=[("BDQ", pb), "Us"], w=[("psc", pb, ri)])
                        X = Xr if ri == 0 else Xi
                        if S5_CUT < 3:
                            continue
                        if S5_CUT != 4:
                            P.add("dve", lambda e, X=X, q=q, px_=px_, xo=xo: e.tensor_copy(X[:, q, xo:xo + NX], px_[:]),
                                  r=[("psx", pb, ri)], w=[("X", ri)])
                        if S5_CUT != 5:
                            P.add("dve", lambda e, X=X, q=q, pb=pb, ri=ri, co=co: e.tensor_copy(X[:, q, co:co + NC], psc[pb][:, ri, :]),
                                  r=[("psc", pb, ri)], w=[("X", ri)])
                if 'dem' not in S5_PH:
                    continue
                V = lambda fn, r, w, eng="dve": P.add(eng, fn, r=r, w=w)
                for q in range(4):
                    thq = RT[:, 8 + d_ * 4 + q:9 + d_ * 4 + q]
                    V(lambda e, q=q, thq=thq: e.tensor_scalar_mul(ANGS[:, q, 0:16], midx[:, 0, 0:16], thq), ["midx", "RT"], ["ANGS"])
                    V(lambda e, q=q, thq=thq: e.tensor_scalar(ANGS[:, q, 16:49], midx[:, 0, 0:33], thq, 16.0, ALU.mult, ALU.mult),
                      ["midx", "RT"], ["ANGS"])
                fs = lambda a: a.rearrange("p q n -> p (q n)")
                emit_sincos(P, fs(ANGS[:]), fs(SINS[:]), fs(COSS[:]), fs(stf[:]), fs(sti[:]), fs(stm[:]), cpi[:, 0:1],
                            ["ANGS"], ["SINS"], ["COSS"], "sc_s")
                if d_ == 0:
                    c0, s0 = COSS[:, :, 0:16], SINS[:, :, 0:16]
                    c1, s1 = COSS[:, :, 16:49], SINS[:, :, 16:49]
                else:
                    c0, s0 = COSS[:, :, 15::-1] if False else COSS[:, :, 0:16][:, :, ::-1], SINS[:, :, 0:16][:, :, ::-1]
                    c1, s1 = COSS[:, :, 16:49][:, :, ::-1], SINS[:, :, 16:49][:, :, ::-1]
                b0 = lambda a: a.unsqueeze(2).broadcast_to([128, 4, 33, 16])
                b1 = lambda a: a.unsqueeze(3).broadcast_to([128, 4, 33, 16])
                v4 = lambda a: a.rearrange("p q (a b) -> p q a b", b=16)
                V(lambda e, c0=c0, c1=c1: e.tensor_tensor(v4(Ec[:]), b1(c1), b0(c0), ALU.mult), ["COSS", "SINS", "Hdone"], ["Ec"])
                V(lambda e, s0=s0, s1=s1: e.tensor_tensor(v4(tf[:]), b1(s1), b0(s0), ALU.mult), ["COSS", "SINS", "Hdone"], [("sc_b", "tf")], "pool")
                V(lambda e: e.tensor_tensor(f2(Ec[:]), f2(Ec[:]), f2(tf[:]), ALU.subtract), ["Ec", ("sc_b", "tf")], ["Ec"])
                V(lambda e, c0=c0, s1=s1: e.tensor_tensor(v4(Es[:]), b1(s1), b0(c0), ALU.mult), ["COSS", "SINS", "Hdone"], ["Es"])
                V(lambda e, s0=s0, c1=c1: e.tensor_tensor(v4(tm[:]), b1(c1), b0(s0), ALU.mult), ["COSS", "SINS", "Hdone"], [("sc_b", "tm")], "pool")
                V(lambda e: e.tensor_tensor(f2(Es[:]), f2(Es[:]), f2(tm[:]), ALU.add), ["Es", ("sc_b", "tm")], ["Es"])
                V(lambda e: e.tensor_tensor(f2(tf[:]), f2(Ec[:]), f2(Xr[:]), ALU.mult), ["Ec", ("X", 0), ("sc_b", "tf")], [("sc_b", "tf")])
                V(lambda e: e.tensor_tensor(f2(tm[:]), f2(Es[:]), f2(Xi[:]), ALU.mult), ["Es", ("X", 1), ("sc_b", "tm")], [("sc_b", "tm")], "pool")
                V(lambda e: e.tensor_tensor(f2(tf[:]), f2(tf[:]), f2(tm[:]), ALU.add), [("sc_b", "tf"), ("sc_b", "tm")], [("sc_b", "tf")])
                V(lambda e: e.tensor_tensor(f2(tm[:]), f2(Ec[:]), f2(Xi[:]), ALU.mult), ["Ec", ("X", 1), ("sc_b", "tf")], [("sc_b", "tm")])
                V(lambda e: e.tensor_tensor(f2(Xr[:]), f2(Es[:]), f2(Xr[:]), ALU.mult), ["Es", ("X", 0), ("sc_b", "tf")], [("X", 0)], "pool")
                V(lambda e: e.tensor_tensor(f2(tm[:]), f2(tm[:]), f2(Xr[:]), ALU.subtract), [("sc_b", "tm"), ("X", 0)], [("sc_b", "tm")])
                for q in range(4):
                    rq = RT[:, d_ * 4 + q:d_ * 4 + q + 1].broadcast_to([128, NCH])
                    for src, dst, kk in ((tf, Xr, 0), (tm, Xi, 1)):
                        if d_ == 0:
                            o_, i_ = dst[:, q, :], src[:, q, :]
                        else:
                            o_, i_ = dst[:, q, ::-1], src[:, q, ::-1]
                        V(lambda e, o_=o_, i_=i_, rq=rq: e.tensor_tensor_scan(o_, rq, i_, 0.0, ALU.mult, ALU.add),
                          ["RT", ("sc_b", "tf"), ("sc_b", "tm"), ("X", kk)], [("X", kk)])
                hr = Hb[:, 0, d_].rearrange("p q n -> p (q n)")
                hi = Hb[:, 1, d_].rearrange("p q n -> p (q n)")
                V(lambda e: e.tensor_tensor(f2(tf[:]), f2(Ec[:]), f2(Xr[:]), ALU.mult), ["Ec", ("X", 0), ("sc_b", "tf")], [("sc_b", "tf")])
                V(lambda e: e.tensor_tensor(f2(tm[:]), f2(Es[:]), f2(Xi[:]), ALU.mult), ["Es", ("X", 1), ("sc_b", "tm")], [("sc_b", "tm")], "pool")
                V(lambda e, hr=hr: e.tensor_tensor(hr, f2(tf[:]), f2(tm[:]), ALU.subtract), [("sc_b", "tf"), ("sc_b", "tm")], [("Hb", d_, 0)])
                V(lambda e: e.tensor_tensor(f2(tf[:]), f2(Ec[:]), f2(Xi[:]), ALU.mult), ["Ec", ("X", 1), ("Hb", d_, 0)], [("sc_b", "tf")])
                V(lambda e: e.tensor_tensor(f2(tm[:]), f2(Es[:]), f2(Xr[:]), ALU.mult), ["Es", ("X", 0), ("Hb", d_, 0)], [("sc_b", "tm")], "pool")
                V(lambda e, hi=hi: e.tensor_tensor(hi, f2(tf[:]), f2(tm[:]), ALU.add), [("sc_b", "tf"), ("sc_b", "tm")], [("Hb", d_, 1), "Hdone"])
            hbk = [("Hb", a, b) for a in range(2) for b in range(2)]
            for s in (range(TS5) if 'inter' in S5_PH else ()):
                pp = psy[s % 2]
                for q in range(4):
                    n_mm = 0
                    for d_ in range(2):
                        j = (s + 1) if d_ == 0 else (TS5 - s)
                        c0 = (NC - 1) if d_ == 0 else 1
                        for ri in range(2):
                            P.add("pe", lambda e, pp=pp, q=q, d_=d_, ri=ri, j=j, c0=c0, n_mm=n_mm: e.matmul(
                                pp[32 * q:32 * q + 32, :], MG[:, ri, d_, j - 1, 32 * q:32 * q + 32],
                                Hb[:, ri, d_, q, c0:c0 + NX], start=(n_mm == 0), stop=(n_mm == 3),
                                tile_position=(0, 32 * q)),
                                r=["MG"] + hbk, w=[("psy", s % 2)])
                            n_mm += 1
                P.add("act", lambda e, pp=pp, s=s: e.copy(YI[:, :, s], pp[:]), r=[("psy", s % 2)], w=["YI", "Us"])
            for tb in (range(16) if 'lag' in S5_PH else ()):
                pp = psy[tb % 2]
                b = tb % 2
                u0 = CTX + tb * 512
                uv = Ub[:, u0:u0 + 512].rearrange("p (n s) -> p n s", s=TS5)
                pv = pp[:].rearrange("p (n s) -> p n s", s=TS5)
                P.add("pe", lambda e, pp=pp, u0=u0: e.matmul(pp[:], DG[:], Ub[:, u0:u0 + 512], start=True, stop=False),
                      r=["DG", "Ub"], w=[("psy", b)])
                for j in range(TS5):
                    P.add("pe", lambda e, pv=pv, uv=uv, j=j: e.matmul(pv[:, :, j:TS5], LAG[:, 0, j, :], uv[:, :, 0:TS5 - j],
                                                                      start=False, stop=False),
                          r=["LAG", "Ub"], w=[("psy", b)])
                    P.add("pe", lambda e, pv=pv, uv=uv, j=j: e.matmul(pv[:, :, 0:TS5 - j], LAG[:, 1, j, :], uv[:, :, j:TS5],
                                                                      start=False, stop=(j == TS5 - 1)),
                          r=["LAG", "Ub"], w=[("psy", b)])
                if S5_CUT < 2:
                    continue
                yiv = YI[:, tb * 32:(tb + 1) * 32, :].rearrange("p n s -> p (n s)")
                P.add("dve", lambda e, pp=pp, b=b, yiv=yiv: e.tensor_tensor(ys[b][:], pp[:], yiv, ALU.add),
                      r=[("psy", b), "YI"], w=[("ys", b)])
                P.add("act", lambda e, b=b: e.activation(out=y2[b][:], in_=ys[b][:], func=AF.Square), r=[("ys", b)], w=[("y2", b)])
                P.add("dve", lambda e, b=b: e.tensor_scalar(y2[b][:], y2[b][:], 0.044715, 1.0, ALU.mult, ALU.add),
                      r=[("y2", b)], w=[("y2", b)])
                P.add("dve", lambda e, b=b: e.tensor_tensor(y2[b][:], y2[b][:], ys[b][:], ALU.mult), r=[("y2", b), ("ys", b)], w=[("y2", b)])
                P.add("act", lambda e, b=b: e.activation(out=y2[b][:], in_=y2[b][:], func=AF.Sigmoid, scale=1.5957691216),
                      r=[("y2", b)], w=[("y2", b)])
                P.add("dve", lambda e, b=b: e.tensor_tensor(yo[b][:], y2[b][:], ys[b][:], ALU.mult), r=[("y2", b), ("ys", b)], w=[("yo", b)])
                if S5_CUT < 3:
                    continue
                P.dma("sp", t["YG"][blk][:, tb * 512:(tb + 1) * 512], yo[b][:], r=[("yo", b)], w=[("YG", blk, tb)])
        P.flush()


def declare_ml(k):
    k.din("convw", [16, 128, 5])
    k.din("convb", [16, 128, 1])
    k.din("gateb", [128, 16])
    k.din("mlng", [128, D_ML])
    k.din("c_tri", [128, 128])
    k.din("c_trit", [128, 128])
    k.din("c_ones", [128, 128])
    k.dscr("QKT", [16, 128, NT], BF16)
    k.dscr("HDIR", [NH, 2, L, DH])
    k.dscr("YML", [L, D_ML], BF16)


def stage4a(k):
    nc, P, t = k.nc, k.P, k.t
    PADW = 2 + CTX + 2 + 2 + L + 2
    with ExitStack() as st:
        sb = lambda n, s, d=F32: st.enter_context(nc.sbuf_tensor(n, s, d))
        identf = sb("c_idf", [128, 128])
        pp = [sb("c_pp%d" % i, [128, PADW], BF16) for i in range(2)]
        cw = [sb("c_cw%d" % i, [128, 5]) for i in range(2)]
        cb_ = [sb("c_cb%d" % i, [128, 1]) for i in range(2)]
        dg = [sb("c_dg%d" % i, [128, 5, 128], BF16) for i in range(2)]
        of = [sb("c_of%d" % i, [128, 512]) for i in range(2)]
        ob = [sb("c_ob%d" % i, [128, NT], BF16) for i in range(2)]
        ps = [st.enter_context(nc.psum_tensor("c_ps%d" % i, [128, 512], F32)) for i in range(2)]
        P.dma("sp", identf[:], t["ident_f"][:, :], w=["identf"])
        for b in range(2):
            for (a0, a1) in ((0, 2), (2 + CTX, 2 + CTX + 4), (PADW - 2, PADW)):
                P.add("pool", lambda e, b=b, a0=a0, a1=a1: e.memset(pp[b][:, a0:a1], 0.0), w=[("pad", b)])
        segs = [(0, CTX, 2)] + [(CTX + 512 * i, 512, 2 + CTX + 4 + 512 * i) for i in range(16)]
        for cb in range(16):
            b = cb % 2
            P.dma("sp", pp[b][:, 2:2 + CTX], t["QKPRE"][cb][:, 0:CTX], r=[("QKPRE", 0), ("pad", b)], w=[("pp", b)])
            P.dma("sp", pp[b][:, 2 + CTX + 4:2 + CTX + 4 + L], t["QKPRE"][cb][:, CTX:NT],
                  r=[("QKPRE", g) for g in range(1, 17)] + [("pad", b)], w=[("pp", b)])
            P.dma("sp", cw[b][:], t["convw"][cb], w=[("cw", b)])
            P.dma("sp", cb_[b][:], t["convb"][cb], w=[("cb", b)])
            for kk in range(5):
                P.add("dve", lambda e, b=b, kk=kk: e.tensor_scalar_mul(dg[b][:, kk, :], identf[:], cw[b][:, kk:kk + 1]),
                      r=["identf", ("cw", b)], w=[("dg", b)])
            for si, (t0, n, po) in enumerate(segs):
                pb = si % 2
                for kk in range(5):
                    P.add("pe", lambda e, pb=pb, b=b, kk=kk, po=po, n=n: e.matmul(
                        ps[pb][:, 0:n], dg[b][:, kk, :], pp[b][:, po + kk - 2:po + kk - 2 + n], start=(kk == 0), stop=(kk == 4)),
                        r=[("dg", b), ("pp", b)], w=[("ps", pb)])
                if cb < 8:
                    P.add("act", lambda e, pb=pb, b=b, n=n: e.activation(out=of[pb][:, 0:n], in_=ps[pb][:, 0:n], func=AF.Silu,
                                                                         bias=cb_[b][:, 0:1], scale=1.0),
                          r=[("ps", pb), ("cb", b)], w=[("of", pb)])
                    P.add("dve", lambda e, pb=pb, b=b, n=n, t0=t0: e.tensor_scalar_mul(ob[b][:, t0:t0 + n], of[pb][:, 0:n], 1.0 / 16.0),
                          r=[("of", pb)], w=[("ob", b)])
                else:
                    P.add("act", lambda e, pb=pb, b=b, n=n, t0=t0: e.activation(out=ob[b][:, t0:t0 + n], in_=ps[pb][:, 0:n], func=AF.Silu,
                                                                               bias=cb_[b][:, 0:1], scale=1.0),
                          r=[("ps", pb), ("cb", b)], w=[("ob", b)])
            P.dma(k.q(), t["QKT"][cb], ob[b][:], r=[("ob", b)], w=[("QKT", cb)])
        P.flush()


def stage4b(k):
    nc, P, t = k.nc, k.P, k.t
    NG_ = NTILE * NH
    with ExitStack() as st:
        sb = lambda n, s, d=F32: st.enter_context(nc.sbuf_tensor(n, s, d))
        G = sb("m_G", [128, NTILE, 16])
        gb = sb("m_gb", [128, 16])
        tri = sb("m_tri", [128, 128])
        trit = sb("m_trit", [128, 128])
        ones = sb("m_ones", [128, 128])
        trib = sb("m_trib", [128, 2, 128], BF16)
        identb = sb("m_idb", [128, 128], BF16)
        one1 = sb("m_one1", [128, 1])
        LF = sb("m_LF", [128, 2, NTILE, NH])
        SC = sb("m_SC", [128, 2, 4, NTILE, NH])
        tmpg = sb("m_tmpg", [128, NTILE, NH])
        pso_full = [st.enter_context(nc.psum_tensor("m_pso%d" % i, [128, NG_], F32)) for i in range(2)]
        psg = pso_full
        P.dma("sp", G[:], t["GATES"][:, :, :], r=["GATES"], w=["G"])
        P.dma("sp", gb[:], t["gateb"][:, :], w=["gb"])
        P.dma("act", tri[:], t["c_tri"][:, :], w=["tri"])
        P.dma("act", trit[:], t["c_trit"][:, :], w=["trit"])
        P.dma("sp", ones[:], t["c_ones"][:, :], w=["ones"])
        P.dma("sp", identb[:], t["ident_bf"][:, :], w=["identb"])
        P.add("pool", lambda e: e.memset(one1[:], 1.0), w=["one1"])
        P.add("dve", lambda e: e.tensor_copy(trib[:, 0, :], tri[:]), r=["tri"], w=["trib"])
        P.add("dve", lambda e: e.tensor_copy(trib[:, 1, :], trit[:]), r=["trit"], w=["trib"])
        P.add("dve", lambda e: e.tensor_tensor(G[:], G[:], gb[:].unsqueeze(1).broadcast_to([128, NTILE, 16]), ALU.add),
              r=["G", "gb"], w=["G"])
        for d_ in range(2):
            ipre = G[:, :, 8 * d_:8 * d_ + 4]
            fpre = G[:, :, 8 * d_ + 4:8 * d_ + 8]
            lf = LF[:, d_]
            P.add("act", lambda e, lf=lf, fpre=fpre: e.activation(out=lf, in_=fpre, func=AF.Exp, scale=-1.0), r=["G"], w=[("LF", d_)])
            P.add("act", lambda e, lf=lf: e.activation(out=lf, in_=lf, func=AF.Ln, bias=one1[:, 0:1], scale=1.0),
                  r=[("LF", d_), "one1"], w=[("LF", d_)])
            lff = lf.rearrange("p n h -> p (n h)")
            P.add("dve", lambda e, lff=lff: e.tensor_scalar_mul(lff, lff, -1.0), r=[("LF", d_)], w=[("LF", d_)])
            m_ = tri if d_ == 0 else trit
            P.add("pe", lambda e, m_=m_, lff=lff: e.matmul(psg[0][:], m_[:], lff, start=True, stop=True),
                  r=["tri", "trit", ("LF", d_)], w=["psg0"])
            P.add("pe", lambda e, lff=lff: e.matmul(psg[1][:], ones[:], lff, start=True, stop=True),
                  r=["ones", ("LF", d_)], w=["psg1"])
            u_ = SC[:, d_, 0].rearrange("p n h -> p (n h)")
            fl_ = SC[:, d_, 1].rearrange("p n h -> p (n h)")
            dec_ = SC[:, d_, 2].rearrange("p n h -> p (n h)")
            wt_ = SC[:, d_, 3].rearrange("p n h -> p (n h)")
            tg = tmpg[:].rearrange("p n h -> p (n h)")
            P.add("dve", lambda e, ipre=ipre: e.tensor_copy(tmpg[:], ipre), r=["G"], w=["tmpg"])
            P.add("dve", lambda e, tg=tg: e.tensor_tensor(tg, tg, psg[0][:], ALU.subtract), r=["tmpg", "psg0"], w=["tmpg"])
            P.add("act", lambda e, u_=u_, tg=tg: e.activation(out=u_, in_=tg, func=AF.Exp), r=["tmpg"], w=[("SC", d_)])
            P.add("act", lambda e, fl_=fl_: e.activation(out=fl_, in_=psg[0][:], func=AF.Exp, scale=-1.0), r=["psg0"], w=[("SC", d_)])
            P.add("act", lambda e, dec_=dec_: e.activation(out=dec_, in_=psg[1][:], func=AF.Exp), r=["psg1"], w=[("SC", d_)])
            P.add("dve", lambda e, wt_=wt_, u_=u_, dec_=dec_: e.tensor_tensor(wt_, u_, dec_, ALU.mult), r=[("SC", d_)], w=[("SC", d_)])
        QT = sb("m_QT", [128, 2, NT], BF16)
        KT = sb("m_KT", [128, 2, NT], BF16)
        Vh = sb("m_Vh", [128, NTILE, DH + 1], BF16)
        CT = [sb("m_CT%d" % i, [128, 2, DH + 1]) for i in range(2)]
        CTb = [sb("m_CTb%d" % i, [128, 2, DH + 1], BF16) for i in range(2)]
        kt = [sb("m_kt%d" % i, [128, DH], BF16) for i in range(2)]
        SW = [sb("m_SW%d" % i, [128, 128], BF16) for i in range(2)]
        vw = [sb("m_vw%d" % i, [128, DH + 1], BF16) for i in range(2)]
        dn = [sb("m_dn%d" % i, [128, 2]) for i in range(2)]
        ho = [sb("m_ho%d" % i, [128, DH]) for i in range(2)]
        pst = [st.enter_context(nc.psum_tensor("m_pst%d" % i, [128, DH], BF16)) for i in range(2)]
        pss = [st.enter_context(nc.psum_tensor("m_pss%d" % i, [128, 128], F32)) for i in range(2)]
        pso = [pso_full[i][:, 0:DH + 1] for i in range(2)]
        psc_ = [st.enter_context(nc.psum_tensor("m_psc%d" % i, [128, DH + 1], F32)) for i in range(2)]
        for h in range(NH):
            for dc in range(2):
                P.dma("sp", QT[:, dc, :], t["QKT"][2 * h + dc], r=[("QKT", 2 * h + dc)], w=["QT"])
                P.dma("act", KT[:, dc, :], t["QKT"][8 + 2 * h + dc], r=[("QKT", 8 + 2 * h + dc)], w=["KT"])
            for n0 in range(0, NTILE, 11):
                P.dma(k.q(), Vh[:, n0:n0 + 11, 0:DH],
                      t["V"].rearrange("(n p) c -> p n c", p=128)[:, n0:n0 + 11, h * DH:(h + 1) * DH],
                      r=[("V", ti) for ti in range(n0, n0 + 11)], w=["Vh"])
            P.add("pool", lambda e: e.memset(Vh[:, :, DH:DH + 1], 1.0), w=["Vh1"])
            for d_ in range(2):
                P.add("pool", lambda e, d_=d_: e.memset(CT[d_][:], 0.0), w=[("CT", d_)])
                P.add("pool", lambda e, d_=d_: e.memset(CTb[d_][:], 0.0), w=[("CTb", d_)])
            order_f = list(range(NTILE))
            order_b = [1, 0] + list(range(NTILE - 1, 1, -1))
            for step in range(NTILE):
                for d_ in range(2):
                    ti = order_f[step] if d_ == 0 else order_b[step]
                    c0 = ti * 128
                    col = ti * NH + h
                    sc = lambda kind, d_=d_, col=col: SC[:, d_, kind].rearrange("p n h -> p (n h)")[:, col:col + 1]
                    for dc in range(2):
                        P.add("pe", lambda e, d_=d_, dc=dc, c0=c0: e.transpose(pst[d_][:, dc * 128:(dc + 1) * 128],
                                                                               KT[:, dc, c0:c0 + 128], identb[:]),
                              r=["KT", "identb"], w=[("pst", d_)])
                    P.add("act", lambda e, d_=d_: e.copy(kt[d_][:], pst[d_][:]), r=[("pst", d_)], w=[("kt", d_)])
                    if ti >= 2:
                        for dc in range(2):
                            P.add("pe", lambda e, d_=d_, dc=dc, c0=c0: e.matmul(pss[d_][:], KT[:, dc, c0:c0 + 128], QT[:, dc, c0:c0 + 128],
                                                                                start=(dc == 0), stop=(dc == 1)),
                                  r=["KT", "QT"], w=[("pss", d_)])
                        P.add("dve", lambda e, d_=d_, sc=sc: e.scalar_tensor_tensor(SW[d_][:], pss[d_][:], sc(0), trib[:, d_, :],
                                                                                    ALU.mult, ALU.mult),
                              r=[("pss", d_), ("SC", d_), "trib"], w=[("SW", d_)])
                        P.add("pe", lambda e, d_=d_, ti=ti: e.matmul(pso[d_][:], SW[d_][:], Vh[:, ti, :], start=True, stop=False),
                              r=[("SW", d_), "Vh", "Vh1"], w=[("pso", d_), "psg%d" % d_])
                        for dc in range(2):
                            P.add("pe", lambda e, d_=d_, dc=dc, c0=c0: e.matmul(pso[d_][:], QT[:, dc, c0:c0 + 128], CTb[d_][:, dc, :],
                                                                                start=False, stop=(dc == 1)),
                                  r=["QT", ("CTb", d_)], w=[("pso", d_)])
                        P.add("act", lambda e, d_=d_: e.activation(out=dn[d_][:, 0:1], in_=pso[d_][:, DH:DH + 1], func=AF.Abs),
                              r=[("pso", d_)], w=[("dn", d_)])
                        P.add("dve", lambda e, d_=d_, sc=sc: e.tensor_tensor(dn[d_][:, 0:1], dn[d_][:, 0:1], sc(1), ALU.max),
                              r=[("dn", d_), ("SC", d_)], w=[("dn", d_)])
                        P.add("dve", lambda e, d_=d_: e.reciprocal(dn[d_][:, 1:2], dn[d_][:, 0:1]), r=[("dn", d_)], w=[("dn1", d_)])
                        P.add("act", lambda e, d_=d_: e.activation(out=ho[d_][:], in_=pso[d_][:, 0:DH], func=AF.Copy,
                                                                   scale=dn[d_][:, 1:2]),
                              r=[("pso", d_), ("dn1", d_)], w=[("ho", d_)])
                        P.dma("sp", t["HDIR"][h, d_, (ti - 2) * 128:(ti - 1) * 128, :], ho[d_][:], r=[("ho", d_)],
                              w=[("HDIR", h, d_, ti)])
                    P.add("dve", lambda e, d_=d_, ti=ti, sc=sc: e.tensor_scalar_mul(vw[d_][:], Vh[:, ti, :], sc(3)),
                          r=["Vh", "Vh1", ("SC", d_)], w=[("vw", d_)])
                    for dc in range(2):
                        pc = psc_[dc]
                        P.add("pe", lambda e, d_=d_, dc=dc, pc=pc: e.matmul(pc[:], kt[d_][:, dc * 128:(dc + 1) * 128], vw[d_][:],
                                                                            start=True, stop=True),
                              r=[("kt", d_), ("vw", d_)], w=[("psc", dc)])
                        P.add("dve", lambda e, d_=d_, dc=dc, pc=pc, sc=sc: e.scalar_tensor_tensor(
                            CT[d_][:, dc, :], CT[d_][:, dc, :], sc(2), pc[:], ALU.mult, ALU.add),
                            r=[("psc", dc), ("CT", d_), ("SC", d_)], w=[("CT", d_)])
                        P.add("act", lambda e, d_=d_, dc=dc: e.copy(CTb[d_][:, dc, :], CT[d_][:, dc, :]),
                              r=[("CT", d_)], w=[("CTb", d_)])
        P.flush()


def stage4c(k):
    nc, P, t = k.nc, k.P, k.t
    with ExitStack() as st:
        sb = lambda n, s, d=F32: st.enter_context(nc.sbuf_tensor(n, s, d))
        ng = sb("n_ng", [128, D_ML])
        epst = sb("n_eps", [128, 1])
        hf = [sb("n_hf%d" % i, [128, NH, DH]) for i in range(2)]
        hb = [sb("n_hb%d" % i, [128, NH, DH]) for i in range(2)]
        so = [sb("n_so%d" % i, [128, D_ML], BF16) for i in range(2)]
        junk = sb("n_junk", [128, DH])
        ss = [sb("n_ss%d" % i, [128, 2, NH]) for i in range(2)]
        yo = [sb("n_yo%d" % i, [128, D_ML], BF16) for i in range(2)]
        P.dma("sp", ng[:], t["mlng"][:, :], w=["ng"])
        P.add("pool", lambda e: e.memset(epst[:], EPS), w=["epst"])
        for i in range(L // 128):
            b = i % 2
            P.dma("sp", hf[b][:], t["HDIR"][:, 0, i * 128:(i + 1) * 128, :].rearrange("h p d -> p h d"),
                  r=[("HDIR", h, 0, i + 2) for h in range(NH)], w=[("hf", b)])
            P.dma("sp", hb[b][:], t["HDIR"][:, 1, i * 128:(i + 1) * 128, :].rearrange("h p d -> p h d"),
                  r=[("HDIR", h, 1, i + 2) for h in range(NH)], w=[("hb", b)])
            P.dma("sp", so[b][:], t["SO"][(i + 2) * 128:(i + 3) * 128, :], r=[("SO", i + 2)], w=[("so", b)])
            hff = hf[b][:].rearrange("p h d -> p (h d)")
            hbf = hb[b][:].rearrange("p h d -> p (h d)")
            P.add("dve", lambda e, hff=hff, hbf=hbf: e.tensor_tensor(hff, hff, hbf, ALU.add), r=[("hf", b), ("hb", b)], w=[("hf", b)])
            for h in range(NH):
                P.add("act", lambda e, b=b, h=h: e.activation(out=junk[:], in_=hf[b][:, h, :], func=AF.Square, scale=float(DH ** -0.5),
                                                              accum_out=ss[b][:, 0, h:h + 1]),
                      r=[("hf", b)], w=["junk", ("ss", b)])
            P.add("act", lambda e, b=b: e.activation(out=ss[b][:, 1, :], in_=ss[b][:, 0, :], func=AF.Sqrt, bias=epst[:, 0:1], scale=1.0),
                  r=[("ss", b), "epst"], w=[("ss1", b)])
            P.add("dve", lambda e, b=b: e.reciprocal(ss[b][:, 1, :], ss[b][:, 1, :]), r=[("ss1", b)], w=[("ss1", b)])
            for h in range(NH):
                P.add("dve", lambda e, b=b, h=h: e.scalar_tensor_tensor(hf[b][:, h, :], hf[b][:, h, :], ss[b][:, 1, h:h + 1],
                                                                        ng[:, h * DH:(h + 1) * DH], ALU.mult, ALU.mult),
                      r=[("hf", b), ("ss1", b), "ng"], w=[("hf", b)])
            P.add("pool", lambda e, b=b, hff=hff: e.tensor_tensor(yo[b][:], hff, so[b][:], ALU.mult), r=[("hf", b), ("so", b)], w=[("yo", b)])
            P.dma("sp", t["YML"][i * 128:(i + 1) * 128, :], yo[b][:], r=[("yo", b)], w=[("YML", i)])
        P.flush()


def declare_s5post(k):
    k.din("gluw", [D_S5, D_S5])
    k.din("glub", [128, 8])
    k.din("w_out", [D, D])
    k.din("rw", [128, 16, NE])
    k.dscr("HX2", [L, D], BF16)
    k.dscr("X1", [L, D])
    k.dscr("AFFD", [128, L // 128, NE])


def stage5(k):
    nc, P, t = k.nc, k.P, k.t
    with ExitStack() as st:
        sb = lambda n, s, d=F32: st.enter_context(nc.sbuf_tensor(n, s, d))
        Wg = sb("p_Wg", [128, 8, D_S5], BF16)
        Wo = sb("p_Wo", [128, 16, D], BF16)
        M2 = [sb("p_M%d" % i, [128, D]) for i in range(3)]
        RW = sb("p_RW", [128, 16, NE])
        glub = sb("p_glub", [128, 8])
        identb = sb("p_idb", [128, 128], BF16)
        identf = sb("p_idf", [128, 128])
        epst = sb("p_eps", [128, 1])
        ygT = [sb("p_yg%d" % i, [128, 8, 512], BF16) for i in range(2)]
        sig = sb("p_sig", [128, 512], BF16)
        yglu = sb("p_yglu", [128, 8, 512], BF16)
        yml = [sb("p_yml%d" % i, [128, D_ML], BF16) for i in range(2)]
        ymlT = [sb("p_ymlT%d" % i, [128, 8, 128], BF16) for i in range(2)]
        yx = sb("p_yx", [128, D])
        xt = [sb("p_xt%d" % i, [128, D]) for i in range(2)]
        x1 = [sb("p_x1%d" % i, [128, D]) for i in range(2)]
        hx2 = sb("p_hx2", [128, D])
        hx2b = sb("p_hx2b", [128, D], BF16)
        junk = sb("p_junk", [128, D], BF16)
        hx2T = sb("p_hx2T", [128, 16, 128])
        ss = sb("p_ss", [128, 8])
        AFF = sb("p_AFF", [128, L // 128, NE])
        pw = [st.enter_context(nc.psum_tensor("p_pw%d" % i, [128, 512], F32)) for i in range(4)]
        pz = [st.enter_context(nc.psum_tensor("p_pz%d" % i, [128, 512], F32)) for i in range(2)]
        pt = st.enter_context(nc.psum_tensor("p_pt", [128, 8, 128], BF16))
        pl = st.enter_context(nc.psum_tensor("p_pl", [128, NE], F32))
        gw = t["gluw"].rearrange("(c p) n -> p c n", p=128)
        for c in range(8):
            P.dma("pool", Wg[:, c, :], gw[:, c, :], w=[("Wg", c)])
        wo = t["w_out"].rearrange("(c p) n -> p c n", p=128)
        for c in range(16):
            for h_ in range(2):
                P.dma("pool", Wo[:, c, h_ * 1024:(h_ + 1) * 1024], wo[:, c, h_ * 1024:(h_ + 1) * 1024], w=[("Wo", c, h_)])
        for i in range(3):
            P.dma("sp", M2[i][:], t["MODS2"][i], r=["M2_%d" % i], w=[("M2", i)])
        P.dma("sp", RW[:], t["rw"][:, :, :], w=["RW"])
        P.dma("sp", glub[:], t["glub"][:, :], w=["glub"])
        P.dma("act", identb[:], t["ident_bf"][:, :], w=["identb"])
        P.dma("act", identf[:], t["ident_f"][:, :], w=["identf"])
        P.add("pool", lambda e: e.memset(epst[:], EPS), w=["epst"])
        ygsrc = t["YG"].rearrange("c p t -> p c t")
        ymlsrc = t["YML"].rearrange("(w r) c -> r w c", r=128)
        ymlkeys = [("YML", i) for i in range(L // 128)]
        def phase_G(g5):
            gb_ = g5 % 2
            P.dma("sp", ygT[gb_][:], ygsrc[:, :, g5 * 512:(g5 + 1) * 512], r=[("YG", blk, g5) for blk in range(8)], w=[("ygT", gb_)])
            for c2 in range(8):
                pzz = pz[c2 % 2]
                for c in range(8):
                    P.add("pe", lambda e, pzz=pzz, c=c, c2=c2, gb_=gb_: e.matmul(pzz[:], Wg[:, c, c2 * 128:(c2 + 1) * 128], ygT[gb_][:, c, :],
                                                                                 start=(c == 0), stop=(c == 7)),
                          r=[("Wg", c), ("ygT", gb_)], w=[("pz", c2 % 2)])
                P.add("act", lambda e, pzz=pzz, c2=c2: e.activation(out=sig[:], in_=pzz[:], func=AF.Sigmoid, bias=glub[:, c2:c2 + 1], scale=1.0),
                      r=[("pz", c2 % 2), "glub"], w=["sig"])
                P.add("dve", lambda e, c2=c2, gb_=gb_: e.tensor_tensor(yglu[:, c2, :], ygT[gb_][:, c2, :], sig[:], ALU.mult),
                      r=["sig", ("ygT", gb_)], w=["yglu"])

        def phase_L(ti):
            tb_ = ti % 2
            for a in range(2):
                P.dma("sp", yml[tb_][64 * a:64 * (a + 1), :], ymlsrc[ti * 2 + a], r=ymlkeys, w=[("yml", tb_)])
            P.dma("sp", xt[tb_][:], t["x"][ti * 128:(ti + 1) * 128, :], w=[("xt", tb_)])

        def phase_A(ti):
            j = ti % 4
            tb_ = ti % 2
            xb = ti % 2
            r0 = ti * 2
            for c in range(8):
                P.add("pe", lambda e, c=c, tb_=tb_: e.transpose(pt[:, c, :], yml[tb_][:, c * 128:(c + 1) * 128], identb[:]),
                      r=[("yml", tb_), "identb"], w=["pt"])
            P.add("act", lambda e, tb_=tb_: e.copy(ymlT[tb_][:], pt[:]), r=["pt"], w=[("ymlT", tb_)])
            for nb in range(4):
                for c in range(16):
                    lhs = yglu[:, c, j * 128:(j + 1) * 128] if c < 8 else ymlT[tb_][:, c - 8, :]
                    P.add("pe", lambda e, nb=nb, c=c, lhs=lhs: e.matmul(pw[nb][:], lhs, Wo[:, c, nb * 512:(nb + 1) * 512],
                                                                        start=(c == 0), stop=(c == 15)),
                          r=["yglu", ("ymlT", tb_), ("Wo", c, nb // 2)], w=[("pw", nb)])
                if nb % 2 == 0:
                    P.add("act", lambda e, nb=nb: e.copy(yx[:, nb * 512:(nb + 1) * 512], pw[nb][:]), r=[("pw", nb)], w=[("yx", nb)])
                else:
                    P.add("dve", lambda e, nb=nb: e.tensor_copy(yx[:, nb * 512:(nb + 1) * 512], pw[nb][:]), r=[("pw", nb)], w=[("yx", nb)])
            yxk = [("yx", nb) for nb in range(4)]
            P.add("act", lambda e: e.activation(out=junk[:], in_=yx[:], func=AF.Square, scale=float(D ** -0.5), accum_out=ss[:, 0:1]),
                  r=yxk, w=["junk", "ss0"])
            P.add("act", lambda e: e.activation(out=ss[:, 1:2], in_=ss[:, 0:1], func=AF.Sqrt, bias=epst[:, 0:1], scale=1.0),
                  r=["ss0", "epst"], w=["ss1"])
            P.add("dve", lambda e: e.reciprocal(ss[:, 1:2], ss[:, 1:2]), r=["ss1"], w=["ss1"])
            P.add("dve", lambda e, xb=xb: e.scalar_tensor_tensor(x1[xb][:], yx[:], ss[:, 1:2], M2[0][:], ALU.mult, ALU.mult),
                  r=yxk + ["ss1", ("M2", 0)], w=[("x1", xb)])
            P.add("pool", lambda e, xb=xb: e.tensor_tensor(x1[xb][:], x1[xb][:], xt[xb][:], ALU.add), r=[("x1", xb), ("xt", xb)], w=[("x1", xb)])
            P.dma("sp", t["X1"][ti * 128:(ti + 1) * 128, :], x1[xb][:], r=[("x1", xb)], w=[("X1", ti)])

        def phase_B(ti):
            xb = ti % 2
            P.add("act", lambda e, xb=xb: e.activation(out=hx2b[:], in_=x1[xb][:], func=AF.Square, scale=float(D ** -0.5), accum_out=ss[:, 2:3]),
                  r=[("x1", xb)], w=["hx2b", "ss2"])
            P.add("act", lambda e: e.activation(out=ss[:, 3:4], in_=ss[:, 2:3], func=AF.Sqrt, bias=epst[:, 0:1], scale=1.0),
                  r=["ss2", "epst"], w=["ss3"])
            P.add("dve", lambda e: e.reciprocal(ss[:, 3:4], ss[:, 3:4]), r=["ss3"], w=["ss3"])
            P.add("dve", lambda e, xb=xb: e.scalar_tensor_tensor(hx2[:], x1[xb][:], ss[:, 3:4], M2[1][:], ALU.mult, ALU.mult),
                  r=[("x1", xb), "ss3", ("M2", 1)], w=["hx2"])
            P.add("pool", lambda e: e.tensor_tensor(hx2[:], hx2[:], M2[2][:], ALU.add), r=["hx2", ("M2", 2)], w=["hx2"])
            P.add("act", lambda e: e.copy(hx2b[:], hx2[:]), r=["hx2"], w=["hx2b"])
            P.dma("sp", t["HX2"][ti * 128:(ti + 1) * 128, :], hx2b[:], r=["hx2b"], w=[("HX2", ti)])
            for g4 in range(4):
                pzz = pz[g4 % 2]
                for c in range(4):
                    kc = g4 * 4 + c
                    P.add("pe", lambda e, pzz=pzz, c=c, kc=kc: e.transpose(pzz[:, c * 128:(c + 1) * 128], hx2[:, kc * 128:(kc + 1) * 128], identf[:]),
                          r=["hx2", "identf"], w=[("pz", g4 % 2)])
                P.add("dve", lambda e, pzz=pzz, g4=g4: e.tensor_copy(hx2T[:, g4 * 4:(g4 + 1) * 4, :].rearrange("p c t -> p (c t)"), pzz[:]),
                      r=[("pz", g4 % 2)], w=[("hx2T", g4)])
            for kc in range(16):
                P.add("pe", lambda e, kc=kc: e.matmul(pl[:], hx2T[:, kc, :], RW[:, kc, :], start=(kc == 0), stop=(kc == 15)),
                      r=[("hx2T", kc // 4), "RW"], w=["pl"])
            P.add("dve", lambda e: e.tensor_reduce(ss[:, 4:5], pl[:], AX.X, ALU.max), r=["pl"], w=["ss4"])
            P.add("dve", lambda e: e.tensor_scalar_mul(ss[:, 4:5], ss[:, 4:5], -1.0), r=["ss4"], w=["ss4"])
            P.add("act", lambda e, ti=ti: e.activation(out=AFF[:, ti, :], in_=pl[:], func=AF.Exp, bias=ss[:, 4:5], scale=1.0,
                                                       accum_out=ss[:, 5:6]),
                  r=["pl", "ss4"], w=["AFF", "ss5"])
            P.add("dve", lambda e: e.reciprocal(ss[:, 5:6], ss[:, 5:6]), r=["ss5"], w=["ss5"])
            P.add("dve", lambda e, ti=ti: e.tensor_scalar_mul(AFF[:, ti, :], AFF[:, ti, :], ss[:, 5:6]), r=["AFF", "ss5"], w=["AFF"])

        phase_L(0)
        for ti in range(L // 128):
            if ti % 4 == 0:
                phase_G(ti // 4)
            if ti + 1 < L // 128:
                phase_L(ti + 1)
            phase_A(ti)
            if ti > 0:
                phase_B(ti - 1)
        phase_B(L // 128 - 1)
        P.dma("sp", t["AFFD"][:, :, :], AFF[:], r=["AFF"], w=["AFFD"])
        P.flush()


def declare_moe(k):
    k.din("ewg", [NE, D, FF])
    k.din("ewu", [NE, D, FF])
    k.din("ewd", [NE, FF, D])
    k.din("ownidx", [128, OWN // 128], I32)
    k.din("c_slt", [128, 128], BF16)
    k.din("c_onesb", [128, 128], BF16)
    k.din("c_iota", [128, CAPL])
    k.dscr("AFFT", [L, NE])
    k.dscr("TH", [128, NE])
    k.dscr("YGALL", [NE, 3, 128, D], BF16)
    k.dscr("OHTALL", [OWN // 128, 128, NE * 3, 128], BF16)
    k.dscr("MOEO", [OWN, D])


def stage6(k):
    nc, P, t = k.nc, k.P, k.t
    NTL = L // 128
    with ExitStack() as st:
        sb = lambda n, s, d=F32: st.enter_context(nc.sbuf_tensor(n, s, d))
        A = sb("t_A", [128, NTL, NE])
        cmp_ = sb("t_cmp", [128, NTL, NE], BF16)
        onesb = sb("t_onesb", [128, 128])
        v = sb("t_v", [128, 8, NE])
        cntp = sb("t_cntp", [128, NE])
        pc = st.enter_context(nc.psum_tensor("t_pc", [128, NE], F32))
        P.dma("sp", A[:], t["AFFD"][:, :, :], r=["AFFD"], w=["A"])
        P.dma("act", t["AFFT"].rearrange("(n p) e -> p n e", p=128), A[:], r=["A"], w=["AFFT"])
        P.dma("sp", onesb[:], t["c_ones"][:, :], w=["onesb"])
        P.add("pool", lambda e: e.memset(v[:, 0, :], 0.0), w=["lo"])
        P.add("pool", lambda e: e.memset(v[:, 1, :], 1.0), w=["hi"])
        lo, hi, th, ge, d1, d2 = (v[:, i, :] for i in range(6))
        for it in range(30):
            P.add("dve", lambda e: e.tensor_tensor(th, lo, hi, ALU.add), r=["lo", "hi"], w=["th"])
            P.add("dve", lambda e: e.tensor_scalar_mul(th, th, 0.5), r=["th"], w=["th"])
            P.add("dve", lambda e: e.tensor_tensor(cmp_[:], A[:], th.unsqueeze(1).broadcast_to([128, NTL, NE]), ALU.is_ge),
                  r=["A", "th"], w=["cmp"])
            P.add("dve", lambda e: e.tensor_reduce(cntp[:], cmp_[:].rearrange("p n e -> p e n"), AX.X, ALU.add), r=["cmp"], w=["cntp"])
            P.add("pe", lambda e: e.matmul(pc[:], onesb[:], cntp[:], start=True, stop=True), r=["onesb", "cntp"], w=["pc"])
            P.add("dve", lambda e: e.tensor_single_scalar(ge, pc[:], float(CAPE) - 0.5, ALU.is_gt), r=["pc"], w=["ge"])
            P.add("dve", lambda e: e.tensor_tensor(d1, th, lo, ALU.subtract), r=["th", "lo"], w=["d1"])
            P.add("dve", lambda e: e.tensor_tensor(d1, d1, ge, ALU.mult), r=["d1", "ge"], w=["d1"])
            P.add("dve", lambda e: e.tensor_tensor(d2, hi, th, ALU.subtract), r=["th", "hi"], w=["d2"])
            P.add("dve", lambda e: e.tensor_tensor(d2, d2, ge, ALU.mult), r=["d2", "ge"], w=["d2"])
            P.add("dve", lambda e: e.tensor_tensor(lo, lo, d1, ALU.add), r=["lo", "d1"], w=["lo"])
            P.add("dve", lambda e: e.tensor_tensor(hi, th, d2, ALU.add), r=["th", "d2"], w=["hi"])
        P.dma("sp", t["TH"][:, :], lo, r=["lo"], w=["TH"])
        P.flush()


def stage7(k):
    nc, P, t = k.nc, k.P, k.t
    NO = OWN // 128
    with ExitStack() as st:
        sb = lambda n, s, d=F32: st.enter_context(nc.sbuf_tensor(n, s, d))
        oidx = sb("e_oidx", [128, NO], I32)
        HX = sb("e_HX", [128, NO, D], BF16)
        Ao = sb("e_Ao", [128, NO, NE])
        th = sb("e_th", [128, NE])
        sel = sb("e_sel", [128, NO, NE])
        selb = sb("e_selb", [128, NO, NE], BF16)
        slot = sb("e_slot", [128, NO, NE])
        cum = sb("e_cum", [128, NO, NE])
        tot = sb("e_tot", [128, NO, NE])
        AHL = sb("e_AHL", [128, NO, NE, 2], BF16)
        ares = sb("e_ares", [128, NO, NE])
        slt = sb("e_slt", [128, 128], BF16)
        onesb = sb("e_onesb", [128, 128], BF16)
        identb = sb("e_idb", [128, 128], BF16)
        iota = sb("e_iota", [128, CAPL])
        onef = sb("e_onef", [128, 1])
        OH = sb("e_OH", [128, NO, CAPL], BF16)
        XT = sb("e_XT", [128, 16, CAPL], BF16)
        HT = sb("e_HT", [128, 16, CAPL], BF16)
        Wg = [sb("e_Wg%d" % i, [128, 16, 256], BF16) for i in range(2)]
        Wu = [sb("e_Wu%d" % i, [128, 16, 256], BF16) for i in range(2)]
        Wd = [sb("e_Wd%d" % i, [128, 16, 512], BF16) for i in range(2)]
        asb = sb("e_asb", [128, CAPL])
        gs = sb("e_gs", [128, 3, 2])
        Yg = sb("e_Yg", [128, 3, D], BF16)
        OHT = sb("e_OHT", [128, NO, 3, 128], BF16)
        pa = [st.enter_context(nc.psum_tensor("e_pa%d" % i, [128, 512], F32)) for i in range(2)]
        pu = [st.enter_context(nc.psum_tensor("e_pu%d" % i, [128, 512], F32)) for i in range(2)]
        py = [st.enter_context(nc.psum_tensor("e_py%d" % i, [128, 512], F32)) for i in range(2)]
        pg = st.enter_context(nc.psum_tensor("e_pg", [128, 3, 2], F32))
        pt = st.enter_context(nc.psum_tensor("e_pt", [128, 3, 128], BF16))
        P.dma("sp", oidx[:], t["ownidx"][:, :], w=["oidx"])
        P.dma("sp", th[:], t["TH"][:, :], r=["TH"], w=["th"])
        P.dma("act", slt[:], t["c_slt"][:, :], w=["slt"])
        P.dma("act", onesb[:], t["c_onesb"][:, :], w=["onesb"])
        P.dma("act", identb[:], t["ident_bf"][:, :], w=["identb"])
        P.dma("sp", iota[:], t["c_iota"][:, :], w=["iota"])
        P.add("pool", lambda e: e.memset(onef[:], 1.0), w=["onef"])
        hx2keys = [("HX2", ti) for ti in range(L // 128)]
        for i in range(NO):
            P.add("pool", lambda e, i=i: e.indirect_dma_start(out=HX[:, i, :], out_offset=None, in_=t["HX2"][:, :],
                                                             in_offset=bass.IndirectOffsetOnAxis(ap=oidx[:, i:i + 1], axis=0)),
                  r=["oidx"] + hx2keys, w=[("HX", i)], dma=True)
            P.add("pool", lambda e, i=i: e.indirect_dma_start(out=Ao[:, i, :], out_offset=None, in_=t["AFFT"][:, :],
                                                             in_offset=bass.IndirectOffsetOnAxis(ap=oidx[:, i:i + 1], axis=0)),
                  r=["oidx", "AFFT"], w=["Ao"], dma=True)
        fl = lambda a: a.rearrange("p n e -> p (n e)")
        P.add("dve", lambda e: e.tensor_tensor(sel[:], Ao[:], th[:].unsqueeze(1).broadcast_to([128, NO, NE]), ALU.is_ge),
              r=["Ao", "th"], w=["sel"])
        P.add("dve", lambda e: e.tensor_copy(selb[:], sel[:]), r=["sel"], w=["selb"])
        P.add("pe", lambda e: e.matmul(pa[0][:, 0:NO * NE], slt[:], fl(selb[:]), start=True, stop=True), r=["slt", "selb"], w=["pa0"])
        P.add("pe", lambda e: e.matmul(pu[0][:, 0:NO * NE], onesb[:], fl(selb[:]), start=True, stop=True), r=["onesb", "selb"], w=["pu0"])
        P.add("dve", lambda e: e.tensor_copy(fl(tot[:]), pu[0][:, 0:NO * NE]), r=["pu0"], w=["tot"])
        for e_ in range(NE):
            P.add("dve", lambda e, e_=e_: e.tensor_tensor_scan(cum[:, :, e_], onef[:, 0:1].broadcast_to([128, NO]), tot[:, :, e_], 0.0,
                                                               ALU.mult, ALU.add),
                  r=["tot", "onef"], w=["cum"])
        P.add("dve", lambda e: e.tensor_tensor(fl(slot[:]), fl(cum[:]), fl(tot[:]), ALU.subtract), r=["cum", "tot"], w=["slot"])
        P.add("dve", lambda e: e.tensor_tensor(fl(slot[:]), fl(slot[:]), pa[0][:, 0:NO * NE], ALU.add), r=["slot", "pa0"], w=["slot"])
        P.add("dve", lambda e: e.scalar_tensor_tensor(fl(slot[:]), fl(slot[:]), 1.0, fl(sel[:]), ALU.add, ALU.mult), r=["slot", "sel"], w=["slot"])
        P.add("dve", lambda e: e.tensor_scalar_add(fl(slot[:]), fl(slot[:]), -1.0), r=["slot"], w=["slot"])
        P.add("dve", lambda e: e.tensor_copy(AHL[:, :, :, 0], Ao[:]), r=["Ao"], w=["AHL0"])
        P.add("dve", lambda e: e.tensor_copy(ares[:], AHL[:, :, :, 0]), r=["AHL0"], w=["ares"])
        P.add("dve", lambda e: e.tensor_tensor(ares[:], Ao[:], ares[:], ALU.subtract), r=["Ao", "ares"], w=["ares"])
        P.add("dve", lambda e: e.tensor_copy(AHL[:, :, :, 1], ares[:]), r=["ares"], w=["AHL1"])
        hxk = [("HX", i) for i in range(NO)]
        nld = 0
        for e_ in range(NE):
            for i in range(NO):
                P.add("dve", lambda e, i=i, e_=e_: e.tensor_single_scalar(OH[:, i, :], iota[:], slot[:, i, e_:e_ + 1], ALU.is_equal),
                      r=["iota", "slot"], w=[("OH", i)])
            ohk = [("OH", i) for i in range(NO)]
            for kc in range(16):
                pp = pa[kc % 2]
                for i in range(NO):
                    P.add("pe", lambda e, pp=pp, i=i, kc=kc: e.matmul(pp[:, 0:CAPL], HX[:, i, kc * 128:(kc + 1) * 128], OH[:, i, :],
                                                                      start=(i == 0), stop=(i == NO - 1)),
                          r=[("HX", i), ("OH", i)], w=[("pa", kc % 2)])
                if kc % 2 == 0:
                    P.add("act", lambda e, pp=pp, kc=kc: e.copy(XT[:, kc, :], pp[:, 0:CAPL]), r=[("pa", kc % 2)], w=[("XT", kc)])
                else:
                    P.add("dve", lambda e, pp=pp, kc=kc: e.tensor_copy(XT[:, kc, :], pp[:, 0:CAPL]), r=[("pa", kc % 2)], w=[("XT", kc)])
            for sc in range(3):
                for i in range(NO):
                    P.add("pe", lambda e, sc=sc, i=i, e_=e_: e.matmul(pg[:, sc, :], OH[:, i, sc * 128:(sc + 1) * 128], AHL[:, i, e_, :],
                                                                      start=(i == 0), stop=(i == NO - 1)),
                          r=[("OH", i), "AHL0", "AHL1"], w=["pg"])
            P.add("dve", lambda e: e.tensor_copy(gs[:], pg[:]), r=["pg"], w=["gs"])
            P.add("dve", lambda e: e.tensor_tensor(gs[:, :, 0], gs[:, :, 0], gs[:, :, 1], ALU.add), r=["gs"], w=["gs"])
            for i in range(NO):
                for sc in range(3):
                    P.add("pe", lambda e, i=i, sc=sc: e.transpose(pt[:, sc, :], OH[:, i, sc * 128:(sc + 1) * 128], identb[:]),
                          r=[("OH", i), "identb"], w=["pt"])
                P.add("act", lambda e, i=i: e.copy(OHT[:, i, :, :], pt[:]), r=["pt"], w=[("OHT", i)])
                P.dma("sp", t["OHTALL"][i][:, e_ * 3:(e_ + 1) * 3, :], OHT[:, i, :, :], r=[("OHT", i)], w=[("OHTALL", i, e_)])
            xtk = [("XT", kc) for kc in range(16)]
            wgv = t["ewg"][e_].rearrange("(kc p) f -> p kc f", p=128)
            wuv = t["ewu"][e_].rearrange("(kc p) f -> p kc f", p=128)
            for fb in range(8):
                wb = nld % 2
                nld += 1
                P.dma("pool", Wg[wb][:], wgv[:, :, fb * 256:(fb + 1) * 256], w=[("Wg", wb)])
                P.dma("pool", Wu[wb][:], wuv[:, :, fb * 256:(fb + 1) * 256], w=[("Wu", wb)])
                for f2_ in range(2):
                    fc = fb * 2 + f2_
                    pb = fc % 2
                    for kc in range(16):
                        P.add("pe", lambda e, pb=pb, wb=wb, kc=kc, f2_=f2_: e.matmul(pa[pb][:, 0:CAPL], Wg[wb][:, kc, f2_ * 128:(f2_ + 1) * 128],
                                                                                     XT[:, kc, :], start=(kc == 0), stop=(kc == 15)),
                              r=[("Wg", wb), ("XT", kc)], w=[("pa", pb)])
                    for kc in range(16):
                        P.add("pe", lambda e, pb=pb, wb=wb, kc=kc, f2_=f2_: e.matmul(pu[pb][:, 0:CAPL], Wu[wb][:, kc, f2_ * 128:(f2_ + 1) * 128],
                                                                                     XT[:, kc, :], start=(kc == 0), stop=(kc == 15)),
                              r=[("Wu", wb), ("XT", kc)], w=[("pu", pb)])
                    P.add("act", lambda e, pb=pb: e.activation(out=asb[:], in_=pa[pb][:, 0:CAPL], func=AF.Silu), r=[("pa", pb)], w=["asb"])
                    P.add("dve", lambda e, pb=pb, fc=fc: e.tensor_tensor(HT[:, fc, :], asb[:], pu[pb][:, 0:CAPL], ALU.mult),
                          r=["asb", ("pu", pb)], w=[("HT", fc)])
            wdv = t["ewd"][e_].rearrange("(fc p) d -> p fc d", p=128)
            for db in range(4):
                wb = db % 2
                P.dma("pool", Wd[wb][:], wdv[:, :, db * 512:(db + 1) * 512], w=[("Wd", wb)])
                for sc in range(3):
                    pp = py[sc % 2]
                    for fc in range(16):
                        P.add("pe", lambda e, pp=pp, wb=wb, fc=fc, sc=sc: e.matmul(pp[:], HT[:, fc, sc * 128:(sc + 1) * 128], Wd[wb][:, fc, :],
                                                                                   start=(fc == 0), stop=(fc == 15)),
                              r=[("HT", fc), ("Wd", wb)], w=[("py", sc % 2)])
                    P.add("act", lambda e, pp=pp, sc=sc, db=db: e.activation(out=Yg[:, sc, db * 512:(db + 1) * 512], in_=pp[:], func=AF.Copy,
                                                                             scale=gs[:, sc, 0:1]),
                          r=[("py", sc % 2), "gs"], w=["Yg"])
            P.dma("sp", t["YGALL"][e_].rearrange("c s d -> s c d"), Yg[:], r=["Yg"], w=[("YGALL", e_)])
        P.flush()


def stage8(k):
    nc, P, t = k.nc, k.P, k.t
    NO = OWN // 128
    with ExitStack() as st:
        sb = lambda n, s, d=F32: st.enter_context(nc.sbuf_tensor(n, s, d))
        YGb = sb("f_YG", [128, NE * 3, 512], BF16)
        OT = [sb("f_OT%d" % i, [128, NE * 3, 128], BF16) for i in range(2)]
        ob = [sb("f_ob%d" % i, [128, 512]) for i in range(2)]
        ps = [st.enter_context(nc.psum_tensor("f_ps%d" % i, [128, 512], F32)) for i in range(2)]
        ygv = t["YGALL"].rearrange("e c s d -> s (e c) d")
        for db in range(4):
            for e_ in range(NE):
                P.dma(k.q(), YGb[:, e_ * 3:(e_ + 1) * 3, :], ygv[:, e_ * 3:(e_ + 1) * 3, db * 512:(db + 1) * 512],
                      r=[("YGALL", e_)], w=[("YGb", e_)])
            for i in range(NO):
                b = i % 2
                P.dma(k.q(), OT[b][:], t["OHTALL"][i], r=[("OHTALL", i, e_) for e_ in range(NE)], w=[("OT", b)])
                for j in range(NE * 3):
                    P.add("pe", lambda e, b=b, j=j: e.matmul(ps[b][:], OT[b][:, j, :], YGb[:, j, :], start=(j == 0), stop=(j == NE * 3 - 1)),
                          r=[("OT", b), ("YGb", j // 3)], w=[("ps", b)])
                P.add("act", lambda e, b=b: e.copy(ob[b][:], ps[b][:]), r=[("ps", b)], w=[("ob", b)])
                P.dma("sp", t["MOEO"][i * 128:(i + 1) * 128, db * 512:(db + 1) * 512], ob[b][:], r=[("ob", b)], w=[("MOEO", i, db)])
        P.flush()


def stage9(k):
    nc, P, t = k.nc, k.P, k.t
    NO = OWN // 128
    with ExitStack() as st:
        sb = lambda n, s, d=F32: st.enter_context(nc.sbuf_tensor(n, s, d))
        GN3 = sb("g_GN3", [128, D])
        oidx = sb("g_oidx", [128, NO], I32)
        epst = sb("g_eps", [128, 1])
        mo = [sb("g_mo%d" % i, [128, D]) for i in range(2)]
        x1 = [sb("g_x1%d" % i, [128, D]) for i in range(2)]
        junk = sb("g_junk", [128, D], BF16)
        ss = [sb("g_ss%d" % i, [128, 2]) for i in range(2)]
        P.dma("sp", GN3[:], t["MODS2"][3], r=["M2_3"], w=["GN3"])
        P.dma("sp", oidx[:], t["ownidx"][:, :], w=["oidx"])
        P.add("pool", lambda e: e.memset(epst[:], EPS), w=["epst"])
        x1keys = [("X1", ti) for ti in range(L // 128)]
        outs = []
        for i in range(NO):
            b = i % 2
            P.dma("sp", mo[b][:], t["MOEO"][i * 128:(i + 1) * 128, :], r=[("MOEO", i, db) for db in range(4)], w=[("mo", b)])
            P.add("pool", lambda e, b=b, i=i: e.indirect_dma_start(out=x1[b][:], out_offset=None, in_=t["X1"][:, :],
                                                                  in_offset=bass.IndirectOffsetOnAxis(ap=oidx[:, i:i + 1], axis=0)),
                  r=["oidx"] + x1keys, w=[("x1", b)], dma=True)
            P.add("act", lambda e, b=b: e.activation(out=junk[:], in_=mo[b][:], func=AF.Square, scale=float(D ** -0.5), accum_out=ss[b][:, 0:1]),
                  r=[("mo", b)], w=["junk", ("ss", b)])
            P.add("act", lambda e, b=b: e.activation(out=ss[b][:, 1:2], in_=ss[b][:, 0:1], func=AF.Sqrt, bias=epst[:, 0:1], scale=1.0),
                  r=[("ss", b), "epst"], w=[("ss1", b)])
            P.add("dve", lambda e, b=b: e.reciprocal(ss[b][:, 1:2], ss[b][:, 1:2]), r=[("ss1", b)], w=[("ss1", b)])
            P.add("dve", lambda e, b=b: e.scalar_tensor_tensor(mo[b][:], mo[b][:], ss[b][:, 1:2], GN3[:], ALU.mult, ALU.mult),
                  r=[("mo", b), ("ss1", b), "GN3"], w=[("mo", b)])
            P.add("pool", lambda e, b=b: e.tensor_tensor(mo[b][:], mo[b][:], x1[b][:], ALU.add), r=[("mo", b), ("x1", b)], w=[("mo", b)])
            outs.append(P.dma("sp", t["out"][i * 128:(i + 1) * 128, :], mo[b][:], r=[("mo", b)], w=[("out", i)]))
        P.flush(final_wait=outs)


def _host_s5(inp):
    are, aim, ldt = inp["s5_a_re"][0], inp["s5_a_im"][0], inp["s5_log_dt"][0]
    bre, bim = inp["s5_b_re"][0], inp["s5_b_im"][0]
    cre, cim = inp["s5_c_re"][0], inp["s5_c_im"][0]

    def gp(a):
        sh = a.shape[:-2]
        a = a.reshape(*sh, 8, 4, 2, 64)
        a = np.moveaxis(a, [-4, -2, -1, -3], [0, 1, 2, -1])
        return a.reshape(8, 128, *sh, 4)

    s5p = np.concatenate([gp(are).reshape(8, 128, 8), gp(aim).reshape(8, 128, 8),
                          gp(np.broadcast_to(ldt[..., None], (2, 64, 64))).reshape(8, 128, 8)], -1)

    def bc(r, i, cfirst):
        out = []
        for a in (r, i):
            if cfirst:
                a = a.transpose(0, 2, 1)
            a = a.reshape(8, 4, 2, 64, 16).transpose(0, 2, 3, 1, 4).reshape(8, 128, 4, 16)
            out.append(a)
        return np.ascontiguousarray(np.stack(out, 2))

    d = {"s5p": np.ascontiguousarray(s5p.astype(np.float32)), "s5b": bc(bre, bim, False),
         "s5c": bc(cre, cim, True), "s5d": np.ascontiguousarray(inp["s5_d"][0].reshape(8, 128, 1))}
    d["c_jidx"] = np.broadcast_to(np.arange(17, dtype=np.float32), (128, 17)).copy()
    pm = np.zeros((128, 2), np.float32)
    pm[:64, 0] = 1
    pm[64:, 1] = 1
    d["c_parmask"] = pm
    d["c_blockmask"] = np.kron(np.eye(8, dtype=np.float32), np.ones((16, 16), np.float32))
    m = np.arange(NCH, dtype=np.float32)
    d["c_midx"] = np.broadcast_to(np.stack([m, m[::-1]]), (128, 2, NCH)).copy()
    d["c_rowmask"] = np.kron(np.eye(4, dtype=np.float32), np.ones((32, 1), np.float32))
    return d


def _core_inputs(inp, core, shared):
    b, j = divmod(core, 4)
    cond = np.stack([inp["c"][b], inp["c_ctx"]], -1).reshape(16, 128, 2).transpose(1, 0, 2)
    d = {"x": inp["x"][b], "ctx": inp["ctx"][b], "cond": np.ascontiguousarray(cond),
         "xown": inp["x"][b][j * OWN:(j + 1) * OWN]}
    d.update(shared)
    return d


def _host_rest(inp):
    d = {}
    d["convw"] = np.ascontiguousarray(inp["ml_conv_w"][0].T.reshape(16, 128, 5))
    d["convb"] = np.ascontiguousarray(inp["ml_conv_b"][0].reshape(16, 128, 1))
    d["gateb"] = np.broadcast_to(inp["ml_gate_b"][0].reshape(1, 16), (128, 16)).copy()
    d["mlng"] = np.broadcast_to(inp["ml_norm_g"][0][None], (128, D_ML)).copy()
    tri = np.triu(np.ones((128, 128), np.float32))
    d["c_tri"] = tri
    d["c_trit"] = np.ascontiguousarray(tri.T)
    d["c_ones"] = np.ones((128, 128), np.float32)
    d["gluw"] = inp["s5_glu_w"][0]
    d["w_out"] = inp["w_out"][0]
    d["glub"] = np.ascontiguousarray(inp["s5_glu_b"][0].reshape(8, 128).T)
    d["rw"] = np.ascontiguousarray(inp["router_w"][0].reshape(16, 128, 16).transpose(1, 0, 2))
    d["ewg"] = inp["exp_w_gate"][0]
    d["ewu"] = inp["exp_w_up"][0]
    d["ewd"] = inp["exp_w_down"][0]
    d["c_slt"] = np.triu(np.ones((128, 128), np.float32), 1).astype(ml_dtypes.bfloat16)
    d["c_onesb"] = np.ones((128, 128), ml_dtypes.bfloat16)
    d["c_iota"] = np.broadcast_to(np.arange(CAPL, dtype=np.float32), (128, CAPL)).copy()
    return d


def build_full():
    k = K()
    declare_io(k)
    declare_s5(k)
    declare_ml(k)
    declare_s5post(k)
    declare_moe(k)
    for f in (stage0, stage1, stage2, stage3a, stage3b, stage4a, stage4b, stage4c, stage5, stage6, stage7, stage8, stage9):
        f(k)
    return k


def host_inputs(inp, cores):
    shared = {"ada_w": inp["ada_w"][0], "ada_b": inp["ada_b"][0][None], "norm_g": inp["norm_g"][0],
              "w_in": inp["w_in"][0], "ident_bf": np.eye(128, dtype=ml_dtypes.bfloat16),
              "ident_f": np.eye(128, dtype=np.float32)}
    shared.update(_host_s5(inp))
    shared.update(_host_rest(inp))
    maps = []
    for core in cores:
        b, j = divmod(core, 4)
        cond = np.stack([inp["c"][b], inp["c_ctx"]], -1).reshape(16, 128, 2).transpose(1, 0, 2)
        d = {"x": inp["x"][b], "ctx": inp["ctx"][b], "cond": np.ascontiguousarray(cond)}
        d["ownidx"] = (j * OWN + np.arange(OWN, dtype=np.int32).reshape(OWN // 128, 128).T).astype(np.int32).copy()
        d.update(shared)
        maps.append(d)
    return maps


def kernel(**inputs):
    inp = {k_: np.asarray(v) for k_, v in inputs.items()}
    k = build_full()
    names = set(k.t.keys())
    in_maps = [{n: np.ascontiguousarray(v) for n, v in m.items() if n in names} for m in host_inputs(inp, range(8))]
    res = run_bass_kernel_spmd(k.nc, in_maps, core_ids=list(range(8)))
    out = np.zeros((2, L, D), np.float32)
    for core in range(8):
        b, j = divmod(core, 4)
        out[b, j * OWN:(j + 1) * OWN] = np.asarray(res.results[core]["out"])
    return out
```

```python
from contextlib import ExitStack
import math
import numpy as np
import ml_dtypes
import concourse.bass as bass
import concourse.mybir as mybir
from concourse.bass_utils import run_bass_kernel_spmd

F32 = mybir.dt.float32
BF16 = mybir.dt.bfloat16
I32 = mybir.dt.int32
U32 = mybir.dt.uint32
ALU = mybir.AluOpType
AF = mybir.ActivationFunctionType
AX = mybir.AxisListType

ENGS = ("sp", "act", "dve", "pool", "pe")

D = 2048
L = 8192
CTX = 256
NT = L + CTX
NTILE = NT // 128
GW = 64
D_S5 = 1024
D_ML = 1024
NH = 4
DH = 256
D_IN = D_S5 + 4 * D_ML + 16
NE = 16
CAPE = 1024
FF = 2048
EPS = 1e-6
OWN = 2048
CAPL = 384


class _Op:
    __slots__ = ("eng", "fn", "deps", "dma", "needed", "sem", "val", "prev_val", "inc")

    def __init__(self, eng, fn, deps, dma, inc):
        self.eng = eng
        self.fn = fn
        self.deps = deps
        self.dma = dma
        self.needed = False
        self.sem = None
        self.val = 0
        self.prev_val = 0
        self.inc = inc


class Prog:
    def __init__(self, nc, n_dma_sems=8):
        self.nc = nc
        self.ops = []
        self.pending = []
        self.last_w = {}
        self.readers = {}
        self.st = ExitStack()
        self.esem = {e: self.st.enter_context(nc.semaphore("es_" + e)) for e in ENGS}
        self.ecnt = {e: 0 for e in ENGS}
        self.dsems = [self.st.enter_context(nc.semaphore("ds%d" % i)) for i in range(n_dma_sems)]
        self.dval = [0] * n_dma_sems
        self.drr = 0
        self.waited = {e: {} for e in ENGS}
        self.n_ins = 0

    def add(self, eng, fn, r=(), w=(), dma=False, inc=16):
        deps = set()
        for x in r:
            if x in self.last_w:
                deps.add(self.last_w[x])
        for x in w:
            if x in self.last_w:
                deps.add(self.last_w[x])
            deps.update(self.readers.get(x, {}).values())
        if eng == "pe" and not dma:
            deps = {d for d in deps if self.ops[d].dma or self.ops[d].eng != "pe"}
        op = _Op(eng, fn, deps, dma, inc)
        oid = len(self.ops)
        self.ops.append(op)
        self.pending.append(oid)
        rk = ("dma", oid) if dma else eng
        for x in r:
            self.readers.setdefault(x, {})[rk] = oid
        for x in w:
            self.last_w[x] = oid
            self.readers[x] = {}
        return oid

    def dma(self, eng, out, in_, r=(), w=(), **kw):
        return self.add(eng, lambda e: e.dma_start(out=out, in_=in_, **kw), r=r, w=w, dma=True)

    def flush(self, final_wait=()):
        ops = self.ops
        pend = self.pending
        self.pending = []
        live = set(self.last_w.values())
        for lst in self.readers.values():
            live.update(lst.values())
        for oid in pend:
            for d in ops[oid].deps:
                ops[d].needed = True
        for oid in pend:
            op = ops[oid]
            if op.dma or oid in live:
                op.needed = True
        for oid in pend:
            op = ops[oid]
            if op.dma:
                k = self.drr
                self.drr = (self.drr + 1) % len(self.dsems)
                op.sem = k
                op.prev_val = self.dval[k]
                self.dval[k] += op.inc
                op.val = self.dval[k]
            elif op.needed:
                self.ecnt[op.eng] += 1
                op.val = self.ecnt[op.eng]
        per = {e: [] for e in ENGS}
        for oid in pend:
            per[ops[oid].eng].append(oid)
        fw = list(final_wait)

        def run(ename, e):
            wd = self.waited[ename]

            def wait(key, sem, val):
                if wd.get(key, 0) >= val:
                    return
                wd[key] = val
                e.wait_ge(sem, val)
                self.n_ins += 1

            def wait_op(p):
                if p.dma:
                    wait(("d", p.sem), self.dsems[p.sem], p.val)
                else:
                    assert p.val > 0, "dependency on unsignalled op"
                    wait(("e", p.eng), self.esem[p.eng], p.val)

            for oid in per[ename]:
                op = ops[oid]
                for d in sorted(op.deps):
                    wait_op(ops[d])
                self.n_ins += 1
                if op.dma:
                    if op.prev_val > 0:
                        wait(("d", op.sem), self.dsems[op.sem], op.prev_val)
                    ins = op.fn(e)
                    ins.then_inc(self.dsems[op.sem], op.inc)
                else:
                    ins = op.fn(e)
                    if op.needed:
                        ins.then_inc(self.esem[ename], 1)
            if ename == "sp":
                for d in fw:
                    wait_op(ops[d])

        with self.nc.Block() as block:
            @block.sync
            def _(e):
                run("sp", e)

            @block.scalar
            def _(e):
                run("act", e)

            @block.vector
            def _(e):
                run("dve", e)

            @block.gpsimd
            def _(e):
                run("pool", e)

            @block.tensor
            def _(e):
                run("pe", e)

    def close(self):
        self.st.close()


class K:
    def __init__(self, dbg=()):
        self.nc = bass.Bass("TRN2", target_bir_lowering=False)
        self.P = Prog(self.nc)
        self.dbg = set(dbg)
        self.t = {}
        self._rr = 0

    def din(self, name, shape, dt=F32):
        a = self.nc.dram_tensor(name, list(shape), dt, kind="ExternalInput").ap()
        self.t[name] = a
        return a

    def dout(self, name, shape, dt=F32):
        a = self.nc.dram_tensor(name, list(shape), dt, kind="ExternalOutput").ap()
        self.t[name] = a
        return a

    def dscr(self, name, shape, dt=F32):
        kind = "ExternalOutput" if name in self.dbg else "Internal"
        a = self.nc.dram_tensor(name, list(shape), dt, kind=kind).ap()
        self.t[name] = a
        return a

    def q(self):
        return "sp"

    def q2(self):
        self._rr ^= 1
        return "sp" if self._rr else "act"


def declare_io(k):
    k.din("x", [L, D])
    k.din("ctx", [CTX, D])
    k.din("cond", [128, 16, 2])
    k.din("ada_w", [D, 6 * D])
    k.din("ada_b", [1, 6 * D])
    k.din("norm_g", [4, D])
    k.din("w_in", [D, D_IN])
    k.din("ident_bf", [128, 128], BF16)
    k.din("ident_f", [128, 128])
    k.dout("out", [OWN, D])
    k.dscr("MODS", [6, 128, D])
    k.dscr("MODS2", [4, 128, D])
    k.dscr("HXT", [2, 16, 128, NT], BF16)
    k.dscr("U5", [8, 128, NT], BF16)
    k.dscr("QKPRE", [16, 128, NT], BF16)
    k.dscr("V", [NT, D_ML], BF16)
    k.dscr("SO", [NT, D_ML], BF16)
    k.dscr("GATES", [128, NTILE, 16])


def stage0(k):
    nc, P, t = k.nc, k.P, k.t
    with ExitStack() as st:
        sb = lambda n, s, d=F32: st.enter_context(nc.sbuf_tensor(n, s, d))
        cond = sb("s0_cond", [128, 16, 2])
        sil = sb("s0_sil", [128, 16, 2])
        lx = sb("s0_lx", [128, 16, 128])
        lc = sb("s0_lc", [128, 16, 128])
        wt = [sb("s0_w%d" % i, [128, 16, 512]) for i in range(2)]
        bt = [sb("s0_b%d" % i, [128, 512]) for i in range(2)]
        mx = sb("s0_mx", [128, 6, D])
        mc = sb("s0_mc", [128, 2, D])
        ng = sb("s0_ng", [128, 4, D])
        o1 = sb("s0_o1", [128, D])
        ps = [st.enter_context(nc.psum_tensor("s0_ps%d" % i, [128, 512], F32)) for i in range(2)]

        P.dma("sp", cond[:], t["cond"][:, :, :], w=["cond"])
        P.dma("act", ng[:], t["norm_g"].rearrange("(o g) d -> o g d", o=1).broadcast_to([128, 4, D]), w=["ng"])
        P.add("act", lambda e: e.activation(out=sil[:], in_=cond[:], func=AF.Silu), r=["cond"], w=["sil"])
        P.add("dve", lambda e: e.tensor_copy(lx[:], sil[:, :, 0:1].broadcast_to([128, 16, 128])), r=["sil"], w=["lx"])
        P.add("dve", lambda e: e.tensor_copy(lc[:], sil[:, :, 1:2].broadcast_to([128, 16, 128])), r=["sil"], w=["lc"])
        adaw = t["ada_w"].rearrange("(kc p) n -> p kc n", p=128)
        for nb in range(24):
            b = nb % 2
            P.dma("sp" if b else "act", wt[b][:], adaw[:, :, nb * 512:(nb + 1) * 512], w=[("w", b)])
            P.dma("pool", bt[b][:], t["ada_b"][:, nb * 512:(nb + 1) * 512].broadcast_to([128, 512]), w=[("b", b)])
            for which in range(2 if nb < 8 else 1):
                lhs = lx if which == 0 else lc
                pst = ps[which]
                for kc in range(16):
                    P.add("pe", lambda e, lhs=lhs, kc=kc, b=b, pst=pst: e.matmul(
                        pst[:], lhs[:, kc, :], wt[b][:, kc, :], start=(kc == 0), stop=(kc == 15)),
                        r=["lx", "lc", ("w", b)], w=[("ps", which)])
                dst = mx if which == 0 else mc
                ch, off = divmod(nb * 512, D)
                P.add("dve", lambda e, dst=dst, ch=ch, off=off, pst=pst, b=b: e.tensor_tensor(
                    dst[:, ch, off:off + 512], pst[:], bt[b][:], ALU.add),
                    r=[("ps", which), ("b", b)], w=[("m", which)])
        mods = t["MODS"]
        mods2 = t["MODS2"]

        def emit(dst_ap, fn, key):
            P.add("dve", fn, r=[("m", 0), ("m", 1), "ng"], w=["o1"])
            P.dma("sp", dst_ap, o1[:], r=["o1"], w=[key])

        emit(mods[0], lambda e: e.scalar_tensor_tensor(o1[:], mx[:, 1, :], 1.0, ng[:, 0, :], ALU.add, ALU.mult), "MODS0")
        emit(mods[1], lambda e: e.tensor_copy(o1[:], mx[:, 0, :]), "MODS1")
        emit(mods[2], lambda e: e.scalar_tensor_tensor(o1[:], mc[:, 1, :], 1.0, ng[:, 0, :], ALU.add, ALU.mult), "MODS2")
        emit(mods[3], lambda e: e.tensor_copy(o1[:], mc[:, 0, :]), "MODS3")
        emit(mods2[0], lambda e: e.tensor_tensor(o1[:], mx[:, 2, :], ng[:, 1, :], ALU.mult), "M2_0")
        emit(mods2[1], lambda e: e.scalar_tensor_tensor(o1[:], mx[:, 4, :], 1.0, ng[:, 2, :], ALU.add, ALU.mult), "M2_1")
        emit(mods2[2], lambda e: e.tensor_copy(o1[:], mx[:, 3, :]), "M2_2")
        emit(mods2[3], lambda e: e.tensor_tensor(o1[:], mx[:, 5, :], ng[:, 3, :], ALU.mult), "M2_3")
        P.flush()


def tok_src(k, order, ti):
    t = k.t
    if ti < 2:
        return t["ctx"][ti * 128:(ti + 1) * 128, :]
    xi = ti - 2
    if order == 0:
        return t["x"][xi * 128:(xi + 1) * 128, :]
    return t["x"].rearrange("(r w) d -> w r d", w=GW)[xi]


def stage1(k, orders=(0, 1)):
    nc, P, t = k.nc, k.P, k.t
    with ExitStack() as st:
        sb = lambda n, s, d=F32: st.enter_context(nc.sbuf_tensor(n, s, d))
        A = [sb("s1_A%d" % i, [128, D]) for i in range(4)]
        ident = sb("s1_id", [128, 128], BF16)
        xt = [sb("s1_x%d" % i, [128, D]) for i in range(2)]
        junk = sb("s1_junk", [128, D])
        t1 = [sb("s1_t%d" % i, [128, D]) for i in range(2)]
        hx = [sb("s1_hx%d" % i, [128, D], BF16) for i in range(2)]
        ss = [sb("s1_ss%d" % i, [128, 2]) for i in range(2)]
        blk = [sb("s1_blk%d" % i, [128, 16, 512], BF16) for i in range(2)]
        ps = [st.enter_context(nc.psum_tensor("s1_ps%d" % i, [128, 16, 128], BF16)) for i in range(2)]
        epst = sb("s1_eps", [128, 1])
        P.add("pool", lambda e: e.memset(epst[:], EPS), w=["epst"])
        for i in range(4):
            P.dma("sp", A[i][:], t["MODS"][i], r=["MODS%d" % i], w=[("A", i)])
        P.dma("act", ident[:], t["ident_bf"][:, :], w=["ident"])
        for order in orders:
            groups = [(0, 2)] + [(2 + 4 * g, 4) for g in range(16)]
            for gi, (t0, n) in enumerate(groups):
                bb = gi % 2
                for j in range(n):
                    ti = t0 + j
                    b = ti % 2
                    a_i, b_i = (2, 3) if ti < 2 else (0, 1)
                    P.dma(k.q2(), xt[b][:], tok_src(k, order, ti), w=[("xt", b)])
                    P.add("act", lambda e, b=b: e.activation(out=junk[:], in_=xt[b][:], func=AF.Square,
                                                             scale=float(D ** -0.5), accum_out=ss[b][:, 0:1]),
                          r=[("xt", b)], w=["junk", ("ss", b)])
                    P.add("act", lambda e, b=b: e.activation(out=ss[b][:, 1:2], in_=ss[b][:, 0:1], func=AF.Sqrt,
                                                             bias=epst[:, 0:1], scale=1.0),
                          r=[("ss", b), "epst"], w=[("ss1", b)])
                    P.add("dve", lambda e, b=b: e.reciprocal(ss[b][:, 1:2], ss[b][:, 1:2]),
                          r=[("ss1", b)], w=[("ss1", b)])
                    P.add("dve", lambda e, b=b, a_i=a_i: e.scalar_tensor_tensor(
                        t1[b][:], xt[b][:], ss[b][:, 1:2], A[a_i][:], ALU.mult, ALU.mult),
                        r=[("xt", b), ("ss1", b), ("A", a_i)], w=[("t1", b)])
                    P.add("pool", lambda e, b=b, b_i=b_i: e.tensor_tensor(hx[b][:], t1[b][:], A[b_i][:], ALU.add),
                          r=[("t1", b), ("A", b_i)], w=[("hx", b)])
                    for kc in range(16):
                        P.add("pe", lambda e, b=b, kc=kc: e.transpose(ps[b][:, kc, :], hx[b][:, kc * 128:(kc + 1) * 128],
                                                                      ident[:]),
                              r=[("hx", b), "ident"], w=[("ps", b)])
                    P.add("act", lambda e, b=b, bb=bb, j=j: e.copy(blk[bb][:, :, j * 128:(j + 1) * 128], ps[b][:]),
                          r=[("ps", b)], w=[("blk", bb)])
                dst = t["HXT"][order].rearrange("kc p t -> p kc t")[:, :, t0 * 128:(t0 + n) * 128]
                P.dma(k.q2(), dst, blk[bb][:, :, 0:n * 128], r=[("blk", bb)], w=[("HXT", order, gi)])
        P.flush()


TGROUPS = [(0, 256)] + [(256 + 512 * g, 512) for g in range(16)]


def stage2(k):
    nc, P, t = k.nc, k.P, k.t
    win = t["w_in"].rearrange("(kc p) n -> p kc n", p=128)
    for pname, order, c0, ncb, dst in (("A", 0, 0, 8, "U5"), ("B", 1, D_S5, 16, "QKPRE")):
        with ExitStack() as st:
            sb = lambda n, s, d=F32: st.enter_context(nc.sbuf_tensor(n, s, d))
            W = sb("s2%s_w" % pname, [128, 16, ncb * 128], BF16)
            hb = [sb("s2%s_h%d" % (pname, i), [128, 16, 512], BF16) for i in range(2)]
            ob = [sb("s2%s_o%d" % (pname, i), [128, ncb, 512], BF16) for i in range(2)]
            ps = [st.enter_context(nc.psum_tensor("s2%s_ps%d" % (pname, i), [128, 512], F32)) for i in range(4)]
            for kc in range(16):
                for c1 in range(0, ncb * 128, 1024):
                    P.dma("pool", W[:, kc, c1:c1 + 1024], win[:, kc, c0 + c1:c0 + c1 + 1024], w=[("W", kc, c1 // 1024)])
            hsrc = t["HXT"][order].rearrange("kc p t -> p kc t")
            dview = t[dst].rearrange("cb p t -> p cb t")
            for gi, (t0, gt) in enumerate(TGROUPS):
                b = gi % 2
                P.dma(k.q2(), hb[b][:, :, 0:gt], hsrc[:, :, t0:t0 + gt], r=[("HXT", order, gi)], w=[("hb", b)])
                for cb in range(ncb):
                    pi = cb % 4
                    for kc in range(16):
                        P.add("pe", lambda e, pi=pi, kc=kc, cb=cb, b=b, gt=gt: e.matmul(
                            ps[pi][:, 0:gt], W[:, kc, cb * 128:(cb + 1) * 128], hb[b][:, kc, 0:gt],
                            start=(kc == 0), stop=(kc == 15)),
                            r=[("W", kc, cb // 8), ("hb", b)], w=[("ps", pi)])
                    eng = "act" if cb % 2 == 0 else "dve"
                    if eng == "act":
                        P.add("act", lambda e, pi=pi, cb=cb, b=b, gt=gt: e.copy(ob[b][:, cb, 0:gt], ps[pi][:, 0:gt]),
                              r=[("ps", pi)], w=[("ob", b)])
                    else:
                        P.add("dve", lambda e, pi=pi, cb=cb, b=b, gt=gt: e.tensor_copy(ob[b][:, cb, 0:gt], ps[pi][:, 0:gt]),
                              r=[("ps", pi)], w=[("ob", b)])
                P.dma(k.q2(), dview[:, :, t0:t0 + gt], ob[b][:, :, 0:gt], r=[("ob", b)], w=[(dst, gi)])
            P.flush()
    with ExitStack() as st:
        sb = lambda n, s, d=F32: st.enter_context(nc.sbuf_tensor(n, s, d))
        W = sb("s2C_w", [128, 16, 2 * D_ML + 16], BF16)
        hb = [sb("s2C_h%d" % i, [128, 16, 512], BF16) for i in range(2)]
        vo = [sb("s2C_vo%d" % i, [128, 2 * D_ML], BF16) for i in range(2)]
        gt_sb = sb("s2C_g", [128, NTILE, 16])
        ps = [st.enter_context(nc.psum_tensor("s2C_ps%d" % i, [128, 512], F32)) for i in range(4)]
        psg = st.enter_context(nc.psum_tensor("s2C_psg", [128, 16], F32))
        c0 = D_S5 + 2 * D_ML
        for kc in range(16):
            for c1, cn in ((0, 1024), (1024, 1024), (2048, 16)):
                P.dma("pool", W[:, kc, c1:c1 + cn], win[:, kc, c0 + c1:c0 + c1 + cn], w=[("W", kc, c1 // 1024)])
        hsrc = t["HXT"][1].rearrange("kc p t -> p kc t")
        for gi, (t0, gtk) in enumerate(TGROUPS):
            b = gi % 2
            P.dma(k.q2(), hb[b][:, :, 0:gtk], hsrc[:, :, t0:t0 + gtk], r=[("HXT", 1, gi)], w=[("hb", b)])
            for j in range(gtk // 128):
                ti = t0 // 128 + j
                vb = ti % 2
                for nb in range(4):
                    for kc in range(16):
                        P.add("pe", lambda e, nb=nb, kc=kc, b=b, j=j: e.matmul(
                            ps[nb][:], hb[b][:, kc, j * 128:(j + 1) * 128], W[:, kc, nb * 512:(nb + 1) * 512],
                            start=(kc == 0), stop=(kc == 15)),
                            r=[("W", kc, nb // 2), ("hb", b)], w=[("ps", nb)])
                    if nb < 2:
                        P.add("dve", lambda e, nb=nb, vb=vb: e.tensor_copy(vo[vb][:, nb * 512:(nb + 1) * 512], ps[nb][:]),
                              r=[("ps", nb)], w=[("vo", vb, nb)])
                    else:
                        P.add("act", lambda e, nb=nb, vb=vb: e.activation(out=vo[vb][:, nb * 512:(nb + 1) * 512],
                                                                          in_=ps[nb][:], func=AF.Sigmoid),
                              r=[("ps", nb)], w=[("vo", vb, nb)])
                for kc in range(16):
                    P.add("pe", lambda e, kc=kc, b=b, j=j: e.matmul(
                        psg[:], hb[b][:, kc, j * 128:(j + 1) * 128], W[:, kc, 2 * D_ML:2 * D_ML + 16],
                        start=(kc == 0), stop=(kc == 15)),
                        r=[("W", kc, 2), ("hb", b)], w=["psg"])
                P.add("dve", lambda e, ti=ti: e.tensor_copy(gt_sb[:, ti, :], psg[:]), r=["psg"], w=["gt_sb"])
                P.dma(k.q2(), t["V"][ti * 128:(ti + 1) * 128, :], vo[vb][:, 0:D_ML],
                      r=[("vo", vb, 0), ("vo", vb, 1)], w=[("V", ti)])
                P.dma(k.q2(), t["SO"][ti * 128:(ti + 1) * 128, :], vo[vb][:, D_ML:2 * D_ML],
                      r=[("vo", vb, 2), ("vo", vb, 3)], w=[("SO", ti)])
        P.dma("sp", t["GATES"][:, :, :], gt_sb[:], r=["gt_sb"], w=["GATES"])
        P.flush()


TS5 = 16
NCH = NT // TS5
TWO_PI = 2.0 * math.pi


def emit_sincos(P, x, osin, ocos, tf, ti, tm, cpi0, rk, wk_sin, wk_cos, tkey):
    PI_ = math.pi
    for which, out, off, wk in ((0, osin, 0.0, wk_sin), (1, ocos, 0.5 * PI_, wk_cos)):
        kt = (tkey, "tf")
        P.add("dve", lambda e, off=off: e.tensor_scalar(tf, x, off, 1.0 / TWO_PI, ALU.add, ALU.mult), r=rk, w=[kt])
        P.add("dve", lambda e: e.tensor_copy(ti, tf), r=[kt], w=[(tkey, "ti")])
        P.add("dve", lambda e: e.tensor_copy(tf, ti), r=[(tkey, "ti")], w=[kt])
        P.add("dve", lambda e, off=off: e.tensor_scalar_add(tm, x, off), r=rk, w=[(tkey, "tm")])
        P.add("dve", lambda e: e.scalar_tensor_tensor(tf, tf, -TWO_PI, tm, ALU.mult, ALU.add), r=[kt, (tkey, "tm")], w=[kt])
        P.add("dve", lambda e: e.tensor_single_scalar(tm, tf, PI_, ALU.is_gt), r=[kt], w=[(tkey, "tm")])
        P.add("dve", lambda e: e.scalar_tensor_tensor(tf, tm, -TWO_PI, tf, ALU.mult, ALU.add), r=[kt, (tkey, "tm")], w=[kt])
        P.add("dve", lambda e: e.tensor_single_scalar(tm, tf, -PI_, ALU.is_lt), r=[kt], w=[(tkey, "tm")])
        P.add("dve", lambda e: e.scalar_tensor_tensor(tf, tm, TWO_PI, tf, ALU.mult, ALU.add), r=[kt, (tkey, "tm")], w=[kt])
        P.add("dve", lambda e: e.tensor_scalar(tf, tf, -3.14159, 3.14159, ALU.max, ALU.min), r=[kt], w=[kt])
        P.add("act", lambda e, out=out: e.activation(out=out, in_=tf, func=AF.Sin, bias=cpi0, scale=1.0),
              r=[kt, "cpi"], w=wk)


def declare_s5(k):
    k.din("s5p", [8, 128, 24])
    k.din("s5b", [8, 128, 2, 4, 16])
    k.din("s5c", [8, 128, 2, 4, 16])
    k.din("s5d", [8, 128, 1])
    k.din("c_jidx", [128, 17])
    k.din("c_parmask", [128, 2])
    k.din("c_blockmask", [128, 128])
    k.din("c_midx", [128, 2, NCH])
    k.din("c_rowmask", [128, 4])
    k.dscr("S5T_LAG", [8, 128, 2, 16, 128], BF16)
    k.dscr("S5T_BDT", [8, 128, 2, 16, 2, 128], BF16)
    k.dscr("S5T_MG", [8, 128, 2, 2, 16, 128], BF16)
    k.dscr("S5T_RT", [8, 128, 16])
    k.dscr("S5T_DIAG", [8, 128, 128], BF16)
    k.dscr("YG", [8, 128, L], BF16)


def stage3a(k):
    nc, P, t = k.nc, k.P, k.t
    with ExitStack() as st:
        sb = lambda n, s, d=F32: st.enter_context(nc.sbuf_tensor(n, s, d))
        jidx = sb("a_jidx", [128, 17])
        parm = sb("a_parm", [128, 2])
        bmask = sb("a_bmask", [128, 128])
        identf = sb("a_idf", [128, 128])
        cpi = sb("a_cpi", [128, 2])
        prm = sb("a_prm", [128, 24])
        bb = sb("a_bb", [128, 2, 4, 16])
        cc = sb("a_cc", [128, 2, 4, 16])
        dd = sb("a_dd", [128, 1])
        sm = sb("a_sm", [128, 12, 8])
        JL = sb("a_JL", [128, 17, 8])
        JA = sb("a_JA", [128, 17, 8])
        T1 = sb("a_T1", [128, 17, 8])
        T2 = sb("a_T2", [128, 17, 8])
        TI = sb("a_TI", [128, 17, 8], I32)
        PR = sb("a_PR", [128, 17, 8])
        PI = sb("a_PI", [128, 17, 8])
        BB = sb("a_BB", [128, 2, 8, 16])
        tb = sb("a_tb", [128, 2, 8, 16])
        Wr = sb("a_Wr", [128, 16, 8, 16])
        Wi = sb("a_Wi", [128, 16, 8, 16])
        Wt = sb("a_Wt", [128, 16, 8, 16])
        MWr = sb("a_MWr", [128, 128, 2, 16])
        MWi = sb("a_MWi", [128, 128, 2, 16])
        MC = sb("a_MC", [128, 2, 4, 2, 16])
        Gt = sb("a_Gt", [128, 16, 4, 16])
        Gt2 = sb("a_Gt2", [128, 16, 4, 16])
        MG = sb("a_MG", [128, 2, 2, 16, 128], BF16)
        LAG = sb("a_LAG", [128, 2, 16, 128], BF16)
        BDT = sb("a_BDT", [128, 2, 16, 2, 128], BF16)
        DG = sb("a_DG", [128, 128], BF16)
        RT = sb("a_RT", [128, 16])
        ps = [st.enter_context(nc.psum_tensor("a_ps%d" % i, [128, 128], F32)) for i in range(4)]

        P.dma("sp", jidx[:], t["c_jidx"][:, :], w=["jidx"])
        P.dma("sp", parm[:], t["c_parmask"][:, :], w=["parm"])
        P.dma("sp", bmask[:], t["c_blockmask"][:, :], w=["bmask"])
        P.dma("sp", identf[:], t["ident_f"][:, :], w=["identf"])
        P.add("pool", lambda e: e.memset(cpi[:], 0.0), w=["cpi"])

        def V(fn, r, w, eng="dve"):
            P.add(eng, fn, r=r, w=w)

        for blk in range(8):
            P.dma("sp", prm[:], t["s5p"][blk], w=["prm"])
            P.dma("act", bb[:], t["s5b"][blk], w=["bb"])
            P.dma("sp", cc[:], t["s5c"][blk], w=["cc"])
            P.dma("act", dd[:], t["s5d"][blk], w=["dd"])
            are, aim, ldt = prm[:, 0:8], prm[:, 8:16], prm[:, 16:24]
            dt_, lr, lrdt, ang = sm[:, 0, :], sm[:, 1, :], sm[:, 2, :], sm[:, 3, :]
            nr, den, cr, ci, tmp, tmp2 = sm[:, 4, :], sm[:, 5, :], sm[:, 6, :], sm[:, 7, :], sm[:, 8, :], sm[:, 9, :]
            V(lambda e: e.activation(out=dt_, in_=ldt, func=AF.Exp), ["prm"], ["sm0"], "act")
            V(lambda e: e.tensor_scalar_min(lr, are, -1e-4), ["prm"], ["sm1"])
            V(lambda e: e.tensor_tensor(lrdt, lr, dt_, ALU.mult), ["sm0", "sm1"], ["sm2"])
            V(lambda e: e.tensor_tensor(ang, aim, dt_, ALU.mult), ["sm0", "prm"], ["sm3"])
            b17 = lambda a: a.unsqueeze(1).broadcast_to([128, 17, 8])
            j17 = jidx[:].unsqueeze(2).broadcast_to([128, 17, 8])
            V(lambda e: e.tensor_tensor(JL[:], j17, b17(lrdt), ALU.mult), ["jidx", "sm2"], ["JL"])
            V(lambda e: e.activation(out=JL[:], in_=JL[:], func=AF.Exp), ["JL"], ["JL"], "act")
            V(lambda e: e.tensor_tensor(JA[:], j17, b17(ang), ALU.mult), ["jidx", "sm3"], ["JA"])
            f2 = lambda a: a.rearrange("p j g -> p (j g)")
            emit_sincos(P, f2(JA[:]), f2(PI[:]), f2(PR[:]), f2(T1[:]), f2(TI[:]), f2(T2[:]), cpi[:, 1:2],
                        ["JA"], ["PI"], ["PR"], "sc_a")
            V(lambda e: e.tensor_tensor(PR[:], PR[:], JL[:], ALU.mult), ["PR", "JL"], ["PR"])
            V(lambda e: e.tensor_tensor(PI[:], PI[:], JL[:], ALU.mult), ["PI", "JL"], ["PI"])
            V(lambda e: e.tensor_scalar_add(nr, PR[:, 1, :], -1.0), ["PR"], ["sm4"])
            V(lambda e: e.tensor_tensor(den, lr, lr, ALU.mult), ["sm1"], ["sm5"])
            V(lambda e: e.tensor_tensor(tmp, aim, aim, ALU.mult), ["prm"], ["sm8"])
            V(lambda e: e.tensor_tensor(den, den, tmp, ALU.add), ["sm5", "sm8"], ["sm5"])
            V(lambda e: e.reciprocal(den, den), ["sm5"], ["sm5"])
            V(lambda e: e.tensor_tensor(cr, nr, lr, ALU.mult), ["sm4", "sm1"], ["sm6"])
            V(lambda e: e.tensor_tensor(tmp, PI[:, 1, :], aim, ALU.mult), ["PI", "prm", "sm5"], ["sm8"])
            V(lambda e: e.tensor_tensor(cr, cr, tmp, ALU.add), ["sm6", "sm8"], ["sm6"])
            V(lambda e: e.tensor_tensor(cr, cr, den, ALU.mult), ["sm6", "sm5"], ["sm6"])
            V(lambda e: e.tensor_tensor(ci, PI[:, 1, :], lr, ALU.mult), ["PI", "sm1"], ["sm7"])
            V(lambda e: e.tensor_tensor(tmp2, nr, aim, ALU.mult), ["sm4", "prm"], ["sm9"])
            V(lambda e: e.tensor_tensor(ci, ci, tmp2, ALU.subtract), ["sm7", "sm9"], ["sm7"])
            V(lambda e: e.tensor_tensor(ci, ci, den, ALU.mult), ["sm7", "sm5"], ["sm7"])
            for d_ in range(2):
                crd = cr[:, d_ * 4:(d_ + 1) * 4].unsqueeze(2).broadcast_to([128, 4, 16])
                cid = ci[:, d_ * 4:(d_ + 1) * 4].unsqueeze(2).broadcast_to([128, 4, 16])
                o_r = BB[:, 0, d_ * 4:(d_ + 1) * 4, :]
                o_i = BB[:, 1, d_ * 4:(d_ + 1) * 4, :]
                t_r = tb[:, 0, d_ * 4:(d_ + 1) * 4, :]
                t_i = tb[:, 1, d_ * 4:(d_ + 1) * 4, :]
                V(lambda e, o_r=o_r, crd=crd: e.tensor_tensor(o_r, crd, bb[:, 0], ALU.mult), ["sm6", "bb"], [("BB", d_)])
                V(lambda e, t_r=t_r, cid=cid: e.tensor_tensor(t_r, cid, bb[:, 1], ALU.mult), ["sm7", "bb"], [("tb", d_)])
                V(lambda e, o_r=o_r, t_r=t_r: e.tensor_tensor(o_r, o_r, t_r, ALU.subtract), [("BB", d_), ("tb", d_)], [("BB", d_)])
                V(lambda e, o_i=o_i, crd=crd: e.tensor_tensor(o_i, crd, bb[:, 1], ALU.mult), ["sm6", "bb"], [("BBi", d_)])
                V(lambda e, t_i=t_i, cid=cid: e.tensor_tensor(t_i, cid, bb[:, 0], ALU.mult), ["sm7", "bb"], [("tbi", d_)])
                V(lambda e, o_i=o_i, t_i=t_i: e.tensor_tensor(o_i, o_i, t_i, ALU.add), [("BBi", d_), ("tbi", d_)], [("BBi", d_)])
            BBk = [("BB", 0), ("BB", 1), ("BBi", 0), ("BBi", 1)]
            pr16 = PR[:, 0:16, :].unsqueeze(3).broadcast_to([128, 16, 8, 16])
            pi16 = PI[:, 0:16, :].unsqueeze(3).broadcast_to([128, 16, 8, 16])
            bbr = BB[:, 0].unsqueeze(1).broadcast_to([128, 16, 8, 16])
            bbi = BB[:, 1].unsqueeze(1).broadcast_to([128, 16, 8, 16])
            V(lambda e: e.tensor_tensor(Wr[:], pr16, bbr, ALU.mult), ["PR"] + BBk, ["Wr"])
            V(lambda e: e.tensor_tensor(Wt[:], pi16, bbi, ALU.mult), ["PI"] + BBk, ["Wt"])
            V(lambda e: e.tensor_tensor(Wr[:], Wr[:], Wt[:], ALU.subtract), ["Wr", "Wt"], ["Wr"])
            V(lambda e: e.tensor_tensor(Wi[:], pr16, bbi, ALU.mult), ["PR"] + BBk, ["Wi"])
            V(lambda e: e.tensor_tensor(Wt[:], pi16, bbr, ALU.mult), ["PI", "Wr"] + BBk, ["Wt"])
            V(lambda e: e.tensor_tensor(Wi[:], Wi[:], Wt[:], ALU.add), ["Wi", "Wt"], ["Wi"])
            pm = parm[:].unsqueeze(1).unsqueeze(3).broadcast_to([128, 128, 2, 16])
            wrv = Wr[:].rearrange("p j g c -> p (j g) c").unsqueeze(2).broadcast_to([128, 128, 2, 16])
            wiv = Wi[:].rearrange("p j g c -> p (j g) c").unsqueeze(2).broadcast_to([128, 128, 2, 16])
            V(lambda e: e.tensor_tensor(MWr[:], wrv, pm, ALU.mult), ["Wr", "parm"], ["MWr"])
            V(lambda e: e.tensor_tensor(MWi[:], wiv, pm, ALU.mult), ["Wi", "parm"], ["MWi"], "pool")
            pm2 = parm[:].unsqueeze(1).unsqueeze(3).broadcast_to([128, 4, 2, 16])
            V(lambda e: e.tensor_tensor(MC[:, 0], cc[:, 0].unsqueeze(2).broadcast_to([128, 4, 2, 16]), pm2, ALU.mult),
              ["cc", "parm"], ["MC0"])
            V(lambda e: e.tensor_tensor(MC[:, 1], cc[:, 1].unsqueeze(2).broadcast_to([128, 4, 2, 16]), pm2, ALU.mult),
              ["cc", "parm"], ["MC1"])
            mc1f = MC[:, 1].rearrange("p q a c -> p (q a c)")
            V(lambda e: e.tensor_scalar_mul(mc1f, mc1f, -1.0), ["MC1"], ["MC1"])
            mwr = MWr[:].rearrange("p (j d q) a c -> p j d (q a c)", j=16, d=2)
            mwi = MWi[:].rearrange("p (j d q) a c -> p j d (q a c)", j=16, d=2)
            mcr = MC[:, 0].rearrange("p q a c -> p (q a c)")
            mci = MC[:, 1].rearrange("p q a c -> p (q a c)")
            n = 0
            for d_ in range(2):
                for j in range(16):
                    pa = ps[n % 4]
                    n += 1
                    P.add("pe", lambda e, pa=pa, j=j, d_=d_: e.matmul(pa[:], mwr[:, j, d_, :], mcr, start=True, stop=False),
                          r=["MWr", "MC0"], w=[("ps", id(pa))])
                    P.add("pe", lambda e, pa=pa, j=j, d_=d_: e.matmul(pa[:], mwi[:, j, d_, :], mci, start=False, stop=True),
                          r=["MWi", "MC1"], w=[("ps", id(pa))])
                    V(lambda e, pa=pa, j=j, d_=d_: e.tensor_tensor(LAG[:, d_, j, :], pa[:], bmask[:], ALU.mult),
                      [("ps", id(pa)), "bmask"], ["LAG"])
                    for ri, mw in ((0, mwr), (1, mwi)):
                        pb = ps[n % 4]
                        n += 1
                        P.add("pe", lambda e, pb=pb, mw=mw, j=j, d_=d_: e.transpose(pb[:], mw[:, j, d_, :], identf[:]),
                              r=["MWr", "MWi", "identf"], w=[("ps", id(pb))])
                        V(lambda e, pb=pb, ri=ri, j=j, d_=d_: e.copy(BDT[:, d_, j, ri, :], pb[:]),
                          [("ps", id(pb))], ["BDT"], "act")
            for d_ in range(2):
                prj = PR[:, 1:17, d_ * 4:(d_ + 1) * 4].unsqueeze(3).broadcast_to([128, 16, 4, 16])
                pij = PI[:, 1:17, d_ * 4:(d_ + 1) * 4].unsqueeze(3).broadcast_to([128, 16, 4, 16])
                crv = cc[:, 0].unsqueeze(1).broadcast_to([128, 16, 4, 16])
                civ = cc[:, 1].unsqueeze(1).broadcast_to([128, 16, 4, 16])
                pm3 = parm[:].unsqueeze(1).unsqueeze(3).broadcast_to([128, 64, 2, 16])
                V(lambda e, prj=prj, crv=crv: e.tensor_tensor(Gt[:], prj, crv, ALU.mult), ["PR", "cc"], ["Gt"])
                V(lambda e, pij=pij, civ=civ: e.tensor_tensor(Gt2[:], pij, civ, ALU.mult), ["PI", "cc"], ["Gt2"])
                V(lambda e: e.tensor_tensor(Gt[:], Gt[:], Gt2[:], ALU.subtract), ["Gt", "Gt2"], ["Gt"])
                gv = Gt[:].rearrange("p j q c -> p (j q) c").unsqueeze(2).broadcast_to([128, 64, 2, 16])
                ov = MG[:, 0, d_].rearrange("p j (q a c) -> p (j q) a c", q=4, a=2)
                V(lambda e, ov=ov, gv=gv, pm3=pm3: e.tensor_tensor(ov, gv, pm3, ALU.mult), ["Gt", "parm"], [("MG", 0, d_)])
                V(lambda e, pij=pij, crv=crv: e.tensor_tensor(Gt[:], pij, crv, ALU.mult), ["PI", "cc", ("MG", 0, d_)], ["Gt"])
                V(lambda e, prj=prj, civ=civ: e.tensor_tensor(Gt2[:], prj, civ, ALU.mult), ["PR", "cc"], ["Gt2"])
                V(lambda e: e.tensor_tensor(Gt[:], Gt[:], Gt2[:], ALU.add), ["Gt", "Gt2"], ["Gt"])
                ov2 = MG[:, 1, d_].rearrange("p j (q a c) -> p (j q) a c", q=4, a=2)
                gtf = Gt[:].rearrange("p j q c -> p (j q c)")
                V(lambda e, gtf=gtf: e.tensor_scalar_mul(gtf, gtf, -1.0), ["Gt"], ["Gt"])
                V(lambda e, ov2=ov2, gv=gv, pm3=pm3: e.tensor_tensor(ov2, gv, pm3, ALU.mult),
                  ["Gt", "parm"], [("MG", 1, d_)])
            V(lambda e: e.tensor_copy(RT[:, 0:8], JL[:, 16, :]), ["JL"], ["RT0"])
            V(lambda e: e.tensor_copy(RT[:, 8:16], JA[:, 16, :]), ["JA"], ["RT1"])
            V(lambda e: e.tensor_scalar_mul(DG[:], identf[:], dd[:, 0:1]), ["identf", "dd"], ["DG"])
            P.dma("sp", t["S5T_LAG"][blk], LAG[:], r=["LAG"], w=[("S5T", blk)])
            P.dma("act", t["S5T_BDT"][blk], BDT[:], r=["BDT"], w=[("S5T", blk)])
            P.dma("sp", t["S5T_MG"][blk], MG[:], r=[("MG", a, b) for a in range(2) for b in range(2)], w=[("S5T", blk)])
            P.dma("act", t["S5T_RT"][blk], RT[:], r=["RT0", "RT1"], w=[("S5T", blk)])
            P.dma("sp", t["S5T_DIAG"][blk], DG[:], r=["DG"], w=[("S5T", blk)])
        P.flush()


def build(upto=99, dbg=(), only=None):
    k = K(dbg)
    declare_io(k)
    declare_s5(k)
    declare_ml(k)
    declare_s5post(k)
    declare_moe(k)
    stages = [stage0, stage1, stage2, stage3a, stage3b, stage4a, stage4b, stage4c, stage5, stage6, stage7, stage8, stage9]
    for i, f in enumerate(stages):
        if (only is None and i <= upto) or (only is not None and i in only):
            f(k)
    return k


S5_BLOCKS = list(range(8))
S5_PH = {'X', 'dem', 'inter', 'lag'}
S5_CUT = 9


def stage3b(k, blocks=None):
    blocks = S5_BLOCKS if blocks is None else blocks
    nc, P, t = k.nc, k.P, k.t
    NX = L // TS5
    NC = CTX // TS5
    with ExitStack() as st:
        sb = lambda n, s, d=F32: st.enter_context(nc.sbuf_tensor(n, s, d))
        Ub = sb("b_U", [128, NT], BF16)
        LAG = sb("b_LAG", [128, 2, 16, 128], BF16)
        BDT = sb("b_BDT", [128, 2, 16, 2, 128], BF16)
        MG = sb("b_MG", [128, 2, 2, 16, 128], BF16)
        DG = sb("b_DG", [128, 128], BF16)
        RT = sb("b_RT", [128, 16])
        midx = sb("b_midx", [128, 2, NCH])
        BDQ = [sb("b_BDQ%d" % i, [128, 16, 2, 128], BF16) for i in range(2)]
        rowm = sb("b_rowm", [128, 4])
        cpi = sb("b_cpi", [128, 2])
        Xr = sb("b_Xr", [128, 4, NCH])
        Xi = sb("b_Xi", [128, 4, NCH])
        Ec = sb("b_Ec", [128, 4, NCH])
        Es = sb("b_Es", [128, 4, NCH])
        tf = sb("b_tf", [128, 4, NCH])
        tm = sb("b_tm", [128, 4, NCH])
        ANGS = sb("b_ANGS", [128, 4, 49])
        SINS = sb("b_SINS", [128, 4, 49])
        COSS = sb("b_COSS", [128, 4, 49])
        stf = sb("b_stf", [128, 4, 49])
        stm = sb("b_stm", [128, 4, 49])
        sti = sb("b_sti", [128, 4, 49], I32)
        Hb = sb("b_Hb", [128, 2, 2, 4, NCH], BF16)
        YI = sb("b_YI", [128, NX, TS5])
        Us = YI[:].rearrange("p n s -> p (n s)").bitcast(BF16)[:, 0:TS5 * NCH].rearrange("p (s n) -> p s n", n=NCH)
        ys = [sb("b_ys%d" % i, [128, 512]) for i in range(2)]
        y2 = [sb("b_y2%d" % i, [128, 512]) for i in range(2)]
        yo = [sb("b_yo%d" % i, [128, 512], BF16) for i in range(2)]
        psx = [st.enter_context(nc.psum_tensor("b_psx%d" % i, [128, 512], F32)) for i in range(4)]
        psc = [st.enter_context(nc.psum_tensor("b_psc%d" % i, [128, 2, 16], F32)) for i in range(2)]
        psy = [st.enter_context(nc.psum_tensor("b_psy%d" % i, [128, 512], F32)) for i in range(2)]
        P.dma("sp", midx[:], t["c_midx"][:, :, :], w=["midx"])
        P.dma("sp", rowm[:], t["c_rowmask"][:, :], w=["rowm"])
        P.add("pool", lambda e: e.memset(cpi[:], 0.0), w=["cpi"])
        f2 = lambda a: a.rearrange("p q n -> p (q n)")
        for blk in blocks:
            P.dma("sp", Ub[:], t["U5"][blk], r=[("U5", g) for g in range(17)], w=["Ub"])
            P.dma("act", LAG[:], t["S5T_LAG"][blk], r=[("S5T", blk)], w=["LAG"])
            P.dma("sp", BDT[:], t["S5T_BDT"][blk], r=[("S5T", blk)], w=["BDT"])
            P.dma("act", MG[:], t["S5T_MG"][blk], r=[("S5T", blk)], w=["MG"])
            P.dma("sp", DG[:], t["S5T_DIAG"][blk], r=[("S5T", blk)], w=["DG"])
            P.dma("act", RT[:], t["S5T_RT"][blk], r=[("S5T", blk)], w=["RT"])
            P.add("dve", lambda e: e.tensor_copy(Us, Ub[:].rearrange("p (n s) -> p s n", s=TS5)), r=["Ub"], w=["Us", "YI"])
            for d_ in range(2):
                xo, co = (NC, 0) if d_ == 0 else (0, NX)
                for q in (range(4) if 'X' in S5_PH else ()):
                    pb = q % 2
                    bq = BDQ[pb]
                    P.add("dve", lambda e, bq=bq, q=q, d_=d_: e.tensor_scalar_mul(
                        bq[:].rearrange("p j r c -> p (j r c)"), BDT[:, d_].rearrange("p j r c -> p (j r c)"),
                        rowm[:, q:q + 1]), r=["BDT", "rowm"], w=[("BDQ", pb)])
                    for ri in (range(2) if S5_CUT >= 2 else ()):
                        px_ = psx[pb * 2 + ri]
                        for s in range(TS5):
                            j = (TS5 - 1 - s) if d_ == 0 else s
                            lhs = bq[:, j, ri, :]
                            P.add("pe", lambda e, px_=px_, lhs=lhs, q=q, s=s: e.matmul(
                                px_[:], lhs, Us[:, s, NC:NCH],
                                start=(s == 0), stop=(s == TS5 - 1)),
                                r=[("BDQ", pb), "Us"], w=[("psx", pb, ri)])
                            P.add("pe", lambda e, pb=pb, ri=ri, lhs=lhs, q=q, s=s: e.matmul(
                                psc[pb][:, ri, :], lhs, Us[:, s, 0:NC],
                                start=(s == 0), stop=(s == TS5 - 1)),
                                r=[("BDQ", pb), "Us"], w=[("psc", pb, ri)])
                        X = Xr if ri == 0 else Xi
                        if S5_CUT < 3:
                            continue
                        if S5_CUT != 4:
                            P.add("dve", lambda e, X=X, q=q, px_=px_, xo=xo: e.tensor_copy(X[:, q, xo:xo + NX], px_[:]),
                                  r=[("psx", pb, ri)], w=[("X", ri)])
                        if S5_CUT != 5:
                            P.add("dve", lambda e, X=X, q=q, pb=pb, ri=ri, co=co: e.tensor_copy(X[:, q, co:co + NC], psc[pb][:, ri, :]),
                                  r=[("psc", pb, ri)], w=[("X", ri)])
                if 'dem' not in S5_PH:
                    continue
                V = lambda fn, r, w, eng="dve": P.add(eng, fn, r=r, w=w)
                for q in range(4):
                    thq = RT[:, 8 + d_ * 4 + q:9 + d_ * 4 + q]
                    V(lambda e, q=q, thq=thq: e.tensor_scalar_mul(ANGS[:, q, 0:16], midx[:, 0, 0:16], thq), ["midx", "RT"], ["ANGS"])
                    V(lambda e, q=q, thq=thq: e.tensor_scalar(ANGS[:, q, 16:49], midx[:, 0, 0:33], thq, 16.0, ALU.mult, ALU.mult),
                      ["midx", "RT"], ["ANGS"])
                fs = lambda a: a.rearrange("p q n -> p (q n)")
                emit_sincos(P, fs(ANGS[:]), fs(SINS[:]), fs(COSS[:]), fs(stf[:]), fs(sti[:]), fs(stm[:]), cpi[:, 0:1],
                            ["ANGS"], ["SINS"], ["COSS"], "sc_s")
                if d_ == 0:
                    c0, s0 = COSS[:, :, 0:16], SINS[:, :, 0:16]
                    c1, s1 = COSS[:, :, 16:49], SINS[:, :, 16:49]
                else:
                    c0, s0 = COSS[:, :, 15::-1] if False else COSS[:, :, 0:16][:, :, ::-1], SINS[:, :, 0:16][:, :, ::-1]
                    c1, s1 = COSS[:, :, 16:49][:, :, ::-1], SINS[:, :, 16:49][:, :, ::-1]
                b0 = lambda a: a.unsqueeze(2).broadcast_to([128, 4, 33, 16])
                b1 = lambda a: a.unsqueeze(3).broadcast_to([128, 4, 33, 16])
                v4 = lambda a: a.rearrange("p q (a b) -> p q a b", b=16)
                V(lambda e, c0=c0, c1=c1: e.tensor_tensor(v4(Ec[:]), b1(c1), b0(c0), ALU.mult), ["COSS", "SINS", "Hdone"], ["Ec"])
                V(lambda e, s0=s0, s1=s1: e.tensor_tensor(v4(tf[:]), b1(s1), b0(s0), ALU.mult), ["COSS", "SINS", "Hdone"], [("sc_b", "tf")], "pool")
                V(lambda e: e.tensor_tensor(f2(Ec[:]), f2(Ec[:]), f2(tf[:]), ALU.subtract), ["Ec", ("sc_b", "tf")], ["Ec"])
                V(lambda e, c0=c0, s1=s1: e.tensor_tensor(v4(Es[:]), b1(s1), b0(c0), ALU.mult), ["COSS", "SINS", "Hdone"], ["Es"])
                V(lambda e, s0=s0, c1=c1: e.tensor_tensor(v4(tm[:]), b1(c1), b0(s0), ALU.mult), ["COSS", "SINS", "Hdone"], [("sc_b", "tm")], "pool")
                V(lambda e: e.tensor_tensor(f2(Es[:]), f2(Es[:]), f2(tm[:]), ALU.add), ["Es", ("sc_b", "tm")], ["Es"])
                V(lambda e: e.tensor_tensor(f2(tf[:]), f2(Ec[:]), f2(Xr[:]), ALU.mult), ["Ec", ("X", 0), ("sc_b", "tf")], [("sc_b", "tf")])
                V(lambda e: e.tensor_tensor(f2(tm[:]), f2(Es[:]), f2(Xi[:]), ALU.mult), ["Es", ("X", 1), ("sc_b", "tm")], [("sc_b", "tm")], "pool")
                V(lambda e: e.tensor_tensor(f2(tf[:]), f2(tf[:]), f2(tm[:]), ALU.add), [("sc_b", "tf"), ("sc_b", "tm")], [("sc_b", "tf")])
                V(lambda e: e.tensor_tensor(f2(tm[:]), f2(Ec[:]), f2(Xi[:]), ALU.mult), ["Ec", ("X", 1), ("sc_b", "tf")], [("sc_b", "tm")])
                V(lambda e: e.tensor_tensor(f2(Xr[:]), f2(Es[:]), f2(Xr[:]), ALU.mult), ["Es", ("X", 0), ("sc_b", "tf")], [("X", 0)], "pool")
                V(lambda e: e.tensor_tensor(f2(tm[:]), f2(tm[:]), f2(Xr[:]), ALU.subtract), [("sc_b", "tm"), ("X", 0)], [("sc_b", "tm")])
                for q in range(4):
                    rq = RT[:, d_ * 4 + q:d_ * 4 + q + 1].broadcast_to([128, NCH])
                    for src, dst, kk in ((tf, Xr, 0), (tm, Xi, 1)):
                        if d_ == 0:
                            o_, i_ = dst[:, q, :], src[:, q, :]
                        else:
                            o_, i_ = dst[:, q, ::-1], src[:, q, ::-1]
                        V(lambda e, o_=o_, i_=i_, rq=rq: e.tensor_tensor_scan(o_, rq, i_, 0.0, ALU.mult, ALU.add),
                          ["RT", ("sc_b", "tf"), ("sc_b", "tm"), ("X", kk)], [("X", kk)])
                hr = Hb[:, 0, d_].rearrange("p q n -> p (q n)")
                hi = Hb[:, 1, d_].rearrange("p q n -> p (q n)")
                V(lambda e: e.tensor_tensor(f2(tf[:]), f2(Ec[:]), f2(Xr[:]), ALU.mult), ["Ec", ("X", 0), ("sc_b", "tf")], [("sc_b", "tf")])
                V(lambda e: e.tensor_tensor(f2(tm[:]), f2(Es[:]), f2(Xi[:]), ALU.mult), ["Es", ("X", 1), ("sc_b", "tm")], [("sc_b", "tm")], "pool")
                V(lambda e, hr=hr: e.tensor_tensor(hr, f2(tf[:]), f2(tm[:]), ALU.subtract), [("sc_b", "tf"), ("sc_b", "tm")], [("Hb", d_, 0)])
                V(lambda e: e.tensor_tensor(f2(tf[:]), f2(Ec[:]), f2(Xi[:]), ALU.mult), ["Ec", ("X", 1), ("Hb", d_, 0)], [("sc_b", "tf")])
                V(lambda e: e.tensor_tensor(f2(tm[:]), f2(Es[:]), f2(Xr[:]), ALU.mult), ["Es", ("X", 0), ("Hb", d_, 0)], [("sc_b", "tm")], "pool")
                V(lambda e, hi=hi: e.tensor_tensor(hi, f2(tf[:]), f2(tm[:]), ALU.add), [("sc_b", "tf"), ("sc_b", "tm")], [("Hb", d_, 1), "Hdone"])
            hbk = [("Hb", a, b) for a in range(2) for b in range(2)]
            for s in (range(TS5) if 'inter' in S5_PH else ()):
                pp = psy[s % 2]
                for q in range(4):
                    n_mm = 0
                    for d_ in range(2):
                        j = (s + 1) if d_ == 0 else (TS5 - s)
                        c0 = (NC - 1) if d_ == 0 else 1
                        for ri in range(2):
                            P.add("pe", lambda e, pp=pp, q=q, d_=d_, ri=ri, j=j, c0=c0, n_mm=n_mm: e.matmul(
                                pp[32 * q:32 * q + 32, :], MG[:, ri, d_, j - 1, 32 * q:32 * q + 32],
                                Hb[:, ri, d_, q, c0:c0 + NX], start=(n_mm == 0), stop=(n_mm == 3),
                                tile_position=(0, 32 * q)),
                                r=["MG"] + hbk, w=[("psy", s % 2)])
                            n_mm += 1
                P.add("act", lambda e, pp=pp, s=s: e.copy(YI[:, :, s], pp[:]), r=[("psy", s % 2)], w=["YI", "Us"])
            for tb in (range(16) if 'lag' in S5_PH else ()):
                pp = psy[tb % 2]
                b = tb % 2
                u0 = CTX + tb * 512
                uv = Ub[:, u0:u0 + 512].rearrange("p (n s) -> p n s", s=TS5)
                pv = pp[:].rearrange("p (n s) -> p n s", s=TS5)
                P.add("pe", lambda e, pp=pp, u0=u0: e.matmul(pp[:], DG[:], Ub[:, u0:u0 + 512], start=True, stop=False),
                      r=["DG", "Ub"], w=[("psy", b)])
                for j in range(TS5):
                    P.add("pe", lambda e, pv=pv, uv=uv, j=j: e.matmul(pv[:, :, j:TS5], LAG[:, 0, j, :], uv[:, :, 0:TS5 - j],
                                                                      start=False, stop=False),
                          r=["LAG", "Ub"], w=[("psy", b)])
                    P.add("pe", lambda e, pv=pv, uv=uv, j=j: e.matmul(pv[:, :, 0:TS5 - j], LAG[:, 1, j, :], uv[:, :, j:TS5],
                                                                      start=False, stop=(j == TS5 - 1)),
                          r=["LAG", "Ub"], w=[("psy", b)])
                if S5_CUT < 2:
                    continue
                yiv = YI[:, tb * 32:(tb + 1) * 32, :].rearrange("p n s -> p (n s)")
                P.add("dve", lambda e, pp=pp, b=b, yiv=yiv: e.tensor_tensor(ys[b][:], pp[:], yiv, ALU.add),
                      r=[("psy", b), "YI"], w=[("ys", b)])
                P.add("act", lambda e, b=b: e.activation(out=y2[b][:], in_=ys[b][:], func=AF.Square), r=[("ys", b)], w=[("y2", b)])
                P.add("dve", lambda e, b=b: e.tensor_scalar(y2[b][:], y2[b][:], 0.044715, 1.0, ALU.mult, ALU.add),
                      r=[("y2", b)], w=[("y2", b)])
                P.add("dve", lambda e, b=b: e.tensor_tensor(y2[b][:], y2[b][:], ys[b][:], ALU.mult), r=[("y2", b), ("ys", b)], w=[("y2", b)])
                P.add("act", lambda e, b=b: e.activation(out=y2[b][:], in_=y2[b][:], func=AF.Sigmoid, scale=1.5957691216),
                      r=[("y2", b)], w=[("y2", b)])
                P.add("dve", lambda e, b=b: e.tensor_tensor(yo[b][:], y2[b][:], ys[b][:], ALU.mult), r=[("y2", b), ("ys", b)], w=[("yo", b)])
                if S5_CUT < 3:
                    continue
                P.dma("sp", t["YG"][blk][:, tb * 512:(tb + 1) * 512], yo[b][:], r=[("yo", b)], w=[("YG", blk, tb)])
        P.flush()


def declare_ml(k):
    k.din("convw", [16, 128, 5])
    k.din("convb", [16, 128, 1])
    k.din("gateb", [128, 16])
    k.din("mlng", [128, D_ML])
    k.din("c_tri", [128, 128])
    k.din("c_trit", [128, 128])
    k.din("c_ones", [128, 128])
    k.dscr("QKT", [16, 128, NT], BF16)
    k.dscr("HDIR", [NH, 2, L, DH])
    k.dscr("YML", [L, D_ML], BF16)


def stage4a(k):
    nc, P, t = k.nc, k.P, k.t
    PADW = 2 + CTX + 2 + 2 + L + 2
    with ExitStack() as st:
        sb = lambda n, s, d=F32: st.enter_context(nc.sbuf_tensor(n, s, d))
        identf = sb("c_idf", [128, 128])
        pp = [sb("c_pp%d" % i, [128, PADW], BF16) for i in range(2)]
        cw = [sb("c_cw%d" % i, [128, 5]) for i in range(2)]
        cb_ = [sb("c_cb%d" % i, [128, 1]) for i in range(2)]
        dg = [sb("c_dg%d" % i, [128, 5, 128], BF16) for i in range(2)]
        of = [sb("c_of%d" % i, [128, 512]) for i in range(2)]
        ob = [sb("c_ob%d" % i, [128, NT], BF16) for i in range(2)]
        ps = [st.enter_context(nc.psum_tensor("c_ps%d" % i, [128, 512], F32)) for i in range(2)]
        P.dma("sp", identf[:], t["ident_f"][:, :], w=["identf"])
        for b in range(2):
            for (a0, a1) in ((0, 2), (2 + CTX, 2 + CTX + 4), (PADW - 2, PADW)):
                P.add("pool", lambda e, b=b, a0=a0, a1=a1: e.memset(pp[b][:, a0:a1], 0.0), w=[("pad", b)])
        segs = [(0, CTX, 2)] + [(CTX + 512 * i, 512, 2 + CTX + 4 + 512 * i) for i in range(16)]
        for cb in range(16):
            b = cb % 2
            P.dma("sp", pp[b][:, 2:2 + CTX], t["QKPRE"][cb][:, 0:CTX], r=[("QKPRE", 0), ("pad", b)], w=[("pp", b)])
            P.dma("sp", pp[b][:, 2 + CTX + 4:2 + CTX + 4 + L], t["QKPRE"][cb][:, CTX:NT],
                  r=[("QKPRE", g) for g in range(1, 17)] + [("pad", b)], w=[("pp", b)])
            P.dma("sp", cw[b][:], t["convw"][cb], w=[("cw", b)])
            P.dma("sp", cb_[b][:], t["convb"][cb], w=[("cb", b)])
            for kk in range(5):
                P.add("dve", lambda e, b=b, kk=kk: e.tensor_scalar_mul(dg[b][:, kk, :], identf[:], cw[b][:, kk:kk + 1]),
                      r=["identf", ("cw", b)], w=[("dg", b)])
            for si, (t0, n, po) in enumerate(segs):
                pb = si % 2
                for kk in range(5):
                    P.add("pe", lambda e, pb=pb, b=b, kk=kk, po=po, n=n: e.matmul(
                        ps[pb][:, 0:n], dg[b][:, kk, :], pp[b][:, po + kk - 2:po + kk - 2 + n], start=(kk == 0), stop=(kk == 4)),
                        r=[("dg", b), ("pp", b)], w=[("ps", pb)])
                if cb < 8:
                    P.add("act", lambda e, pb=pb, b=b, n=n: e.activation(out=of[pb][:, 0:n], in_=ps[pb][:, 0:n], func=AF.Silu,
                                                                         bias=cb_[b][:, 0:1], scale=1.0),
                          r=[("ps", pb), ("cb", b)], w=[("of", pb)])
                    P.add("dve", lambda e, pb=pb, b=b, n=n, t0=t0: e.tensor_scalar_mul(ob[b][:, t0:t0 + n], of[pb][:, 0:n], 1.0 / 16.0),
                          r=[("of", pb)], w=[("ob", b)])
                else:
                    P.add("act", lambda e, pb=pb, b=b, n=n, t0=t0: e.activation(out=ob[b][:, t0:t0 + n], in_=ps[pb][:, 0:n], func=AF.Silu,
                                                                               bias=cb_[b][:, 0:1], scale=1.0),
                          r=[("ps", pb), ("cb", b)], w=[("ob", b)])
            P.dma(k.q(), t["QKT"][cb], ob[b][:], r=[("ob", b)], w=[("QKT", cb)])
        P.flush()


def stage4b(k):
    nc, P, t = k.nc, k.P, k.t
    NG_ = NTILE * NH
    with ExitStack() as st:
        sb = lambda n, s, d=F32: st.enter_context(nc.sbuf_tensor(n, s, d))
        G = sb("m_G", [128, NTILE, 16])
        gb = sb("m_gb", [128, 16])
        tri = sb("m_tri", [128, 128])
        trit = sb("m_trit", [128, 128])
        ones = sb("m_ones", [128, 128])
        trib = sb("m_trib", [128, 2, 128], BF16)
        identb = sb("m_idb", [128, 128], BF16)
        one1 = sb("m_one1", [128, 1])
        LF = sb("m_LF", [128, 2, NTILE, NH])
        SC = sb("m_SC", [128, 2, 4, NTILE, NH])
        tmpg = sb("m_tmpg", [128, NTILE, NH])
        pso_full = [st.enter_context(nc.psum_tensor("m_pso%d" % i, [128, NG_], F32)) for i in range(2)]
        psg = pso_full
        P.dma("sp", G[:], t["GATES"][:, :, :], r=["GATES"], w=["G"])
        P.dma("sp", gb[:], t["gateb"][:, :], w=["gb"])
        P.dma("act", tri[:], t["c_tri"][:, :], w=["tri"])
        P.dma("act", trit[:], t["c_trit"][:, :], w=["trit"])
        P.dma("sp", ones[:], t["c_ones"][:, :], w=["ones"])
        P.dma("sp", identb[:], t["ident_bf"][:, :], w=["identb"])
        P.add("pool", lambda e: e.memset(one1[:], 1.0), w=["one1"])
        P.add("dve", lambda e: e.tensor_copy(trib[:, 0, :], tri[:]), r=["tri"], w=["trib"])
        P.add("dve", lambda e: e.tensor_copy(trib[:, 1, :], trit[:]), r=["trit"], w=["trib"])
        P.add("dve", lambda e: e.tensor_tensor(G[:], G[:], gb[:].unsqueeze(1).broadcast_to([128, NTILE, 16]), ALU.add),
              r=["G", "gb"], w=["G"])
        for d_ in range(2):
            ipre = G[:, :, 8 * d_:8 * d_ + 4]
            fpre = G[:, :, 8 * d_ + 4:8 * d_ + 8]
            lf = LF[:, d_]
            P.add("act", lambda e, lf=lf, fpre=fpre: e.activation(out=lf, in_=fpre, func=AF.Exp, scale=-1.0), r=["G"], w=[("LF", d_)])
            P.add("act", lambda e, lf=lf: e.activation(out=lf, in_=lf, func=AF.Ln, bias=one1[:, 0:1], scale=1.0),
                  r=[("LF", d_), "one1"], w=[("LF", d_)])
            lff = lf.rearrange("p n h -> p (n h)")
            P.add("dve", lambda e, lff=lff: e.tensor_scalar_mul(lff, lff, -1.0), r=[("LF", d_)], w=[("LF", d_)])
            m_ = tri if d_ == 0 else trit
            P.add("pe", lambda e, m_=m_, lff=lff: e.matmul(psg[0][:], m_[:], lff, start=True, stop=True),
                  r=["tri", "trit", ("LF", d_)], w=["psg0"])
            P.add("pe", lambda e, lff=lff: e.matmul(psg[1][:], ones[:], lff, start=True, stop=True),
                  r=["ones", ("LF", d_)], w=["psg1"])
            u_ = SC[:, d_, 0].rearrange("p n h -> p (n h)")
            fl_ = SC[:, d_, 1].rearrange("p n h -> p (n h)")
            dec_ = SC[:, d_, 2].rearrange("p n h -> p (n h)")
            wt_ = SC[:, d_, 3].rearrange("p n h -> p (n h)")
            tg = tmpg[:].rearrange("p n h -> p (n h)")
            P.add("dve", lambda e, ipre=ipre: e.tensor_copy(tmpg[:], ipre), r=["G"], w=["tmpg"])
            P.add("dve", lambda e, tg=tg: e.tensor_tensor(tg, tg, psg[0][:], ALU.subtract), r=["tmpg", "psg0"], w=["tmpg"])
            P.add("act", lambda e, u_=u_, tg=tg: e.activation(out=u_, in_=tg, func=AF.Exp), r=["tmpg"], w=[("SC", d_)])
            P.add("act", lambda e, fl_=fl_: e.activation(out=fl_, in_=psg[0][:], func=AF.Exp, scale=-1.0), r=["psg0"], w=[("SC", d_)])
            P.add("act", lambda e, dec_=dec_: e.activation(out=dec_, in_=psg[1][:], func=AF.Exp), r=["psg1"], w=[("SC", d_)])
            P.add("dve", lambda e, wt_=wt_, u_=u_, dec_=dec_: e.tensor_tensor(wt_, u_, dec_, ALU.mult), r=[("SC", d_)], w=[("SC", d_)])
        QT = sb("m_QT", [128, 2, NT], BF16)
        KT = sb("m_KT", [128, 2, NT], BF16)
        Vh = sb("m_Vh", [128, NTILE, DH + 1], BF16)
        CT = [sb("m_CT%d" % i, [128, 2, DH + 1]) for i in range(2)]
        CTb = [sb("m_CTb%d" % i, [128, 2, DH + 1], BF16) for i in range(2)]
        kt = [sb("m_kt%d" % i, [128, DH], BF16) for i in range(2)]
        SW = [sb("m_SW%d" % i, [128, 128], BF16) for i in range(2)]
        vw = [sb("m_vw%d" % i, [128, DH + 1], BF16) for i in range(2)]
        dn = [sb("m_dn%d" % i, [128, 2]) for i in range(2)]
        ho = [sb("m_ho%d" % i, [128, DH]) for i in range(2)]
        pst = [st.enter_context(nc.psum_tensor("m_pst%d" % i, [128, DH], BF16)) for i in range(2)]
        pss = [st.enter_context(nc.psum_tensor("m_pss%d" % i, [128, 128], F32)) for i in range(2)]
        pso = [pso_full[i][:, 0:DH + 1] for i in range(2)]
        psc_ = [st.enter_context(nc.psum_tensor("m_psc%d" % i, [128, DH + 1], F32)) for i in range(2)]
        for h in range(NH):
            for dc in range(2):
                P.dma("sp", QT[:, dc, :], t["QKT"][2 * h + dc], r=[("QKT", 2 * h + dc)], w=["QT"])
                P.dma("act", KT[:, dc, :], t["QKT"][8 + 2 * h + dc], r=[("QKT", 8 + 2 * h + dc)], w=["KT"])
            for n0 in range(0, NTILE, 11):
                P.dma(k.q(), Vh[:, n0:n0 + 11, 0:DH],
                      t["V"].rearrange("(n p) c -> p n c", p=128)[:, n0:n0 + 11, h * DH:(h + 1) * DH],
                      r=[("V", ti) for ti in range(n0, n0 + 11)], w=["Vh"])
            P.add("pool", lambda e: e.memset(Vh[:, :, DH:DH + 1], 1.0), w=["Vh1"])
            for d_ in range(2):
                P.add("pool", lambda e, d_=d_: e.memset(CT[d_][:], 0.0), w=[("CT", d_)])
                P.add("pool", lambda e, d_=d_: e.memset(CTb[d_][:], 0.0), w=[("CTb", d_)])
            order_f = list(range(NTILE))
            order_b = [1, 0] + list(range(NTILE - 1, 1, -1))
            for step in range(NTILE):
                for d_ in range(2):
                    ti = order_f[step] if d_ == 0 else order_b[step]
                    c0 = ti * 128
                    col = ti * NH + h
                    sc = lambda kind, d_=d_, col=col: SC[:, d_, kind].rearrange("p n h -> p (n h)")[:, col:col + 1]
                    for dc in range(2):
                        P.add("pe", lambda e, d_=d_, dc=dc, c0=c0: e.transpose(pst[d_][:, dc * 128:(dc + 1) * 128],
                                                                               KT[:, dc, c0:c0 + 128], identb[:]),
                              r=["KT", "identb"], w=[("pst", d_)])
                    P.add("act", lambda e, d_=d_: e.copy(kt[d_][:], pst[d_][:]), r=[("pst", d_)], w=[("kt", d_)])
                    if ti >= 2:
                        for dc in range(2):
                            P.add("pe", lambda e, d_=d_, dc=dc, c0=c0: e.matmul(pss[d_][:], KT[:, dc, c0:c0 + 128], QT[:, dc, c0:c0 + 128],
                                                                                start=(dc == 0), stop=(dc == 1)),
                                  r=["KT", "QT"], w=[("pss", d_)])
                        P.add("dve", lambda e, d_=d_, sc=sc: e.scalar_tensor_tensor(SW[d_][:], pss[d_][:], sc(0), trib[:, d_, :],
                                                                                    ALU.mult, ALU.mult),
                              r=[("pss", d_), ("SC", d_), "trib"], w=[("SW", d_)])
                        P.add("pe", lambda e, d_=d_, ti=ti: e.matmul(pso[d_][:], SW[d_][:], Vh[:, ti, :], start=True, stop=False),
                              r=[("SW", d_), "Vh", "Vh1"], w=[("pso", d_), "psg%d" % d_])
                        for dc in range(2):
                            P.add("pe", lambda e, d_=d_, dc=dc, c0=c0: e.matmul(pso[d_][:], QT[:, dc, c0:c0 + 128], CTb[d_][:, dc, :],
                                                                                start=False, stop=(dc == 1)),
                                  r=["QT", ("CTb", d_)], w=[("pso", d_)])
                        P.add("act", lambda e, d_=d_: e.activation(out=dn[d_][:, 0:1], in_=pso[d_][:, DH:DH + 1], func=AF.Abs),
                              r=[("pso", d_)], w=[("dn", d_)])
                        P.add("dve", lambda e, d_=d_, sc=sc: e.tensor_tensor(dn[d_][:, 0:1], dn[d_][:, 0:1], sc(1), ALU.max),
                              r=[("dn", d_), ("SC", d_)], w=[("dn", d_)])
                        P.add("dve", lambda e, d_=d_: e.reciprocal(dn[d_][:, 1:2], dn[d_][:, 0:1]), r=[("dn", d_)], w=[("dn1", d_)])
                        P.add("act", lambda e, d_=d_: e.activation(out=ho[d_][:], in_=pso[d_][:, 0:DH], func=AF.Copy,
                                                                   scale=dn[d_][:, 1:2]),
                              r=[("pso", d_), ("dn1", d_)], w=[("ho", d_)])
                        P.dma("sp", t["HDIR"][h, d_, (ti - 2) * 128:(ti - 1) * 128, :], ho[d_][:], r=[("ho", d_)],
                              w=[("HDIR", h, d_, ti)])
                    P.add("dve", lambda e, d_=d_, ti=ti, sc=sc: e.tensor_scalar_mul(vw[d_][:], Vh[:, ti, :], sc(3)),
                          r=["Vh", "Vh1", ("SC", d_)], w=[("vw", d_)])
                    for dc in range(2):
                        pc = psc_[dc]
                        P.add("pe", lambda e, d_=d_, dc=dc, pc=pc: e.matmul(pc[:], kt[d_][:, dc * 128:(dc + 1) * 128], vw[d_][:],
                                                                            start=True, stop=True),
                              r=[("kt", d_), ("vw", d_)], w=[("psc", dc)])
                        P.add("dve", lambda e, d_=d_, dc=dc, pc=pc, sc=sc: e.scalar_tensor_tensor(
                            CT[d_][:, dc, :], CT[d_][:, dc, :], sc(2), pc[:], ALU.mult, ALU.add),
                            r=[("psc", dc), ("CT", d_), ("SC", d_)], w=[("CT", d_)])
                        P.add("act", lambda e, d_=d_, dc=dc: e.copy(CTb[d_][:, dc, :], CT[d_][:, dc, :]),
                              r=[("CT", d_)], w=[("CTb", d_)])
        P.flush()


def stage4c(k):
    nc, P, t = k.nc, k.P, k.t
    with ExitStack() as st:
        sb = lambda n, s, d=F32: st.enter_context(nc.sbuf_tensor(n, s, d))
        ng = sb("n_ng", [128, D_ML])
        epst = sb("n_eps", [128, 1])
        hf = [sb("n_hf%d" % i, [128, NH, DH]) for i in range(2)]
        hb = [sb("n_hb%d" % i, [128, NH, DH]) for i in range(2)]
        so = [sb("n_so%d" % i, [128, D_ML], BF16) for i in range(2)]
        junk = sb("n_junk", [128, DH])
        ss = [sb("n_ss%d" % i, [128, 2, NH]) for i in range(2)]
        yo = [sb("n_yo%d" % i, [128, D_ML], BF16) for i in range(2)]
        P.dma("sp", ng[:], t["mlng"][:, :], w=["ng"])
        P.add("pool", lambda e: e.memset(epst[:], EPS), w=["epst"])
        for i in range(L // 128):
            b = i % 2
            P.dma("sp", hf[b][:], t["HDIR"][:, 0, i * 128:(i + 1) * 128, :].rearrange("h p d -> p h d"),
                  r=[("HDIR", h, 0, i + 2) for h in range(NH)], w=[("hf", b)])
            P.dma("sp", hb[b][:], t["HDIR"][:, 1, i * 128:(i + 1) * 128, :].rearrange("h p d -> p h d"),
                  r=[("HDIR", h, 1, i + 2) for h in range(NH)], w=[("hb", b)])
            P.dma("sp", so[b][:], t["SO"][(i + 2) * 128:(i + 3) * 128, :], r=[("SO", i + 2)], w=[("so", b)])
            hff = hf[b][:].rearrange("p h d -> p (h d)")
            hbf = hb[b][:].rearrange("p h d -> p (h d)")
            P.add("dve", lambda e, hff=hff, hbf=hbf: e.tensor_tensor(hff, hff, hbf, ALU.add), r=[("hf", b), ("hb", b)], w=[("hf", b)])
            for h in range(NH):
                P.add("act", lambda e, b=b, h=h: e.activation(out=junk[:], in_=hf[b][:, h, :], func=AF.Square, scale=float(DH ** -0.5),
                                                              accum_out=ss[b][:, 0, h:h + 1]),
                      r=[("hf", b)], w=["junk", ("ss", b)])
            P.add("act", lambda e, b=b: e.activation(out=ss[b][:, 1, :], in_=ss[b][:, 0, :], func=AF.Sqrt, bias=epst[:, 0:1], scale=1.0),
                  r=[("ss", b), "epst"], w=[("ss1", b)])
            P.add("dve", lambda e, b=b: e.reciprocal(ss[b][:, 1, :], ss[b][:, 1, :]), r=[("ss1", b)], w=[("ss1", b)])
            for h in range(NH):
                P.add("dve", lambda e, b=b, h=h: e.scalar_tensor_tensor(hf[b][:, h, :], hf[b][:, h, :], ss[b][:, 1, h:h + 1],
                                                                        ng[:, h * DH:(h + 1) * DH], ALU.mult, ALU.mult),
                      r=[("hf", b), ("ss1", b), "ng"], w=[("hf", b)])
            P.add("pool", lambda e, b=b, hff=hff: e.tensor_tensor(yo[b][:], hff, so[b][:], ALU.mult), r=[("hf", b), ("so", b)], w=[("yo", b)])
            P.dma("sp", t["YML"][i * 128:(i + 1) * 128, :], yo[b][:], r=[("yo", b)], w=[("YML", i)])
        P.flush()


def declare_s5post(k):
    k.din("gluw", [D_S5, D_S5])
    k.din("glub", [128, 8])
    k.din("w_out", [D, D])
    k.din("rw", [128, 16, NE])
    k.dscr("HX2", [L, D], BF16)
    k.dscr("X1", [L, D])
    k.dscr("AFFD", [128, L // 128, NE])


def stage5(k):
    nc, P, t = k.nc, k.P, k.t
    with ExitStack() as st:
        sb = lambda n, s, d=F32: st.enter_context(nc.sbuf_tensor(n, s, d))
        Wg = sb("p_Wg", [128, 8, D_S5], BF16)
        Wo = sb("p_Wo", [128, 16, D], BF16)
        M2 = [sb("p_M%d" % i, [128, D]) for i in range(3)]
        RW = sb("p_RW", [128, 16, NE])
        glub = sb("p_glub", [128, 8])
        identb = sb("p_idb", [128, 128], BF16)
        identf = sb("p_idf", [128, 128])
        epst = sb("p_eps", [128, 1])
        ygT = [sb("p_yg%d" % i, [128, 8, 512], BF16) for i in range(2)]
        sig = sb("p_sig", [128, 512], BF16)
        yglu = sb("p_yglu", [128, 8, 512], BF16)
        yml = [sb("p_yml%d" % i, [128, D_ML], BF16) for i in range(2)]
        ymlT = [sb("p_ymlT%d" % i, [128, 8, 128], BF16) for i in range(2)]
        yx = sb("p_yx", [128, D])
        xt = [sb("p_xt%d" % i, [128, D]) for i in range(2)]
        x1 = [sb("p_x1%d" % i, [128, D]) for i in range(2)]
        hx2 = sb("p_hx2", [128, D])
        hx2b = sb("p_hx2b", [128, D], BF16)
        junk = sb("p_junk", [128, D], BF16)
        hx2T = sb("p_hx2T", [128, 16, 128])
        ss = sb("p_ss", [128, 8])
        AFF = sb("p_AFF", [128, L // 128, NE])
        pw = [st.enter_context(nc.psum_tensor("p_pw%d" % i, [128, 512], F32)) for i in range(4)]
        pz = [st.enter_context(nc.psum_tensor("p_pz%d" % i, [128, 512], F32)) for i in range(2)]
        pt = st.enter_context(nc.psum_tensor("p_pt", [128, 8, 128], BF16))
        pl = st.enter_context(nc.psum_tensor("p_pl", [128, NE], F32))
        gw = t["gluw"].rearrange("(c p) n -> p c n", p=128)
        for c in range(8):
            P.dma("pool", Wg[:, c, :], gw[:, c, :], w=[("Wg", c)])
        wo = t["w_out"].rearrange("(c p) n -> p c n", p=128)
        for c in range(16):
            for h_ in range(2):
                P.dma("pool", Wo[:, c, h_ * 1024:(h_ + 1) * 1024], wo[:, c, h_ * 1024:(h_ + 1) * 1024], w=[("Wo", c, h_)])
        for i in range(3):
            P.dma("sp", M2[i][:], t["MODS2"][i], r=["M2_%d" % i], w=[("M2", i)])
        P.dma("sp", RW[:], t["rw"][:, :, :], w=["RW"])
        P.dma("sp", glub[:], t["glub"][:, :], w=["glub"])
        P.dma("act", identb[:], t["ident_bf"][:, :], w=["identb"])
        P.dma("act", identf[:], t["ident_f"][:, :], w=["identf"])
        P.add("pool", lambda e: e.memset(epst[:], EPS), w=["epst"])
        ygsrc = t["YG"].rearrange("c p t -> p c t")
        ymlsrc = t["YML"].rearrange("(w r) c -> r w c", r=128)
        ymlkeys = [("YML", i) for i in range(L // 128)]
        def phase_G(g5):
            gb_ = g5 % 2
            P.dma("sp", ygT[gb_][:], ygsrc[:, :, g5 * 512:(g5 + 1) * 512], r=[("YG", blk, g5) for blk in range(8)], w=[("ygT", gb_)])
            for c2 in range(8):
                pzz = pz[c2 % 2]
                for c in range(8):
                    P.add("pe", lambda e, pzz=pzz, c=c, c2=c2, gb_=gb_: e.matmul(pzz[:], Wg[:, c, c2 * 128:(c2 + 1) * 128], ygT[gb_][:, c, :],
                                                                                 start=(c == 0), stop=(c == 7)),
                          r=[("Wg", c), ("ygT", gb_)], w=[("pz", c2 % 2)])
                P.add("act", lambda e, pzz=pzz, c2=c2: e.activation(out=sig[:], in_=pzz[:], func=AF.Sigmoid, bias=glub[:, c2:c2 + 1], scale=1.0),
                      r=[("pz", c2 % 2), "glub"], w=["sig"])
                P.add("dve", lambda e, c2=c2, gb_=gb_: e.tensor_tensor(yglu[:, c2, :], ygT[gb_][:, c2, :], sig[:], ALU.mult),
                      r=["sig", ("ygT", gb_)], w=["yglu"])

        def phase_L(ti):
            tb_ = ti % 2
            for a in range(2):
                P.dma("sp", yml[tb_][64 * a:64 * (a + 1), :], ymlsrc[ti * 2 + a], r=ymlkeys, w=[("yml", tb_)])
            P.dma("sp", xt[tb_][:], t["x"][ti * 128:(ti + 1) * 128, :], w=[("xt", tb_)])

        def phase_A(ti):
            j = ti % 4
            tb_ = ti % 2
            xb = ti % 2
            r0 = ti * 2
            for c in range(8):
                P.add("pe", lambda e, c=c, tb_=tb_: e.transpose(pt[:, c, :], yml[tb_][:, c * 128:(c + 1) * 128], identb[:]),
                      r=[("yml", tb_), "identb"], w=["pt"])
            P.add("act", lambda e, tb_=tb_: e.copy(ymlT[tb_][:], pt[:]), r=["pt"], w=[("ymlT", tb_)])
            for nb in range(4):
                for c in range(16):
                    lhs = yglu[:, c, j * 128:(j + 1) * 128] if c < 8 else ymlT[tb_][:, c - 8, :]
                    P.add("pe", lambda e, nb=nb, c=c, lhs=lhs: e.matmul(pw[nb][:], lhs, Wo[:, c, nb * 512:(nb + 1) * 512],
                                                                        start=(c == 0), stop=(c == 15)),
                          r=["yglu", ("ymlT", tb_), ("Wo", c, nb // 2)], w=[("pw", nb)])
                if nb % 2 == 0:
                    P.add("act", lambda e, nb=nb: e.copy(yx[:, nb * 512:(nb + 1) * 512], pw[nb][:]), r=[("pw", nb)], w=[("yx", nb)])
                else:
                    P.add("dve", lambda e, nb=nb: e.tensor_copy(yx[:, nb * 512:(nb + 1) * 512], pw[nb][:]), r=[("pw", nb)], w=[("yx", nb)])
            yxk = [("yx", nb) for nb in range(4)]
            P.add("act", lambda e: e.activation(out=junk[:], in_=yx[:], func=AF.Square, scale=float(D ** -0.5), accum_out=ss[:, 0:1]),
                  r=yxk, w=["junk", "ss0"])
            P.add("act", lambda e: e.activation(out=ss[:, 1:2], in_=ss[:, 0:1], func=AF.Sqrt, bias=epst[:, 0:1], scale=1.0),
                  r=["ss0", "epst"], w=["ss1"])
            P.add("dve", lambda e: e.reciprocal(ss[:, 1:2], ss[:, 1:2]), r=["ss1"], w=["ss1"])
            P.add("dve", lambda e, xb=xb: e.scalar_tensor_tensor(x1[xb][:], yx[:], ss[:, 1:2], M2[0][:], ALU.mult, ALU.mult),
                  r=yxk + ["ss1", ("M2", 0)], w=[("x1", xb)])
            P.add("pool", lambda e, xb=xb: e.tensor_tensor(x1[xb][:], x1[xb][:], xt[xb][:], ALU.add), r=[("x1", xb), ("xt", xb)], w=[("x1", xb)])
            P.dma("sp", t["X1"][ti * 128:(ti + 1) * 128, :], x1[xb][:], r=[("x1", xb)], w=[("X1", ti)])

        def phase_B(ti):
            xb = ti % 2
            P.add("act", lambda e, xb=xb: e.activation(out=hx2b[:], in_=x1[xb][:], func=AF.Square, scale=float(D ** -0.5), accum_out=ss[:, 2:3]),
                  r=[("x1", xb)], w=["hx2b", "ss2"])
            P.add("act", lambda e: e.activation(out=ss[:, 3:4], in_=ss[:, 2:3], func=AF.Sqrt, bias=epst[:, 0:1], scale=1.0),
                  r=["ss2", "epst"], w=["ss3"])
            P.add("dve", lambda e: e.reciprocal(ss[:, 3:4], ss[:, 3:4]), r=["ss3"], w=["ss3"])
            P.add("dve", lambda e, xb=xb: e.scalar_tensor_tensor(hx2[:], x1[xb][:], ss[:, 3:4], M2[1][:], ALU.mult, ALU.mult),
                  r=[("x1", xb), "ss3", ("M2", 1)], w=["hx2"])
            P.add("pool", lambda e: e.tensor_tensor(hx2[:], hx2[:], M2[2][:], ALU.add), r=["hx2", ("M2", 2)], w=["hx2"])
            P.add("act", lambda e: e.copy(hx2b[:], hx2[:]), r=["hx2"], w=["hx2b"])
            P.dma("sp", t["HX2"][ti * 128:(ti + 1) * 128, :], hx2b[:], r=["hx2b"], w=[("HX2", ti)])
            for g4 in range(4):
                pzz = pz[g4 % 2]
                for c in range(4):
                    kc = g4 * 4 + c
                    P.add("pe", lambda e, pzz=pzz, c=c, kc=kc: e.transpose(pzz[:, c * 128:(c + 1) * 128], hx2[:, kc * 128:(kc + 1) * 128], identf[:]),
                          r=["hx2", "identf"], w=[("pz", g4 % 2)])
                P.add("dve", lambda e, pzz=pzz, g4=g4: e.tensor_copy(hx2T[:, g4 * 4:(g4 + 1) * 4, :].rearrange("p c t -> p (c t)"), pzz[:]),
                      r=[("pz", g4 % 2)], w=[("hx2T", g4)])
            for kc in range(16):
                P.add("pe", lambda e, kc=kc: e.matmul(pl[:], hx2T[:, kc, :], RW[:, kc, :], start=(kc == 0), stop=(kc == 15)),
                      r=[("hx2T", kc // 4), "RW"], w=["pl"])
            P.add("dve", lambda e: e.tensor_reduce(ss[:, 4:5], pl[:], AX.X, ALU.max), r=["pl"], w=["ss4"])
            P.add("dve", lambda e: e.tensor_scalar_mul(ss[:, 4:5], ss[:, 4:5], -1.0), r=["ss4"], w=["ss4"])
            P.add("act", lambda e, ti=ti: e.activation(out=AFF[:, ti, :], in_=pl[:], func=AF.Exp, bias=ss[:, 4:5], scale=1.0,
                                                       accum_out=ss[:, 5:6]),
                  r=["pl", "ss4"], w=["AFF", "ss5"])
            P.add("dve", lambda e: e.reciprocal(ss[:, 5:6], ss[:, 5:6]), r=["ss5"], w=["ss5"])
            P.add("dve", lambda e, ti=ti: e.tensor_scalar_mul(AFF[:, ti, :], AFF[:, ti, :], ss[:, 5:6]), r=["AFF", "ss5"], w=["AFF"])

        phase_L(0)
        for ti in range(L // 128):
            if ti % 4 == 0:
                phase_G(ti // 4)
            if ti + 1 < L // 128:
                phase_L(ti + 1)
            phase_A(ti)
            if ti > 0:
                phase_B(ti - 1)
        phase_B(L // 128 - 1)
        P.dma("sp", t["AFFD"][:, :, :], AFF[:], r=["AFF"], w=["AFFD"])
        P.flush()


def declare_moe(k):
    k.din("ewg", [NE, D, FF])
    k.din("ewu", [NE, D, FF])
    k.din("ewd", [NE, FF, D])
    k.din("ownidx", [128, OWN // 128], I32)
    k.din("c_slt", [128, 128], BF16)
    k.din("c_onesb", [128, 128], BF16)
    k.din("c_iota", [128, CAPL])
    k.dscr("AFFT", [L, NE])
    k.dscr("TH", [128, NE])
    k.dscr("YGALL", [NE, 3, 128, D], BF16)
    k.dscr("OHTALL", [OWN // 128, 128, NE * 3, 128], BF16)
    k.dscr("MOEO", [OWN, D])


def stage6(k):
    nc, P, t = k.nc, k.P, k.t
    NTL = L // 128
    with ExitStack() as st:
        sb = lambda n, s, d=F32: st.enter_context(nc.sbuf_tensor(n, s, d))
        A = sb("t_A", [128, NTL, NE])
        cmp_ = sb("t_cmp", [128, NTL, NE], BF16)
        onesb = sb("t_onesb", [128, 128])
        v = sb("t_v", [128, 8, NE])
        cntp = sb("t_cntp", [128, NE])
        pc = st.enter_context(nc.psum_tensor("t_pc", [128, NE], F32))
        P.dma("sp", A[:], t["AFFD"][:, :, :], r=["AFFD"], w=["A"])
        P.dma("act", t["AFFT"].rearrange("(n p) e -> p n e", p=128), A[:], r=["A"], w=["AFFT"])
        P.dma("sp", onesb[:], t["c_ones"][:, :], w=["onesb"])
        P.add("pool", lambda e: e.memset(v[:, 0, :], 0.0), w=["lo"])
        P.add("pool", lambda e: e.memset(v[:, 1, :], 1.0), w=["hi"])
        lo, hi, th, ge, d1, d2 = (v[:, i, :] for i in range(6))
        for it in range(30):
            P.add("dve", lambda e: e.tensor_tensor(th, lo, hi, ALU.add), r=["lo", "hi"], w=["th"])
            P.add("dve", lambda e: e.tensor_scalar_mul(th, th, 0.5), r=["th"], w=["th"])
            P.add("dve", lambda e: e.tensor_tensor(cmp_[:], A[:], th.unsqueeze(1).broadcast_to([128, NTL, NE]), ALU.is_ge),
                  r=["A", "th"], w=["cmp"])
            P.add("dve", lambda e: e.tensor_reduce(cntp[:], cmp_[:].rearrange("p n e -> p e n"), AX.X, ALU.add), r=["cmp"], w=["cntp"])
            P.add("pe", lambda e: e.matmul(pc[:], onesb[:], cntp[:], start=True, stop=True), r=["onesb", "cntp"], w=["pc"])
            P.add("dve", lambda e: e.tensor_single_scalar(ge, pc[:], float(CAPE) - 0.5, ALU.is_gt), r=["pc"], w=["ge"])
            P.add("dve", lambda e: e.tensor_tensor(d1, th, lo, ALU.subtract), r=["th", "lo"], w=["d1"])
            P.add("dve", lambda e: e.tensor_tensor(d1, d1, ge, ALU.mult), r=["d1", "ge"], w=["d1"])
            P.add("dve", lambda e: e.tensor_tensor(d2, hi, th, ALU.subtract), r=["th", "hi"], w=["d2"])
            P.add("dve", lambda e: e.tensor_tensor(d2, d2, ge, ALU.mult), r=["d2", "ge"], w=["d2"])
            P.add("dve", lambda e: e.tensor_tensor(lo, lo, d1, ALU.add), r=["lo", "d1"], w=["lo"])
            P.add("dve", lambda e: e.tensor_tensor(hi, th, d2, ALU.add), r=["th", "d2"], w=["hi"])
        P.dma("sp", t["TH"][:, :], lo, r=["lo"], w=["TH"])
        P.flush()


def stage7(k):
    nc, P, t = k.nc, k.P, k.t
    NO = OWN // 128
    with ExitStack() as st:
        sb = lambda n, s, d=F32: st.enter_context(nc.sbuf_tensor(n, s, d))
        oidx = sb("e_oidx", [128, NO], I32)
        HX = sb("e_HX", [128, NO, D], BF16)
        Ao = sb("e_Ao", [128, NO, NE])
        th = sb("e_th", [128, NE])
        sel = sb("e_sel", [128, NO, NE])
        selb = sb("e_selb", [128, NO, NE], BF16)
        slot = sb("e_slot", [128, NO, NE])
        cum = sb("e_cum", [128, NO, NE])
        tot = sb("e_tot", [128, NO, NE])
        AHL = sb("e_AHL", [128, NO, NE, 2], BF16)
        ares = sb("e_ares", [128, NO, NE])
        slt = sb("e_slt", [128, 128], BF16)
        onesb = sb("e_onesb", [128, 128], BF16)
        identb = sb("e_idb", [128, 128], BF16)
        iota = sb("e_iota", [128, CAPL])
        onef = sb("e_onef", [128, 1])
        OH = sb("e_OH", [128, NO, CAPL], BF16)
        XT = sb("e_XT", [128, 16, CAPL], BF16)
        HT = sb("e_HT", [128, 16, CAPL], BF16)
        Wg = [sb("e_Wg%d" % i, [128, 16, 256], BF16) for i in range(2)]
        Wu = [sb("e_Wu%d" % i, [128, 16, 256], BF16) for i in range(2)]
        Wd = [sb("e_Wd%d" % i, [128, 16, 512], BF16) for i in range(2)]
        asb = sb("e_asb", [128, CAPL])
        gs = sb("e_gs", [128, 3, 2])
        Yg = sb("e_Yg", [128, 3, D], BF16)
        OHT = sb("e_OHT", [128, NO, 3, 128], BF16)
        pa = [st.enter_context(nc.psum_tensor("e_pa%d" % i, [128, 512], F32)) for i in range(2)]
        pu = [st.enter_context(nc.psum_tensor("e_pu%d" % i, [128, 512], F32)) for i in range(2)]
        py = [st.enter_context(nc.psum_tensor("e_py%d" % i, [128, 512], F32)) for i in range(2)]
        pg = st.enter_context(nc.psum_tensor("e_pg", [128, 3, 2], F32))
        pt = st.enter_context(nc.psum_tensor("e_pt", [128, 3, 128], BF16))
        P.dma("sp", oidx[:], t["ownidx"][:, :], w=["oidx"])
        P.dma("sp", th[:], t["TH"][:, :], r=["TH"], w=["th"])
        P.dma("act", slt[:], t["c_slt"][:, :], w=["slt"])
        P.dma("act", onesb[:], t["c_onesb"][:, :], w=["onesb"])
        P.dma("act", identb[:], t["ident_bf"][:, :], w=["identb"])
        P.dma("sp", iota[:], t["c_iota"][:, :], w=["iota"])
        P.add("pool", lambda e: e.memset(onef[:], 1.0), w=["onef"])
        hx2keys = [("HX2", ti) for ti in range(L // 128)]
        for i in range(NO):
            P.add("pool", lambda e, i=i: e.indirect_dma_start(out=HX[:, i, :], out_offset=None, in_=t["HX2"][:, :],
                                                             in_offset=bass.IndirectOffsetOnAxis(ap=oidx[:, i:i + 1], axis=0)),
                  r=["oidx"] + hx2keys, w=[("HX", i)], dma=True)
            P.add("pool", lambda e, i=i: e.indirect_dma_start(out=Ao[:, i, :], out_offset=None, in_=t["AFFT"][:, :],
                                                             in_offset=bass.IndirectOffsetOnAxis(ap=oidx[:, i:i + 1], axis=0)),
                  r=["oidx", "AFFT"], w=["Ao"], dma=True)
        fl = lambda a: a.rearrange("p n e -> p (n e)")
        P.add("dve", lambda e: e.tensor_tensor(sel[:], Ao[:], th[:].unsqueeze(1).broadcast_to([128, NO, NE]), ALU.is_ge),
              r=["Ao", "th"], w=["sel"])
        P.add("dve", lambda e: e.tensor_copy(selb[:], sel[:]), r=["sel"], w=["selb"])
        P.add("pe", lambda e: e.matmul(pa[0][:, 0:NO * NE], slt[:], fl(selb[:]), start=True, stop=True), r=["slt", "selb"], w=["pa0"])
        P.add("pe", lambda e: e.matmul(pu[0][:, 0:NO * NE], onesb[:], fl(selb[:]), start=True, stop=True), r=["onesb", "selb"], w=["pu0"])
        P.add("dve", lambda e: e.tensor_copy(fl(tot[:]), pu[0][:, 0:NO * NE]), r=["pu0"], w=["tot"])
        for e_ in range(NE):
            P.add("dve", lambda e, e_=e_: e.tensor_tensor_scan(cum[:, :, e_], onef[:, 0:1].broadcast_to([128, NO]), tot[:, :, e_], 0.0,
                                                               ALU.mult, ALU.add),
                  r=["tot", "onef"], w=["cum"])
        P.add("dve", lambda e: e.tensor_tensor(fl(slot[:]), fl(cum[:]), fl(tot[:]), ALU.subtract), r=["cum", "tot"], w=["slot"])
        P.add("dve", lambda e: e.tensor_tensor(fl(slot[:]), fl(slot[:]), pa[0][:, 0:NO * NE], ALU.add), r=["slot", "pa0"], w=["slot"])
        P.add("dve", lambda e: e.scalar_tensor_tensor(fl(slot[:]), fl(slot[:]), 1.0, fl(sel[:]), ALU.add, ALU.mult), r=["slot", "sel"], w=["slot"])
        P.add("dve", lambda e: e.tensor_scalar_add(fl(slot[:]), fl(slot[:]), -1.0), r=["slot"], w=["slot"])
        P.add("dve", lambda e: e.tensor_copy(AHL[:, :, :, 0], Ao[:]), r=["Ao"], w=["AHL0"])
        P.add("dve", lambda e: e.tensor_copy(ares[:], AHL[:, :, :, 0]), r=["AHL0"], w=["ares"])
        P.add("dve", lambda e: e.tensor_tensor(ares[:], Ao[:], ares[:], ALU.subtract), r=["Ao", "ares"], w=["ares"])
        P.add("dve", lambda e: e.tensor_copy(AHL[:, :, :, 1], ares[:]), r=["ares"], w=["AHL1"])
        hxk = [("HX", i) for i in range(NO)]
        nld = 0
        for e_ in range(NE):
            for i in range(NO):
                P.add("dve", lambda e, i=i, e_=e_: e.tensor_single_scalar(OH[:, i, :], iota[:], slot[:, i, e_:e_ + 1], ALU.is_equal),
                      r=["iota", "slot"], w=[("OH", i)])
            ohk = [("OH", i) for i in range(NO)]
            for kc in range(16):
                pp = pa[kc % 2]
                for i in range(NO):
                    P.add("pe", lambda e, pp=pp, i=i, kc=kc: e.matmul(pp[:, 0:CAPL], HX[:, i, kc * 128:(kc + 1) * 128], OH[:, i, :],
                                                                      start=(i == 0), stop=(i == NO - 1)),
                          r=[("HX", i), ("OH", i)], w=[("pa", kc % 2)])
                if kc % 2 == 0:
                    P.add("act", lambda e, pp=pp, kc=kc: e.copy(XT[:, kc, :], pp[:, 0:CAPL]), r=[("pa", kc % 2)], w=[("XT", kc)])
                else:
                    P.add("dve", lambda e, pp=pp, kc=kc: e.tensor_copy(XT[:, kc, :], pp[:, 0:CAPL]), r=[("pa", kc % 2)], w=[("XT", kc)])
            for sc in range(3):
                for i in range(NO):
                    P.add("pe", lambda e, sc=sc, i=i, e_=e_: e.matmul(pg[:, sc, :], OH[:, i, sc * 128:(sc + 1) * 128], AHL[:, i, e_, :],
                                                                      start=(i == 0), stop=(i == NO - 1)),
                          r=[("OH", i), "AHL0", "AHL1"], w=["pg"])
            P.add("dve", lambda e: e.tensor_copy(gs[:], pg[:]), r=["pg"], w=["gs"])
            P.add("dve", lambda e: e.tensor_tensor(gs[:, :, 0], gs[:, :, 0], gs[:, :, 1], ALU.add), r=["gs"], w=["gs"])
            for i in range(NO):
                for sc in range(3):
                    P.add("pe", lambda e, i=i, sc=sc: e.transpose(pt[:, sc, :], OH[:, i, sc * 128:(sc + 1) * 128], identb[:]),
                          r=[("OH", i), "identb"], w=["pt"])
                P.add("act", lambda e, i=i: e.copy(OHT[:, i, :, :], pt[:]), r=["pt"], w=[("OHT", i)])
                P.dma("sp", t["OHTALL"][i][:, e_ * 3:(e_ + 1) * 3, :], OHT[:, i, :, :], r=[("OHT", i)], w=[("OHTALL", i, e_)])
            xtk = [("XT", kc) for kc in range(16)]
            wgv = t["ewg"][e_].rearrange("(kc p) f -> p kc f", p=128)
            wuv = t["ewu"][e_].rearrange("(kc p) f -> p kc f", p=128)
            for fb in range(8):
                wb = nld % 2
                nld += 1
                P.dma("pool", Wg[wb][:], wgv[:, :, fb * 256:(fb + 1) * 256], w=[("Wg", wb)])
                P.dma("pool", Wu[wb][:], wuv[:, :, fb * 256:(fb + 1) * 256], w=[("Wu", wb)])
                for f2_ in range(2):
                    fc = fb * 2 + f2_
                    pb = fc % 2
                    for kc in range(16):
                        P.add("pe", lambda e, pb=pb, wb=wb, kc=kc, f2_=f2_: e.matmul(pa[pb][:, 0:CAPL], Wg[wb][:, kc, f2_ * 128:(f2_ + 1) * 128],
                                                                                     XT[:, kc, :], start=(kc == 0), stop=(kc == 15)),
                              r=[("Wg", wb), ("XT", kc)], w=[("pa", pb)])
                    for kc in range(16):
                        P.add("pe", lambda e, pb=pb, wb=wb, kc=kc, f2_=f2_: e.matmul(pu[pb][:, 0:CAPL], Wu[wb][:, kc, f2_ * 128:(f2_ + 1) * 128],
                                                                                     XT[:, kc, :], start=(kc == 0), stop=(kc == 15)),
                              r=[("Wu", wb), ("XT", kc)], w=[("pu", pb)])
                    P.add("act", lambda e, pb=pb: e.activation(out=asb[:], in_=pa[pb][:, 0:CAPL], func=AF.Silu), r=[("pa", pb)], w=["asb"])
                    P.add("dve", lambda e, pb=pb, fc=fc: e.tensor_tensor(HT[:, fc, :], asb[:], pu[pb][:, 0:CAPL], ALU.mult),
                          r=["asb", ("pu", pb)], w=[("HT", fc)])
            wdv = t["ewd"][e_].rearrange("(fc p) d -> p fc d", p=128)
            for db in range(4):
                wb = db % 2
                P.dma("pool", Wd[wb][:], wdv[:, :, db * 512:(db + 1) * 512], w=[("Wd", wb)])
                for sc in range(3):
                    pp = py[sc % 2]
                    for fc in range(16):
                        P.add("pe", lambda e, pp=pp, wb=wb, fc=fc, sc=sc: e.matmul(pp[:], HT[:, fc, sc * 128:(sc + 1) * 128], Wd[wb][:, fc, :],
                                                                                   start=(fc == 0), stop=(fc == 15)),
                              r=[("HT", fc), ("Wd", wb)], w=[("py", sc % 2)])
                    P.add("act", lambda e, pp=pp, sc=sc, db=db: e.activation(out=Yg[:, sc, db * 512:(db + 1) * 512], in_=pp[:], func=AF.Copy,
                                                                             scale=gs[:, sc, 0:1]),
                          r=[("py", sc % 2), "gs"], w=["Yg"])
            P.dma("sp", t["YGALL"][e_].rearrange("c s d -> s c d"), Yg[:], r=["Yg"], w=[("YGALL", e_)])
        P.flush()


def stage8(k):
    nc, P, t = k.nc, k.P, k.t
    NO = OWN // 128
    with ExitStack() as st:
        sb = lambda n, s, d=F32: st.enter_context(nc.sbuf_tensor(n, s, d))
        YGb = sb("f_YG", [128, NE * 3, 512], BF16)
        OT = [sb("f_OT%d" % i, [128, NE * 3, 128], BF16) for i in range(2)]
        ob = [sb("f_ob%d" % i, [128, 512]) for i in range(2)]
        ps = [st.enter_context(nc.psum_tensor("f_ps%d" % i, [128, 512], F32)) for i in range(2)]
        ygv = t["YGALL"].rearrange("e c s d -> s (e c) d")
        for db in range(4):
            for e_ in range(NE):
                P.dma(k.q(), YGb[:, e_ * 3:(e_ + 1) * 3, :], ygv[:, e_ * 3:(e_ + 1) * 3, db * 512:(db + 1) * 512],
                      r=[("YGALL", e_)], w=[("YGb", e_)])
            for i in range(NO):
                b = i % 2
                P.dma(k.q(), OT[b][:], t["OHTALL"][i], r=[("OHTALL", i, e_) for e_ in range(NE)], w=[("OT", b)])
                for j in range(NE * 3):
                    P.add("pe", lambda e, b=b, j=j: e.matmul(ps[b][:], OT[b][:, j, :], YGb[:, j, :], start=(j == 0), stop=(j == NE * 3 - 1)),
                          r=[("OT", b), ("YGb", j // 3)], w=[("ps", b)])
                P.add("act", lambda e, b=b: e.copy(ob[b][:], ps[b][:]), r=[("ps", b)], w=[("ob", b)])
                P.dma("sp", t["MOEO"][i * 128:(i + 1) * 128, db * 512:(db + 1) * 512], ob[b][:], r=[("ob", b)], w=[("MOEO", i, db)])
        P.flush()


def stage9(k):
    nc, P, t = k.nc, k.P, k.t
    NO = OWN // 128
    with ExitStack() as st:
        sb = lambda n, s, d=F32: st.enter_context(nc.sbuf_tensor(n, s, d))
        GN3 = sb("g_GN3", [128, D])
        oidx = sb("g_oidx", [128, NO], I32)
        epst = sb("g_eps", [128, 1])
        mo = [sb("g_mo%d" % i, [128, D]) for i in range(2)]
        x1 = [sb("g_x1%d" % i, [128, D]) for i in range(2)]
        junk = sb("g_junk", [128, D], BF16)
        ss = [sb("g_ss%d" % i, [128, 2]) for i in range(2)]
        P.dma("sp", GN3[:], t["MODS2"][3], r=["M2_3"], w=["GN3"])
        P.dma("sp", oidx[:], t["ownidx"][:, :], w=["oidx"])
        P.add("pool", lambda e: e.memset(epst[:], EPS), w=["epst"])
        x1keys = [("X1", ti) for ti in range(L // 128)]
        outs = []
        for i in range(NO):
            b = i % 2
            P.dma("sp", mo[b][:], t["MOEO"][i * 128:(i + 1) * 128, :], r=[("MOEO", i, db) for db in range(4)], w=[("mo", b)])
            P.add("pool", lambda e, b=b, i=i: e.indirect_dma_start(out=x1[b][:], out_offset=None, in_=t["X1"][:, :],
                                                                  in_offset=bass.IndirectOffsetOnAxis(ap=oidx[:, i:i + 1], axis=0)),
                  r=["oidx"] + x1keys, w=[("x1", b)], dma=True)
            P.add("act", lambda e, b=b: e.activation(out=junk[:], in_=mo[b][:], func=AF.Square, scale=float(D ** -0.5), accum_out=ss[b][:, 0:1]),
                  r=[("mo", b)], w=["junk", ("ss", b)])
            P.add("act", lambda e, b=b: e.activation(out=ss[b][:, 1:2], in_=ss[b][:, 0:1], func=AF.Sqrt, bias=epst[:, 0:1], scale=1.0),
                  r=[("ss", b), "epst"], w=[("ss1", b)])
            P.add("dve", lambda e, b=b: e.reciprocal(ss[b][:, 1:2], ss[b][:, 1:2]), r=[("ss1", b)], w=[("ss1", b)])
            P.add("dve", lambda e, b=b: e.scalar_tensor_tensor(mo[b][:], mo[b][:], ss[b][:, 1:2], GN3[:], ALU.mult, ALU.mult),
                  r=[("mo", b), ("ss1", b), "GN3"], w=[("mo", b)])
            P.add("pool", lambda e, b=b: e.tensor_tensor(mo[b][:], mo[b][:], x1[b][:], ALU.add), r=[("mo", b), ("x1", b)], w=[("mo", b)])
            outs.append(P.dma("sp", t["out"][i * 128:(i + 1) * 128, :], mo[b][:], r=[("mo", b)], w=[("out", i)]))
        P.flush(final_wait=outs)


def _host_s5(inp):
    are, aim, ldt = inp["s5_a_re"][0], inp["s5_a_im"][0], inp["s5_log_dt"][0]
    bre, bim = inp["s5_b_re"][0], inp["s5_b_im"][0]
    cre, cim = inp["s5_c_re"][0], inp["s5_c_im"][0]

    def gp(a):
        sh = a.shape[:-2]
        a = a.reshape(*sh, 8, 4, 2, 64)
        a = np.moveaxis(a, [-4, -2, -1, -3], [0, 1, 2, -1])
        return a.reshape(8, 128, *sh, 4)

    s5p = np.concatenate([gp(are).reshape(8, 128, 8), gp(aim).reshape(8, 128, 8),
                          gp(np.broadcast_to(ldt[..., None], (2, 64, 64))).reshape(8, 128, 8)], -1)

    def bc(r, i, cfirst):
        out = []
        for a in (r, i):
            if cfirst:
                a = a.transpose(0, 2, 1)
            a = a.reshape(8, 4, 2, 64, 16).transpose(0, 2, 3, 1, 4).reshape(8, 128, 4, 16)
            out.append(a)
        return np.ascontiguousarray(np.stack(out, 2))

    d = {"s5p": np.ascontiguousarray(s5p.astype(np.float32)), "s5b": bc(bre, bim, False),
         "s5c": bc(cre, cim, True), "s5d": np.ascontiguousarray(inp["s5_d"][0].reshape(8, 128, 1))}
    d["c_jidx"] = np.broadcast_to(np.arange(17, dtype=np.float32), (128, 17)).copy()
    pm = np.zeros((128, 2), np.float32)
    pm[:64, 0] = 1
    pm[64:, 1] = 1
    d["c_parmask"] = pm
    d["c_blockmask"] = np.kron(np.eye(8, dtype=np.float32), np.ones((16, 16), np.float32))
    m = np.arange(NCH, dtype=np.float32)
    d["c_midx"] = np.broadcast_to(np.stack([m, m[::-1]]), (128, 2, NCH)).copy()
    d["c_rowmask"] = np.kron(np.eye(4, dtype=np.float32), np.ones((32, 1), np.float32))
    return d


def _core_inputs(inp, core, shared):
    b, j = divmod(core, 4)
    cond = np.stack([inp["c"][b], inp["c_ctx"]], -1).reshape(16, 128, 2).transpose(1, 0, 2)
    d = {"x": inp["x"][b], "ctx": inp["ctx"][b], "cond": np.ascontiguousarray(cond),
         "xown": inp["x"][b][j * OWN:(j + 1) * OWN]}
    d.update(shared)
    return d


def _host_rest(inp):
    d = {}
    d["convw"] = np.ascontiguousarray(inp["ml_conv_w"][0].T.reshape(16, 128, 5))
    d["convb"] = np.ascontiguousarray(inp["ml_conv_b"][0].reshape(16, 128, 1))
    d["gateb"] = np.broadcast_to(inp["ml_gate_b"][0].reshape(1, 16), (128, 16)).copy()
    d["mlng"] = np.broadcast_to(inp["ml_norm_g"][0][None], (128, D_ML)).copy()
    tri = np.triu(np.ones((128, 128), np.float32))
    d["c_tri"] = tri
    d["c_trit"] = np.ascontiguousarray(tri.T)
    d["c_ones"] = np.ones((128, 128), np.float32)
    d["gluw"] = inp["s5_glu_w"][0]
    d["w_out"] = inp["w_out"][0]
    d["glub"] = np.ascontiguousarray(inp["s5_glu_b"][0].reshape(8, 128).T)
    d["rw"] = np.ascontiguousarray(inp["router_w"][0].reshape(16, 128, 16).transpose(1, 0, 2))
    d["ewg"] = inp["exp_w_gate"][0]
    d["ewu"] = inp["exp_w_up"][0]
    d["ewd"] = inp["exp_w_down"][0]
    d["c_slt"] = np.triu(np.ones((128, 128), np.float32), 1).astype(ml_dtypes.bfloat16)
    d["c_onesb"] = np.ones((128, 128), ml_dtypes.bfloat16)
    d["c_iota"] = np.broadcast_to(np.arange(CAPL, dtype=np.float32), (128, CAPL)).copy()
    return d


def build_full():
    k = K()
    declare_io(k)
    declare_s5(k)
    declare_ml(k)
    declare_s5post(k)
    declare_moe(k)
    for f in (stage0, stage1, stage2, stage3a, stage3b, stage4a, stage4b, stage4c, stage5, stage6, stage7, stage8, stage9):
        f(k)
    return k


def host_inputs(inp, cores):
    shared = {"ada_w": inp["ada_w"][0], "ada_b": inp["ada_b"][0][None], "norm_g": inp["norm_g"][0],
              "w_in": inp["w_in"][0], "ident_bf": np.eye(128, dtype=ml_dtypes.bfloat16),
              "ident_f": np.eye(128, dtype=np.float32)}
    shared.update(_host_s5(inp))
    shared.update(_host_rest(inp))
    maps = []
    for core in cores:
        b, j = divmod(core, 4)
        cond = np.stack([inp["c"][b], inp["c_ctx"]], -1).reshape(16, 128, 2).transpose(1, 0, 2)
        d = {"x": inp["x"][b], "ctx": inp["ctx"][b], "cond": np.ascontiguousarray(cond)}
        d["ownidx"] = (j * OWN + np.arange(OWN, dtype=np.int32).reshape(OWN // 128, 128).T).astype(np.int32).copy()
        d.update(shared)
        maps.append(d)
    return maps


def kernel(**inputs):
    inp = {k_: np.asarray(v) for k_, v in inputs.items()}
    k = build_full()
    names = set(k.t.keys())
    in_maps = [{n: np.ascontiguousarray(v) for n, v in m.items() if n in names} for m in host_inputs(inp, range(8))]
    res = run_bass_kernel_spmd(k.nc, in_maps, core_ids=list(range(8)))
    out = np.zeros((2, L, D), np.float32)
    for core in range(8):
        b, j = divmod(core, 4)
        out[b, j * OWN:(j + 1) * OWN] = np.asarray(res.results[core]["out"])
    return out
```
